# Optimizing a Trainium2 kernel written in Bass

```python
import jax, jax.numpy as jnp
from jax import lax
import numpy as np

D_MODEL = 2048
BATCH = 8
SEQ = 2048
DEPTH = 1

GRID_W = 64
CTX_LEN = 256
D_MIX = D_MODEL
RET_HEADS = 8
RET_HEAD_DIM = 128
D_RET = RET_HEADS * RET_HEAD_DIM
D_CONV = D_MIX - D_RET
D_IN = 4 * D_RET + 2 * D_CONV
CONV_WIDTH = 31
CONV_PAD = CONV_WIDTH // 2
CHUNK = 128
ROPE_BASE = 10000.0
N_EXPERTS = 32
TOP_K = 4
D_FF = D_MODEL
SWIGLU_ALPHA = 1.702
SWIGLU_LIMIT = 7.0
EPS = 1e-6
GN_EPS = 1e-5

kernel_name = 'hybrid_retention_conformer_moe_dit_layer'


def rms_norm(x, w):
    xf = x.astype(jnp.float32)
    y = xf * lax.rsqrt(jnp.mean(xf * xf, axis=-1, keepdims=True) + EPS)
    return (y * w.astype(jnp.float32)).astype(x.dtype)


def modulate(h, shift, scale):
    return h * (1 + scale) + shift


def to_heads(t):
    b, n, _ = t.shape
    return t.reshape(b, n, RET_HEADS, RET_HEAD_DIM).transpose(0, 2, 1, 3).astype(jnp.float32)


def rotary(t, pos):
    half = RET_HEAD_DIM // 2
    inv = ROPE_BASE ** (-jnp.arange(half, dtype=jnp.float32) / half)
    ang = pos[:, None] * inv[None, :]
    cos, sin = jnp.cos(ang), jnp.sin(ang)
    t1, t2 = t[..., :half], t[..., half:]
    return jnp.concatenate([t1 * cos - t2 * sin, t1 * sin + t2 * cos], axis=-1)


def retention_chunked(q, k, v, log_g, state0):
    b, h, n, d = q.shape
    nc = n // CHUNK

    def chunks(t):
        return t.reshape(b, h, nc, CHUNK, d).transpose(2, 0, 1, 3, 4)

    idx = jnp.arange(CHUNK, dtype=jnp.float32)
    diff = idx[:, None] - idx[None, :]
    decay_in = jnp.where(diff >= 0, jnp.exp(log_g[:, None, None] * jnp.maximum(diff, 0.0)), 0.0)
    decay_q = jnp.exp(log_g[:, None] * (idx + 1.0))[:, :, None]
    decay_k = jnp.exp(log_g[:, None] * (CHUNK - 1.0 - idx))[:, :, None]
    decay_c = jnp.exp(log_g * CHUNK)[:, None, None]

    def step(state, qkv):
        qi, ki, vi = qkv
        scores = jnp.einsum('bhid,bhjd->bhij', qi, ki) * decay_in
        out = (jnp.einsum('bhij,bhje->bhie', scores, vi)
               + jnp.einsum('bhid,bhde->bhie', qi, state) * decay_q)
        state = state * decay_c + jnp.einsum('bhjd,bhje->bhde', ki * decay_k, vi)
        return state, out

    _, out = lax.scan(step, state0, (chunks(q), chunks(k), chunks(v)))
    return out.transpose(1, 2, 0, 3, 4).reshape(b, h, n, d)


def retention_state(k, v, log_g):
    n = k.shape[2]
    w = jnp.exp(log_g[:, None] * (n - 1.0 - jnp.arange(n, dtype=jnp.float32)))
    return jnp.einsum('bhtd,bhte,ht->bhde', k, v, w)


def bidirectional_retention(q, k, v, lg_f, lg_b, state_f, state_b):
    fwd = retention_chunked(q, k, v, lg_f, state_f)
    bwd = retention_chunked(q[:, :, ::-1], k[:, :, ::-1], v[:, :, ::-1], lg_b, state_b)
    return fwd + bwd[:, :, ::-1]


def retention_group_out(o, g, gn_w):
    mu = jnp.mean(o, axis=-1, keepdims=True)
    var = jnp.mean(jnp.square(o - mu), axis=-1, keepdims=True)
    on = (o - mu) * lax.rsqrt(var + GN_EPS)
    b, h, n, d = o.shape
    on = on.transpose(0, 2, 1, 3).reshape(b, n, h * d) * gn_w.astype(jnp.float32)
    return on * jax.nn.silu(g.astype(jnp.float32))


def conformer_conv(a, gate, conv_w, conv_b, ln_w, ln_b, n_seq, seq_len):
    u = a * jax.nn.sigmoid(gate)
    b, n, ch = u.shape
    u = u.reshape(n_seq, seq_len, ch)
    u = lax.conv_general_dilated(u, conv_w[:, None, :].astype(u.dtype), window_strides=(1,),
                                 padding=[(CONV_PAD, CONV_PAD)],
                                 dimension_numbers=('NWC', 'WIO', 'NWC'),
                                 feature_group_count=ch)
    u = (u + conv_b).reshape(b, n, ch).astype(jnp.float32)
    mu = jnp.mean(u, axis=-1, keepdims=True)
    var = jnp.mean(jnp.square(u - mu), axis=-1, keepdims=True)
    u = (u - mu) * lax.rsqrt(var + EPS) * ln_w.astype(jnp.float32) + ln_b.astype(jnp.float32)
    return jax.nn.silu(u)


def parallel_mixer(h_lat, h_ctx, w_in, dec_f, dec_b, gn_w, conv_w, conv_b, ln_w, ln_b, w_out,
                   need_ctx_out):
    b, s, _ = h_lat.shape
    n_ctx = h_ctx.shape[1]
    rows = s // GRID_W
    pos_ctx = jnp.arange(n_ctx, dtype=jnp.float32)
    pos_lat = n_ctx + jnp.arange(s, dtype=jnp.float32)
    lg_f = jax.nn.log_sigmoid(dec_f.astype(jnp.float32))
    lg_b = jax.nn.log_sigmoid(dec_b.astype(jnp.float32))
    splits = [D_RET, 2 * D_RET, 3 * D_RET, 4 * D_RET, 4 * D_RET + D_CONV]
    q, k, v, g, ca, cb = jnp.split(h_lat @ w_in, splits, axis=-1)
    if need_ctx_out:
        qc, kc, vc, gc, cac, cbc = jnp.split(h_ctx @ w_in, splits, axis=-1)
    else:
        kc, vc = jnp.split(h_ctx @ w_in[:, D_RET:3 * D_RET], 2, axis=-1)
    q_scale = RET_HEAD_DIM ** -0.5
    kc = rotary(to_heads(kc), pos_ctx)
    vc = to_heads(vc)
    state_f = retention_state(kc, vc, lg_f)
    state_b = retention_state(kc[:, :, ::-1], vc[:, :, ::-1], lg_b)
    o_lat = bidirectional_retention(rotary(to_heads(q), pos_lat) * q_scale, rotary(to_heads(k), pos_lat),
                                    to_heads(v), lg_f, lg_b, state_f, state_b)
    y_lat = jnp.concatenate([retention_group_out(o_lat, g, gn_w),
                             conformer_conv(ca, cb, conv_w, conv_b, ln_w, ln_b, b * rows, GRID_W)], axis=-1)
    mix_lat = y_lat.astype(h_lat.dtype) @ w_out
    if not need_ctx_out:
        return mix_lat, None
    zero = jnp.zeros((b, RET_HEADS, RET_HEAD_DIM, RET_HEAD_DIM), jnp.float32)
    o_ctx = bidirectional_retention(rotary(to_heads(qc), pos_ctx) * q_scale, kc, vc, lg_f, lg_b, zero, zero)
    y_ctx = jnp.concatenate([retention_group_out(o_ctx, gc, gn_w),
                             conformer_conv(cac, cbc, conv_w, conv_b, ln_w, ln_b, b, n_ctx)], axis=-1)
    return mix_lat, y_ctx.astype(h_ctx.dtype) @ w_out


def moe_ffn(h, router_w, router_b, w1, b1, w2, b2):
    shp = h.shape
    hf = h.reshape(-1, shp[-1])
    logits = (hf @ router_w + router_b).astype(jnp.float32)
    top_val, top_idx = lax.top_k(logits, TOP_K)
    top_w = jax.nn.softmax(top_val, axis=-1)
    gates = jnp.einsum('nk,nke->ne', top_w, jax.nn.one_hot(top_idx, N_EXPERTS, dtype=jnp.float32))
    out = jnp.zeros(hf.shape, jnp.float32)
    for e in range(N_EXPERTS):
        u = hf @ w1[e] + b1[e]
        glu = jnp.minimum(u[:, :D_FF], SWIGLU_LIMIT)
        lin = jnp.clip(u[:, D_FF:], -SWIGLU_LIMIT, SWIGLU_LIMIT)
        act = glu * jax.nn.sigmoid(SWIGLU_ALPHA * glu) * (lin + 1)
        out = out + gates[:, e:e + 1] * (act @ w2[e] + b2[e]).astype(jnp.float32)
    return out.reshape(shp).astype(h.dtype)


def setup_inputs(seed: int = 0) -> dict:
    key = jax.random.key(seed)
    ks = jax.random.split(key, 26)

    def nrm(k, shape, scale):
        return scale * jax.random.normal(k, shape, jnp.float32)

    base = 1.0 - 2.0 ** (-5.0 - np.arange(RET_HEADS, dtype=np.float32))
    decay_logit = jnp.asarray(np.log(base / (1.0 - base)), jnp.float32)
    return {
        'x': nrm(ks[0], (BATCH, SEQ, D_MODEL), 1.0),
        'c': nrm(ks[1], (BATCH, D_MODEL), 1.0),
        'ctx': nrm(ks[2], (BATCH, CTX_LEN, D_MODEL), 1.0),
        'c_ctx': nrm(ks[3], (D_MODEL,), 1.0),
        'ada_w': nrm(ks[4], (DEPTH, D_MODEL, 6 * D_MODEL), 0.5 * D_MODEL ** -0.5),
        'ada_b': nrm(ks[5], (DEPTH, 6 * D_MODEL), 0.02),
        'pre_mix_norm': 1.0 + nrm(ks[6], (DEPTH, D_MODEL), 0.05),
        'post_mix_norm': 1.0 + nrm(ks[7], (DEPTH, D_MODEL), 0.05),
        'pre_ffn_norm': 1.0 + nrm(ks[8], (DEPTH, D_MODEL), 0.05),
        'post_ffn_norm': 1.0 + nrm(ks[9], (DEPTH, D_MODEL), 0.05),
        'w_in': nrm(ks[10], (DEPTH, D_MODEL, D_IN), D_MODEL ** -0.5),
        'ret_decay_fwd': decay_logit[None, :] + nrm(ks[11], (DEPTH, RET_HEADS), 0.1),
        'ret_decay_bwd': decay_logit[None, :] + nrm(ks[12], (DEPTH, RET_HEADS), 0.1),
        'ret_gn_w': 1.0 + nrm(ks[13], (DEPTH, D_RET), 0.05),
        'conv_w': nrm(ks[14], (DEPTH, CONV_WIDTH, D_CONV), CONV_WIDTH ** -0.5),
        'conv_b': nrm(ks[15], (DEPTH, D_CONV), 0.02),
        'conv_ln_w': 1.0 + nrm(ks[16], (DEPTH, D_CONV), 0.05),
        'conv_ln_b': nrm(ks[17], (DEPTH, D_CONV), 0.02),
        'w_out': nrm(ks[18], (DEPTH, D_MIX, D_MODEL), D_MIX ** -0.5),
        'router_w': nrm(ks[19], (DEPTH, D_MODEL, N_EXPERTS), D_MODEL ** -0.5),
        'router_b': nrm(ks[20], (DEPTH, N_EXPERTS), 0.01),
        'w1': nrm(ks[21], (DEPTH, N_EXPERTS, D_MODEL, 2 * D_FF), D_MODEL ** -0.5),
        'b1': nrm(ks[22], (DEPTH, N_EXPERTS, 2 * D_FF), 0.02),
        'w2': nrm(ks[23], (DEPTH, N_EXPERTS, D_FF, D_MODEL), D_FF ** -0.5),
        'b2': nrm(ks[24], (DEPTH, N_EXPERTS, D_MODEL), 0.02),
    }


def reference(x, c, ctx, c_ctx, ada_w, ada_b, pre_mix_norm, post_mix_norm, pre_ffn_norm, post_ffn_norm,
              w_in, ret_decay_fwd, ret_decay_bwd, ret_gn_w, conv_w, conv_b, conv_ln_w, conv_ln_b, w_out,
              router_w, router_b, w1, b1, w2, b2):
    ctx_h = ctx
    for l in range(DEPTH):
        last = l == DEPTH - 1
        mod_lat = jax.nn.silu(c) @ ada_w[l] + ada_b[l]
        sh1, sc1, g1, sh2, sc2, g2 = [m[:, None, :] for m in jnp.split(mod_lat, 6, axis=-1)]
        mod_ctx = jax.nn.silu(c_ctx) @ ada_w[l] + ada_b[l]
        csh1, csc1, cg1, csh2, csc2, cg2 = jnp.split(mod_ctx, 6, axis=-1)

        h_lat = modulate(rms_norm(x, pre_mix_norm[l]), sh1, sc1)
        h_ctx = modulate(rms_norm(ctx_h, pre_mix_norm[l]), csh1, csc1)
        mix_lat, mix_ctx = parallel_mixer(h_lat, h_ctx, w_in[l], ret_decay_fwd[l], ret_decay_bwd[l],
                                          ret_gn_w[l], conv_w[l], conv_b[l], conv_ln_w[l], conv_ln_b[l],
                                          w_out[l], not last)
        x = x + g1 * rms_norm(mix_lat, post_mix_norm[l])

        h = modulate(rms_norm(x, pre_ffn_norm[l]), sh2, sc2)
        x = x + g2 * rms_norm(moe_ffn(h, router_w[l], router_b[l], w1[l], b1[l], w2[l], b2[l]),
                              post_ffn_norm[l])

        if not last:
            ctx_h = ctx_h + cg1 * rms_norm(mix_ctx, post_mix_norm[l])
            hc = modulate(rms_norm(ctx_h, pre_ffn_norm[l]), csh2, csc2)
            ctx_h = ctx_h + cg2 * rms_norm(moe_ffn(hc, router_w[l], router_b[l], w1[l], b1[l], w2[l], b2[l]),
                                            post_ffn_norm[l])
    return x
```

```python
import numpy as np
import concourse.bass as bass
import concourse.mybir as mybir
from concourse.alu_op_type import AluOpType as ALU
from concourse.bass_utils import run_bass_kernel_spmd

F32 = mybir.dt.float32
BF16 = mybir.dt.bfloat16
I32 = mybir.dt.int32
AF = mybir.ActivationFunctionType
AX = mybir.AxisListType

D = 2048
SEQ = 2048
NB = 8
NCTX = 256
H = 8
HD = 128
DRET = 1024
DCONV = 1024
DIN = 6144
CW = 31
NE = 32
DFF = 2048
NT = SEQ // 128
KC = D // 128
EPS = 1e-6
GN_EPS = 1e-5
ALPHA = 1.702
LIMIT = 7.0
QSCALE = HD ** -0.5

ROUNDS = 1
CAP = ROUNDS * 1024
BIG = 4.0e6

ENGS = ("pe", "act", "dve", "pool", "sp")
EPOCH = 6000


class Buf:
    __slots__ = ("name", "excl", "last_w", "readers")

    def __init__(self, name, excl=False):
        self.name = name
        self.excl = excl
        self.last_w = None
        self.readers = []


class Op:
    __slots__ = ("eng", "fn", "deps", "signal", "done", "is_dma", "chan", "chan_prev", "grp")

    def __init__(self, eng, fn):
        self.eng = eng
        self.fn = fn
        self.deps = []
        self.signal = False
        self.done = None
        self.is_dma = False
        self.chan = None
        self.chan_prev = None
        self.grp = None


class Chan:
    def __init__(self, name):
        self.name = name
        self.sem = None
        self.last_grp = None


class Sched:
    def __init__(self, nc):
        self.nc = nc
        self.ops = {e: [] for e in ENGS}
        self.chans = []
        self.final_waits = []
        self.nrec = 0

    def chan(self, name):
        c = Chan(name)
        self.chans.append(c)
        return c

    def _deps(self, op, reads, writes):
        for b in reads:
            if b.last_w is not None:
                op.deps.append((b.last_w, "RAW"))
            if b.excl:
                for r in b.readers:
                    op.deps.append((r, "RAR"))
        for b in writes:
            if b.last_w is not None:
                op.deps.append((b.last_w, "WAW"))
            for r in b.readers:
                op.deps.append((r, "WAR"))
        for b in reads:
            b.readers.append(op)
        for b in writes:
            b.last_w = op
            b.readers = []

    def op(self, eng, fn, reads=(), writes=()):
        o = Op(eng, fn)
        self._deps(o, list(reads), list(writes))
        self.ops[eng].append(o)
        self.nrec += 1
        return o

    def dma(self, eng, chan, fn, reads=(), writes=(), cont=False):
        o = Op(eng, fn)
        o.is_dma = True
        o.chan = chan
        if cont and chan.last_grp is not None:
            o.grp = chan.last_grp
            o.chan_prev = o.grp[0].chan_prev
        else:
            o.chan_prev = chan.last_grp
            o.grp = []
            chan.last_grp = o.grp
        o.grp.append(o)
        self._deps(o, list(reads), list(writes))
        self.ops[eng].append(o)
        self.nrec += 1
        return o

    def barrier(self):
        lasts = []
        for e in ENGS:
            for o in reversed(self.ops[e]):
                if not o.is_dma and o.fn is not None:
                    lasts.append(o)
                    break
        for c in self.chans:
            if c.last_grp:
                lasts.append(c.last_grp[0])
        for e in ENGS:
            o = Op(e, None)
            for d in lasts:
                o.deps.append((d, "RAW"))
            self.ops[e].append(o)

    def wait_final(self, eng, ops):
        self.final_waits.append((eng, list(ops)))

    def emit(self):
        nc = self.nc
        for e in ENGS:
            for o in self.ops[e]:
                for (d, kind) in o.deps:
                    if d.is_dma:
                        continue
                    if d.eng != o.eng or kind == "RAW":
                        d.signal = True
        for (e, ops) in self.final_waits:
            for d in ops:
                if not d.is_dma:
                    d.signal = True
        sems = []
        for e in ENGS:
            cnt = 0
            sem = None
            for o in self.ops[e]:
                if o.is_dma or o.fn is None:
                    continue
                if o.signal:
                    if sem is None or cnt >= EPOCH:
                        sem = nc.alloc_semaphore(f"s_{e}_{len(sems)}")
                        sems.append(sem)
                        cnt = 0
                    cnt += 1
                    o.done = (sem, cnt)
        for c in self.chans:
            if c.last_grp is None:
                continue
            c.sem = nc.alloc_semaphore(f"c_{c.name}")
            chain = []
            g = c.last_grp
            while g is not None:
                chain.append(g)
                g = g[0].chan_prev
            chain.reverse()
            v = 0
            for g in chain:
                v += 16 * len(g)
                for o in g:
                    o.done = (c.sem, v)
        lists = self.ops
        finals = self.final_waits

        def run(e, h):
            seen = {}

            def need(sem, val):
                k = id(sem)
                if seen.get(k, 0) < val:
                    h.wait_ge(sem, val)
                    seen[k] = val

            for o in lists[e]:
                if o.is_dma and o.chan_prev is not None and o is o.grp[0]:
                    need(*o.chan_prev[0].done)
                for (d, kind) in o.deps:
                    if d.is_dma:
                        if d.grp is o.grp:
                            continue
                        need(*d.done)
                    elif d.eng != e or kind == "RAW":
                        need(*d.done)
                if o.fn is None:
                    continue
                ins = o.fn(h)
                if o.is_dma:
                    ins.then_inc(o.chan.sem, 16)
                elif o.signal:
                    ins.then_inc(o.done[0], 1)
            for (fe, ops) in finals:
                if fe == e:
                    for d in ops:
                        need(*d.done)

        with nc.Block() as block:
            @block.tensor
            def _(h):
                run("pe", h)

            @block.scalar
            def _(h):
                run("act", h)

            @block.vector
            def _(h):
                run("dve", h)

            @block.gpsimd
            def _(h):
                run("pool", h)

            @block.sync
            def _(h):
                run("sp", h)


class Arena:
    def __init__(self, nc, nbytes):
        self.nc = nc
        left = nc._sbuf_addr_for_side("left")
        self.base = (left + 63) // 64 * 64
        nbytes = nbytes // 64 * 64
        self.slab = nc.alloc_sbuf_tensor("arena", [128, nbytes // 4], F32)
        self.size = nbytes - 64
        self.top = 0
        self.n = 0

    def alloc(self, name, shape, dtype):
        esz = 2 if dtype == BF16 else 4
        nb = esz
        for s in shape[1:]:
            nb *= s
        nb = (nb + 63) // 64 * 64
        off = self.top
        assert off + nb <= self.size, (name, off, nb, self.size)
        self.top += nb
        self.n += 1
        return self.nc.alloc_sbuf_tensor_at(f"{name}_{self.n}", list(shape), dtype, offset=self.base + off)

    def alloc_at(self, name, shape, dtype, off):
        self.n += 1
        return self.nc.alloc_sbuf_tensor_at(f"{name}_{self.n}", list(shape), dtype, offset=self.base + off)

    def mark(self):
        return self.top

    def reset(self, m):
        self.top = m


def build_program(stage=99, debug=False, ne_decl=NE):
    nc = bass.Bass("TRN2", target_bir_lowering=False)
    S = Sched(nc)

    def din(name, shape, dt=F32):
        return nc.dram_tensor(name, list(shape), dt, kind="ExternalInput").ap()

    x_d = din("x", [SEQ, D])
    c_d = din("c", [16, 128])
    ctx_d = din("ctx", [NCTX, D])
    cctx_d = din("c_ctx", [16, 128])
    adaw_d = din("ada_w", [D, 6 * D])
    adab_d = din("ada_b", [1, 6 * D])
    pmw_d = din("pre_mix_norm", [1, D])
    pomw_d = din("post_mix_norm", [1, D])
    pfw_d = din("pre_ffn_norm", [1, D])
    pofw_d = din("post_ffn_norm", [1, D])
    win_d = din("w_in", [D, DIN])
    dec_d = din("ret_decay", [1, 16])
    gnw_d = din("ret_gn_w", [1, DRET])
    convw_d = din("conv_w", [CW, DCONV])
    cvec_d = din("conv_vecs", [24, 128])
    wout_d = din("w_out", [D, D])
    rw_d = din("router_w", [D, NE])
    rb_d = din("router_b", [1, NE])
    w1_d = din("w1", [ne_decl, D, 2 * DFF])
    b1_d = din("b1", [NE * 32, 128])
    w2_d = din("w2", [ne_decl, DFF, D])
    b2_d = din("b2", [NE, D])
    cos_d = din("rope_cos", [NCTX + SEQ, 64])
    sin_d = din("rope_sin", [NCTX + SEQ, 64])
    out_d = nc.dram_tensor("out", [SEQ, D], F32, kind="ExternalOutput").ap()
    dbg = {}

    def dbg_out(name, shape, dt=F32):
        t = nc.dram_tensor("dbg_" + name, list(shape), dt, kind="ExternalOutput").ap()
        dbg[name] = t
        return t

    mod_s = nc.dram_tensor("mod_s", [2, 6 * D], F32).ap()
    yT_s = nc.dram_tensor("yT_s", [D, SEQ], BF16).ap()
    x1_s = nc.dram_tensor("x1_s", [SEQ, D], F32).ap()
    hsel_s = nc.dram_tensor("hsel_s", [NE * CAP, D], BF16).ap()
    y_s = nc.dram_tensor("y_s", [NE * CAP, D], F32).ap()

    AR = Arena(nc, nc.sbuf_bytes_remaining - 6144)

    psf = [nc.alloc_psum_tensor(f"psf{i}", [128, 512], F32) for i in range(6)]
    psb = [nc.alloc_psum_tensor(f"psb{i}", [128, 1024], BF16) for i in range(2)]
    Bpsf = [Buf(f"psf{i}", excl=True) for i in range(6)]
    Bpsb = [Buf(f"psb{i}", excl=True) for i in range(2)]
    final_ops = []
    _regs = {}

    def bound_reg(h, val):
        if val not in _regs:
            _regs[val] = h.to_reg(val)
        return _regs[val]

    ident = AR.alloc("ident", [128, 128], F32)
    identb = AR.alloc("identb", [128, 128], BF16)
    onesb = AR.alloc("onesb", [128, 128], BF16)
    ones32 = AR.alloc("ones32", [128, 128], F32)
    trib = AR.alloc("trib", [128, 128], BF16)
    iorow = AR.alloc("iorow", [128, 128], F32)
    iocol = AR.alloc("iocol", [128, 1], F32)
    b1T = AR.alloc("b1T", [128, NE * 32], F32)
    gate4 = AR.alloc("gate4", [128, NT, 4], F32)
    idx4 = AR.alloc("idx4", [128, NT * 4], I32)
    gatesA = AR.alloc("gatesA", [128, NT, NE], F32)
    Bc = Buf("consts")
    Bb1T = Buf("b1T")
    Bgate4 = Buf("gate4")
    Bidx4 = Buf("idx4")
    BgatesA = Buf("gatesA")

    S.op("pool", lambda h: h.memset(ident[:], 0.0), writes=[Bc])
    S.op("pool", lambda h: h.affine_select(out=ident[:], in_=ident[:], pattern=[[-1, 128]], compare_op=ALU.not_equal,
                                            fill=1.0, base=0, channel_multiplier=1), reads=[Bc], writes=[Bc])
    S.op("pool", lambda h: h.memset(ones32[:], 1.0), writes=[Bc])
    S.op("pool", lambda h: h.affine_select(out=iorow[:], in_=ones32[:], pattern=[[1, 128]], compare_op=ALU.is_gt,
                                            fill=0.0, base=0, channel_multiplier=-1), reads=[Bc], writes=[Bc])
    S.op("dve", lambda h: h.tensor_copy(out=trib[:], in_=iorow[:]), reads=[Bc], writes=[Bc])
    S.op("dve", lambda h: h.tensor_copy(out=identb[:], in_=ident[:]), reads=[Bc], writes=[Bc])
    S.op("dve", lambda h: h.tensor_copy(out=onesb[:], in_=ones32[:]), reads=[Bc], writes=[Bc])
    ioi = AR.alloc("ioi", [128, 128], I32)
    S.op("pool", lambda h: h.iota(ioi[:], pattern=[[1, 128]], base=0, channel_multiplier=0), writes=[Bc])
    S.op("dve", lambda h: h.tensor_copy(out=iorow[:], in_=ioi[:]), reads=[Bc], writes=[Bc])
    S.op("pool", lambda h: h.iota(ioi[:, 0:1], pattern=[[0, 1]], base=0, channel_multiplier=1), reads=[Bc], writes=[Bc])
    S.op("dve", lambda h: h.tensor_copy(out=iocol[:], in_=ioi[:, 0:1]), reads=[Bc], writes=[Bc])

    ch_small = S.chan("small")
    persist_mark = AR.mark()

    def transpose_rows(rows_ap, nrows, out_ap, bank, Bbank, reads, writes, evac="act"):
        S.op("pe", lambda h: h.transpose(out=psf[bank][:, 0:nrows], in_=rows_ap, identity=ident[0:nrows, 0:nrows]),
             reads=reads + [Bc], writes=[Bbank])
        if evac == "act":
            S.op("act", lambda h: h.copy(out=out_ap, in_=psf[bank][:, 0:nrows]), reads=[Bbank], writes=writes)
        else:
            S.op("dve", lambda h: h.tensor_copy(out=out_ap, in_=psf[bank][:, 0:nrows]), reads=[Bbank], writes=writes)

    m0 = AR.mark()
    rows32 = AR.alloc("rows32", [32, 128], F32)
    cT = AR.alloc("cT", [128, 32], F32)
    sil = AR.alloc("sil", [128, 32], F32)
    adab = AR.alloc("adab", [2, 6 * D], F32)
    modsb = AR.alloc("modsb", [2, 6 * D], F32)
    adaw = [AR.alloc(f"adaw{i}", [128, KC, 512], F32) for i in range(2)]
    b1rows = AR.alloc("b1rows", [128, 8, 128], F32)
    Brows32, BcT, Bsil, Badab, Bmodsb, Bb1rows = Buf("rows32"), Buf("cT"), Buf("sil"), Buf("adab"), Buf("modsb"), Buf("b1rows")
    Badaw = [Buf("adaw0"), Buf("adaw1")]
    ch_adaw = [S.chan("adaw0"), S.chan("adaw1")]

    S.dma("sp", ch_small, lambda h: h.dma_start(out=rows32[0:16, :], in_=c_d), writes=[Brows32])
    S.dma("sp", ch_small, lambda h: h.dma_start(out=rows32[16:32, :], in_=cctx_d), writes=[Brows32], cont=True)
    S.dma("sp", ch_small, lambda h: h.dma_start(out=adab[:], in_=adab_d.partition_broadcast(2)), writes=[Badab], cont=True)
    S.dma("sp", ch_small, lambda h: h.dma_start(out=b1rows[:], in_=b1_d.rearrange("(t p) f -> p t f", p=128)), writes=[Bb1rows], cont=True)
    transpose_rows(rows32[:], 32, cT[:], 0, Bpsf[0], [Brows32], [BcT])
    S.op("act", lambda h: h.activation(out=sil[:], in_=cT[:], func=AF.Silu), reads=[BcT], writes=[Bsil])
    for t in range(8):
        S.op("pe", lambda h, t=t: h.transpose(out=psf[1 + (t % 2)][:, 0:128], in_=b1rows[:, t, :], identity=ident[:]),
             reads=[Bb1rows, Bc], writes=[Bpsf[1 + (t % 2)]])
        S.op("act", lambda h, t=t: h.copy(out=b1T[:, t * 128:(t + 1) * 128], in_=psf[1 + (t % 2)][:, 0:128]),
             reads=[Bpsf[1 + (t % 2)]], writes=[Bb1T])
    sil2 = AR.alloc("sil2", [128, KC, 2], F32)
    Bsil2 = Buf("sil2")
    S.op("dve", lambda h: h.tensor_copy(out=sil2[:, :, 0], in_=sil[:, 0:16]), reads=[Bsil], writes=[Bsil2])
    S.op("dve", lambda h: h.tensor_copy(out=sil2[:, :, 1], in_=sil[:, 16:32]), reads=[Bsil], writes=[Bsil2])
    adaw_v = adaw_d.rearrange("(k p) n -> p k n", p=128)
    for j in range(24):
        bi = j % 2
        S.dma("sp", ch_adaw[bi], lambda h, j=j, bi=bi: h.dma_start(out=adaw[bi][:], in_=adaw_v[:, :, j * 512:(j + 1) * 512]),
              writes=[Badaw[bi]])
        bank = 2 + (j % 2)
        for kc in range(KC):
            S.op("pe", lambda h, kc=kc, bi=bi, bank=bank: h.matmul(psf[bank][0:2, :], lhsT=sil2[:, kc, :], rhs=adaw[bi][:, kc, :],
                                                                     start=(kc == 0), stop=(kc == KC - 1)),
                 reads=[Bsil2, Badaw[bi]], writes=[Bpsf[bank]])
        S.op("dve", lambda h, j=j, bank=bank: h.tensor_tensor(out=modsb[:, j * 512:(j + 1) * 512], in0=psf[bank][0:2, :],
                                                               in1=adab[:, j * 512:(j + 1) * 512], op=ALU.add),
             reads=[Bpsf[bank], Badab], writes=[Bmodsb])
    Bmod_s = Buf("mod_s")
    S.dma("sp", ch_small, lambda h: h.dma_start(out=mod_s, in_=modsb[:]), reads=[Bmodsb], writes=[Bmod_s])
    if debug:
        d_mod = dbg_out("mod", [2, 6 * D])
        final_ops.append(S.dma("sp", S.chan("dbgmod"), lambda h: h.dma_start(out=d_mod, in_=modsb[:]), reads=[Bmodsb]))
    S.barrier()
    AR.reset(m0)
    if stage <= 0:
        return finish(nc, S, final_ops), dbg

    def load_mod_bcast(dst, row, g, chan, Bdst):
        return S.dma("sp", chan, lambda h: h.dma_start(out=dst[:], in_=mod_s[row:row + 1, g * D:(g + 1) * D].partition_broadcast(128)),
                     reads=[Bmod_s], writes=[Bdst])

    def load_row_bcast(dst, row_ap, chan, Bdst, cont=False):
        return S.dma("sp", chan, lambda h: h.dma_start(out=dst[:], in_=row_ap.partition_broadcast(128)), writes=[Bdst], cont=cont)

    hT = AR.alloc("hT", [128, KC, SEQ], BF16)
    hcT_off = AR.mark()
    hcT = AR.alloc("hcT", [128, KC, NCTX], BF16)
    BhT = [Buf(f"hT{i}") for i in range(NT)]
    BhcT = Buf("hcT")
    m1 = AR.mark()
    A1 = AR.alloc("A1", [128, D], F32)
    S1 = AR.alloc("S1", [128, D], F32)
    A1c = AR.alloc("A1c", [128, D], F32)
    S1c = AR.alloc("S1c", [128, D], F32)
    wtmp = AR.alloc("wtmp", [128, D], F32)
    xb = [AR.alloc(f"xb{i}", [128, D], F32) for i in range(2)]
    t32 = AR.alloc("t32", [128, D], F32)
    hb = [AR.alloc(f"hb{i}", [128, D], BF16) for i in range(2)]
    junkb = AR.alloc("junkb", [128, D], BF16)
    stat = AR.alloc("stat", [128, 8], F32)
    BA1, BS1, BA1c, BS1c, Bwtmp, Bt32, Bjunk, Bstat = (Buf(n) for n in ["A1", "S1", "A1c", "S1c", "wtmp", "t32", "junk", "stat"])
    Bxb = [Buf("xb0"), Buf("xb1")]
    Bhb = [Buf("hb0"), Buf("hb1")]
    ch_x = [S.chan("x0"), S.chan("x1")]
    ch_v = S.chan("vecs")

    load_row_bcast(wtmp, pmw_d, ch_v, Bwtmp)
    load_mod_bcast(A1, 0, 1, ch_v, BA1)
    load_mod_bcast(S1, 0, 0, ch_v, BS1)
    load_mod_bcast(A1c, 1, 1, ch_v, BA1c)
    load_mod_bcast(S1c, 1, 0, ch_v, BS1c)
    S.op("dve", lambda h: h.scalar_tensor_tensor(out=A1[:], in0=A1[:], scalar=1.0, in1=wtmp[:], op0=ALU.add, op1=ALU.mult),
         reads=[BA1, Bwtmp], writes=[BA1])
    S.op("dve", lambda h: h.scalar_tensor_tensor(out=A1c[:], in0=A1c[:], scalar=1.0, in1=wtmp[:], op0=ALU.add, op1=ALU.mult),
         reads=[BA1c, Bwtmp], writes=[BA1c])

    def norm_mod_tile(src_ap_dram, xbuf, Bx, chan, Avec, BAv, Svec, BSv, hbuf, Bh, sidx):
        S.dma("sp", chan, lambda h: h.dma_start(out=xbuf[:], in_=src_ap_dram), writes=[Bx])
        S.op("act", lambda h: h.activation(out=junkb[:], in_=xbuf[:], func=AF.Square, accum_out=stat[:, sidx:sidx + 1]),
             reads=[Bx], writes=[Bjunk, Bstat])
        S.op("act", lambda h: h.activation(out=stat[:, sidx:sidx + 1], in_=stat[:, sidx:sidx + 1], func=AF.Sqrt, scale=1.0 / D, bias=EPS),
             reads=[Bstat], writes=[Bstat])
        S.op("dve", lambda h: h.reciprocal(out=stat[:, sidx:sidx + 1], in_=stat[:, sidx:sidx + 1]), reads=[Bstat], writes=[Bstat])
        S.op("dve", lambda h: h.scalar_tensor_tensor(out=t32[:], in0=xbuf[:], scalar=stat[:, sidx:sidx + 1], in1=Avec[:],
                                                     op0=ALU.mult, op1=ALU.mult), reads=[Bx, Bstat, BAv], writes=[Bt32])
        S.op("pool", lambda h: h.tensor_tensor(out=hbuf[:], in0=t32[:], in1=Svec[:], op=ALU.add), reads=[Bt32, BSv], writes=[Bh])

    def transpose_tile_bf16(hbuf, Bh, dstT, col0, Bdst):
        for half in range(2):
            pb = half
            for q in range(8):
                kc = half * 8 + q
                S.op("pe", lambda h, kc=kc, q=q, pb=pb: h.transpose(out=psb[pb][:, q * 128:(q + 1) * 128], in_=hbuf[:, kc * 128:(kc + 1) * 128],
                                                                       identity=identb[:]),
                     reads=[Bh, Bc], writes=[Bpsb[pb]])
            eng = "act" if half == 0 else "dve"
            if eng == "act":
                S.op("act", lambda h, half=half, pb=pb: h.copy(out=dstT[:, half * 8:(half + 1) * 8, col0:col0 + 128],
                                                                 in_=psb[pb][:, :].rearrange("p (q t) -> p q t", q=8)),
                     reads=[Bpsb[pb]], writes=[Bdst])
            else:
                S.op("dve", lambda h, half=half, pb=pb: h.tensor_copy(out=dstT[:, half * 8:(half + 1) * 8, col0:col0 + 128],
                                                                        in_=psb[pb][:, :].rearrange("p (q t) -> p q t", q=8)),
                     reads=[Bpsb[pb]], writes=[Bdst])

    for i in range(2):
        bi = i % 2
        norm_mod_tile(ctx_d[i * 128:(i + 1) * 128, :], xb[bi], Bxb[bi], ch_x[bi], A1c, BA1c, S1c, BS1c, hb[bi], Bhb[bi], i % 8)
        transpose_tile_bf16(hb[bi], Bhb[bi], hcT, i * 128, BhcT)
    for i in range(NT):
        bi = i % 2
        norm_mod_tile(x_d[i * 128:(i + 1) * 128, :], xb[bi], Bxb[bi], ch_x[bi], A1, BA1, S1, BS1, hb[bi], Bhb[bi], i % 8)
        transpose_tile_bf16(hb[bi], Bhb[bi], hT, i * 128, BhT[i])
    if debug:
        d_hT = dbg_out("hT", [128, KC, SEQ], BF16)
        final_ops.append(S.dma("sp", S.chan("dbghT"), lambda h: h.dma_start(out=d_hT, in_=hT[:]), reads=BhT))
    S.barrier()
    AR.reset(m1)
    if stage <= 1:
        return finish(nc, S, final_ops), dbg

    ByT_s = Buf("yT_s")
    win_v = win_d.rearrange("(k p) n -> p k n", p=128)

    m2 = AR.mark()
    decb = AR.alloc("decb", [128, 16], F32)
    Mh = AR.alloc("Mh", [128, H, 128], F32)
    dcol = AR.alloc("dcol", [128, H, 6], F32)
    wctx = AR.alloc("wctx", [128, H, 4], F32)
    Bdecb, BMh, Bdcol, Bwctx = Buf("decb"), Buf("Mh"), Buf("dcol"), Buf("wctx")
    tA = AR.alloc("tA", [128, 128], F32)
    tB = AR.alloc("tB", [128, 128], F32)
    tC = AR.alloc("tC", [128, 128], F32)
    tD = AR.alloc("tD", [128, 128], F32)
    cols = AR.alloc("cols", [128, 8], F32)
    BtA, BtB, BtC, BtD, Bcols = Buf("tA"), Buf("tB"), Buf("tC"), Buf("tD"), Buf("cols")
    S.dma("sp", ch_v, lambda h: h.dma_start(out=decb[:], in_=dec_d.partition_broadcast(128)), writes=[Bdecb])
    S.op("act", lambda h: h.activation(out=decb[:], in_=decb[:], func=AF.Exp, scale=-1.0), reads=[Bdecb], writes=[Bdecb])
    S.op("act", lambda h: h.activation(out=decb[:], in_=decb[:], func=AF.Ln, bias=1.0), reads=[Bdecb], writes=[Bdecb])
    S.op("dve", lambda h: h.tensor_scalar(out=decb[:], in0=decb[:], scalar1=-1.0, scalar2=None, op0=ALU.mult), reads=[Bdecb], writes=[Bdecb])
    S.op("dve", lambda h: h.tensor_scalar(out=tA[:], in0=iorow[:], scalar1=iocol[:, 0:1], scalar2=0.0, op0=ALU.subtract, op1=ALU.max),
         reads=[Bc], writes=[BtA])
    S.op("dve", lambda h: h.tensor_scalar(out=tB[:], in0=iorow[:], scalar1=iocol[:, 0:1], scalar2=-1.0, op0=ALU.subtract, op1=ALU.mult),
         reads=[Bc], writes=[BtB])
    S.op("dve", lambda h: h.tensor_scalar(out=tB[:], in0=tB[:], scalar1=0.0, scalar2=None, op0=ALU.max), reads=[BtB], writes=[BtB])
    S.op("dve", lambda h: h.tensor_scalar(out=tC[:], in0=iorow[:], scalar1=iocol[:, 0:1], scalar2=None, op0=ALU.is_ge), reads=[Bc], writes=[BtC])
    S.op("dve", lambda h: h.tensor_scalar(out=tD[:], in0=iorow[:], scalar1=iocol[:, 0:1], scalar2=None, op0=ALU.is_le), reads=[Bc], writes=[BtD])
    for ci, (mul, add) in enumerate([(1.0, 1.0), (-1.0, 128.0), (-1.0, 127.0), (1.0, 0.0), (0.0, 128.0), (-1.0, 255.0), (-1.0, 127.0), (1.0, 128.0)]):
        S.op("dve", lambda h, ci=ci, mul=mul, add=add: h.tensor_scalar(out=cols[:, ci:ci + 1], in0=iocol[:, 0:1], scalar1=mul, scalar2=add,
                                                                      op0=ALU.mult, op1=ALU.add), reads=[Bc], writes=[Bcols])
    ex1 = AR.alloc("ex1", [128, 128], F32)
    ex2 = AR.alloc("ex2", [128, 128], F32)
    Bex1, Bex2 = Buf("ex1"), Buf("ex2")
    for hh in range(H):
        lf = decb[:, hh:hh + 1]
        lb = decb[:, 8 + hh:9 + hh]
        S.op("act", lambda h, lf=lf: h.activation(out=ex1[:], in_=tA[:], func=AF.Exp, scale=lf), reads=[BtA, Bdecb], writes=[Bex1])
        S.op("act", lambda h, lb=lb: h.activation(out=ex2[:], in_=tB[:], func=AF.Exp, scale=lb), reads=[BtB, Bdecb], writes=[Bex2])
        S.op("dve", lambda h: h.tensor_tensor(out=ex1[:], in0=ex1[:], in1=tC[:], op=ALU.mult), reads=[Bex1, BtC], writes=[Bex1])
        S.op("dve", lambda h: h.tensor_tensor(out=ex2[:], in0=ex2[:], in1=tD[:], op=ALU.mult), reads=[Bex2, BtD], writes=[Bex2])
        S.op("dve", lambda h, hh=hh: h.tensor_tensor(out=Mh[:, hh, :], in0=ex1[:], in1=ex2[:], op=ALU.add), reads=[Bex1, Bex2], writes=[BMh])
        for (dst, ci, lg) in [(0, 0, lf), (1, 1, lb), (2, 2, lf), (3, 3, lb), (4, 4, lf), (5, 4, lb)]:
            S.op("act", lambda h, hh=hh, dst=dst, ci=ci, lg=lg: h.activation(out=dcol[:, hh, dst:dst + 1], in_=cols[:, ci:ci + 1], func=AF.Exp, scale=lg),
                 reads=[Bcols, Bdecb], writes=[Bdcol])
        for (dst, ci, lg) in [(0, 5, lf), (1, 6, lf), (2, 3, lb), (3, 7, lb)]:
            S.op("act", lambda h, hh=hh, dst=dst, ci=ci, lg=lg: h.activation(out=wctx[:, hh, dst:dst + 1], in_=cols[:, ci:ci + 1], func=AF.Exp, scale=lg),
                 reads=[Bcols, Bdecb], writes=[Bwctx])

    cosL = AR.alloc("cosL", [128, NT, 64], F32)
    sinL = AR.alloc("sinL", [128, NT, 64], F32)
    cosT = cosL[:, :, :].unsqueeze(2).to_broadcast([128, NT, 2, 64])
    sinT = sinL[:, :, :].unsqueeze(2).to_broadcast([128, NT, 2, 64])
    cosC = AR.alloc("cosC", [128, 2, 64], F32)
    sinC = AR.alloc("sinC", [128, 2, 64], F32)
    Brope = Buf("rope")
    cos_lat = cos_d[NCTX:NCTX + SEQ, :].rearrange("(t p) f -> p t f", p=128)
    sin_lat = sin_d[NCTX:NCTX + SEQ, :].rearrange("(t p) f -> p t f", p=128)
    S.dma("sp", ch_v, lambda h: h.dma_start(out=cosL[:], in_=cos_lat), writes=[Brope])
    S.dma("sp", ch_v, lambda h: h.dma_start(out=sinL[:], in_=sin_lat), writes=[Brope], cont=True)
    S.dma("sp", ch_v, lambda h: h.dma_start(out=cosC[:], in_=cos_d[0:NCTX, :].rearrange("(t p) f -> p t f", p=128)), writes=[Brope], cont=True)
    S.dma("sp", ch_v, lambda h: h.dma_start(out=sinC[:], in_=sin_d[0:NCTX, :].rearrange("(t p) f -> p t f", p=128)), writes=[Brope], cont=True)
    gnwb = AR.alloc("gnwb", [128, DRET], F32)
    Bgnwb = Buf("gnwb")
    load_row_bcast(gnwb, gnw_d, ch_v, Bgnwb)

    R0 = AR.alloc("R0", [128, H, 2, 128], F32)
    BR0 = Buf("R0")
    m2b = AR.mark()
    wkv = [AR.alloc(f"wkv{i}", [128, KC, 256], BF16) for i in range(2)]
    Bwkv = [Buf("wkv0"), Buf("wkv1")]
    ch_wkv = [S.chan("wkv0"), S.chan("wkv1")]
    kc32 = AR.alloc("kc32", [128, 2, 128], F32)
    kcr = AR.alloc("kcr", [128, 2, 128], BF16)
    vwf = AR.alloc("vwf", [128, 2, 128], BF16)
    vwb = AR.alloc("vwb", [128, 2, 128], BF16)
    rt1 = AR.alloc("rt1", [128, 2, 64], F32)
    rt2 = AR.alloc("rt2", [128, 2, 64], F32)
    Bkc32, Bkcr, Bvwf, Bvwb, Brt1, Brt2 = (Buf(n) for n in ["kc32", "kcr", "vwf", "vwb", "rt1", "rt2"])
    for hh in range(H):
        bi = hh % 2
        S.dma("pool", ch_wkv[bi], lambda h, hh=hh, bi=bi: h.dma_start(out=wkv[bi][:, :, 0:128], in_=win_v[:, :, DRET + hh * 128:DRET + (hh + 1) * 128]),
              writes=[Bwkv[bi]])
        S.dma("pool", ch_wkv[bi], lambda h, hh=hh, bi=bi: h.dma_start(out=wkv[bi][:, :, 128:256], in_=win_v[:, :, 2 * DRET + hh * 128:2 * DRET + (hh + 1) * 128]),
              writes=[Bwkv[bi]], cont=True)
        for t in range(2):
            bank = t
            for kc in range(KC):
                S.op("pe", lambda h, kc=kc, t=t, bi=bi, bank=bank: h.matmul(psf[bank][:, 0:256], lhsT=hcT[:, kc, t * 128:(t + 1) * 128], rhs=wkv[bi][:, kc, :],
                                                                             start=(kc == 0), stop=(kc == KC - 1)),
                     reads=[BhcT, Bwkv[bi]], writes=[Bpsf[bank]])
            S.op("act", lambda h, t=t, bank=bank: h.copy(out=kc32[:, t, :], in_=psf[bank][:, 0:128]), reads=[Bpsf[bank]], writes=[Bkc32])
            S.op("dve", lambda h, t=t, bank=bank, hh=hh: h.tensor_scalar(out=vwf[:, t, :], in0=psf[bank][:, 128:256], scalar1=wctx[:, hh, t:t + 1], scalar2=None, op0=ALU.mult),
                 reads=[Bpsf[bank], Bwctx], writes=[Bvwf])
            S.op("dve", lambda h, t=t, bank=bank, hh=hh: h.tensor_scalar(out=vwb[:, t, :], in0=psf[bank][:, 128:256], scalar1=wctx[:, hh, 2 + t:3 + t], scalar2=None, op0=ALU.mult),
                 reads=[Bpsf[bank], Bwctx], writes=[Bvwb])
        k1 = kc32[:, :, 0:64]
        k2 = kc32[:, :, 64:128]
        S.op("dve", lambda h: h.tensor_tensor(out=rt1[:], in0=k1, in1=cosC[:], op=ALU.mult), reads=[Bkc32, Brope], writes=[Brt1])
        S.op("pool", lambda h: h.tensor_tensor(out=rt2[:], in0=k2, in1=sinC[:], op=ALU.mult), reads=[Bkc32, Brope], writes=[Brt2])
        S.op("dve", lambda h: h.tensor_tensor(out=kcr[:, :, 0:64], in0=rt1[:], in1=rt2[:], op=ALU.subtract), reads=[Brt1, Brt2], writes=[Bkcr])
        S.op("dve", lambda h: h.tensor_tensor(out=rt1[:], in0=k1, in1=sinC[:], op=ALU.mult), reads=[Bkc32, Brope, Bkcr], writes=[Brt1])
        S.op("pool", lambda h: h.tensor_tensor(out=rt2[:], in0=k2, in1=cosC[:], op=ALU.mult), reads=[Bkc32, Brope, Bkcr], writes=[Brt2])
        S.op("dve", lambda h: h.tensor_tensor(out=kcr[:, :, 64:128], in0=rt1[:], in1=rt2[:], op=ALU.add), reads=[Brt1, Brt2], writes=[Bkcr])
        for di, vw, Bvw in [(0, vwf, Bvwf), (1, vwb, Bvwb)]:
            bank = 2 + di
            for t in range(2):
                S.op("pe", lambda h, t=t, bank=bank, vw=vw: h.matmul(psf[bank][:, 0:128], lhsT=kcr[:, t, :], rhs=vw[:, t, :], start=(t == 0), stop=(t == 1)),
                     reads=[Bkcr, Bvw], writes=[Bpsf[bank]])
            S.op("act", lambda h, hh=hh, di=di, bank=bank: h.copy(out=R0[:, hh, di, :], in_=psf[bank][:, 0:128]), reads=[Bpsf[bank]], writes=[BR0])
    if debug:
        d_R0 = dbg_out("R0", [128, H, 2, 128])
        final_ops.append(S.dma("sp", S.chan("dbgR0"), lambda h: h.dma_start(out=d_R0, in_=R0[:]), reads=[BR0]))
    S.barrier()
    AR.reset(m2b)
    if stage <= 2:
        return finish(nc, S, final_ops), dbg

    m2c = AR.mark()
    wq_off = AR.mark()
    wq1 = AR.alloc("wq", [128, KC, 512], BF16)
    wq = [wq1, wq1]
    Bwq1 = Buf("wq")
    Bwq = [Bwq1, Bwq1]
    ch_wq1 = S.chan("wq")
    ch_wq = [ch_wq1, ch_wq1]
    qT = AR.alloc_at("qT", [128, SEQ], BF16, wq_off)
    qdfT = AR.alloc_at("qdfT", [128, SEQ], BF16, wq_off + 4096)
    qdbT = AR.alloc_at("qdbT", [128, SEQ], BF16, wq_off + 8192)
    kT = AR.alloc_at("kT", [128, SEQ], BF16, wq_off + 12288)
    BqT = BqdfT = BqdbT = BkT = Bwq1
    qk_off = AR.mark()
    qk32 = AR.alloc("qk32", [128, NT, 2, 2, 64], F32)
    Bqk32 = Buf("qk32")
    o32 = AR.alloc_at("o32", [128, NT, 128], F32, qk_off)
    ytok = AR.alloc_at("ytok", [128, NT, 128], BF16, qk_off + 8192)
    yTh = AR.alloc_at("yTh", [128, SEQ], BF16, qk_off + 12288)
    Bo32 = Bytok = ByTh = Bqk32
    ta_off = AR.mark()
    ta = AR.alloc("ta", [128, NT, 2, 64], F32)
    Bta = Buf("ta")
    Sfb = AR.alloc_at("Sfb", [128, NT, 128], BF16, ta_off)
    Sbb = AR.alloc_at("Sbb", [128, NT, 128], BF16, ta_off + 4096)
    BSfb = BSbb = Bta
    tb = AR.alloc("tb", [128, NT, 2, 64], F32)
    rotb = AR.alloc("rotb", [128, NT, 2, 2, 64], BF16)
    qdf = AR.alloc("qdf", [128, NT, 2, 64], BF16)
    qdb = AR.alloc("qdb", [128, NT, 2, 64], BF16)
    kdf = AR.alloc("kdf", [128, NT, 2, 64], BF16)
    kdb = AR.alloc("kdb", [128, NT, 2, 64], BF16)
    vtok = AR.alloc("vtok", [128, NT, 128], BF16)
    sg = AR.alloc_at("sg", [128, NT, 128], F32, hcT_off)
    Rf = AR.alloc("Rf", [128, 128], F32)
    Rb = AR.alloc("Rb", [128, 128], F32)
    SM = [AR.alloc(f"SM{i}", [128, 128], BF16) for i in range(2)]
    bnst = AR.alloc("bnst", [128, NT, 6], F32)
    mv = AR.alloc("mv", [128, NT, 2], F32)
    rstd = AR.alloc("rstd", [128, NT], F32)
    (Btb, Brotb, Bqdf, Bqdb, Bkdf, Bkdb, Bvtok, Bsg, BRf, BRb, Bbnst, Bmv, Brstd) = (
        Buf(n) for n in ["tb", "rotb", "qdf", "qdb", "kdf", "kdb", "vtok", "sg", "Rf", "Rb", "bnst", "mv", "rstd"])
    BSM = [Buf("SM0"), Buf("SM1")]
    ch_yT = S.chan("yTout")

    for hh in range(H):
        bi = hh % 2
        for seg in range(4):
            S.dma("pool", ch_wq[bi], lambda h, hh=hh, bi=bi, seg=seg: h.dma_start(out=wq[bi][:, :, seg * 128:(seg + 1) * 128],
                                                                                    in_=win_v[:, :, seg * DRET + hh * 128:seg * DRET + (hh + 1) * 128]),
                  writes=[Bwq[bi]], cont=(seg > 0))
        for i in range(NT):
            bank = i % 4
            for kc in range(KC):
                S.op("pe", lambda h, kc=kc, i=i, bi=bi, bank=bank: h.matmul(psf[bank][:, :], lhsT=hT[:, kc, i * 128:(i + 1) * 128], rhs=wq[bi][:, kc, :],
                                                                             start=(kc == 0), stop=(kc == KC - 1)),
                     reads=[BhT[i], Bwq[bi]], writes=[Bpsf[bank]])
            S.op("act", lambda h, i=i, bank=bank: h.copy(out=qk32[:, i, :, :, :], in_=psf[bank][:, 0:256].rearrange("p (a b c) -> p a b c", a=2, b=2)),
                 reads=[Bpsf[bank]], writes=[Bqk32])
            S.op("act", lambda h, i=i, bank=bank: h.copy(out=vtok[:, i, :], in_=psf[bank][:, 256:384]), reads=[Bpsf[bank]], writes=[Bvtok])
            S.op("act", lambda h, i=i, bank=bank: h.activation(out=sg[:, i, :], in_=psf[bank][:, 384:512], func=AF.Silu), reads=[Bpsf[bank]], writes=[Bsg])
        X1 = qk32[:, :, :, 0, :]
        X2 = qk32[:, :, :, 1, :]
        S.op("dve", lambda h: h.tensor_tensor(out=ta[:], in0=X1, in1=cosT, op=ALU.mult), reads=[Bqk32, Brope], writes=[Bta])
        S.op("pool", lambda h: h.tensor_tensor(out=tb[:], in0=X2, in1=sinT, op=ALU.mult), reads=[Bqk32, Brope], writes=[Btb])
        S.op("dve", lambda h: h.tensor_tensor(out=ta[:], in0=ta[:], in1=tb[:], op=ALU.subtract), reads=[Bta, Btb], writes=[Bta])
        S.op("pool", lambda h: h.tensor_tensor(out=tb[:], in0=X1, in1=sinT, op=ALU.mult), reads=[Bqk32, Brope, Bta], writes=[Btb])
        S.op("dve", lambda h: h.tensor_tensor(out=X1, in0=X2, in1=cosT, op=ALU.mult), reads=[Bqk32, Brope, Btb], writes=[Bqk32])
        S.op("dve", lambda h: h.tensor_tensor(out=tb[:], in0=tb[:], in1=X1, op=ALU.add), reads=[Btb, Bqk32], writes=[Btb])
        S.op("act", lambda h: h.mul(out=rotb[:, :, 0, 0, :], in_=ta[:, :, 0, :], mul=QSCALE), reads=[Bta], writes=[Brotb])
        S.op("act", lambda h: h.copy(out=rotb[:, :, 1, 0, :], in_=ta[:, :, 1, :]), reads=[Bta], writes=[Brotb])
        S.op("act", lambda h: h.mul(out=rotb[:, :, 0, 1, :], in_=tb[:, :, 0, :], mul=QSCALE), reads=[Btb], writes=[Brotb])
        S.op("act", lambda h: h.copy(out=rotb[:, :, 1, 1, :], in_=tb[:, :, 1, :]), reads=[Btb], writes=[Brotb])
        for (dst, Bd, src_i, col, sc) in [(qdf, Bqdf, 0, 0, QSCALE), (qdb, Bqdb, 0, 1, QSCALE), (kdf, Bkdf, 1, 2, 1.0), (kdb, Bkdb, 1, 3, 1.0)]:
            S.op("dve", lambda h, dst=dst, src_i=src_i, col=col, hh=hh, sc=sc: h.tensor_scalar(out=dst[:, :, 0, :], in0=ta[:, :, src_i, :], scalar1=dcol[:, hh, col:col + 1],
                                                                                              scalar2=sc, op0=ALU.mult, op1=ALU.mult), reads=[Bta, Bdcol], writes=[Bd])
            S.op("pool", lambda h, dst=dst, src_i=src_i, col=col, hh=hh, sc=sc: h.tensor_scalar(out=dst[:, :, 1, :], in0=tb[:, :, src_i, :], scalar1=dcol[:, hh, col:col + 1],
                                                                                               scalar2=sc, op0=ALU.mult, op1=ALU.mult), reads=[Btb, Bdcol], writes=[Bd])
        srcs = [(lambda i: rotb[:, i, 0, :, :].rearrange("p a b -> p (a b)"), Brotb, qT, BqT),
                (lambda i: qdf[:, i, :, :].rearrange("p a b -> p (a b)"), Bqdf, qdfT, BqdfT),
                (lambda i: qdb[:, i, :, :].rearrange("p a b -> p (a b)"), Bqdb, qdbT, BqdbT),
                (lambda i: rotb[:, i, 1, :, :].rearrange("p a b -> p (a b)"), Brotb, kT, BkT)]
        cnt = 0
        for (srcf, Bsrc, dstT, BdstT) in srcs:
            for half in range(2):
                pb = cnt % 2
                cnt += 1
                for q in range(8):
                    i = half * 8 + q
                    S.op("pe", lambda h, i=i, q=q, pb=pb, srcf=srcf: h.transpose(out=psb[pb][:, q * 128:(q + 1) * 128], in_=srcf(i), identity=identb[:]),
                         reads=[Bsrc, Bc], writes=[Bpsb[pb]])
                if pb == 0:
                    S.op("act", lambda h, half=half, pb=pb, dstT=dstT: h.copy(out=dstT[:, half * 1024:(half + 1) * 1024], in_=psb[pb][:, :]),
                         reads=[Bpsb[pb]], writes=[BdstT])
                else:
                    S.op("dve", lambda h, half=half, pb=pb, dstT=dstT: h.tensor_copy(out=dstT[:, half * 1024:(half + 1) * 1024], in_=psb[pb][:, :]),
                         reads=[Bpsb[pb]], writes=[BdstT])
        S.op("dve", lambda h, hh=hh: h.tensor_copy(out=Rf[:], in_=R0[:, hh, 0, :]), reads=[BR0], writes=[BRf])
        S.op("dve", lambda h, hh=hh: h.tensor_copy(out=Rb[:], in_=R0[:, hh, 1, :]), reads=[BR0], writes=[BRb])
        S.op("act", lambda h: h.copy(out=Sfb[:, 0, :], in_=Rf[:]), reads=[BRf], writes=[BSfb])
        S.op("act", lambda h: h.copy(out=Sbb[:, NT - 1, :], in_=Rb[:]), reads=[BRb], writes=[BSbb])
        for step in range(NT - 1):
            i_f = step
            i_b = NT - 1 - step
            S.op("pe", lambda h, i=i_f: h.matmul(psf[4][:, 0:128], lhsT=kdf[:, i, :, :].rearrange("p a b -> p (a b)"), rhs=vtok[:, i, :], start=True, stop=True),
                 reads=[Bkdf, Bvtok], writes=[Bpsf[4]])
            S.op("dve", lambda h, hh=hh: h.scalar_tensor_tensor(out=Rf[:], in0=Rf[:], scalar=dcol[:, hh, 4:5], in1=psf[4][:, 0:128], op0=ALU.mult, op1=ALU.add),
                 reads=[BRf, Bdcol, Bpsf[4]], writes=[BRf])
            S.op("act", lambda h, i=i_f: h.copy(out=Sfb[:, i + 1, :], in_=Rf[:]), reads=[BRf], writes=[BSfb])
            S.op("pe", lambda h, i=i_b: h.matmul(psf[5][:, 0:128], lhsT=kdb[:, i, :, :].rearrange("p a b -> p (a b)"), rhs=vtok[:, i, :], start=True, stop=True),
                 reads=[Bkdb, Bvtok], writes=[Bpsf[5]])
            S.op("dve", lambda h, hh=hh: h.scalar_tensor_tensor(out=Rb[:], in0=Rb[:], scalar=dcol[:, hh, 5:6], in1=psf[5][:, 0:128], op0=ALU.mult, op1=ALU.add),
                 reads=[BRb, Bdcol, Bpsf[5]], writes=[BRb])
            S.op("act", lambda h, i=i_b: h.copy(out=Sbb[:, i - 1, :], in_=Rb[:]), reads=[BRb], writes=[BSbb])
        for i in range(NT):
            sb_ = i % 2
            bs = i % 2
            bo = 2 + (i % 2)
            cs = slice(i * 128, (i + 1) * 128)
            S.op("pe", lambda h, cs=cs, bs=bs: h.matmul(psf[bs][:, 0:128], lhsT=kT[:, cs], rhs=qT[:, cs], start=True, stop=True),
                 reads=[BkT, BqT], writes=[Bpsf[bs]])
            S.op("dve", lambda h, bs=bs, sb_=sb_, hh=hh: h.tensor_tensor(out=SM[sb_][:], in0=psf[bs][:, 0:128], in1=Mh[:, hh, :], op=ALU.mult),
                 reads=[Bpsf[bs], BMh], writes=[BSM[sb_]])
            S.op("pe", lambda h, i=i, sb_=sb_, bo=bo: h.matmul(psf[bo][:, 0:128], lhsT=SM[sb_][:], rhs=vtok[:, i, :], start=True, stop=False),
                 reads=[BSM[sb_], Bvtok], writes=[Bpsf[bo]])
            S.op("pe", lambda h, i=i, cs=cs, bo=bo: h.matmul(psf[bo][:, 0:128], lhsT=qdfT[:, cs], rhs=Sfb[:, i, :], start=False, stop=False),
                 reads=[BqdfT, BSfb], writes=[Bpsf[bo]])
            S.op("pe", lambda h, i=i, cs=cs, bo=bo: h.matmul(psf[bo][:, 0:128], lhsT=qdbT[:, cs], rhs=Sbb[:, i, :], start=False, stop=True),
                 reads=[BqdbT, BSbb], writes=[Bpsf[bo]])
            S.op("act", lambda h, i=i, bo=bo: h.copy(out=o32[:, i, :], in_=psf[bo][:, 0:128]), reads=[Bpsf[bo]], writes=[Bo32])
            S.op("dve", lambda h, i=i: h.bn_stats(out=bnst[:, i, :], in_=o32[:, i, :]), reads=[Bo32], writes=[Bbnst])
            S.op("dve", lambda h, i=i: h.bn_aggr(out=mv[:, i, :], in_=bnst[:, i, :]), reads=[Bbnst], writes=[Bmv])
        S.op("act", lambda h: h.activation(out=rstd[:], in_=mv[:, :, 1], func=AF.Sqrt, bias=GN_EPS), reads=[Bmv], writes=[Brstd])
        S.op("dve", lambda h: h.reciprocal(out=rstd[:], in_=rstd[:]), reads=[Brstd], writes=[Brstd])
        S.op("pool", lambda h, hh=hh: h.tensor_tensor(out=sg[:], in0=sg[:], in1=gnwb[:, hh * 128:(hh + 1) * 128].unsqueeze(1).to_broadcast([128, NT, 128]), op=ALU.mult),
             reads=[Bsg, Bgnwb], writes=[Bsg])
        for i in range(NT):
            S.op("dve", lambda h, i=i: h.tensor_scalar(out=o32[:, i, :], in0=o32[:, i, :], scalar1=mv[:, i, 0:1], scalar2=rstd[:, i:i + 1], op0=ALU.subtract, op1=ALU.mult),
                 reads=[Bo32, Bmv, Brstd], writes=[Bo32])
        S.op("pool", lambda h: h.tensor_tensor(out=ytok[:], in0=o32[:], in1=sg[:], op=ALU.mult), reads=[Bo32, Bsg], writes=[Bytok])
        for half in range(2):
            pb = half
            for q in range(8):
                i = half * 8 + q
                S.op("pe", lambda h, i=i, q=q, pb=pb: h.transpose(out=psb[pb][:, q * 128:(q + 1) * 128], in_=ytok[:, i, :], identity=identb[:]),
                     reads=[Bytok, Bc], writes=[Bpsb[pb]])
            if half == 0:
                S.op("act", lambda h, half=half, pb=pb: h.copy(out=yTh[:, half * 1024:(half + 1) * 1024], in_=psb[pb][:, :]), reads=[Bpsb[pb]], writes=[ByTh])
            else:
                S.op("dve", lambda h, half=half, pb=pb: h.tensor_copy(out=yTh[:, half * 1024:(half + 1) * 1024], in_=psb[pb][:, :]), reads=[Bpsb[pb]], writes=[ByTh])
        S.dma("sp", ch_yT, lambda h, hh=hh: h.dma_start(out=yT_s[hh * 128:(hh + 1) * 128, :], in_=yTh[:]), reads=[ByTh], writes=[ByT_s])
    S.barrier()
    AR.reset(m2c)
    if stage <= 3:
        if debug:
            d_yT = dbg_out("yT", [D, SEQ], BF16)
            ld = AR.alloc("dbgld", [128, KC, SEQ], BF16)
            Bld = Buf("dbgld")
            S.dma("sp", S.chan("dbgy1"), lambda h: h.dma_start(out=ld[:], in_=yT_s.rearrange("(c p) t -> p c t", p=128)), reads=[ByT_s], writes=[Bld])
            final_ops.append(S.dma("sp", S.chan("dbgy2"), lambda h: h.dma_start(out=d_yT.rearrange("(c p) t -> p c t", p=128), in_=ld[:]), reads=[Bld]))
        return finish(nc, S, final_ops), dbg

    m2d = AR.mark()
    cvrows = AR.alloc("cvrows", [24, 128], F32)
    cvT = AR.alloc("cvT", [128, 24], F32)
    cwrows = AR.alloc("cwrows", [CW, DCONV], F32)
    cwT = AR.alloc("cwT", [128, 8, CW], F32)
    Bcvrows, BcvT, Bcwrows, BcwT = Buf("cvrows"), Buf("cvT"), Buf("cwrows"), Buf("cwT")
    S.dma("sp", ch_v, lambda h: h.dma_start(out=cvrows[:], in_=cvec_d), writes=[Bcvrows])
    S.dma("sp", ch_v, lambda h: h.dma_start(out=cwrows[:], in_=convw_d), writes=[Bcwrows], cont=True)
    transpose_rows(cvrows[:], 24, cvT[:], 0, Bpsf[0], [Bcvrows], [BcvT])
    for cc in range(8):
        S.op("pe", lambda h, cc=cc: h.transpose(out=psf[1][:, 0:CW], in_=cwrows[:, cc * 128:(cc + 1) * 128], identity=ident[0:CW, 0:CW]),
             reads=[Bcwrows, Bc], writes=[Bpsf[1]])
        S.op("act", lambda h, cc=cc: h.copy(out=cwT[:, cc, :], in_=psf[1][:, 0:CW]), reads=[Bpsf[1]], writes=[BcwT])
    wc = [AR.alloc(f"wc{i}", [128, KC, 256], BF16) for i in range(2)]
    Bwc = [Buf("wc0"), Buf("wc1")]
    ch_wc = [S.chan("wc0"), S.chan("wc1")]
    sig = AR.alloc("sig", [128, 512], F32)
    uu = AR.alloc("uu", [128, 8, 64], F32)
    cvo = AR.alloc("cvo", [128, 8, 8, 64], F32)
    sq = AR.alloc("sq", [128, 512], F32)
    mean = AR.alloc("mean", [128, 512], F32)
    msq = AR.alloc("msq", [128, 512], F32)
    rsd = AR.alloc("rsd", [128, 512], F32)
    tn = AR.alloc("tn", [128, 512], F32)
    ycv = [AR.alloc(f"ycv{i}", [128, 512], BF16) for i in range(2)]
    Bsig, Buu, Bcvo, Bsq, Bmean, Bmsq, Brsd, Btn = (Buf(n) for n in ["sig", "uu", "cvo", "sq", "mean", "msq", "rsd", "tn"])
    Bycv = [Buf("ycv0"), Buf("ycv1")]
    ch_ycv = [S.chan("ycv0"), S.chan("ycv1")]
    pcount = 0
    for tbk in range(4):
        tsl = slice(tbk * 512, (tbk + 1) * 512)
        for cc in range(8):
            bi = pcount % 2
            pcount += 1
            S.dma("pool", ch_wc[bi], lambda h, cc=cc, bi=bi: h.dma_start(out=wc[bi][:, :, 0:128], in_=win_v[:, :, 4 * DRET + cc * 128:4 * DRET + (cc + 1) * 128]),
                  writes=[Bwc[bi]])
            S.dma("pool", ch_wc[bi], lambda h, cc=cc, bi=bi: h.dma_start(out=wc[bi][:, :, 128:256],
                                                                          in_=win_v[:, :, 4 * DRET + DCONV + cc * 128:4 * DRET + DCONV + (cc + 1) * 128]),
                  writes=[Bwc[bi]], cont=True)
            ba, bb = 0 + 2 * (cc % 2), 1 + 2 * (cc % 2)
            for kc in range(KC):
                S.op("pe", lambda h, kc=kc, bi=bi, ba=ba, tsl=tsl: h.matmul(psf[ba][:, :], lhsT=wc[bi][:, kc, 0:128], rhs=hT[:, kc, tsl], start=(kc == 0), stop=(kc == KC - 1)),
                     reads=[Bwc[bi]] + BhT[tbk * 4:(tbk + 1) * 4], writes=[Bpsf[ba]])
            for kc in range(KC):
                S.op("pe", lambda h, kc=kc, bi=bi, bb=bb, tsl=tsl: h.matmul(psf[bb][:, :], lhsT=wc[bi][:, kc, 128:256], rhs=hT[:, kc, tsl], start=(kc == 0), stop=(kc == KC - 1)),
                     reads=[Bwc[bi]] + BhT[tbk * 4:(tbk + 1) * 4], writes=[Bpsf[bb]])
            S.op("act", lambda h, bb=bb: h.activation(out=sig[:], in_=psf[bb][:, :], func=AF.Sigmoid), reads=[Bpsf[bb]], writes=[Bsig])
            S.op("dve", lambda h, ba=ba: h.tensor_tensor(out=uu[:].rearrange("p a b -> p (a b)"), in0=psf[ba][:, :], in1=sig[:], op=ALU.mult),
                 reads=[Bpsf[ba], Bsig], writes=[Buu])
            acc = cvo[:, cc, :, :]
            S.op("dve", lambda h, cc=cc, acc=acc: h.tensor_scalar(out=acc, in0=uu[:], scalar1=cwT[:, cc, 15:16], scalar2=cvT[:, cc:cc + 1], op0=ALU.mult, op1=ALU.add),
                 reads=[Buu, BcwT, BcvT], writes=[Bcvo])
            for k in range(CW):
                o = k - 15
                if o == 0:
                    continue
                lo, hi = max(0, -o), min(64, 64 - o)
                S.op("dve", lambda h, cc=cc, k=k, o=o, lo=lo, hi=hi: h.scalar_tensor_tensor(out=cvo[:, cc, :, lo:hi], in0=uu[:, :, lo + o:hi + o], scalar=cwT[:, cc, k:k + 1],
                                                                                             in1=cvo[:, cc, :, lo:hi], op0=ALU.mult, op1=ALU.add),
                     reads=[Buu, BcwT, Bcvo], writes=[Bcvo])
            accf = cvo[:, cc, :, :].rearrange("p a b -> p (a b)")
            S.op("act", lambda h, accf=accf: h.activation(out=sq[:], in_=accf, func=AF.Square), reads=[Bcvo], writes=[Bsq])
            S.op("pe", lambda h, accf=accf, cc=cc: h.matmul(psf[4][:, :], lhsT=ones32[:], rhs=accf, start=(cc == 0), stop=(cc == 7)), reads=[Bcvo, Bc], writes=[Bpsf[4]])
            S.op("pe", lambda h, cc=cc: h.matmul(psf[5][:, :], lhsT=ones32[:], rhs=sq[:], start=(cc == 0), stop=(cc == 7)), reads=[Bsq, Bc], writes=[Bpsf[5]])
        S.op("act", lambda h: h.activation(out=mean[:], in_=psf[4][:, :], func=AF.Identity, scale=1.0 / DCONV), reads=[Bpsf[4]], writes=[Bmean])
        S.op("dve", lambda h: h.tensor_tensor(out=msq[:], in0=mean[:], in1=mean[:], op=ALU.mult), reads=[Bmean], writes=[Bmsq])
        S.op("dve", lambda h: h.scalar_tensor_tensor(out=rsd[:], in0=psf[5][:, :], scalar=1.0 / DCONV, in1=msq[:], op0=ALU.mult, op1=ALU.subtract),
             reads=[Bpsf[5], Bmsq], writes=[Brsd])
        S.op("act", lambda h: h.activation(out=rsd[:], in_=rsd[:], func=AF.Sqrt, bias=EPS), reads=[Brsd], writes=[Brsd])
        S.op("dve", lambda h: h.reciprocal(out=rsd[:], in_=rsd[:]), reads=[Brsd], writes=[Brsd])
        for cc in range(8):
            yb = cc % 2
            accf = cvo[:, cc, :, :].rearrange("p a b -> p (a b)")
            S.op("dve", lambda h, accf=accf: h.tensor_tensor(out=tn[:], in0=accf, in1=mean[:], op=ALU.subtract), reads=[Bcvo, Bmean], writes=[Btn])
            S.op("pool", lambda h: h.tensor_tensor(out=tn[:], in0=tn[:], in1=rsd[:], op=ALU.mult), reads=[Btn, Brsd], writes=[Btn])
            S.op("act", lambda h, cc=cc, yb=yb: h.activation(out=ycv[yb][:], in_=tn[:], func=AF.Silu, scale=cvT[:, 8 + cc:9 + cc], bias=cvT[:, 16 + cc:17 + cc]),
                 reads=[Btn, BcvT], writes=[Bycv[yb]])
            S.dma("sp", ch_ycv[yb], lambda h, cc=cc, yb=yb, tsl=tsl: h.dma_start(out=yT_s[DRET + cc * 128:DRET + (cc + 1) * 128, tsl], in_=ycv[yb][:]),
                  reads=[Bycv[yb]], writes=[ByT_s])
    S.barrier()
    AR.reset(m2)
    AR.reset(persist_mark)
    if stage <= 4:
        if debug:
            d_yT = dbg_out("yT", [D, SEQ], BF16)
            ld = AR.alloc("dbgld", [128, KC, SEQ], BF16)
            Bld = Buf("dbgld")
            S.dma("sp", S.chan("dbgy1"), lambda h: h.dma_start(out=ld[:], in_=yT_s.rearrange("(c p) t -> p c t", p=128)), reads=[ByT_s], writes=[Bld])
            final_ops.append(S.dma("sp", S.chan("dbgy2"), lambda h: h.dma_start(out=d_yT.rearrange("(c p) t -> p c t", p=128), in_=ld[:]), reads=[Bld]))
        return finish(nc, S, final_ops), dbg

    m3 = AR.mark()
    wo = AR.alloc("wo", [128, KC, D], BF16)
    Bwo = Buf("wo")
    ch_wo = S.chan("wo")
    wout_v = wout_d.rearrange("(k p) n -> p k n", p=128)
    for j in range(4):
        S.dma("pool", ch_wo, lambda h, j=j: h.dma_start(out=wo[:, :, j * 512:(j + 1) * 512], in_=wout_v[:, :, j * 512:(j + 1) * 512]), writes=[Bwo], cont=(j > 0))
    G1 = AR.alloc("G1", [128, D], F32)
    A2 = AR.alloc("A2", [128, D], F32)
    S2 = AR.alloc("S2", [128, D], F32)
    wt3 = AR.alloc("wt3", [128, D], F32)
    BG1, BA2, BS2, Bwt3 = Buf("G1"), Buf("A2"), Buf("S2"), Buf("wt3")
    load_mod_bcast(G1, 0, 2, ch_v, BG1)
    load_row_bcast(wt3, pomw_d, ch_v, Bwt3)
    S.op("dve", lambda h: h.tensor_tensor(out=G1[:], in0=G1[:], in1=wt3[:], op=ALU.mult), reads=[BG1, Bwt3], writes=[BG1])
    load_mod_bcast(A2, 0, 4, ch_v, BA2)
    load_row_bcast(wt3, pfw_d, ch_v, Bwt3)
    S.op("dve", lambda h: h.scalar_tensor_tensor(out=A2[:], in0=A2[:], scalar=1.0, in1=wt3[:], op0=ALU.add, op1=ALU.mult), reads=[BA2, Bwt3], writes=[BA2])
    load_mod_bcast(S2, 0, 3, ch_v, BS2)
    rw32 = AR.alloc("rw32", [128, KC, NE], F32)
    rbb = AR.alloc("rbb", [128, NE], F32)
    ebase = AR.alloc("ebase", [128, NE], F32)
    Brw, Brbb, Bebase = Buf("rw32"), Buf("rbb"), Buf("ebase")
    S.dma("sp", ch_v, lambda h: h.dma_start(out=rw32[:], in_=rw_d.rearrange("(k p) e -> p k e", p=128)), writes=[Brw])
    S.dma("sp", ch_v, lambda h: h.dma_start(out=rbb[:], in_=rb_d.partition_broadcast(128)), writes=[Brbb], cont=True)
    S.op("dve", lambda h: h.tensor_scalar(out=ebase[:], in0=iorow[:, 0:NE], scalar1=float(CAP), scalar2=None, op0=ALU.mult), reads=[Bc], writes=[Bebase])
    yTt = [AR.alloc(f"yTt{i}", [128, KC, 128], BF16) for i in range(2)]
    ByTt = [Buf("yTt0"), Buf("yTt1")]
    ch_yTt = [S.chan("yTt0"), S.chan("yTt1")]
    xr = [AR.alloc(f"xr{i}", [128, D], F32) for i in range(2)]
    Bxr = [Buf("xr0"), Buf("xr1")]
    ch_xr = [S.chan("xr0"), S.chan("xr1")]
    t3 = AR.alloc("t3", [128, D], F32)
    x1t = [AR.alloc(f"x1t{i}", [128, D], F32) for i in range(2)]
    h2f = AR.alloc("h2f", [128, D], F32)
    h2b = [AR.alloc(f"h2b{i}", [128, D], BF16) for i in range(2)]
    h2T = AR.alloc("h2T", [128, KC, 128], F32)
    junk3 = AR.alloc("junk3", [128, D], BF16)
    st3 = AR.alloc("st3", [128, 8], F32)
    lg = AR.alloc("lg", [128, NE], F32)
    mx8 = AR.alloc("mx8", [128, 8], F32)
    nmx = AR.alloc("nmx", [128, 1], F32)
    msk = AR.alloc("msk", [128, NE], F32)
    mskb = AR.alloc("mskb", [128, NT, NE], BF16)
    exv = AR.alloc("exv", [128, NE], F32)
    den = AR.alloc("den", [128, 1], F32)
    posC = AR.alloc("posC", [128, NE], F32)
    ovf = AR.alloc("ovf", [128, NE], F32)
    oh = AR.alloc("oh", [128, NE], F32)
    jk = AR.alloc("jk", [128, NE], F32)
    idxf = AR.alloc("idxf", [128, 4], F32)
    (Bt3, Bh2f, Bh2T, Bjunk3, Bst3, Blg, Bmx8, Bnmx, Bmsk, Bmskb, Bexv, Bden, BposC, Bovf, Boh, Bjk, Bidxf) = (
        Buf(n) for n in ["t3", "h2f", "h2T", "junk3", "st3", "lg", "mx8", "nmx", "msk", "mskb", "exv", "den", "posC", "ovf", "oh", "jk", "idxf"])
    Bx1t = [Buf("x1t0"), Buf("x1t1")]
    Bh2b = [Buf("h2b0"), Buf("h2b1")]
    ch_x1 = [S.chan("x1w0"), S.chan("x1w1")]
    ch_sc = [S.chan(f"scat{i}") for i in range(2)]
    Bx1_s = [Buf(f"x1_s{i}") for i in range(NT)]
    Bhsel = Buf("hsel_s")
    yT_v = yT_s.rearrange("(c p) t -> p c t", p=128)
    mixps = [psf[0], psf[1], psf[2], psf[3]]
    for i in range(NT):
        bi = i % 2
        S.dma("sp", ch_yTt[bi], lambda h, i=i, bi=bi: h.dma_start(out=yTt[bi][:], in_=yT_v[:, :, i * 128:(i + 1) * 128]), reads=[ByT_s], writes=[ByTt[bi]])
        S.dma("sp", ch_xr[bi], lambda h, i=i, bi=bi: h.dma_start(out=xr[bi][:], in_=x_d[i * 128:(i + 1) * 128, :]), writes=[Bxr[bi]])
        for cb in range(4):
            for c in range(KC):
                S.op("pe", lambda h, c=c, cb=cb, bi=bi: h.matmul(psf[cb][:, :], lhsT=yTt[bi][:, c, :], rhs=wo[:, c, cb * 512:(cb + 1) * 512], start=(c == 0), stop=(c == KC - 1)),
                     reads=[ByTt[bi], Bwo], writes=[Bpsf[cb]])
        for cb in range(4):
            S.op("act", lambda h, cb=cb: h.activation(out=junk3[:, cb * 512:(cb + 1) * 512], in_=psf[cb][:, :], func=AF.Square, accum_out=st3[:, cb:cb + 1]),
                 reads=[Bpsf[cb]], writes=[Bjunk3, Bst3])
        S.op("dve", lambda h: h.tensor_reduce(out=st3[:, 4:5], in_=st3[:, 0:4], axis=AX.X, op=ALU.add), reads=[Bst3], writes=[Bst3])
        S.op("act", lambda h: h.activation(out=st3[:, 4:5], in_=st3[:, 4:5], func=AF.Sqrt, scale=1.0 / D, bias=EPS), reads=[Bst3], writes=[Bst3])
        S.op("dve", lambda h: h.reciprocal(out=st3[:, 4:5], in_=st3[:, 4:5]), reads=[Bst3], writes=[Bst3])
        for cb in range(4):
            S.op("dve", lambda h, cb=cb: h.scalar_tensor_tensor(out=t3[:, cb * 512:(cb + 1) * 512], in0=psf[cb][:, :], scalar=st3[:, 4:5], in1=G1[:, cb * 512:(cb + 1) * 512],
                                                                op0=ALU.mult, op1=ALU.mult), reads=[Bpsf[cb], Bst3, BG1], writes=[Bt3])
        S.op("pool", lambda h, bi=bi: h.tensor_tensor(out=x1t[bi][:], in0=t3[:], in1=xr[bi][:], op=ALU.add), reads=[Bt3, Bxr[bi]], writes=[Bx1t[bi]])
        S.dma("sp", ch_x1[bi], lambda h, i=i, bi=bi: h.dma_start(out=x1_s[i * 128:(i + 1) * 128, :], in_=x1t[bi][:]), reads=[Bx1t[bi]], writes=[Bx1_s[i]])
        S.op("act", lambda h, bi=bi: h.activation(out=junk3[:], in_=x1t[bi][:], func=AF.Square, accum_out=st3[:, 5:6]), reads=[Bx1t[bi]], writes=[Bjunk3, Bst3])
        S.op("act", lambda h: h.activation(out=st3[:, 5:6], in_=st3[:, 5:6], func=AF.Sqrt, scale=1.0 / D, bias=EPS), reads=[Bst3], writes=[Bst3])
        S.op("dve", lambda h: h.reciprocal(out=st3[:, 5:6], in_=st3[:, 5:6]), reads=[Bst3], writes=[Bst3])
        S.op("dve", lambda h, bi=bi: h.scalar_tensor_tensor(out=t3[:], in0=x1t[bi][:], scalar=st3[:, 5:6], in1=A2[:], op0=ALU.mult, op1=ALU.mult),
             reads=[Bx1t[bi], Bst3, BA2], writes=[Bt3])
        S.op("pool", lambda h: h.tensor_tensor(out=h2f[:], in0=t3[:], in1=S2[:], op=ALU.add), reads=[Bt3, BS2], writes=[Bh2f])
        S.op("act", lambda h, bi=bi: h.copy(out=h2b[bi][:], in_=h2f[:]), reads=[Bh2f], writes=[Bh2b[bi]])
        for q4 in range(4):
            bank = 4 + (q4 % 2)
            for q in range(4):
                kc = q4 * 4 + q
                S.op("pe", lambda h, kc=kc, q=q, bank=bank: h.transpose(out=psf[bank][:, q * 128:(q + 1) * 128], in_=h2f[:, kc * 128:(kc + 1) * 128], identity=ident[:]),
                     reads=[Bh2f, Bc], writes=[Bpsf[bank]])
            if q4 % 2 == 0:
                S.op("act", lambda h, q4=q4, bank=bank: h.copy(out=h2T[:, q4 * 4:(q4 + 1) * 4, :], in_=psf[bank][:, :].rearrange("p (q t) -> p q t", q=4)),
                     reads=[Bpsf[bank]], writes=[Bh2T])
            else:
                S.op("dve", lambda h, q4=q4, bank=bank: h.tensor_copy(out=h2T[:, q4 * 4:(q4 + 1) * 4, :], in_=psf[bank][:, :].rearrange("p (q t) -> p q t", q=4)),
                     reads=[Bpsf[bank]], writes=[Bh2T])
        for kc in range(KC):
            S.op("pe", lambda h, kc=kc: h.matmul(psf[4][:, 0:NE], lhsT=h2T[:, kc, :], rhs=rw32[:, kc, :], start=(kc == 0), stop=(kc == KC - 1)),
                 reads=[Bh2T, Brw], writes=[Bpsf[4]])
        S.op("dve", lambda h: h.tensor_tensor(out=lg[:], in0=psf[4][:, 0:NE], in1=rbb[:], op=ALU.add), reads=[Bpsf[4], Brbb], writes=[Blg])
        S.op("dve", lambda h: h.max(out=mx8[:], in_=lg[:]), reads=[Blg], writes=[Bmx8])
        S.op("dve", lambda h: h.tensor_scalar(out=msk[:], in0=lg[:], scalar1=mx8[:, 3:4], scalar2=None, op0=ALU.is_ge), reads=[Blg, Bmx8], writes=[Bmsk])
        S.op("dve", lambda h, i=i: h.tensor_copy(out=mskb[:, i, :], in_=msk[:]), reads=[Bmsk], writes=[Bmskb])
        S.op("dve", lambda h: h.tensor_scalar(out=nmx[:], in0=mx8[:, 0:1], scalar1=-1.0, scalar2=None, op0=ALU.mult), reads=[Bmx8], writes=[Bnmx])
        S.op("act", lambda h: h.activation(out=exv[:], in_=lg[:], func=AF.Exp, bias=nmx[:, 0:1]), reads=[Blg, Bnmx], writes=[Bexv])
        S.op("dve", lambda h: h.tensor_tensor(out=exv[:], in0=exv[:], in1=msk[:], op=ALU.mult), reads=[Bexv, Bmsk], writes=[Bexv])
        S.op("dve", lambda h: h.tensor_reduce(out=den[:], in_=exv[:], axis=AX.X, op=ALU.add), reads=[Bexv], writes=[Bden])
        S.op("dve", lambda h: h.reciprocal(out=den[:], in_=den[:]), reads=[Bden], writes=[Bden])
        S.op("pe", lambda h, i=i: h.matmul(psf[5][:, 0:NE], lhsT=trib[:], rhs=mskb[:, i, :], start=True, stop=(i == 0)), reads=[Bmskb, Bc], writes=[Bpsf[5]])
        for j in range(i):
            S.op("pe", lambda h, j=j, i=i: h.matmul(psf[5][:, 0:NE], lhsT=onesb[:], rhs=mskb[:, j, :], start=False, stop=(j == i - 1)), reads=[Bmskb, Bc], writes=[Bpsf[5]])
        S.op("dve", lambda h: h.tensor_scalar(out=ovf[:], in0=psf[5][:, 0:NE], scalar1=float(CAP) - 0.5, scalar2=None, op0=ALU.is_gt), reads=[Bpsf[5]], writes=[Bovf])
        S.op("dve", lambda h: h.tensor_tensor(out=posC[:], in0=psf[5][:, 0:NE], in1=ebase[:], op=ALU.add), reads=[Bpsf[5], Bebase], writes=[BposC])
        S.op("dve", lambda h: h.scalar_tensor_tensor(out=posC[:], in0=ovf[:], scalar=BIG, in1=posC[:], op0=ALU.mult, op1=ALU.add), reads=[Bovf, BposC], writes=[BposC])
        S.op("dve", lambda h: h.tensor_scalar(out=ovf[:], in0=ovf[:], scalar1=-1.0, scalar2=1.0, op0=ALU.mult, op1=ALU.add), reads=[Bovf], writes=[Bovf])
        S.op("dve", lambda h, i=i: h.scalar_tensor_tensor(out=gatesA[:, i, :], in0=exv[:], scalar=den[:, 0:1], in1=ovf[:], op0=ALU.mult, op1=ALU.mult),
             reads=[Bexv, Bden, Bovf], writes=[BgatesA])
        for k in range(4):
            S.op("dve", lambda h, k=k: h.tensor_scalar(out=oh[:], in0=lg[:], scalar1=mx8[:, k:k + 1], scalar2=None, op0=ALU.is_equal), reads=[Blg, Bmx8], writes=[Boh])
            S.op("dve", lambda h, k=k: h.scalar_tensor_tensor(out=jk[:], in0=oh[:], scalar=1.0, in1=posC[:], op0=ALU.mult, op1=ALU.mult, accum_out=idxf[:, k:k + 1]),
                 reads=[Boh, BposC], writes=[Bjk, Bidxf])
            S.op("dve", lambda h, k=k, i=i: h.scalar_tensor_tensor(out=jk[:], in0=oh[:], scalar=1.0, in1=gatesA[:, i, :], op0=ALU.mult, op1=ALU.mult,
                                                                 accum_out=gate4[:, i, k:k + 1]), reads=[Boh, BgatesA], writes=[Bjk, Bgate4])
        S.op("dve", lambda h, i=i: h.tensor_copy(out=idx4[:, i * 4:(i + 1) * 4], in_=idxf[:]), reads=[Bidxf], writes=[Bidx4])
        for k in range(4):
            S.dma("pool", ch_sc[bi], lambda h, i=i, k=k, bi=bi: h.indirect_dma_start(out=hsel_s, out_offset=bass.IndirectOffsetOnAxis(ap=idx4[:, i * 4 + k:i * 4 + k + 1], axis=0),
                                                                                      in_=h2b[bi][:], in_offset=None, bounds_check=bound_reg(h, NE * CAP - 1), oob_is_err=False),
                  reads=[Bh2b[bi], Bidx4], writes=[Bhsel], cont=(k > 0))
    if debug:
        d_lg = dbg_out("gates", [128, NT, NE])
        final_ops.append(S.dma("sp", S.chan("dbglg"), lambda h: h.dma_start(out=d_lg, in_=gatesA[:]), reads=[BgatesA]))
        d_idx = dbg_out("idx4", [128, NT * 4], I32)
        final_ops.append(S.dma("sp", S.chan("dbgidx"), lambda h: h.dma_start(out=d_idx, in_=idx4[:]), reads=[Bidx4]))
        d_g4 = dbg_out("gate4", [128, NT, 4])
        final_ops.append(S.dma("sp", S.chan("dbgg4"), lambda h: h.dma_start(out=d_g4, in_=gate4[:]), reads=[Bgate4]))
    S.barrier()
    AR.reset(m3)
    if stage <= 5:
        if debug:
            d_x1 = dbg_out("x1", [SEQ, D])
            ld = AR.alloc("dbgld", [128, NT, D], F32)
            Bld = Buf("dbgld")
            S.dma("sp", S.chan("dbgx1"), lambda h: h.dma_start(out=ld[:], in_=x1_s.rearrange("(t p) d -> p t d", p=128)), reads=Bx1_s, writes=[Bld])
            final_ops.append(S.dma("sp", S.chan("dbgx2"), lambda h: h.dma_start(out=d_x1.rearrange("(t p) d -> p t d", p=128), in_=ld[:]), reads=[Bld]))
        return finish(nc, S, final_ops), dbg

    m5 = AR.mark()
    RS = 1024
    hselT = [AR.alloc(f"hselT{i}", [128, KC, RS], BF16) for i in range(2)]
    BhselT = [Buf("hselT0"), Buf("hselT1")]
    actT = AR.alloc("actT", [128, KC, RS], BF16)
    BactT = Buf("actT")
    w1u = [AR.alloc(f"w1u{i}", [128, KC, 512], BF16) for i in range(2)]
    Bw1u = [Buf("w1u0"), Buf("w1u1")]
    ch_w1 = [S.chan("w1u0"), S.chan("w1u1")]
    w2p = [AR.alloc(f"w2p{i}", [128, KC, 512], BF16) for i in range(2)]
    Bw2p = [Buf("w2p0"), Buf("w2p1")]
    ch_w2 = [S.chan("w2p0"), S.chan("w2p1")]
    hrow = [AR.alloc(f"hrow{i}", [128, D], BF16) for i in range(2)]
    Bhrow = [Buf("hrow0"), Buf("hrow1")]
    ch_hrow = [S.chan("hrow0"), S.chan("hrow1")]
    ysb = [AR.alloc(f"ysb{i}", [128, 512], F32) for i in range(4)]
    Bysb = [Buf(f"ysb{i}") for i in range(4)]
    ch_y = [S.chan(f"yw{i}") for i in range(4)]
    g1 = [AR.alloc(f"g1_{i}", [128, 512], F32) for i in range(2)]
    sgm = [AR.alloc(f"sgm{i}", [128, 512], F32) for i in range(2)]
    l2 = [AR.alloc(f"l2_{i}", [128, 512], F32) for i in range(2)]
    Bg1 = [Buf("g1_0"), Buf("g1_1")]
    Bsgm = [Buf("sgm0"), Buf("sgm1")]
    Bl2 = [Buf("l2_0"), Buf("l2_1")]
    By_s = Buf("y_s")
    w1_v = [w1_d[e].rearrange("(k p) n -> p k n", p=128) for e in range(NE)]
    w2_v = [w2_d[e].rearrange("(k p) n -> p k n", p=128) for e in range(NE)]

    def build_hselT(e, r, buf_i):
        for st in range(RS // 128):
            hb_i = st % 2
            row0 = e * CAP + r * RS + st * 128
            S.dma("sp", ch_hrow[hb_i], lambda h, row0=row0, hb_i=hb_i: h.dma_start(out=hrow[hb_i][:], in_=hsel_s[row0:row0 + 128, :]), reads=[Bhsel], writes=[Bhrow[hb_i]])
            for half in range(2):
                pb = half
                for q in range(8):
                    kc = half * 8 + q
                    S.op("pe", lambda h, kc=kc, q=q, pb=pb, hb_i=hb_i: h.transpose(out=psb[pb][:, q * 128:(q + 1) * 128], in_=hrow[hb_i][:, kc * 128:(kc + 1) * 128], identity=identb[:]),
                         reads=[Bhrow[hb_i], Bc], writes=[Bpsb[pb]])
                if half == 0:
                    S.op("act", lambda h, half=half, pb=pb, st=st, buf_i=buf_i: h.copy(out=hselT[buf_i][:, half * 8:(half + 1) * 8, st * 128:(st + 1) * 128],
                                                                                    in_=psb[pb][:, :].rearrange("p (q t) -> p q t", q=8)), reads=[Bpsb[pb]], writes=[BhselT[buf_i]])
                else:
                    S.op("dve", lambda h, half=half, pb=pb, st=st, buf_i=buf_i: h.tensor_copy(out=hselT[buf_i][:, half * 8:(half + 1) * 8, st * 128:(st + 1) * 128],
                                                                                           in_=psb[pb][:, :].rearrange("p (q t) -> p q t", q=8)), reads=[Bpsb[pb]], writes=[BhselT[buf_i]])

    ucount = 0
    pcount2 = 0
    acnt = 0
    ycnt = 0
    er_list = [(e, r) for e in range(NE) for r in range(ROUNDS)]
    build_hselT(er_list[0][0], er_list[0][1], 0)
    for n, (e, r) in enumerate(er_list):
        hb_cur = n % 2
        for u in range(8):
            bi = ucount % 2
            ucount += 1
            S.dma("pool", ch_w1[bi], lambda h, e=e, u=u, bi=bi: h.dma_start(out=w1u[bi][:, :, 0:256], in_=w1_v[e][:, :, u * 256:(u + 1) * 256]), writes=[Bw1u[bi]])
            S.dma("pool", ch_w1[bi], lambda h, e=e, u=u, bi=bi: h.dma_start(out=w1u[bi][:, :, 256:512], in_=w1_v[e][:, :, DFF + u * 256:DFF + (u + 1) * 256]),
                  writes=[Bw1u[bi]], cont=True)
            for j in range(2):
                fc = u * 2 + j
                for g in range(RS // 512):
                    ab = acnt % 2
                    acnt += 1
                    bg_, bl_ = (0, 1) if ab == 0 else (2, 3)
                    gs = slice(g * 512, (g + 1) * 512)
                    for kc in range(KC):
                        S.op("pe", lambda h, kc=kc, bi=bi, j=j, gs=gs, bg_=bg_, hb_cur=hb_cur: h.matmul(psf[bg_][:, :], lhsT=w1u[bi][:, kc, j * 128:(j + 1) * 128],
                                                                                                     rhs=hselT[hb_cur][:, kc, gs], start=(kc == 0), stop=(kc == KC - 1)),
                             reads=[Bw1u[bi], BhselT[hb_cur]], writes=[Bpsf[bg_]])
                    for kc in range(KC):
                        S.op("pe", lambda h, kc=kc, bi=bi, j=j, gs=gs, bl_=bl_, hb_cur=hb_cur: h.matmul(psf[bl_][:, :], lhsT=w1u[bi][:, kc, 256 + j * 128:256 + (j + 1) * 128],
                                                                                                     rhs=hselT[hb_cur][:, kc, gs], start=(kc == 0), stop=(kc == KC - 1)),
                             reads=[Bw1u[bi], BhselT[hb_cur]], writes=[Bpsf[bl_]])
                    cg = e * 32 + fc
                    cl = e * 32 + 16 + fc
                    S.op("dve", lambda h, ab=ab, bg_=bg_, cg=cg: h.tensor_scalar(out=g1[ab][:], in0=psf[bg_][:, :], scalar1=b1T[:, cg:cg + 1], scalar2=LIMIT, op0=ALU.add, op1=ALU.min),
                         reads=[Bpsf[bg_], Bb1T], writes=[Bg1[ab]])
                    S.op("act", lambda h, ab=ab: h.activation(out=sgm[ab][:], in_=g1[ab][:], func=AF.Sigmoid, scale=ALPHA), reads=[Bg1[ab]], writes=[Bsgm[ab]])
                    S.op("dve", lambda h, ab=ab, bl_=bl_, cl=cl: h.tensor_scalar(out=l2[ab][:], in0=psf[bl_][:, :], scalar1=b1T[:, cl:cl + 1], scalar2=LIMIT, op0=ALU.add, op1=ALU.min),
                         reads=[Bpsf[bl_], Bb1T], writes=[Bl2[ab]])
                    S.op("pool", lambda h, ab=ab: h.tensor_scalar(out=l2[ab][:], in0=l2[ab][:], scalar1=-LIMIT, scalar2=1.0, op0=ALU.max, op1=ALU.add), reads=[Bl2[ab]], writes=[Bl2[ab]])
                    S.op("pool", lambda h, ab=ab: h.tensor_tensor(out=g1[ab][:], in0=g1[ab][:], in1=sgm[ab][:], op=ALU.mult), reads=[Bg1[ab], Bsgm[ab]], writes=[Bg1[ab]])
                    S.op("dve", lambda h, ab=ab, fc=fc, gs=gs: h.tensor_tensor(out=actT[:, fc, gs], in0=g1[ab][:], in1=l2[ab][:], op=ALU.mult),
                         reads=[Bg1[ab], Bl2[ab]], writes=[BactT])
        if n + 1 < len(er_list):
            build_hselT(er_list[n + 1][0], er_list[n + 1][1], (n + 1) % 2)
        for db in range(4):
            bi = pcount2 % 2
            pcount2 += 1
            S.dma("pool", ch_w2[bi], lambda h, e=e, db=db, bi=bi: h.dma_start(out=w2p[bi][:], in_=w2_v[e][:, :, db * 512:(db + 1) * 512]), writes=[Bw2p[bi]])
            for st in range(RS // 128):
                bank = 4 + (ycnt % 2)
                yb = ycnt % 4
                ycnt += 1
                for fc in range(KC):
                    S.op("pe", lambda h, fc=fc, st=st, bi=bi, bank=bank: h.matmul(psf[bank][:, :], lhsT=actT[:, fc, st * 128:(st + 1) * 128], rhs=w2p[bi][:, fc, :],
                                                                                 start=(fc == 0), stop=(fc == KC - 1)),
                         reads=[BactT, Bw2p[bi]], writes=[Bpsf[bank]])
                S.op("act", lambda h, bank=bank, yb=yb: h.copy(out=ysb[yb][:], in_=psf[bank][:, :]), reads=[Bpsf[bank]], writes=[Bysb[yb]])
                row0 = e * CAP + r * RS + st * 128
                S.dma("sp", ch_y[yb], lambda h, row0=row0, db=db, yb=yb: h.dma_start(out=y_s[row0:row0 + 128, db * 512:(db + 1) * 512], in_=ysb[yb][:]),
                      reads=[Bysb[yb]], writes=[By_s])
    S.barrier()
    AR.reset(m5)

    G2 = AR.alloc("G2", [128, D], F32)
    wt6 = AR.alloc("wt6", [128, D], F32)
    b2sb = AR.alloc("b2sb", [NE, D], F32)
    BG2, Bwt6, Bb2 = Buf("G2"), Buf("wt6"), Buf("b2sb")
    load_mod_bcast(G2, 0, 5, ch_v, BG2)
    load_row_bcast(wt6, pofw_d, ch_v, Bwt6)
    S.op("dve", lambda h: h.tensor_tensor(out=G2[:], in0=G2[:], in1=wt6[:], op=ALU.mult), reads=[BG2, Bwt6], writes=[BG2])
    S.dma("sp", ch_v, lambda h: h.dma_start(out=b2sb[:], in_=b2_d), writes=[Bb2])
    yk = [[AR.alloc(f"yk{b}_{k}", [128, D], F32) for k in range(4)] for b in range(2)]
    Byk = [[Buf(f"yk{b}_{k}") for k in range(4)] for b in range(2)]
    ch_g = [S.chan("gath0"), S.chan("gath1")]
    x1r = [AR.alloc(f"x1r{i}", [128, D], F32) for i in range(2)]
    Bx1r = [Buf("x1r0"), Buf("x1r1")]
    ch_x1r = [S.chan("x1r0"), S.chan("x1r1")]
    ff = AR.alloc("ff", [128, D], F32)
    ot = [AR.alloc(f"ot{i}", [128, D], F32) for i in range(2)]
    gT = AR.alloc("gT", [NE, 128], F32)
    junk6 = AR.alloc("junk6", [128, D], BF16)
    st6 = AR.alloc("st6", [128, 2], F32)
    Bff, BgT, Bjunk6, Bst6 = Buf("ff"), Buf("gT"), Buf("junk6"), Buf("st6")
    Bot = [Buf("ot0"), Buf("ot1")]
    ch_out = [S.chan("out0"), S.chan("out1")]
    for i in range(NT):
        bi = i % 2
        for k in range(4):
            S.op("pool", lambda h, bi=bi, k=k: h.memset(yk[bi][k][:], 0.0), writes=[Byk[bi][k]])
            S.dma("pool", ch_g[bi], lambda h, i=i, k=k, bi=bi: h.indirect_dma_start(out=yk[bi][k][:], out_offset=None, in_=y_s,
                                                                                     in_offset=bass.IndirectOffsetOnAxis(ap=idx4[:, i * 4 + k:i * 4 + k + 1], axis=0),
                                                                                     bounds_check=bound_reg(h, NE * CAP - 1), oob_is_err=False),
                  reads=[By_s, Bidx4], writes=[Byk[bi][k]], cont=(k > 0))
        S.dma("sp", ch_x1r[bi], lambda h, i=i, bi=bi: h.dma_start(out=x1r[bi][:], in_=x1_s[i * 128:(i + 1) * 128, :]), reads=[Bx1_s[i]], writes=[Bx1r[bi]])
        S.op("pe", lambda h, i=i: h.transpose(out=psf[4][0:NE, 0:128], in_=gatesA[:, i, :], identity=ident[:]), reads=[BgatesA, Bc], writes=[Bpsf[4]])
        S.op("act", lambda h: h.copy(out=gT[:], in_=psf[4][0:NE, 0:128]), reads=[Bpsf[4]], writes=[BgT])
        for cb in range(4):
            S.op("pe", lambda h, cb=cb: h.matmul(psf[cb][:, :], lhsT=gT[:], rhs=b2sb[:, cb * 512:(cb + 1) * 512], start=True, stop=True), reads=[BgT, Bb2], writes=[Bpsf[cb]])
            S.op("dve", lambda h, cb=cb, bi=bi, i=i: h.scalar_tensor_tensor(out=ff[:, cb * 512:(cb + 1) * 512], in0=yk[bi][0][:, cb * 512:(cb + 1) * 512], scalar=gate4[:, i, 0:1],
                                                                          in1=psf[cb][:, :], op0=ALU.mult, op1=ALU.add), reads=[Byk[bi][0], Bgate4, Bpsf[cb]], writes=[Bff])
        for k in range(1, 4):
            S.op("dve", lambda h, k=k, bi=bi, i=i: h.scalar_tensor_tensor(out=ff[:], in0=yk[bi][k][:], scalar=gate4[:, i, k:k + 1], in1=ff[:], op0=ALU.mult, op1=ALU.add),
                 reads=[Byk[bi][k], Bgate4, Bff], writes=[Bff])
        S.op("act", lambda h: h.activation(out=junk6[:], in_=ff[:], func=AF.Square, accum_out=st6[:, 0:1]), reads=[Bff], writes=[Bjunk6, Bst6])
        S.op("act", lambda h: h.activation(out=st6[:, 0:1], in_=st6[:, 0:1], func=AF.Sqrt, scale=1.0 / D, bias=EPS), reads=[Bst6], writes=[Bst6])
        S.op("dve", lambda h: h.reciprocal(out=st6[:, 0:1], in_=st6[:, 0:1]), reads=[Bst6], writes=[Bst6])
        S.op("dve", lambda h: h.scalar_tensor_tensor(out=ff[:], in0=ff[:], scalar=st6[:, 0:1], in1=G2[:], op0=ALU.mult, op1=ALU.mult), reads=[Bff, Bst6, BG2], writes=[Bff])
        S.op("pool", lambda h, bi=bi: h.tensor_tensor(out=ot[bi][:], in0=ff[:], in1=x1r[bi][:], op=ALU.add), reads=[Bff, Bx1r[bi]], writes=[Bot[bi]])
        final_ops.append(S.dma("sp", ch_out[bi], lambda h, i=i, bi=bi: h.dma_start(out=out_d[i * 128:(i + 1) * 128, :], in_=ot[bi][:]), reads=[Bot[bi]]))
    return finish(nc, S, final_ops), dbg


def finish(nc, S, final_ops):
    S.wait_final("sp", final_ops)
    S.emit()
    return nc


def _rope_tables():
    half = 64
    inv = (10000.0 ** (-np.arange(half, dtype=np.float32) / np.float32(half))).astype(np.float32)
    pos = np.arange(NCTX + SEQ, dtype=np.float32)
    ang = (pos[:, None] * inv[None, :]).astype(np.float32)
    return np.cos(ang).astype(np.float32), np.sin(ang).astype(np.float32)


def make_in_maps(inp, ne_decl=NE):
    f = lambda a: np.ascontiguousarray(np.asarray(a, dtype=np.float32))
    cos, sin = _rope_tables()
    shared = {
        "c_ctx": f(inp["c_ctx"]).reshape(16, 128),
        "ada_w": f(inp["ada_w"][0]),
        "ada_b": f(inp["ada_b"][0]).reshape(1, 6 * D),
        "pre_mix_norm": f(inp["pre_mix_norm"][0]).reshape(1, D),
        "post_mix_norm": f(inp["post_mix_norm"][0]).reshape(1, D),
        "pre_ffn_norm": f(inp["pre_ffn_norm"][0]).reshape(1, D),
        "post_ffn_norm": f(inp["post_ffn_norm"][0]).reshape(1, D),
        "w_in": f(inp["w_in"][0]),
        "ret_decay": np.concatenate([f(inp["ret_decay_fwd"][0]), f(inp["ret_decay_bwd"][0])]).reshape(1, 16),
        "ret_gn_w": f(inp["ret_gn_w"][0]).reshape(1, DRET),
        "conv_w": f(inp["conv_w"][0]),
        "conv_vecs": np.concatenate([f(inp["conv_b"][0]).reshape(8, 128), f(inp["conv_ln_w"][0]).reshape(8, 128), f(inp["conv_ln_b"][0]).reshape(8, 128)], axis=0),
        "w_out": f(inp["w_out"][0]),
        "router_w": f(inp["router_w"][0]),
        "router_b": f(inp["router_b"][0]).reshape(1, NE),
        "w1": f(inp["w1"][0][:ne_decl]),
        "b1": f(inp["b1"][0]).reshape(NE * 32, 128),
        "w2": f(inp["w2"][0][:ne_decl]),
        "b2": f(inp["b2"][0]),
        "rope_cos": cos,
        "rope_sin": sin,
    }
    maps = []
    for b in range(NB):
        m = dict(shared)
        m["x"] = f(inp["x"][b])
        m["c"] = f(inp["c"][b]).reshape(16, 128)
        m["ctx"] = f(inp["ctx"][b])
        maps.append(m)
    return maps


_NC_CACHE = {}


def kernel(**inputs):
    if "nc" not in _NC_CACHE:
        _NC_CACHE["nc"] = build_program()[0]
    nc = _NC_CACHE["nc"]
    in_maps = make_in_maps(inputs)
    res = run_bass_kernel_spmd(nc, in_maps, core_ids=list(range(NB)))
    out = np.stack([np.asarray(res.results[b]["out"], dtype=np.float32) for b in range(NB)], axis=0)
    return out
```

```python
import numpy as np
import concourse.bass as bass
import concourse.mybir as mybir
from concourse.alu_op_type import AluOpType as ALU
from concourse.bass_utils import run_bass_kernel_spmd

F32 = mybir.dt.float32
BF16 = mybir.dt.bfloat16
I32 = mybir.dt.int32
AF = mybir.ActivationFunctionType
AX = mybir.AxisListType

D = 2048
SEQ = 2048
NB = 8
NCTX = 256
H = 8
HD = 128
DRET = 1024
DCONV = 1024
DIN = 6144
CW = 31
NE = 32
DFF = 2048
NT = SEQ // 128
KC = D // 128
EPS = 1e-6
GN_EPS = 1e-5
ALPHA = 1.702
LIMIT = 7.0
QSCALE = HD ** -0.5

ROUNDS = 2
RS = 1024
CAP = ROUNDS * RS
BLK = 256
NBLK = RS // BLK
NHS = 2
NYS = 4
BIG = 4.0e6

ENGS = ("pe", "act", "dve", "pool", "sp")
EPOCH = 1 << 30


class Buf:
    __slots__ = ("name", "excl", "last_w", "readers")

    def __init__(self, name, excl=False):
        self.name = name
        self.excl = excl
        self.last_w = None
        self.readers = []


class Op:
    __slots__ = ("eng", "fn", "deps", "signal", "done", "is_dma", "chan", "chan_prev", "grp", "guard")

    def __init__(self, eng, fn):
        self.eng = eng
        self.fn = fn
        self.deps = []
        self.signal = False
        self.done = None
        self.is_dma = False
        self.chan = None
        self.chan_prev = None
        self.grp = None
        self.guard = None


class Chan:
    def __init__(self, name):
        self.name = name
        self.sem = None
        self.last_grp = None


class Sched:
    def __init__(self, nc):
        self.nc = nc
        self.ops = {e: [] for e in ENGS}
        self.chans = []
        self.final_waits = []
        self.nrec = 0
        self.cur_guard = None
        self.cnt_ap = None
        self.cnt_op = None

    def chan(self, name):
        c = Chan(name)
        self.chans.append(c)
        return c

    def _deps(self, op, reads, writes):
        for b in reads:
            if b.last_w is not None:
                op.deps.append((b.last_w, "RAW"))
            if b.excl:
                for r in b.readers:
                    op.deps.append((r, "RAR"))
        for b in writes:
            if b.last_w is not None:
                op.deps.append((b.last_w, "WAW"))
            for r in b.readers:
                op.deps.append((r, "WAR"))
        for b in reads:
            b.readers.append(op)
        for b in writes:
            b.last_w = op
            b.readers = []

    def op(self, eng, fn, reads=(), writes=()):
        o = Op(eng, fn)
        o.guard = self.cur_guard
        self._deps(o, list(reads), list(writes))
        self.ops[eng].append(o)
        self.nrec += 1
        return o

    def dma(self, eng, chan, fn, reads=(), writes=(), cont=False):
        o = Op(eng, fn)
        o.guard = self.cur_guard
        o.is_dma = True
        o.chan = chan
        if cont and chan.last_grp is not None:
            o.grp = chan.last_grp
            o.chan_prev = o.grp[0].chan_prev
        else:
            o.chan_prev = chan.last_grp
            o.grp = []
            chan.last_grp = o.grp
        o.grp.append(o)
        self._deps(o, list(reads), list(writes))
        self.ops[eng].append(o)
        self.nrec += 1
        return o

    def barrier(self):
        lasts = []
        for e in ENGS:
            for o in reversed(self.ops[e]):
                if not o.is_dma and o.fn is not None:
                    lasts.append(o)
                    break
        for c in self.chans:
            if c.last_grp:
                lasts.append(c.last_grp[0])
        for e in ENGS:
            o = Op(e, None)
            for d in lasts:
                o.deps.append((d, "RAW"))
            self.ops[e].append(o)

    def wait_final(self, eng, ops):
        self.final_waits.append((eng, list(ops)))

    def emit(self):
        nc = self.nc
        for e in ENGS:
            for o in self.ops[e]:
                for (d, kind) in o.deps:
                    if d.is_dma:
                        continue
                    if d.eng != o.eng or kind == "RAW":
                        d.signal = True
        for (e, ops) in self.final_waits:
            for d in ops:
                if not d.is_dma:
                    d.signal = True
        if self.cnt_op is not None:
            self.cnt_op.signal = True
        sems = []
        for e in ENGS:
            cnt = 0
            sem = None
            for o in self.ops[e]:
                if o.is_dma or o.fn is None:
                    continue
                if o.signal:
                    if sem is None or cnt >= EPOCH:
                        sem = nc.alloc_semaphore(f"s_{e}_{len(sems)}")
                        sems.append(sem)
                        cnt = 0
                    cnt += 1
                    o.done = (sem, cnt)
        for c in self.chans:
            if c.last_grp is None:
                continue
            c.sem = nc.alloc_semaphore(f"c_{c.name}")
            chain = []
            g = c.last_grp
            while g is not None:
                chain.append(g)
                g = g[0].chan_prev
            chain.reverse()
            v = 0
            for g in chain:
                v += 16 * len(g)
                for o in g:
                    o.done = (c.sem, v)
        lists = self.ops
        finals = self.final_waits

        cnt_ap = self.cnt_ap
        cnt_op = self.cnt_op

        def run(e, h):
            seen = {}
            state = {"greg": None, "loaded": None, "last_sig": None}

            def need(sem, val):
                k = id(sem)
                if seen.get(k, 0) < val:
                    h.wait_ge(sem, val)
                    seen[k] = val

            def emit_op(o):
                if o.is_dma and o.chan_prev is not None and o is o.grp[0]:
                    need(*o.chan_prev[0].done)
                for (d, kind) in o.deps:
                    if d.is_dma:
                        if d.grp is o.grp:
                            continue
                        need(*d.done)
                    elif d.eng != e or kind == "RAW":
                        need(*d.done)
                if o.fn is None:
                    return
                ins = o.fn(h)
                if o.is_dma:
                    ins.then_inc(o.chan.sem, 16)
                elif o.signal:
                    ins.then_inc(o.done[0], 1)
                    state["last_sig"] = o.done

            ops = lists[e]
            i = 0
            n = len(ops)
            while i < n:
                o = ops[i]
                if o.guard is None:
                    emit_op(o)
                    i += 1
                    continue
                j = i
                while j < n and ops[j].guard == o.guard:
                    j += 1
                grp = ops[i:j]
                (ge, thr) = o.guard
                if state["greg"] is None:
                    state["greg"] = h.alloc_register(f"greg_{e}")
                if state["loaded"] != ge:
                    need(*cnt_op.done)
                    h.reg_load(state["greg"], cnt_ap(ge))
                    state["loaded"] = ge
                saved = dict(seen)
                pre_sig = state["last_sig"]
                with h.If_cmp(state["greg"], thr, "IS_GT"):
                    for g in grp:
                        emit_op(g)
                nsig = 0
                sig_sem = None
                ndma = 0
                for g in grp:
                    if g.fn is None:
                        continue
                    if g.is_dma:
                        ndma += 1
                    elif g.signal:
                        nsig += 1
                        sig_sem = g.done[0]
                        last_in = g.done
                if nsig or ndma:
                    with h.Else():
                        if nsig:
                            if pre_sig is not None:
                                h.wait_ge(*pre_sig)
                            h.sem_inc(sig_sem, nsig)
                        for g in grp:
                            if g.fn is not None and g.is_dma:
                                if g is g.grp[0] and g.chan_prev is not None:
                                    h.wait_ge(*g.chan_prev[0].done)
                                h.sem_inc(g.chan.sem, 16)
                if nsig:
                    state["last_sig"] = last_in
                seen.clear()
                seen.update(saved)
                i = j
            for (fe, fops) in finals:
                if fe == e:
                    for d in fops:
                        need(*d.done)

        with nc.Block() as block:
            @block.tensor
            def _(h):
                run("pe", h)

            @block.scalar
            def _(h):
                run("act", h)

            @block.vector
            def _(h):
                run("dve", h)

            @block.gpsimd
            def _(h):
                run("pool", h)

            @block.sync
            def _(h):
                run("sp", h)


class Arena:
    def __init__(self, nc, nbytes):
        self.nc = nc
        left = nc._sbuf_addr_for_side("left")
        self.base = (left + 63) // 64 * 64
        nbytes = nbytes // 64 * 64
        self.slab = nc.alloc_sbuf_tensor("arena", [128, nbytes // 4], F32)
        self.size = nbytes - 64
        self.top = 0
        self.n = 0

    def alloc(self, name, shape, dtype):
        esz = 2 if dtype == BF16 else 4
        nb = esz
        for s in shape[1:]:
            nb *= s
        nb = (nb + 63) // 64 * 64
        off = self.top
        assert off + nb <= self.size, (name, off, nb, self.size)
        self.top += nb
        self.n += 1
        return self.nc.alloc_sbuf_tensor_at(f"{name}_{self.n}", list(shape), dtype, offset=self.base + off)

    def alloc_at(self, name, shape, dtype, off):
        self.n += 1
        return self.nc.alloc_sbuf_tensor_at(f"{name}_{self.n}", list(shape), dtype, offset=self.base + off)

    def mark(self):
        return self.top

    def reset(self, m):
        self.top = m


def build_program(stage=99, debug=False, ne_decl=NE):
    nc = bass.Bass("TRN2", target_bir_lowering=False)
    S = Sched(nc)

    def din(name, shape, dt=F32):
        return nc.dram_tensor(name, list(shape), dt, kind="ExternalInput").ap()

    x_d = din("x", [SEQ, D])
    c_d = din("c", [16, 128])
    ctx_d = din("ctx", [NCTX, D])
    cctx_d = din("c_ctx", [16, 128])
    adaw_d = din("ada_w", [D, 6 * D])
    adab_d = din("ada_b", [1, 6 * D])
    pmw_d = din("pre_mix_norm", [1, D])
    pomw_d = din("post_mix_norm", [1, D])
    pfw_d = din("pre_ffn_norm", [1, D])
    pofw_d = din("post_ffn_norm", [1, D])
    win_d = din("w_in", [D, DIN])
    dec_d = din("ret_decay", [1, 16])
    gnw_d = din("ret_gn_w", [1, DRET])
    convw_d = din("conv_w", [CW, DCONV])
    cvec_d = din("conv_vecs", [24, 128])
    wout_d = din("w_out", [D, D])
    rw_d = din("router_w", [D, NE])
    rb_d = din("router_b", [1, NE])
    w1_d = din("w1", [ne_decl, D, 2 * DFF])
    b1_d = din("b1", [NE * 32, 128])
    w2_d = din("w2", [ne_decl, DFF, D])
    b2_d = din("b2", [NE, D])
    cos_d = din("rope_cos", [NCTX + SEQ, 64])
    sin_d = din("rope_sin", [NCTX + SEQ, 64])
    out_d = nc.dram_tensor("out", [SEQ, D], F32, kind="ExternalOutput").ap()
    dbg = {}

    def dbg_out(name, shape, dt=F32):
        t = nc.dram_tensor("dbg_" + name, list(shape), dt, kind="ExternalOutput").ap()
        dbg[name] = t
        return t

    mod_s = nc.dram_tensor("mod_s", [2, 6 * D], F32).ap()
    yT_s = nc.dram_tensor("yT_s", [D, SEQ], BF16).ap()
    x1_s = nc.dram_tensor("x1_s", [SEQ, D], F32).ap()
    hsel_s = [nc.dram_tensor(f"hsel_s{j}", [(NE // NHS) * CAP, D], BF16).ap() for j in range(NHS)]
    y_s = [nc.dram_tensor(f"y_s{j}", [(NE // NYS) * CAP, D], F32).ap() for j in range(NYS)]

    AR = Arena(nc, nc.sbuf_bytes_remaining - 6144)

    psf = [nc.alloc_psum_tensor(f"psf{i}", [128, 512], F32) for i in range(6)]
    psb = [nc.alloc_psum_tensor(f"psb{i}", [128, 1024], BF16) for i in range(2)]
    Bpsf = [Buf(f"psf{i}", excl=True) for i in range(6)]
    Bpsb = [Buf(f"psb{i}", excl=True) for i in range(2)]
    final_ops = []
    _regs = {}

    def bound_reg(h, val):
        if val not in _regs:
            _regs[val] = h.to_reg(val)
        return _regs[val]

    ident = AR.alloc("ident", [128, 128], F32)
    identb = AR.alloc("identb", [128, 128], BF16)
    onesb = AR.alloc("onesb", [128, 128], BF16)
    ones32 = AR.alloc("ones32", [128, 128], F32)
    trib = AR.alloc("trib", [128, 128], BF16)
    iorow = AR.alloc("iorow", [128, 128], F32)
    iocol = AR.alloc("iocol", [128, 1], F32)
    b1T = AR.alloc("b1T", [128, NE * 32], F32)
    gate4 = AR.alloc("gate4", [128, NT, 4], F32)
    idxH = [AR.alloc(f"idxH{j}", [128, NT * 4], I32) for j in range(NHS)]
    idxY = [AR.alloc(f"idxY{j}", [128, NT * 4], I32) for j in range(NYS)]
    cnt_i = AR.alloc("cnt_i", [128, NE], I32)
    Bcnt = Buf("cnt_i")
    gatesA = AR.alloc("gatesA", [128, NT, NE], F32)
    Bc = Buf("consts")
    Bb1T = Buf("b1T")
    Bgate4 = Buf("gate4")
    Bidx = [Buf(f"idx{i}") for i in range(NT)]
    BgatesA = Buf("gatesA")

    S.op("pool", lambda h: h.memset(ident[:], 0.0), writes=[Bc])
    S.op("pool", lambda h: h.affine_select(out=ident[:], in_=ident[:], pattern=[[-1, 128]], compare_op=ALU.not_equal,
                                            fill=1.0, base=0, channel_multiplier=1), reads=[Bc], writes=[Bc])
    S.op("pool", lambda h: h.memset(ones32[:], 1.0), writes=[Bc])
    S.op("pool", lambda h: h.affine_select(out=iorow[:], in_=ones32[:], pattern=[[1, 128]], compare_op=ALU.is_gt,
                                            fill=0.0, base=0, channel_multiplier=-1), reads=[Bc], writes=[Bc])
    S.op("dve", lambda h: h.tensor_copy(out=trib[:], in_=iorow[:]), reads=[Bc], writes=[Bc])
    S.op("dve", lambda h: h.tensor_copy(out=identb[:], in_=ident[:]), reads=[Bc], writes=[Bc])
    S.op("dve", lambda h: h.tensor_copy(out=onesb[:], in_=ones32[:]), reads=[Bc], writes=[Bc])
    ioi = AR.alloc("ioi", [128, 128], I32)
    S.op("pool", lambda h: h.iota(ioi[:], pattern=[[1, 128]], base=0, channel_multiplier=0), writes=[Bc])
    S.op("dve", lambda h: h.tensor_copy(out=iorow[:], in_=ioi[:]), reads=[Bc], writes=[Bc])
    S.op("pool", lambda h: h.iota(ioi[:, 0:1], pattern=[[0, 1]], base=0, channel_multiplier=1), reads=[Bc], writes=[Bc])
    S.op("dve", lambda h: h.tensor_copy(out=iocol[:], in_=ioi[:, 0:1]), reads=[Bc], writes=[Bc])

    ch_small = S.chan("small")
    persist_mark = AR.mark()

    def transpose_rows(rows_ap, nrows, out_ap, bank, Bbank, reads, writes, evac="act"):
        S.op("pe", lambda h: h.transpose(out=psf[bank][:, 0:nrows], in_=rows_ap, identity=ident[0:nrows, 0:nrows]),
             reads=reads + [Bc], writes=[Bbank])
        if evac == "act":
            S.op("act", lambda h: h.copy(out=out_ap, in_=psf[bank][:, 0:nrows]), reads=[Bbank], writes=writes)
        else:
            S.op("dve", lambda h: h.tensor_copy(out=out_ap, in_=psf[bank][:, 0:nrows]), reads=[Bbank], writes=writes)

    m0 = AR.mark()
    rows32 = AR.alloc("rows32", [32, 128], F32)
    cT = AR.alloc("cT", [128, 32], F32)
    sil = AR.alloc("sil", [128, 32], F32)
    adab = AR.alloc("adab", [2, 6 * D], F32)
    modsb = AR.alloc("modsb", [2, 6 * D], F32)
    adaw = [AR.alloc(f"adaw{i}", [128, KC, 512], F32) for i in range(2)]
    b1rows = AR.alloc("b1rows", [128, 8, 128], F32)
    Brows32, BcT, Bsil, Badab, Bmodsb, Bb1rows = Buf("rows32"), Buf("cT"), Buf("sil"), Buf("adab"), Buf("modsb"), Buf("b1rows")
    Badaw = [Buf("adaw0"), Buf("adaw1")]
    ch_adaw = [S.chan("adaw0"), S.chan("adaw1")]

    S.dma("sp", ch_small, lambda h: h.dma_start(out=rows32[0:16, :], in_=c_d), writes=[Brows32])
    S.dma("sp", ch_small, lambda h: h.dma_start(out=rows32[16:32, :], in_=cctx_d), writes=[Brows32], cont=True)
    S.dma("sp", ch_small, lambda h: h.dma_start(out=adab[:], in_=adab_d.partition_broadcast(2)), writes=[Badab], cont=True)
    S.dma("sp", ch_small, lambda h: h.dma_start(out=b1rows[:], in_=b1_d.rearrange("(t p) f -> p t f", p=128)), writes=[Bb1rows], cont=True)
    transpose_rows(rows32[:], 32, cT[:], 0, Bpsf[0], [Brows32], [BcT])
    S.op("act", lambda h: h.activation(out=sil[:], in_=cT[:], func=AF.Silu), reads=[BcT], writes=[Bsil])
    for t in range(8):
        S.op("pe", lambda h, t=t: h.transpose(out=psf[1 + (t % 2)][:, 0:128], in_=b1rows[:, t, :], identity=ident[:]),
             reads=[Bb1rows, Bc], writes=[Bpsf[1 + (t % 2)]])
        S.op("act", lambda h, t=t: h.copy(out=b1T[:, t * 128:(t + 1) * 128], in_=psf[1 + (t % 2)][:, 0:128]),
             reads=[Bpsf[1 + (t % 2)]], writes=[Bb1T])
    b1T3 = b1T[:, :].rearrange("p (e f) -> p e f", e=NE)
    S.op("dve", lambda h: h.tensor_scalar(out=b1T3[:, :, 16:32], in0=b1T3[:, :, 16:32], scalar1=1.0, scalar2=None, op0=ALU.add), reads=[Bb1T], writes=[Bb1T])
    sil2 = AR.alloc("sil2", [128, KC, 2], F32)
    Bsil2 = Buf("sil2")
    S.op("dve", lambda h: h.tensor_copy(out=sil2[:, :, 0], in_=sil[:, 0:16]), reads=[Bsil], writes=[Bsil2])
    S.op("dve", lambda h: h.tensor_copy(out=sil2[:, :, 1], in_=sil[:, 16:32]), reads=[Bsil], writes=[Bsil2])
    adaw_v = adaw_d.rearrange("(k p) n -> p k n", p=128)
    for j in range(24):
        bi = j % 2
        S.dma("sp", ch_adaw[bi], lambda h, j=j, bi=bi: h.dma_start(out=adaw[bi][:], in_=adaw_v[:, :, j * 512:(j + 1) * 512]),
              writes=[Badaw[bi]])
        bank = 2 + (j % 2)
        for kc in range(KC):
            S.op("pe", lambda h, kc=kc, bi=bi, bank=bank: h.matmul(psf[bank][0:2, :], lhsT=sil2[:, kc, :], rhs=adaw[bi][:, kc, :],
                                                                     start=(kc == 0), stop=(kc == KC - 1)),
                 reads=[Bsil2, Badaw[bi]], writes=[Bpsf[bank]])
        S.op("dve", lambda h, j=j, bank=bank: h.tensor_tensor(out=modsb[:, j * 512:(j + 1) * 512], in0=psf[bank][0:2, :],
                                                               in1=adab[:, j * 512:(j + 1) * 512], op=ALU.add),
             reads=[Bpsf[bank], Badab], writes=[Bmodsb])
    Bmod_s = Buf("mod_s")
    S.dma("sp", ch_small, lambda h: h.dma_start(out=mod_s, in_=modsb[:]), reads=[Bmodsb], writes=[Bmod_s])
    if debug:
        d_mod = dbg_out("mod", [2, 6 * D])
        final_ops.append(S.dma("sp", S.chan("dbgmod"), lambda h: h.dma_start(out=d_mod, in_=modsb[:]), reads=[Bmodsb]))
    S.barrier()
    AR.reset(m0)
    if stage <= 0:
        return finish(nc, S, final_ops), dbg

    def load_mod_bcast(dst, row, g, chan, Bdst):
        return S.dma("sp", chan, lambda h: h.dma_start(out=dst[:], in_=mod_s[row:row + 1, g * D:(g + 1) * D].partition_broadcast(128)),
                     reads=[Bmod_s], writes=[Bdst])

    def load_row_bcast(dst, row_ap, chan, Bdst, cont=False):
        return S.dma("sp", chan, lambda h: h.dma_start(out=dst[:], in_=row_ap.partition_broadcast(128)), writes=[Bdst], cont=cont)

    hT = AR.alloc("hT", [128, KC, SEQ], BF16)
    hcT_off = AR.mark()
    hcT = AR.alloc("hcT", [128, KC, NCTX], BF16)
    BhT = [Buf(f"hT{i}") for i in range(NT)]
    BhcT = Buf("hcT")
    m1 = AR.mark()
    A1 = AR.alloc("A1", [128, D], F32)
    S1 = AR.alloc("S1", [128, D], F32)
    A1c = AR.alloc("A1c", [128, D], F32)
    S1c = AR.alloc("S1c", [128, D], F32)
    wtmp = AR.alloc("wtmp", [128, D], F32)
    xb = [AR.alloc(f"xb{i}", [128, D], F32) for i in range(2)]
    t32 = AR.alloc("t32", [128, D], F32)
    hb = [AR.alloc(f"hb{i}", [128, D], BF16) for i in range(2)]
    junkb = AR.alloc("junkb", [128, D], BF16)
    stat = AR.alloc("stat", [128, 8], F32)
    BA1, BS1, BA1c, BS1c, Bwtmp, Bt32, Bjunk, Bstat = (Buf(n) for n in ["A1", "S1", "A1c", "S1c", "wtmp", "t32", "junk", "stat"])
    Bxb = [Buf("xb0"), Buf("xb1")]
    Bhb = [Buf("hb0"), Buf("hb1")]
    ch_x = [S.chan("x0"), S.chan("x1")]
    ch_v = S.chan("vecs")

    load_row_bcast(wtmp, pmw_d, ch_v, Bwtmp)
    load_mod_bcast(A1, 0, 1, ch_v, BA1)
    load_mod_bcast(S1, 0, 0, ch_v, BS1)
    load_mod_bcast(A1c, 1, 1, ch_v, BA1c)
    load_mod_bcast(S1c, 1, 0, ch_v, BS1c)
    S.op("dve", lambda h: h.scalar_tensor_tensor(out=A1[:], in0=A1[:], scalar=1.0, in1=wtmp[:], op0=ALU.add, op1=ALU.mult),
         reads=[BA1, Bwtmp], writes=[BA1])
    S.op("dve", lambda h: h.scalar_tensor_tensor(out=A1c[:], in0=A1c[:], scalar=1.0, in1=wtmp[:], op0=ALU.add, op1=ALU.mult),
         reads=[BA1c, Bwtmp], writes=[BA1c])

    def norm_mod_tile(src_ap_dram, xbuf, Bx, chan, Avec, BAv, Svec, BSv, hbuf, Bh, sidx):
        S.dma("sp", chan, lambda h: h.dma_start(out=xbuf[:], in_=src_ap_dram), writes=[Bx])
        S.op("act", lambda h: h.activation(out=junkb[:], in_=xbuf[:], func=AF.Square, accum_out=stat[:, sidx:sidx + 1]),
             reads=[Bx], writes=[Bjunk, Bstat])
        S.op("act", lambda h: h.activation(out=stat[:, sidx:sidx + 1], in_=stat[:, sidx:sidx + 1], func=AF.Sqrt, scale=1.0 / D, bias=EPS),
             reads=[Bstat], writes=[Bstat])
        S.op("dve", lambda h: h.reciprocal(out=stat[:, sidx:sidx + 1], in_=stat[:, sidx:sidx + 1]), reads=[Bstat], writes=[Bstat])
        S.op("dve", lambda h: h.scalar_tensor_tensor(out=t32[:], in0=xbuf[:], scalar=stat[:, sidx:sidx + 1], in1=Avec[:],
                                                     op0=ALU.mult, op1=ALU.mult), reads=[Bx, Bstat, BAv], writes=[Bt32])
        S.op("pool", lambda h: h.tensor_tensor(out=hbuf[:], in0=t32[:], in1=Svec[:], op=ALU.add), reads=[Bt32, BSv], writes=[Bh])

    def transpose_tile_bf16(hbuf, Bh, dstT, col0, Bdst):
        for half in range(2):
            pb = half
            for q in range(8):
                kc = half * 8 + q
                S.op("pe", lambda h, kc=kc, q=q, pb=pb: h.transpose(out=psb[pb][:, q * 128:(q + 1) * 128], in_=hbuf[:, kc * 128:(kc + 1) * 128],
                                                                       identity=identb[:]),
                     reads=[Bh, Bc], writes=[Bpsb[pb]])
            eng = "act" if half == 0 else "dve"
            if eng == "act":
                S.op("act", lambda h, half=half, pb=pb: h.copy(out=dstT[:, half * 8:(half + 1) * 8, col0:col0 + 128],
                                                                 in_=psb[pb][:, :].rearrange("p (q t) -> p q t", q=8)),
                     reads=[Bpsb[pb]], writes=[Bdst])
            else:
                S.op("dve", lambda h, half=half, pb=pb: h.tensor_copy(out=dstT[:, half * 8:(half + 1) * 8, col0:col0 + 128],
                                                                        in_=psb[pb][:, :].rearrange("p (q t) -> p q t", q=8)),
                     reads=[Bpsb[pb]], writes=[Bdst])

    for i in range(2):
        bi = i % 2
        norm_mod_tile(ctx_d[i * 128:(i + 1) * 128, :], xb[bi], Bxb[bi], ch_x[bi], A1c, BA1c, S1c, BS1c, hb[bi], Bhb[bi], i % 8)
        transpose_tile_bf16(hb[bi], Bhb[bi], hcT, i * 128, BhcT)
    for i in range(NT):
        bi = i % 2
        norm_mod_tile(x_d[i * 128:(i + 1) * 128, :], xb[bi], Bxb[bi], ch_x[bi], A1, BA1, S1, BS1, hb[bi], Bhb[bi], i % 8)
        transpose_tile_bf16(hb[bi], Bhb[bi], hT, i * 128, BhT[i])
    if debug:
        d_hT = dbg_out("hT", [128, KC, SEQ], BF16)
        final_ops.append(S.dma("sp", S.chan("dbghT"), lambda h: h.dma_start(out=d_hT, in_=hT[:]), reads=BhT))
    S.barrier()
    AR.reset(m1)
    if stage <= 1:
        return finish(nc, S, final_ops), dbg

    ByT_s = Buf("yT_s")
    win_v = win_d.rearrange("(k p) n -> p k n", p=128)

    m2 = AR.mark()
    decb = AR.alloc("decb", [128, 16], F32)
    Mh = AR.alloc("Mh", [128, H, 128], F32)
    dcol = AR.alloc("dcol", [128, H, 6], F32)
    wctx = AR.alloc("wctx", [128, H, 4], F32)
    Bdecb, BMh, Bdcol, Bwctx = Buf("decb"), Buf("Mh"), Buf("dcol"), Buf("wctx")
    tA = AR.alloc("tA", [128, 128], F32)
    tB = AR.alloc("tB", [128, 128], F32)
    tC = AR.alloc("tC", [128, 128], F32)
    tD = AR.alloc("tD", [128, 128], F32)
    cols = AR.alloc("cols", [128, 8], F32)
    BtA, BtB, BtC, BtD, Bcols = Buf("tA"), Buf("tB"), Buf("tC"), Buf("tD"), Buf("cols")
    S.dma("sp", ch_v, lambda h: h.dma_start(out=decb[:], in_=dec_d.partition_broadcast(128)), writes=[Bdecb])
    S.op("act", lambda h: h.activation(out=decb[:], in_=decb[:], func=AF.Exp, scale=-1.0), reads=[Bdecb], writes=[Bdecb])
    S.op("act", lambda h: h.activation(out=decb[:], in_=decb[:], func=AF.Ln, bias=1.0), reads=[Bdecb], writes=[Bdecb])
    S.op("dve", lambda h: h.tensor_scalar(out=decb[:], in0=decb[:], scalar1=-1.0, scalar2=None, op0=ALU.mult), reads=[Bdecb], writes=[Bdecb])
    S.op("dve", lambda h: h.tensor_scalar(out=tA[:], in0=iorow[:], scalar1=iocol[:, 0:1], scalar2=0.0, op0=ALU.subtract, op1=ALU.max),
         reads=[Bc], writes=[BtA])
    S.op("dve", lambda h: h.tensor_scalar(out=tB[:], in0=iorow[:], scalar1=iocol[:, 0:1], scalar2=-1.0, op0=ALU.subtract, op1=ALU.mult),
         reads=[Bc], writes=[BtB])
    S.op("dve", lambda h: h.tensor_scalar(out=tB[:], in0=tB[:], scalar1=0.0, scalar2=None, op0=ALU.max), reads=[BtB], writes=[BtB])
    S.op("dve", lambda h: h.tensor_scalar(out=tC[:], in0=iorow[:], scalar1=iocol[:, 0:1], scalar2=None, op0=ALU.is_ge), reads=[Bc], writes=[BtC])
    S.op("dve", lambda h: h.tensor_scalar(out=tD[:], in0=iorow[:], scalar1=iocol[:, 0:1], scalar2=None, op0=ALU.is_le), reads=[Bc], writes=[BtD])
    for ci, (mul, add) in enumerate([(1.0, 1.0), (-1.0, 128.0), (-1.0, 127.0), (1.0, 0.0), (0.0, 128.0), (-1.0, 255.0), (-1.0, 127.0), (1.0, 128.0)]):
        S.op("dve", lambda h, ci=ci, mul=mul, add=add: h.tensor_scalar(out=cols[:, ci:ci + 1], in0=iocol[:, 0:1], scalar1=mul, scalar2=add,
                                                                      op0=ALU.mult, op1=ALU.add), reads=[Bc], writes=[Bcols])
    ex1 = AR.alloc("ex1", [128, 128], F32)
    ex2 = AR.alloc("ex2", [128, 128], F32)
    Bex1, Bex2 = Buf("ex1"), Buf("ex2")
    for hh in range(H):
        lf = decb[:, hh:hh + 1]
        lb = decb[:, 8 + hh:9 + hh]
        S.op("act", lambda h, lf=lf: h.activation(out=ex1[:], in_=tA[:], func=AF.Exp, scale=lf), reads=[BtA, Bdecb], writes=[Bex1])
        S.op("act", lambda h, lb=lb: h.activation(out=ex2[:], in_=tB[:], func=AF.Exp, scale=lb), reads=[BtB, Bdecb], writes=[Bex2])
        S.op("dve", lambda h: h.tensor_tensor(out=ex1[:], in0=ex1[:], in1=tC[:], op=ALU.mult), reads=[Bex1, BtC], writes=[Bex1])
        S.op("dve", lambda h: h.tensor_tensor(out=ex2[:], in0=ex2[:], in1=tD[:], op=ALU.mult), reads=[Bex2, BtD], writes=[Bex2])
        S.op("dve", lambda h, hh=hh: h.tensor_tensor(out=Mh[:, hh, :], in0=ex1[:], in1=ex2[:], op=ALU.add), reads=[Bex1, Bex2], writes=[BMh])
        for (dst, ci, lg) in [(0, 0, lf), (1, 1, lb), (2, 2, lf), (3, 3, lb), (4, 4, lf), (5, 4, lb)]:
            S.op("act", lambda h, hh=hh, dst=dst, ci=ci, lg=lg: h.activation(out=dcol[:, hh, dst:dst + 1], in_=cols[:, ci:ci + 1], func=AF.Exp, scale=lg),
                 reads=[Bcols, Bdecb], writes=[Bdcol])
        for (dst, ci, lg) in [(0, 5, lf), (1, 6, lf), (2, 3, lb), (3, 7, lb)]:
            S.op("act", lambda h, hh=hh, dst=dst, ci=ci, lg=lg: h.activation(out=wctx[:, hh, dst:dst + 1], in_=cols[:, ci:ci + 1], func=AF.Exp, scale=lg),
                 reads=[Bcols, Bdecb], writes=[Bwctx])

    cosL = AR.alloc("cosL", [128, NT, 64], F32)
    sinL = AR.alloc("sinL", [128, NT, 64], F32)
    cosT = cosL[:, :, :].unsqueeze(2).to_broadcast([128, NT, 2, 64])
    sinT = sinL[:, :, :].unsqueeze(2).to_broadcast([128, NT, 2, 64])
    cosC = AR.alloc("cosC", [128, 2, 64], F32)
    sinC = AR.alloc("sinC", [128, 2, 64], F32)
    Brope = Buf("rope")
    cos_lat = cos_d[NCTX:NCTX + SEQ, :].rearrange("(t p) f -> p t f", p=128)
    sin_lat = sin_d[NCTX:NCTX + SEQ, :].rearrange("(t p) f -> p t f", p=128)
    S.dma("sp", ch_v, lambda h: h.dma_start(out=cosL[:], in_=cos_lat), writes=[Brope])
    S.dma("sp", ch_v, lambda h: h.dma_start(out=sinL[:], in_=sin_lat), writes=[Brope], cont=True)
    S.dma("sp", ch_v, lambda h: h.dma_start(out=cosC[:], in_=cos_d[0:NCTX, :].rearrange("(t p) f -> p t f", p=128)), writes=[Brope], cont=True)
    S.dma("sp", ch_v, lambda h: h.dma_start(out=sinC[:], in_=sin_d[0:NCTX, :].rearrange("(t p) f -> p t f", p=128)), writes=[Brope], cont=True)
    gnwb = AR.alloc("gnwb", [128, DRET], F32)
    Bgnwb = Buf("gnwb")
    load_row_bcast(gnwb, gnw_d, ch_v, Bgnwb)

    R0 = AR.alloc("R0", [128, H, 2, 128], F32)
    BR0 = Buf("R0")
    m2b = AR.mark()
    wkv = [AR.alloc(f"wkv{i}", [128, KC, 256], BF16) for i in range(2)]
    Bwkv = [Buf("wkv0"), Buf("wkv1")]
    ch_wkv = [S.chan("wkv0"), S.chan("wkv1")]
    kc32 = AR.alloc("kc32", [128, 2, 128], F32)
    kcr = AR.alloc("kcr", [128, 2, 128], BF16)
    vwf = AR.alloc("vwf", [128, 2, 128], BF16)
    vwb = AR.alloc("vwb", [128, 2, 128], BF16)
    rt1 = AR.alloc("rt1", [128, 2, 64], F32)
    rt2 = AR.alloc("rt2", [128, 2, 64], F32)
    Bkc32, Bkcr, Bvwf, Bvwb, Brt1, Brt2 = (Buf(n) for n in ["kc32", "kcr", "vwf", "vwb", "rt1", "rt2"])
    for hh in range(H):
        bi = hh % 2
        S.dma("pool", ch_wkv[bi], lambda h, hh=hh, bi=bi: h.dma_start(out=wkv[bi][:, :, 0:128], in_=win_v[:, :, DRET + hh * 128:DRET + (hh + 1) * 128]),
              writes=[Bwkv[bi]])
        S.dma("pool", ch_wkv[bi], lambda h, hh=hh, bi=bi: h.dma_start(out=wkv[bi][:, :, 128:256], in_=win_v[:, :, 2 * DRET + hh * 128:2 * DRET + (hh + 1) * 128]),
              writes=[Bwkv[bi]], cont=True)
        for t in range(2):
            bank = t
            for kc in range(KC):
                S.op("pe", lambda h, kc=kc, t=t, bi=bi, bank=bank: h.matmul(psf[bank][:, 0:256], lhsT=hcT[:, kc, t * 128:(t + 1) * 128], rhs=wkv[bi][:, kc, :],
                                                                             start=(kc == 0), stop=(kc == KC - 1)),
                     reads=[BhcT, Bwkv[bi]], writes=[Bpsf[bank]])
            S.op("act", lambda h, t=t, bank=bank: h.copy(out=kc32[:, t, :], in_=psf[bank][:, 0:128]), reads=[Bpsf[bank]], writes=[Bkc32])
            S.op("dve", lambda h, t=t, bank=bank, hh=hh: h.tensor_scalar(out=vwf[:, t, :], in0=psf[bank][:, 128:256], scalar1=wctx[:, hh, t:t + 1], scalar2=None, op0=ALU.mult),
                 reads=[Bpsf[bank], Bwctx], writes=[Bvwf])
            S.op("dve", lambda h, t=t, bank=bank, hh=hh: h.tensor_scalar(out=vwb[:, t, :], in0=psf[bank][:, 128:256], scalar1=wctx[:, hh, 2 + t:3 + t], scalar2=None, op0=ALU.mult),
                 reads=[Bpsf[bank], Bwctx], writes=[Bvwb])
        k1 = kc32[:, :, 0:64]
        k2 = kc32[:, :, 64:128]
        S.op("dve", lambda h: h.tensor_tensor(out=rt1[:], in0=k1, in1=cosC[:], op=ALU.mult), reads=[Bkc32, Brope], writes=[Brt1])
        S.op("pool", lambda h: h.tensor_tensor(out=rt2[:], in0=k2, in1=sinC[:], op=ALU.mult), reads=[Bkc32, Brope], writes=[Brt2])
        S.op("dve", lambda h: h.tensor_tensor(out=kcr[:, :, 0:64], in0=rt1[:], in1=rt2[:], op=ALU.subtract), reads=[Brt1, Brt2], writes=[Bkcr])
        S.op("dve", lambda h: h.tensor_tensor(out=rt1[:], in0=k1, in1=sinC[:], op=ALU.mult), reads=[Bkc32, Brope, Bkcr], writes=[Brt1])
        S.op("pool", lambda h: h.tensor_tensor(out=rt2[:], in0=k2, in1=cosC[:], op=ALU.mult), reads=[Bkc32, Brope, Bkcr], writes=[Brt2])
        S.op("dve", lambda h: h.tensor_tensor(out=kcr[:, :, 64:128], in0=rt1[:], in1=rt2[:], op=ALU.add), reads=[Brt1, Brt2], writes=[Bkcr])
        for di, vw, Bvw in [(0, vwf, Bvwf), (1, vwb, Bvwb)]:
            bank = 2 + di
            for t in range(2):
                S.op("pe", lambda h, t=t, bank=bank, vw=vw: h.matmul(psf[bank][:, 0:128], lhsT=kcr[:, t, :], rhs=vw[:, t, :], start=(t == 0), stop=(t == 1)),
                     reads=[Bkcr, Bvw], writes=[Bpsf[bank]])
            S.op("act", lambda h, hh=hh, di=di, bank=bank: h.copy(out=R0[:, hh, di, :], in_=psf[bank][:, 0:128]), reads=[Bpsf[bank]], writes=[BR0])
    if debug:
        d_R0 = dbg_out("R0", [128, H, 2, 128])
        final_ops.append(S.dma("sp", S.chan("dbgR0"), lambda h: h.dma_start(out=d_R0, in_=R0[:]), reads=[BR0]))
    S.barrier()
    AR.reset(m2b)
    if stage <= 2:
        return finish(nc, S, final_ops), dbg

    m2c = AR.mark()
    wq_off = AR.mark()
    wq1 = AR.alloc("wq", [128, KC, 512], BF16)
    wq = [wq1, wq1]
    Bwq1 = Buf("wq")
    Bwq = [Bwq1, Bwq1]
    ch_wq1 = S.chan("wq")
    ch_wq = [ch_wq1, ch_wq1]
    qT = AR.alloc_at("qT", [128, SEQ], BF16, wq_off)
    qdfT = AR.alloc_at("qdfT", [128, SEQ], BF16, wq_off + 4096)
    qdbT = AR.alloc_at("qdbT", [128, SEQ], BF16, wq_off + 8192)
    kT = AR.alloc_at("kT", [128, SEQ], BF16, wq_off + 12288)
    BqT = BqdfT = BqdbT = BkT = Bwq1
    qk_off = AR.mark()
    qk32 = AR.alloc("qk32", [128, NT, 2, 2, 64], F32)
    Bqk32 = Buf("qk32")
    o32 = AR.alloc_at("o32", [128, NT, 128], F32, qk_off)
    ytok = AR.alloc_at("ytok", [128, NT, 128], BF16, qk_off + 8192)
    yTh = AR.alloc_at("yTh", [128, SEQ], BF16, qk_off + 12288)
    Bo32 = Bytok = ByTh = Bqk32
    ta_off = AR.mark()
    ta = AR.alloc("ta", [128, NT, 2, 64], F32)
    Bta = Buf("ta")
    Sfb = AR.alloc_at("Sfb", [128, NT, 128], BF16, ta_off)
    Sbb = AR.alloc_at("Sbb", [128, NT, 128], BF16, ta_off + 4096)
    BSfb = BSbb = Bta
    tb = AR.alloc("tb", [128, NT, 2, 64], F32)
    rotb = AR.alloc("rotb", [128, NT, 2, 2, 64], BF16)
    qdf = AR.alloc("qdf", [128, NT, 2, 64], BF16)
    qdb = AR.alloc("qdb", [128, NT, 2, 64], BF16)
    kdf = AR.alloc("kdf", [128, NT, 2, 64], BF16)
    kdb = AR.alloc("kdb", [128, NT, 2, 64], BF16)
    vtok = AR.alloc("vtok", [128, NT, 128], BF16)
    sg = AR.alloc_at("sg", [128, NT, 128], F32, hcT_off)
    Rf = AR.alloc("Rf", [128, 128], F32)
    Rb = AR.alloc("Rb", [128, 128], F32)
    SM = [AR.alloc(f"SM{i}", [128, 128], BF16) for i in range(2)]
    bnst = AR.alloc("bnst", [128, NT, 6], F32)
    mv = AR.alloc("mv", [128, NT, 2], F32)
    rstd = AR.alloc("rstd", [128, NT], F32)
    (Btb, Brotb, Bqdf, Bqdb, Bkdf, Bkdb, Bvtok, Bsg, BRf, BRb, Bbnst, Bmv, Brstd) = (
        Buf(n) for n in ["tb", "rotb", "qdf", "qdb", "kdf", "kdb", "vtok", "sg", "Rf", "Rb", "bnst", "mv", "rstd"])
    BSM = [Buf("SM0"), Buf("SM1")]
    ch_yT = S.chan("yTout")

    for hh in range(H):
        bi = hh % 2
        for seg in range(4):
            S.dma("pool", ch_wq[bi], lambda h, hh=hh, bi=bi, seg=seg: h.dma_start(out=wq[bi][:, :, seg * 128:(seg + 1) * 128],
                                                                                    in_=win_v[:, :, seg * DRET + hh * 128:seg * DRET + (hh + 1) * 128]),
                  writes=[Bwq[bi]], cont=(seg > 0))
        for i in range(NT):
            bank = i % 4
            for kc in range(KC):
                S.op("pe", lambda h, kc=kc, i=i, bi=bi, bank=bank: h.matmul(psf[bank][:, :], lhsT=hT[:, kc, i * 128:(i + 1) * 128], rhs=wq[bi][:, kc, :],
                                                                             start=(kc == 0), stop=(kc == KC - 1)),
                     reads=[BhT[i], Bwq[bi]], writes=[Bpsf[bank]])
            S.op("act", lambda h, i=i, bank=bank: h.copy(out=qk32[:, i, :, :, :], in_=psf[bank][:, 0:256].rearrange("p (a b c) -> p a b c", a=2, b=2)),
                 reads=[Bpsf[bank]], writes=[Bqk32])
            S.op("act", lambda h, i=i, bank=bank: h.copy(out=vtok[:, i, :], in_=psf[bank][:, 256:384]), reads=[Bpsf[bank]], writes=[Bvtok])
            S.op("act", lambda h, i=i, bank=bank: h.activation(out=sg[:, i, :], in_=psf[bank][:, 384:512], func=AF.Silu), reads=[Bpsf[bank]], writes=[Bsg])
        X1 = qk32[:, :, :, 0, :]
        X2 = qk32[:, :, :, 1, :]
        S.op("dve", lambda h: h.tensor_tensor(out=ta[:], in0=X1, in1=cosT, op=ALU.mult), reads=[Bqk32, Brope], writes=[Bta])
        S.op("pool", lambda h: h.tensor_tensor(out=tb[:], in0=X2, in1=sinT, op=ALU.mult), reads=[Bqk32, Brope], writes=[Btb])
        S.op("dve", lambda h: h.tensor_tensor(out=ta[:], in0=ta[:], in1=tb[:], op=ALU.subtract), reads=[Bta, Btb], writes=[Bta])
        S.op("pool", lambda h: h.tensor_tensor(out=tb[:], in0=X1, in1=sinT, op=ALU.mult), reads=[Bqk32, Brope, Bta], writes=[Btb])
        S.op("dve", lambda h: h.tensor_tensor(out=X1, in0=X2, in1=cosT, op=ALU.mult), reads=[Bqk32, Brope, Btb], writes=[Bqk32])
        S.op("dve", lambda h: h.tensor_tensor(out=tb[:], in0=tb[:], in1=X1, op=ALU.add), reads=[Btb, Bqk32], writes=[Btb])
        S.op("act", lambda h: h.mul(out=rotb[:, :, 0, 0, :], in_=ta[:, :, 0, :], mul=QSCALE), reads=[Bta], writes=[Brotb])
        S.op("act", lambda h: h.copy(out=rotb[:, :, 1, 0, :], in_=ta[:, :, 1, :]), reads=[Bta], writes=[Brotb])
        S.op("act", lambda h: h.mul(out=rotb[:, :, 0, 1, :], in_=tb[:, :, 0, :], mul=QSCALE), reads=[Btb], writes=[Brotb])
        S.op("act", lambda h: h.copy(out=rotb[:, :, 1, 1, :], in_=tb[:, :, 1, :]), reads=[Btb], writes=[Brotb])
        for (dst, Bd, src_i, col, sc) in [(qdf, Bqdf, 0, 0, QSCALE), (qdb, Bqdb, 0, 1, QSCALE), (kdf, Bkdf, 1, 2, 1.0), (kdb, Bkdb, 1, 3, 1.0)]:
            S.op("dve", lambda h, dst=dst, src_i=src_i, col=col, hh=hh, sc=sc: h.tensor_scalar(out=dst[:, :, 0, :], in0=ta[:, :, src_i, :], scalar1=dcol[:, hh, col:col + 1],
                                                                                              scalar2=sc, op0=ALU.mult, op1=ALU.mult), reads=[Bta, Bdcol], writes=[Bd])
            S.op("pool", lambda h, dst=dst, src_i=src_i, col=col, hh=hh, sc=sc: h.tensor_scalar(out=dst[:, :, 1, :], in0=tb[:, :, src_i, :], scalar1=dcol[:, hh, col:col + 1],
                                                                                               scalar2=sc, op0=ALU.mult, op1=ALU.mult), reads=[Btb, Bdcol], writes=[Bd])
        srcs = [(lambda i: rotb[:, i, 0, :, :].rearrange("p a b -> p (a b)"), Brotb, qT, BqT),
                (lambda i: qdf[:, i, :, :].rearrange("p a b -> p (a b)"), Bqdf, qdfT, BqdfT),
                (lambda i: qdb[:, i, :, :].rearrange("p a b -> p (a b)"), Bqdb, qdbT, BqdbT),
                (lambda i: rotb[:, i, 1, :, :].rearrange("p a b -> p (a b)"), Brotb, kT, BkT)]
        cnt = 0
        for (srcf, Bsrc, dstT, BdstT) in srcs:
            for half in range(2):
                pb = cnt % 2
                cnt += 1
                for q in range(8):
                    i = half * 8 + q
                    S.op("pe", lambda h, i=i, q=q, pb=pb, srcf=srcf: h.transpose(out=psb[pb][:, q * 128:(q + 1) * 128], in_=srcf(i), identity=identb[:]),
                         reads=[Bsrc, Bc], writes=[Bpsb[pb]])
                if pb == 0:
                    S.op("act", lambda h, half=half, pb=pb, dstT=dstT: h.copy(out=dstT[:, half * 1024:(half + 1) * 1024], in_=psb[pb][:, :]),
                         reads=[Bpsb[pb]], writes=[BdstT])
                else:
                    S.op("dve", lambda h, half=half, pb=pb, dstT=dstT: h.tensor_copy(out=dstT[:, half * 1024:(half + 1) * 1024], in_=psb[pb][:, :]),
                         reads=[Bpsb[pb]], writes=[BdstT])
        S.op("dve", lambda h, hh=hh: h.tensor_copy(out=Rf[:], in_=R0[:, hh, 0, :]), reads=[BR0], writes=[BRf])
        S.op("dve", lambda h, hh=hh: h.tensor_copy(out=Rb[:], in_=R0[:, hh, 1, :]), reads=[BR0], writes=[BRb])
        S.op("act", lambda h: h.copy(out=Sfb[:, 0, :], in_=Rf[:]), reads=[BRf], writes=[BSfb])
        S.op("act", lambda h: h.copy(out=Sbb[:, NT - 1, :], in_=Rb[:]), reads=[BRb], writes=[BSbb])
        for step in range(NT - 1):
            i_f = step
            i_b = NT - 1 - step
            S.op("pe", lambda h, i=i_f: h.matmul(psf[4][:, 0:128], lhsT=kdf[:, i, :, :].rearrange("p a b -> p (a b)"), rhs=vtok[:, i, :], start=True, stop=True),
                 reads=[Bkdf, Bvtok], writes=[Bpsf[4]])
            S.op("dve", lambda h, hh=hh: h.scalar_tensor_tensor(out=Rf[:], in0=Rf[:], scalar=dcol[:, hh, 4:5], in1=psf[4][:, 0:128], op0=ALU.mult, op1=ALU.add),
                 reads=[BRf, Bdcol, Bpsf[4]], writes=[BRf])
            S.op("act", lambda h, i=i_f: h.copy(out=Sfb[:, i + 1, :], in_=Rf[:]), reads=[BRf], writes=[BSfb])
            S.op("pe", lambda h, i=i_b: h.matmul(psf[5][:, 0:128], lhsT=kdb[:, i, :, :].rearrange("p a b -> p (a b)"), rhs=vtok[:, i, :], start=True, stop=True),
                 reads=[Bkdb, Bvtok], writes=[Bpsf[5]])
            S.op("dve", lambda h, hh=hh: h.scalar_tensor_tensor(out=Rb[:], in0=Rb[:], scalar=dcol[:, hh, 5:6], in1=psf[5][:, 0:128], op0=ALU.mult, op1=ALU.add),
                 reads=[BRb, Bdcol, Bpsf[5]], writes=[BRb])
            S.op("act", lambda h, i=i_b: h.copy(out=Sbb[:, i - 1, :], in_=Rb[:]), reads=[BRb], writes=[BSbb])
        for i in range(NT):
            sb_ = i % 2
            bs = i % 2
            bo = 2 + (i % 2)
            cs = slice(i * 128, (i + 1) * 128)
            S.op("pe", lambda h, cs=cs, bs=bs: h.matmul(psf[bs][:, 0:128], lhsT=kT[:, cs], rhs=qT[:, cs], start=True, stop=True),
                 reads=[BkT, BqT], writes=[Bpsf[bs]])
            S.op("dve", lambda h, bs=bs, sb_=sb_, hh=hh: h.tensor_tensor(out=SM[sb_][:], in0=psf[bs][:, 0:128], in1=Mh[:, hh, :], op=ALU.mult),
                 reads=[Bpsf[bs], BMh], writes=[BSM[sb_]])
            S.op("pe", lambda h, i=i, sb_=sb_, bo=bo: h.matmul(psf[bo][:, 0:128], lhsT=SM[sb_][:], rhs=vtok[:, i, :], start=True, stop=False),
                 reads=[BSM[sb_], Bvtok], writes=[Bpsf[bo]])
            S.op("pe", lambda h, i=i, cs=cs, bo=bo: h.matmul(psf[bo][:, 0:128], lhsT=qdfT[:, cs], rhs=Sfb[:, i, :], start=False, stop=False),
                 reads=[BqdfT, BSfb], writes=[Bpsf[bo]])
            S.op("pe", lambda h, i=i, cs=cs, bo=bo: h.matmul(psf[bo][:, 0:128], lhsT=qdbT[:, cs], rhs=Sbb[:, i, :], start=False, stop=True),
                 reads=[BqdbT, BSbb], writes=[Bpsf[bo]])
            S.op("act", lambda h, i=i, bo=bo: h.copy(out=o32[:, i, :], in_=psf[bo][:, 0:128]), reads=[Bpsf[bo]], writes=[Bo32])
            S.op("dve", lambda h, i=i: h.bn_stats(out=bnst[:, i, :], in_=o32[:, i, :]), reads=[Bo32], writes=[Bbnst])
            S.op("dve", lambda h, i=i: h.bn_aggr(out=mv[:, i, :], in_=bnst[:, i, :]), reads=[Bbnst], writes=[Bmv])
        S.op("act", lambda h: h.activation(out=rstd[:], in_=mv[:, :, 1], func=AF.Sqrt, bias=GN_EPS), reads=[Bmv], writes=[Brstd])
        S.op("dve", lambda h: h.reciprocal(out=rstd[:], in_=rstd[:]), reads=[Brstd], writes=[Brstd])
        S.op("pool", lambda h, hh=hh: h.tensor_tensor(out=sg[:], in0=sg[:], in1=gnwb[:, hh * 128:(hh + 1) * 128].unsqueeze(1).to_broadcast([128, NT, 128]), op=ALU.mult),
             reads=[Bsg, Bgnwb], writes=[Bsg])
        for i in range(NT):
            S.op("dve", lambda h, i=i: h.tensor_scalar(out=o32[:, i, :], in0=o32[:, i, :], scalar1=mv[:, i, 0:1], scalar2=rstd[:, i:i + 1], op0=ALU.subtract, op1=ALU.mult),
                 reads=[Bo32, Bmv, Brstd], writes=[Bo32])
        S.op("pool", lambda h: h.tensor_tensor(out=ytok[:], in0=o32[:], in1=sg[:], op=ALU.mult), reads=[Bo32, Bsg], writes=[Bytok])
        for half in range(2):
            pb = half
            for q in range(8):
                i = half * 8 + q
                S.op("pe", lambda h, i=i, q=q, pb=pb: h.transpose(out=psb[pb][:, q * 128:(q + 1) * 128], in_=ytok[:, i, :], identity=identb[:]),
                     reads=[Bytok, Bc], writes=[Bpsb[pb]])
            if half == 0:
                S.op("act", lambda h, half=half, pb=pb: h.copy(out=yTh[:, half * 1024:(half + 1) * 1024], in_=psb[pb][:, :]), reads=[Bpsb[pb]], writes=[ByTh])
            else:
                S.op("dve", lambda h, half=half, pb=pb: h.tensor_copy(out=yTh[:, half * 1024:(half + 1) * 1024], in_=psb[pb][:, :]), reads=[Bpsb[pb]], writes=[ByTh])
        S.dma("sp", ch_yT, lambda h, hh=hh: h.dma_start(out=yT_s[hh * 128:(hh + 1) * 128, :], in_=yTh[:]), reads=[ByTh], writes=[ByT_s])
    S.barrier()
    AR.reset(m2c)
    if stage <= 3:
        if debug:
            d_yT = dbg_out("yT", [D, SEQ], BF16)
            ld = AR.alloc("dbgld", [128, KC, SEQ], BF16)
            Bld = Buf("dbgld")
            S.dma("sp", S.chan("dbgy1"), lambda h: h.dma_start(out=ld[:], in_=yT_s.rearrange("(c p) t -> p c t", p=128)), reads=[ByT_s], writes=[Bld])
            final_ops.append(S.dma("sp", S.chan("dbgy2"), lambda h: h.dma_start(out=d_yT.rearrange("(c p) t -> p c t", p=128), in_=ld[:]), reads=[Bld]))
        return finish(nc, S, final_ops), dbg

    m2d = AR.mark()
    cvrows = AR.alloc("cvrows", [24, 128], F32)
    cvT = AR.alloc("cvT", [128, 24], F32)
    cwrows = AR.alloc("cwrows", [CW, DCONV], F32)
    cwT = AR.alloc("cwT", [128, 8, CW], F32)
    Bcvrows, BcvT, Bcwrows, BcwT = Buf("cvrows"), Buf("cvT"), Buf("cwrows"), Buf("cwT")
    S.dma("sp", ch_v, lambda h: h.dma_start(out=cvrows[:], in_=cvec_d), writes=[Bcvrows])
    S.dma("sp", ch_v, lambda h: h.dma_start(out=cwrows[:], in_=convw_d), writes=[Bcwrows], cont=True)
    transpose_rows(cvrows[:], 24, cvT[:], 0, Bpsf[0], [Bcvrows], [BcvT])
    for cc in range(8):
        S.op("pe", lambda h, cc=cc: h.transpose(out=psf[1][:, 0:CW], in_=cwrows[:, cc * 128:(cc + 1) * 128], identity=ident[0:CW, 0:CW]),
             reads=[Bcwrows, Bc], writes=[Bpsf[1]])
        S.op("act", lambda h, cc=cc: h.copy(out=cwT[:, cc, :], in_=psf[1][:, 0:CW]), reads=[Bpsf[1]], writes=[BcwT])
    wc = [AR.alloc(f"wc{i}", [128, KC, 256], BF16) for i in range(2)]
    Bwc = [Buf("wc0"), Buf("wc1")]
    ch_wc = [S.chan("wc0"), S.chan("wc1")]
    sig = AR.alloc("sig", [128, 512], F32)
    uu = AR.alloc("uu", [128, 8, 64], F32)
    cvo = AR.alloc("cvo", [128, 8, 8, 64], F32)
    sq = AR.alloc("sq", [128, 512], F32)
    mean = AR.alloc("mean", [128, 512], F32)
    msq = AR.alloc("msq", [128, 512], F32)
    rsd = AR.alloc("rsd", [128, 512], F32)
    tn = AR.alloc("tn", [128, 512], F32)
    ycv = [AR.alloc(f"ycv{i}", [128, 512], BF16) for i in range(2)]
    Bsig, Buu, Bcvo, Bsq, Bmean, Bmsq, Brsd, Btn = (Buf(n) for n in ["sig", "uu", "cvo", "sq", "mean", "msq", "rsd", "tn"])
    Bycv = [Buf("ycv0"), Buf("ycv1")]
    ch_ycv = [S.chan("ycv0"), S.chan("ycv1")]
    pcount = 0
    for tbk in range(4):
        tsl = slice(tbk * 512, (tbk + 1) * 512)
        for cc in range(8):
            bi = pcount % 2
            pcount += 1
            S.dma("pool", ch_wc[bi], lambda h, cc=cc, bi=bi: h.dma_start(out=wc[bi][:, :, 0:128], in_=win_v[:, :, 4 * DRET + cc * 128:4 * DRET + (cc + 1) * 128]),
                  writes=[Bwc[bi]])
            S.dma("pool", ch_wc[bi], lambda h, cc=cc, bi=bi: h.dma_start(out=wc[bi][:, :, 128:256],
                                                                          in_=win_v[:, :, 4 * DRET + DCONV + cc * 128:4 * DRET + DCONV + (cc + 1) * 128]),
                  writes=[Bwc[bi]], cont=True)
            ba, bb = 0 + 2 * (cc % 2), 1 + 2 * (cc % 2)
            for kc in range(KC):
                S.op("pe", lambda h, kc=kc, bi=bi, ba=ba, tsl=tsl: h.matmul(psf[ba][:, :], lhsT=wc[bi][:, kc, 0:128], rhs=hT[:, kc, tsl], start=(kc == 0), stop=(kc == KC - 1)),
                     reads=[Bwc[bi]] + BhT[tbk * 4:(tbk + 1) * 4], writes=[Bpsf[ba]])
            for kc in range(KC):
                S.op("pe", lambda h, kc=kc, bi=bi, bb=bb, tsl=tsl: h.matmul(psf[bb][:, :], lhsT=wc[bi][:, kc, 128:256], rhs=hT[:, kc, tsl], start=(kc == 0), stop=(kc == KC - 1)),
                     reads=[Bwc[bi]] + BhT[tbk * 4:(tbk + 1) * 4], writes=[Bpsf[bb]])
            S.op("act", lambda h, bb=bb: h.activation(out=sig[:], in_=psf[bb][:, :], func=AF.Sigmoid), reads=[Bpsf[bb]], writes=[Bsig])
            S.op("dve", lambda h, ba=ba: h.tensor_tensor(out=uu[:].rearrange("p a b -> p (a b)"), in0=psf[ba][:, :], in1=sig[:], op=ALU.mult),
                 reads=[Bpsf[ba], Bsig], writes=[Buu])
            acc = cvo[:, cc, :, :]
            S.op("dve", lambda h, cc=cc, acc=acc: h.tensor_scalar(out=acc, in0=uu[:], scalar1=cwT[:, cc, 15:16], scalar2=cvT[:, cc:cc + 1], op0=ALU.mult, op1=ALU.add),
                 reads=[Buu, BcwT, BcvT], writes=[Bcvo])
            for k in range(CW):
                o = k - 15
                if o == 0:
                    continue
                lo, hi = max(0, -o), min(64, 64 - o)
                S.op("dve", lambda h, cc=cc, k=k, o=o, lo=lo, hi=hi: h.scalar_tensor_tensor(out=cvo[:, cc, :, lo:hi], in0=uu[:, :, lo + o:hi + o], scalar=cwT[:, cc, k:k + 1],
                                                                                             in1=cvo[:, cc, :, lo:hi], op0=ALU.mult, op1=ALU.add),
                     reads=[Buu, BcwT, Bcvo], writes=[Bcvo])
            accf = cvo[:, cc, :, :].rearrange("p a b -> p (a b)")
            S.op("act", lambda h, accf=accf: h.activation(out=sq[:], in_=accf, func=AF.Square), reads=[Bcvo], writes=[Bsq])
            S.op("pe", lambda h, accf=accf, cc=cc: h.matmul(psf[4][:, :], lhsT=ones32[:], rhs=accf, start=(cc == 0), stop=(cc == 7)), reads=[Bcvo, Bc], writes=[Bpsf[4]])
            S.op("pe", lambda h, cc=cc: h.matmul(psf[5][:, :], lhsT=ones32[:], rhs=sq[:], start=(cc == 0), stop=(cc == 7)), reads=[Bsq, Bc], writes=[Bpsf[5]])
        S.op("act", lambda h: h.activation(out=mean[:], in_=psf[4][:, :], func=AF.Identity, scale=1.0 / DCONV), reads=[Bpsf[4]], writes=[Bmean])
        S.op("dve", lambda h: h.tensor_tensor(out=msq[:], in0=mean[:], in1=mean[:], op=ALU.mult), reads=[Bmean], writes=[Bmsq])
        S.op("dve", lambda h: h.scalar_tensor_tensor(out=rsd[:], in0=psf[5][:, :], scalar=1.0 / DCONV, in1=msq[:], op0=ALU.mult, op1=ALU.subtract),
             reads=[Bpsf[5], Bmsq], writes=[Brsd])
        S.op("act", lambda h: h.activation(out=rsd[:], in_=rsd[:], func=AF.Sqrt, bias=EPS), reads=[Brsd], writes=[Brsd])
        S.op("dve", lambda h: h.reciprocal(out=rsd[:], in_=rsd[:]), reads=[Brsd], writes=[Brsd])
        for cc in range(8):
            yb = cc % 2
            accf = cvo[:, cc, :, :].rearrange("p a b -> p (a b)")
            S.op("dve", lambda h, accf=accf: h.tensor_tensor(out=tn[:], in0=accf, in1=mean[:], op=ALU.subtract), reads=[Bcvo, Bmean], writes=[Btn])
            S.op("pool", lambda h: h.tensor_tensor(out=tn[:], in0=tn[:], in1=rsd[:], op=ALU.mult), reads=[Btn, Brsd], writes=[Btn])
            S.op("act", lambda h, cc=cc, yb=yb: h.activation(out=ycv[yb][:], in_=tn[:], func=AF.Silu, scale=cvT[:, 8 + cc:9 + cc], bias=cvT[:, 16 + cc:17 + cc]),
                 reads=[Btn, BcvT], writes=[Bycv[yb]])
            S.dma("sp", ch_ycv[yb], lambda h, cc=cc, yb=yb, tsl=tsl: h.dma_start(out=yT_s[DRET + cc * 128:DRET + (cc + 1) * 128, tsl], in_=ycv[yb][:]),
                  reads=[Bycv[yb]], writes=[ByT_s])
    S.barrier()
    AR.reset(m2)
    AR.reset(persist_mark)
    if stage <= 4:
        if debug:
            d_yT = dbg_out("yT", [D, SEQ], BF16)
            ld = AR.alloc("dbgld", [128, KC, SEQ], BF16)
            Bld = Buf("dbgld")
            S.dma("sp", S.chan("dbgy1"), lambda h: h.dma_start(out=ld[:], in_=yT_s.rearrange("(c p) t -> p c t", p=128)), reads=[ByT_s], writes=[Bld])
            final_ops.append(S.dma("sp", S.chan("dbgy2"), lambda h: h.dma_start(out=d_yT.rearrange("(c p) t -> p c t", p=128), in_=ld[:]), reads=[Bld]))
        return finish(nc, S, final_ops), dbg

    m3 = AR.mark()
    wo = AR.alloc("wo", [128, KC, D], BF16)
    Bwo = Buf("wo")
    ch_wo = S.chan("wo")
    wout_v = wout_d.rearrange("(k p) n -> p k n", p=128)
    for j in range(4):
        S.dma("pool", ch_wo, lambda h, j=j: h.dma_start(out=wo[:, :, j * 512:(j + 1) * 512], in_=wout_v[:, :, j * 512:(j + 1) * 512]), writes=[Bwo], cont=(j > 0))
    G1 = AR.alloc("G1", [128, D], F32)
    A2 = AR.alloc("A2", [128, D], F32)
    S2 = AR.alloc("S2", [128, D], F32)
    wt3 = AR.alloc("wt3", [128, D], F32)
    BG1, BA2, BS2, Bwt3 = Buf("G1"), Buf("A2"), Buf("S2"), Buf("wt3")
    load_mod_bcast(G1, 0, 2, ch_v, BG1)
    load_row_bcast(wt3, pomw_d, ch_v, Bwt3)
    S.op("dve", lambda h: h.tensor_tensor(out=G1[:], in0=G1[:], in1=wt3[:], op=ALU.mult), reads=[BG1, Bwt3], writes=[BG1])
    load_mod_bcast(A2, 0, 4, ch_v, BA2)
    load_row_bcast(wt3, pfw_d, ch_v, Bwt3)
    S.op("dve", lambda h: h.scalar_tensor_tensor(out=A2[:], in0=A2[:], scalar=1.0, in1=wt3[:], op0=ALU.add, op1=ALU.mult), reads=[BA2, Bwt3], writes=[BA2])
    load_mod_bcast(S2, 0, 3, ch_v, BS2)
    rw32 = AR.alloc("rw32", [128, KC, NE], F32)
    rbb = AR.alloc("rbb", [128, NE], F32)
    ebase = AR.alloc("ebase", [128, NE], F32)
    Brw, Brbb, Bebase = Buf("rw32"), Buf("rbb"), Buf("ebase")
    S.dma("sp", ch_v, lambda h: h.dma_start(out=rw32[:], in_=rw_d.rearrange("(k p) e -> p k e", p=128)), writes=[Brw])
    S.dma("sp", ch_v, lambda h: h.dma_start(out=rbb[:], in_=rb_d.partition_broadcast(128)), writes=[Brbb], cont=True)
    S.op("dve", lambda h: h.tensor_scalar(out=ebase[:], in0=iorow[:, 0:NE], scalar1=float(CAP), scalar2=None, op0=ALU.mult), reads=[Bc], writes=[Bebase])
    yTt = [AR.alloc(f"yTt{i}", [128, KC, 128], BF16) for i in range(2)]
    ByTt = [Buf("yTt0"), Buf("yTt1")]
    ch_yTt = [S.chan("yTt0"), S.chan("yTt1")]
    xr = [AR.alloc(f"xr{i}", [128, D], F32) for i in range(2)]
    Bxr = [Buf("xr0"), Buf("xr1")]
    ch_xr = [S.chan("xr0"), S.chan("xr1")]
    t3 = AR.alloc("t3", [128, D], F32)
    x1t = [AR.alloc(f"x1t{i}", [128, D], F32) for i in range(2)]
    h2f = AR.alloc("h2f", [128, D], F32)
    h2b = [AR.alloc(f"h2b{i}", [128, D], BF16) for i in range(2)]
    h2T = AR.alloc("h2T", [128, KC, 128], F32)
    junk3 = AR.alloc("junk3", [128, D], BF16)
    st3 = AR.alloc("st3", [128, 8], F32)
    lg = AR.alloc("lg", [128, NE], F32)
    mx8 = AR.alloc("mx8", [128, 8], F32)
    nmx = AR.alloc("nmx", [128, 1], F32)
    msk = AR.alloc("msk", [128, NE], F32)
    mskb = AR.alloc("mskb", [128, NT, NE], BF16)
    exv = AR.alloc("exv", [128, NE], F32)
    den = AR.alloc("den", [128, 1], F32)
    posC = AR.alloc("posC", [128, NE], F32)
    ovf = AR.alloc("ovf", [128, NE], F32)
    oh = AR.alloc("oh", [128, NE], F32)
    jk = AR.alloc("jk", [128, NE], F32)
    idxf = AR.alloc("idxf", [128, 4], F32)
    idl = AR.alloc("idl", [128, 4], F32)
    idn = AR.alloc("idn", [128, 4], F32)
    Bidl, Bidn = Buf("idl"), Buf("idn")
    (Bt3, Bh2f, Bh2T, Bjunk3, Bst3, Blg, Bmx8, Bnmx, Bmsk, Bmskb, Bexv, Bden, BposC, Bovf, Boh, Bjk, Bidxf) = (
        Buf(n) for n in ["t3", "h2f", "h2T", "junk3", "st3", "lg", "mx8", "nmx", "msk", "mskb", "exv", "den", "posC", "ovf", "oh", "jk", "idxf"])
    Bx1t = [Buf("x1t0"), Buf("x1t1")]
    Bh2b = [Buf("h2b0"), Buf("h2b1")]
    ch_x1 = [S.chan("x1w0"), S.chan("x1w1")]
    ch_sc = [S.chan(f"scat{i}") for i in range(2)]
    Bx1_s = [Buf(f"x1_s{i}") for i in range(NT)]
    Bhsel = Buf("hsel_s")
    yT_v = yT_s.rearrange("(c p) t -> p c t", p=128)
    mixps = [psf[0], psf[1], psf[2], psf[3]]
    for i in range(NT):
        bi = i % 2
        S.dma("sp", ch_yTt[bi], lambda h, i=i, bi=bi: h.dma_start(out=yTt[bi][:], in_=yT_v[:, :, i * 128:(i + 1) * 128]), reads=[ByT_s], writes=[ByTt[bi]])
        S.dma("sp", ch_xr[bi], lambda h, i=i, bi=bi: h.dma_start(out=xr[bi][:], in_=x_d[i * 128:(i + 1) * 128, :]), writes=[Bxr[bi]])
        for cb in range(4):
            for c in range(KC):
                S.op("pe", lambda h, c=c, cb=cb, bi=bi: h.matmul(psf[cb][:, :], lhsT=yTt[bi][:, c, :], rhs=wo[:, c, cb * 512:(cb + 1) * 512], start=(c == 0), stop=(c == KC - 1)),
                     reads=[ByTt[bi], Bwo], writes=[Bpsf[cb]])
        for cb in range(4):
            S.op("act", lambda h, cb=cb: h.activation(out=junk3[:, cb * 512:(cb + 1) * 512], in_=psf[cb][:, :], func=AF.Square, accum_out=st3[:, cb:cb + 1]),
                 reads=[Bpsf[cb]], writes=[Bjunk3, Bst3])
        S.op("dve", lambda h: h.tensor_reduce(out=st3[:, 4:5], in_=st3[:, 0:4], axis=AX.X, op=ALU.add), reads=[Bst3], writes=[Bst3])
        S.op("act", lambda h: h.activation(out=st3[:, 4:5], in_=st3[:, 4:5], func=AF.Sqrt, scale=1.0 / D, bias=EPS), reads=[Bst3], writes=[Bst3])
        S.op("dve", lambda h: h.reciprocal(out=st3[:, 4:5], in_=st3[:, 4:5]), reads=[Bst3], writes=[Bst3])
        for cb in range(4):
            S.op("dve", lambda h, cb=cb: h.scalar_tensor_tensor(out=t3[:, cb * 512:(cb + 1) * 512], in0=psf[cb][:, :], scalar=st3[:, 4:5], in1=G1[:, cb * 512:(cb + 1) * 512],
                                                                op0=ALU.mult, op1=ALU.mult), reads=[Bpsf[cb], Bst3, BG1], writes=[Bt3])
        S.op("pool", lambda h, bi=bi: h.tensor_tensor(out=x1t[bi][:], in0=t3[:], in1=xr[bi][:], op=ALU.add), reads=[Bt3, Bxr[bi]], writes=[Bx1t[bi]])
        S.dma("sp", ch_x1[bi], lambda h, i=i, bi=bi: h.dma_start(out=x1_s[i * 128:(i + 1) * 128, :], in_=x1t[bi][:]), reads=[Bx1t[bi]], writes=[Bx1_s[i]])
        S.op("act", lambda h, bi=bi: h.activation(out=junk3[:], in_=x1t[bi][:], func=AF.Square, accum_out=st3[:, 5:6]), reads=[Bx1t[bi]], writes=[Bjunk3, Bst3])
        S.op("act", lambda h: h.activation(out=st3[:, 5:6], in_=st3[:, 5:6], func=AF.Sqrt, scale=1.0 / D, bias=EPS), reads=[Bst3], writes=[Bst3])
        S.op("dve", lambda h: h.reciprocal(out=st3[:, 5:6], in_=st3[:, 5:6]), reads=[Bst3], writes=[Bst3])
        S.op("dve", lambda h, bi=bi: h.scalar_tensor_tensor(out=t3[:], in0=x1t[bi][:], scalar=st3[:, 5:6], in1=A2[:], op0=ALU.mult, op1=ALU.mult),
             reads=[Bx1t[bi], Bst3, BA2], writes=[Bt3])
        S.op("pool", lambda h: h.tensor_tensor(out=h2f[:], in0=t3[:], in1=S2[:], op=ALU.add), reads=[Bt3, BS2], writes=[Bh2f])
        S.op("act", lambda h, bi=bi: h.copy(out=h2b[bi][:], in_=h2f[:]), reads=[Bh2f], writes=[Bh2b[bi]])
        for q4 in range(4):
            bank = 4 + (q4 % 2)
            for q in range(4):
                kc = q4 * 4 + q
                S.op("pe", lambda h, kc=kc, q=q, bank=bank: h.transpose(out=psf[bank][:, q * 128:(q + 1) * 128], in_=h2f[:, kc * 128:(kc + 1) * 128], identity=ident[:]),
                     reads=[Bh2f, Bc], writes=[Bpsf[bank]])
            if q4 % 2 == 0:
                S.op("act", lambda h, q4=q4, bank=bank: h.copy(out=h2T[:, q4 * 4:(q4 + 1) * 4, :], in_=psf[bank][:, :].rearrange("p (q t) -> p q t", q=4)),
                     reads=[Bpsf[bank]], writes=[Bh2T])
            else:
                S.op("dve", lambda h, q4=q4, bank=bank: h.tensor_copy(out=h2T[:, q4 * 4:(q4 + 1) * 4, :], in_=psf[bank][:, :].rearrange("p (q t) -> p q t", q=4)),
                     reads=[Bpsf[bank]], writes=[Bh2T])
        for kc in range(KC):
            S.op("pe", lambda h, kc=kc: h.matmul(psf[4][:, 0:NE], lhsT=h2T[:, kc, :], rhs=rw32[:, kc, :], start=(kc == 0), stop=(kc == KC - 1)),
                 reads=[Bh2T, Brw], writes=[Bpsf[4]])
        S.op("dve", lambda h: h.tensor_tensor(out=lg[:], in0=psf[4][:, 0:NE], in1=rbb[:], op=ALU.add), reads=[Bpsf[4], Brbb], writes=[Blg])
        S.op("dve", lambda h: h.max(out=mx8[:], in_=lg[:]), reads=[Blg], writes=[Bmx8])
        S.op("dve", lambda h: h.tensor_scalar(out=msk[:], in0=lg[:], scalar1=mx8[:, 3:4], scalar2=None, op0=ALU.is_ge), reads=[Blg, Bmx8], writes=[Bmsk])
        S.op("dve", lambda h, i=i: h.tensor_copy(out=mskb[:, i, :], in_=msk[:]), reads=[Bmsk], writes=[Bmskb])
        S.op("dve", lambda h: h.tensor_scalar(out=nmx[:], in0=mx8[:, 0:1], scalar1=-1.0, scalar2=None, op0=ALU.mult), reads=[Bmx8], writes=[Bnmx])
        S.op("act", lambda h: h.activation(out=exv[:], in_=lg[:], func=AF.Exp, bias=nmx[:, 0:1]), reads=[Blg, Bnmx], writes=[Bexv])
        S.op("dve", lambda h: h.tensor_tensor(out=exv[:], in0=exv[:], in1=msk[:], op=ALU.mult), reads=[Bexv, Bmsk], writes=[Bexv])
        S.op("dve", lambda h: h.tensor_reduce(out=den[:], in_=exv[:], axis=AX.X, op=ALU.add), reads=[Bexv], writes=[Bden])
        S.op("dve", lambda h: h.reciprocal(out=den[:], in_=den[:]), reads=[Bden], writes=[Bden])
        S.op("pe", lambda h, i=i: h.matmul(psf[5][:, 0:NE], lhsT=trib[:], rhs=mskb[:, i, :], start=True, stop=(i == 0)), reads=[Bmskb, Bc], writes=[Bpsf[5]])
        for j in range(i):
            S.op("pe", lambda h, j=j, i=i: h.matmul(psf[5][:, 0:NE], lhsT=onesb[:], rhs=mskb[:, j, :], start=False, stop=(j == i - 1)), reads=[Bmskb, Bc], writes=[Bpsf[5]])
        S.op("dve", lambda h: h.tensor_scalar(out=ovf[:], in0=psf[5][:, 0:NE], scalar1=float(CAP) - 0.5, scalar2=None, op0=ALU.is_gt), reads=[Bpsf[5]], writes=[Bovf])
        S.op("dve", lambda h: h.tensor_tensor(out=posC[:], in0=psf[5][:, 0:NE], in1=ebase[:], op=ALU.add), reads=[Bpsf[5], Bebase], writes=[BposC])
        S.op("dve", lambda h: h.scalar_tensor_tensor(out=posC[:], in0=ovf[:], scalar=BIG, in1=posC[:], op0=ALU.mult, op1=ALU.add), reads=[Bovf, BposC], writes=[BposC])
        S.op("dve", lambda h: h.tensor_scalar(out=ovf[:], in0=ovf[:], scalar1=-1.0, scalar2=1.0, op0=ALU.mult, op1=ALU.add), reads=[Bovf], writes=[Bovf])
        S.op("dve", lambda h, i=i: h.scalar_tensor_tensor(out=gatesA[:, i, :], in0=exv[:], scalar=den[:, 0:1], in1=ovf[:], op0=ALU.mult, op1=ALU.mult),
             reads=[Bexv, Bden, Bovf], writes=[BgatesA])
        for k in range(4):
            S.op("dve", lambda h, k=k: h.tensor_scalar(out=oh[:], in0=lg[:], scalar1=mx8[:, k:k + 1], scalar2=None, op0=ALU.is_equal), reads=[Blg, Bmx8], writes=[Boh])
            S.op("dve", lambda h, k=k: h.scalar_tensor_tensor(out=jk[:], in0=oh[:], scalar=1.0, in1=posC[:], op0=ALU.mult, op1=ALU.mult, accum_out=idxf[:, k:k + 1]),
                 reads=[Boh, BposC], writes=[Bjk, Bidxf])
            S.op("dve", lambda h, k=k, i=i: h.scalar_tensor_tensor(out=jk[:], in0=oh[:], scalar=1.0, in1=gatesA[:, i, :], op0=ALU.mult, op1=ALU.mult,
                                                                 accum_out=gate4[:, i, k:k + 1]), reads=[Boh, BgatesA], writes=[Bjk, Bgate4])
        for (lst, nten, Bl) in [(idxH, NHS, Bidx[i]), (idxY, NYS, Bidx[i])]:
            for j in range(nten):
                shift = float(j * (NE // nten) * CAP)
                S.op("dve", lambda h, shift=shift: h.tensor_scalar(out=idl[:], in0=idxf[:], scalar1=shift, scalar2=None, op0=ALU.subtract), reads=[Bidxf], writes=[Bidl])
                S.op("dve", lambda h: h.tensor_scalar(out=idn[:], in0=idl[:], scalar1=0.0, scalar2=BIG, op0=ALU.is_lt, op1=ALU.mult), reads=[Bidl], writes=[Bidn])
                S.op("dve", lambda h: h.tensor_tensor(out=idl[:], in0=idl[:], in1=idn[:], op=ALU.add), reads=[Bidl, Bidn], writes=[Bidl])
                S.op("dve", lambda h, i=i, t=lst[j]: h.tensor_copy(out=t[:, i * 4:(i + 1) * 4], in_=idl[:]), reads=[Bidl], writes=[Bl])
        first = True
        for k in range(4):
            for j in range(NHS):
                S.dma("pool", ch_sc[bi], lambda h, i=i, k=k, bi=bi, j=j: h.indirect_dma_start(out=hsel_s[j], out_offset=bass.IndirectOffsetOnAxis(ap=idxH[j][:, i * 4 + k:i * 4 + k + 1], axis=0),
                                                                                           in_=h2b[bi][:], in_offset=None, bounds_check=bound_reg(h, (NE // NHS) * CAP - 1), oob_is_err=False),
                      reads=[Bh2b[bi], Bidx[i]], writes=[Bhsel], cont=(not first))
                first = False
    for j in range(NT):
        S.op("pe", lambda h, j=j: h.matmul(psf[5][:, 0:NE], lhsT=onesb[:], rhs=mskb[:, j, :], start=(j == 0), stop=(j == NT - 1)), reads=[Bmskb, Bc], writes=[Bpsf[5]])
    S.cnt_op = S.op("dve", lambda h: h.tensor_copy(out=cnt_i[:], in_=psf[5][:, 0:NE]), reads=[Bpsf[5]], writes=[Bcnt])
    S.cnt_ap = lambda e: cnt_i[0:1, e:e + 1]
    if debug:
        d_lg = dbg_out("gates", [128, NT, NE])
        final_ops.append(S.dma("sp", S.chan("dbglg"), lambda h: h.dma_start(out=d_lg, in_=gatesA[:]), reads=[BgatesA]))
        d_idx = dbg_out("idx4", [128, NT * 4], I32)
        final_ops.append(S.dma("sp", S.chan("dbgidx"), lambda h: h.dma_start(out=d_idx, in_=idxY[0][:]), reads=Bidx))
        d_cnt = dbg_out("cnt", [128, NE], I32)
        final_ops.append(S.dma("sp", S.chan("dbgcnt"), lambda h: h.dma_start(out=d_cnt, in_=cnt_i[:]), reads=[Bcnt]))
        d_g4 = dbg_out("gate4", [128, NT, 4])
        final_ops.append(S.dma("sp", S.chan("dbgg4"), lambda h: h.dma_start(out=d_g4, in_=gate4[:]), reads=[Bgate4]))
    S.barrier()
    AR.reset(m3)
    if stage <= 5:
        if debug:
            d_x1 = dbg_out("x1", [SEQ, D])
            ld = AR.alloc("dbgld", [128, NT, D], F32)
            Bld = Buf("dbgld")
            S.dma("sp", S.chan("dbgx1"), lambda h: h.dma_start(out=ld[:], in_=x1_s.rearrange("(t p) d -> p t d", p=128)), reads=Bx1_s, writes=[Bld])
            final_ops.append(S.dma("sp", S.chan("dbgx2"), lambda h: h.dma_start(out=d_x1.rearrange("(t p) d -> p t d", p=128), in_=ld[:]), reads=[Bld]))
        return finish(nc, S, final_ops), dbg

    m5 = AR.mark()
    hselT = [AR.alloc(f"hselT{i}", [128, KC, RS], BF16) for i in range(2)]
    BhselT = [Buf("hselT0"), Buf("hselT1")]
    actT = AR.alloc("actT", [128, KC, RS], BF16)
    BactT = Buf("actT")
    w1u = [AR.alloc(f"w1u{i}", [128, KC, 512], BF16) for i in range(2)]
    Bw1u = [Buf("w1u0"), Buf("w1u1")]
    ch_w1 = [S.chan("w1u0"), S.chan("w1u1")]
    w2p = [AR.alloc(f"w2p{i}", [128, KC, 512], BF16) for i in range(2)]
    Bw2p = [Buf("w2p0"), Buf("w2p1")]
    ch_w2 = [S.chan("w2p0"), S.chan("w2p1")]
    hrow = [AR.alloc(f"hrow{i}", [128, D], BF16) for i in range(2)]
    Bhrow = [Buf("hrow0"), Buf("hrow1")]
    ch_hrow = [S.chan("hrow0"), S.chan("hrow1")]
    ysb = [AR.alloc(f"ysb{i}", [128, 512], F32) for i in range(4)]
    Bysb = [Buf(f"ysb{i}") for i in range(4)]
    ch_y = [S.chan(f"yw{i}") for i in range(4)]
    g1 = [AR.alloc(f"g1_{i}", [128, BLK], F32) for i in range(2)]
    sgm = [AR.alloc(f"sgm{i}", [128, BLK], F32) for i in range(2)]
    l2 = [AR.alloc(f"l2_{i}", [128, BLK], F32) for i in range(2)]
    wv = [AR.alloc(f"wv_{i}", [128, BLK], F32) for i in range(2)]
    Bg1 = [Buf("g1_0"), Buf("g1_1")]
    Bsgm = [Buf("sgm0"), Buf("sgm1")]
    Bl2 = [Buf("l2_0"), Buf("l2_1")]
    Bwv = [Buf("wv_0"), Buf("wv_1")]
    By_s = Buf("y_s")
    w1_v = [w1_d[e].rearrange("(k p) n -> p k n", p=128) for e in range(NE)]
    w2_v = [w2_d[e].rearrange("(k p) n -> p k n", p=128) for e in range(NE)]

    def blk_guard(e, r, b):
        return (e, b * BLK) if r == 0 else (e, r * RS)

    def rnd_guard(e, r):
        return (e, r * RS) if r > 0 else None

    def build_hselT(e, r, buf_i):
        for b in range(NBLK):
            S.cur_guard = blk_guard(e, r, b)
            for st2 in range(BLK // 128):
                st = b * (BLK // 128) + st2
                hb_i = st % 2
                row0 = (e % (NE // NHS)) * CAP + r * RS + st * 128
                src = hsel_s[e // (NE // NHS)]
                S.dma("sp", ch_hrow[hb_i], lambda h, row0=row0, hb_i=hb_i, src=src: h.dma_start(out=hrow[hb_i][:], in_=src[row0:row0 + 128, :]), reads=[Bhsel], writes=[Bhrow[hb_i]])
                for half in range(2):
                    pb = half
                    for q in range(8):
                        kc = half * 8 + q
                        S.op("pe", lambda h, kc=kc, q=q, pb=pb, hb_i=hb_i: h.transpose(out=psb[pb][:, q * 128:(q + 1) * 128], in_=hrow[hb_i][:, kc * 128:(kc + 1) * 128], identity=identb[:]),
                             reads=[Bhrow[hb_i], Bc], writes=[Bpsb[pb]])
                    if half == 0:
                        S.op("act", lambda h, half=half, pb=pb, st=st, buf_i=buf_i: h.copy(out=hselT[buf_i][:, half * 8:(half + 1) * 8, st * 128:(st + 1) * 128],
                                                                                        in_=psb[pb][:, :].rearrange("p (q t) -> p q t", q=8)), reads=[Bpsb[pb]], writes=[BhselT[buf_i]])
                    else:
                        S.op("dve", lambda h, half=half, pb=pb, st=st, buf_i=buf_i: h.tensor_copy(out=hselT[buf_i][:, half * 8:(half + 1) * 8, st * 128:(st + 1) * 128],
                                                                                               in_=psb[pb][:, :].rearrange("p (q t) -> p q t", q=8)), reads=[Bpsb[pb]], writes=[BhselT[buf_i]])
        S.cur_guard = None

    ucount = 0
    pcount2 = 0
    acnt = 0
    ycnt = 0
    er_list = [(e, r) for e in range(NE) for r in range(ROUNDS)]
    build_hselT(er_list[0][0], er_list[0][1], 0)
    for n, (e, r) in enumerate(er_list):
        hb_cur = n % 2
        for u in range(8):
            bi = ucount % 2
            ucount += 1
            S.cur_guard = rnd_guard(e, r)
            S.dma("pool", ch_w1[bi], lambda h, e=e, u=u, bi=bi: h.dma_start(out=w1u[bi][:, :, 0:256], in_=w1_v[e][:, :, u * 256:(u + 1) * 256]), writes=[Bw1u[bi]])
            S.dma("pool", ch_w1[bi], lambda h, e=e, u=u, bi=bi: h.dma_start(out=w1u[bi][:, :, 256:512], in_=w1_v[e][:, :, DFF + u * 256:DFF + (u + 1) * 256]),
                  writes=[Bw1u[bi]], cont=True)
            for b in range(NBLK):
                S.cur_guard = blk_guard(e, r, b)
                for j in range(2):
                    fc = u * 2 + j
                    ab = acnt % 2
                    bank = acnt % 4
                    acnt += 1
                    bs = slice(b * BLK, (b + 1) * BLK)
                    for kc in range(KC):
                        S.op("pe", lambda h, kc=kc, bi=bi, j=j, bs=bs, bank=bank, hb_cur=hb_cur: h.matmul(psf[bank][:, 0:BLK], lhsT=w1u[bi][:, kc, j * 128:(j + 1) * 128],
                                                                                                     rhs=hselT[hb_cur][:, kc, bs], start=(kc == 0), stop=(kc == KC - 1)),
                             reads=[Bw1u[bi], BhselT[hb_cur]], writes=[Bpsf[bank]])
                    for kc in range(KC):
                        S.op("pe", lambda h, kc=kc, bi=bi, j=j, bs=bs, bank=bank, hb_cur=hb_cur: h.matmul(psf[bank][:, BLK:2 * BLK], lhsT=w1u[bi][:, kc, 256 + j * 128:256 + (j + 1) * 128],
                                                                                                     rhs=hselT[hb_cur][:, kc, bs], start=(kc == 0), stop=(kc == KC - 1)),
                             reads=[Bw1u[bi], BhselT[hb_cur]], writes=[Bpsf[bank]])
                    cg = e * 32 + fc
                    cl = e * 32 + 16 + fc
                    S.op("dve", lambda h, ab=ab, bank=bank, cg=cg: h.tensor_scalar(out=g1[ab][:], in0=psf[bank][:, 0:BLK], scalar1=b1T[:, cg:cg + 1], scalar2=LIMIT, op0=ALU.add, op1=ALU.min),
                         reads=[Bpsf[bank], Bb1T], writes=[Bg1[ab]])
                    S.op("act", lambda h, ab=ab: h.activation(out=sgm[ab][:], in_=g1[ab][:], func=AF.Sigmoid, scale=ALPHA), reads=[Bg1[ab]], writes=[Bsgm[ab]])
                    S.op("dve", lambda h, ab=ab, bank=bank, cl=cl: h.tensor_scalar(out=l2[ab][:], in0=psf[bank][:, BLK:2 * BLK], scalar1=b1T[:, cl:cl + 1], scalar2=LIMIT + 1.0, op0=ALU.add, op1=ALU.min),
                         reads=[Bpsf[bank], Bb1T], writes=[Bl2[ab]])
                    S.op("dve", lambda h, ab=ab: h.scalar_tensor_tensor(out=wv[ab][:], in0=l2[ab][:], scalar=1.0 - LIMIT, in1=g1[ab][:], op0=ALU.max, op1=ALU.mult),
                         reads=[Bl2[ab], Bg1[ab]], writes=[Bwv[ab]])
                    S.op("dve", lambda h, ab=ab, fc=fc, bs=bs: h.tensor_tensor(out=actT[:, fc, bs], in0=wv[ab][:], in1=sgm[ab][:], op=ALU.mult),
                         reads=[Bwv[ab], Bsgm[ab]], writes=[BactT])
        S.cur_guard = None
        if n + 1 < len(er_list):
            build_hselT(er_list[n + 1][0], er_list[n + 1][1], (n + 1) % 2)
        ydst = y_s[e // (NE // NYS)]
        for db in range(4):
            bi = pcount2 % 2
            pcount2 += 1
            S.cur_guard = rnd_guard(e, r)
            S.dma("pool", ch_w2[bi], lambda h, e=e, db=db, bi=bi: h.dma_start(out=w2p[bi][:], in_=w2_v[e][:, :, db * 512:(db + 1) * 512]), writes=[Bw2p[bi]])
            for b in range(NBLK):
                S.cur_guard = blk_guard(e, r, b)
                for st2 in range(BLK // 128):
                    st = b * (BLK // 128) + st2
                    bank = 4 + (ycnt % 2)
                    yb = ycnt % 4
                    ycnt += 1
                    for fc in range(KC):
                        S.op("pe", lambda h, fc=fc, st=st, bi=bi, bank=bank: h.matmul(psf[bank][:, :], lhsT=actT[:, fc, st * 128:(st + 1) * 128], rhs=w2p[bi][:, fc, :],
                                                                                     start=(fc == 0), stop=(fc == KC - 1)),
                             reads=[BactT, Bw2p[bi]], writes=[Bpsf[bank]])
                    S.op("act", lambda h, bank=bank, yb=yb: h.copy(out=ysb[yb][:], in_=psf[bank][:, :]), reads=[Bpsf[bank]], writes=[Bysb[yb]])
                    row0 = (e % (NE // NYS)) * CAP + r * RS + st * 128
                    S.dma("sp", ch_y[yb], lambda h, row0=row0, db=db, yb=yb, ydst=ydst: h.dma_start(out=ydst[row0:row0 + 128, db * 512:(db + 1) * 512], in_=ysb[yb][:]),
                          reads=[Bysb[yb]], writes=[By_s])
        S.cur_guard = None
    S.barrier()
    AR.reset(m5)

    G2 = AR.alloc("G2", [128, D], F32)
    wt6 = AR.alloc("wt6", [128, D], F32)
    b2sb = AR.alloc("b2sb", [NE, D], F32)
    BG2, Bwt6, Bb2 = Buf("G2"), Buf("wt6"), Buf("b2sb")
    load_mod_bcast(G2, 0, 5, ch_v, BG2)
    load_row_bcast(wt6, pofw_d, ch_v, Bwt6)
    S.op("dve", lambda h: h.tensor_tensor(out=G2[:], in0=G2[:], in1=wt6[:], op=ALU.mult), reads=[BG2, Bwt6], writes=[BG2])
    S.dma("sp", ch_v, lambda h: h.dma_start(out=b2sb[:], in_=b2_d), writes=[Bb2])
    yk = [[AR.alloc(f"yk{b}_{k}", [128, D], F32) for k in range(4)] for b in range(2)]
    Byk = [[Buf(f"yk{b}_{k}") for k in range(4)] for b in range(2)]
    ch_g = [S.chan("gath0"), S.chan("gath1")]
    x1r = [AR.alloc(f"x1r{i}", [128, D], F32) for i in range(2)]
    Bx1r = [Buf("x1r0"), Buf("x1r1")]
    ch_x1r = [S.chan("x1r0"), S.chan("x1r1")]
    ff = AR.alloc("ff", [128, D], F32)
    ot = [AR.alloc(f"ot{i}", [128, D], F32) for i in range(2)]
    gT = AR.alloc("gT", [NE, 128], F32)
    junk6 = AR.alloc("junk6", [128, D], BF16)
    st6 = AR.alloc("st6", [128, 2], F32)
    Bff, BgT, Bjunk6, Bst6 = Buf("ff"), Buf("gT"), Buf("junk6"), Buf("st6")
    Bot = [Buf("ot0"), Buf("ot1")]
    ch_out = [S.chan("out0"), S.chan("out1")]
    for i in range(NT):
        bi = i % 2
        first = True
        for k in range(4):
            for j in range(NYS):
                S.dma("pool", ch_g[bi], lambda h, i=i, k=k, bi=bi, j=j: h.indirect_dma_start(out=yk[bi][k][:], out_offset=None, in_=y_s[j],
                                                                                          in_offset=bass.IndirectOffsetOnAxis(ap=idxY[j][:, i * 4 + k:i * 4 + k + 1], axis=0),
                                                                                          bounds_check=bound_reg(h, (NE // NYS) * CAP - 1), oob_is_err=False),
                      reads=[By_s, Bidx[i]], writes=[Byk[bi][k]], cont=(not first))
                first = False
        S.dma("sp", ch_x1r[bi], lambda h, i=i, bi=bi: h.dma_start(out=x1r[bi][:], in_=x1_s[i * 128:(i + 1) * 128, :]), reads=[Bx1_s[i]], writes=[Bx1r[bi]])
        S.op("pe", lambda h, i=i: h.transpose(out=psf[4][0:NE, 0:128], in_=gatesA[:, i, :], identity=ident[:]), reads=[BgatesA, Bc], writes=[Bpsf[4]])
        S.op("act", lambda h: h.copy(out=gT[:], in_=psf[4][0:NE, 0:128]), reads=[Bpsf[4]], writes=[BgT])
        for cb in range(4):
            S.op("pe", lambda h, cb=cb: h.matmul(psf[cb][:, :], lhsT=gT[:], rhs=b2sb[:, cb * 512:(cb + 1) * 512], start=True, stop=True), reads=[BgT, Bb2], writes=[Bpsf[cb]])
            S.op("dve", lambda h, cb=cb, bi=bi, i=i: h.scalar_tensor_tensor(out=ff[:, cb * 512:(cb + 1) * 512], in0=yk[bi][0][:, cb * 512:(cb + 1) * 512], scalar=gate4[:, i, 0:1],
                                                                          in1=psf[cb][:, :], op0=ALU.mult, op1=ALU.add), reads=[Byk[bi][0], Bgate4, Bpsf[cb]], writes=[Bff])
        for k in range(1, 4):
            S.op("dve", lambda h, k=k, bi=bi, i=i: h.scalar_tensor_tensor(out=ff[:], in0=yk[bi][k][:], scalar=gate4[:, i, k:k + 1], in1=ff[:], op0=ALU.mult, op1=ALU.add),
                 reads=[Byk[bi][k], Bgate4, Bff], writes=[Bff])
        S.op("act", lambda h: h.activation(out=junk6[:], in_=ff[:], func=AF.Square, accum_out=st6[:, 0:1]), reads=[Bff], writes=[Bjunk6, Bst6])
        S.op("act", lambda h: h.activation(out=st6[:, 0:1], in_=st6[:, 0:1], func=AF.Sqrt, scale=1.0 / D, bias=EPS), reads=[Bst6], writes=[Bst6])
        S.op("dve", lambda h: h.reciprocal(out=st6[:, 0:1], in_=st6[:, 0:1]), reads=[Bst6], writes=[Bst6])
        S.op("dve", lambda h: h.scalar_tensor_tensor(out=ff[:], in0=ff[:], scalar=st6[:, 0:1], in1=G2[:], op0=ALU.mult, op1=ALU.mult), reads=[Bff, Bst6, BG2], writes=[Bff])
        S.op("pool", lambda h, bi=bi: h.tensor_tensor(out=ot[bi][:], in0=ff[:], in1=x1r[bi][:], op=ALU.add), reads=[Bff, Bx1r[bi]], writes=[Bot[bi]])
        final_ops.append(S.dma("sp", ch_out[bi], lambda h, i=i, bi=bi: h.dma_start(out=out_d[i * 128:(i + 1) * 128, :], in_=ot[bi][:]), reads=[Bot[bi]]))
    return finish(nc, S, final_ops), dbg


def finish(nc, S, final_ops):
    S.wait_final("sp", final_ops)
    S.emit()
    return nc


def _rope_tables():
    half = 64
    inv = (10000.0 ** (-np.arange(half, dtype=np.float32) / np.float32(half))).astype(np.float32)
    pos = np.arange(NCTX + SEQ, dtype=np.float32)
    ang = (pos[:, None] * inv[None, :]).astype(np.float32)
    return np.cos(ang).astype(np.float32), np.sin(ang).astype(np.float32)


def make_in_maps(inp, ne_decl=NE):
    f = lambda a: np.ascontiguousarray(np.asarray(a, dtype=np.float32))
    cos, sin = _rope_tables()
    shared = {
        "c_ctx": f(inp["c_ctx"]).reshape(16, 128),
        "ada_w": f(inp["ada_w"][0]),
        "ada_b": f(inp["ada_b"][0]).reshape(1, 6 * D),
        "pre_mix_norm": f(inp["pre_mix_norm"][0]).reshape(1, D),
        "post_mix_norm": f(inp["post_mix_norm"][0]).reshape(1, D),
        "pre_ffn_norm": f(inp["pre_ffn_norm"][0]).reshape(1, D),
        "post_ffn_norm": f(inp["post_ffn_norm"][0]).reshape(1, D),
        "w_in": f(inp["w_in"][0]),
        "ret_decay": np.concatenate([f(inp["ret_decay_fwd"][0]), f(inp["ret_decay_bwd"][0])]).reshape(1, 16),
        "ret_gn_w": f(inp["ret_gn_w"][0]).reshape(1, DRET),
        "conv_w": f(inp["conv_w"][0]),
        "conv_vecs": np.concatenate([f(inp["conv_b"][0]).reshape(8, 128), f(inp["conv_ln_w"][0]).reshape(8, 128), f(inp["conv_ln_b"][0]).reshape(8, 128)], axis=0),
        "w_out": f(inp["w_out"][0]),
        "router_w": f(inp["router_w"][0]),
        "router_b": f(inp["router_b"][0]).reshape(1, NE),
        "w1": f(inp["w1"][0][:ne_decl]),
        "b1": f(inp["b1"][0]).reshape(NE * 32, 128),
        "w2": f(inp["w2"][0][:ne_decl]),
        "b2": f(inp["b2"][0]),
        "rope_cos": cos,
        "rope_sin": sin,
    }
    maps = []
    for b in range(NB):
        m = dict(shared)
        m["x"] = f(inp["x"][b])
        m["c"] = f(inp["c"][b]).reshape(16, 128)
        m["ctx"] = f(inp["ctx"][b])
        maps.append(m)
    return maps


_NC_CACHE = {}


def kernel(**inputs):
    if "nc" not in _NC_CACHE:
        _NC_CACHE["nc"] = build_program()[0]
    nc = _NC_CACHE["nc"]
    in_maps = make_in_maps(inputs)
    res = run_bass_kernel_spmd(nc, in_maps, core_ids=list(range(NB)))
    out = np.stack([np.asarray(res.results[b]["out"], dtype=np.float32) for b in range(NB)], axis=0)
    return out
```

```python
import numpy as np
import concourse.bass as bass
import concourse.mybir as mybir
from concourse.alu_op_type import AluOpType as ALU
from concourse.bass_utils import run_bass_kernel_spmd

F32 = mybir.dt.float32
BF16 = mybir.dt.bfloat16
I32 = mybir.dt.int32
AF = mybir.ActivationFunctionType
AX = mybir.AxisListType

D = 2048
SEQ = 2048
NB = 8
NCTX = 256
H = 8
HD = 128
DRET = 1024
DCONV = 1024
DIN = 6144
CW = 31
NE = 32
DFF = 2048
NT = SEQ // 128
KC = D // 128
EPS = 1e-6
GN_EPS = 1e-5
ALPHA = 1.702
LIMIT = 7.0
QSCALE = HD ** -0.5

ROUNDS = 4
RS = 512
CAP = ROUNDS * RS
BLK = 256
NBLK = RS // BLK
NHS = 1
NYS = 2
BIG = 4.0e6

ENGS = ("pe", "act", "dve", "pool", "sp")
EPOCH = 1 << 30


class Buf:
    __slots__ = ("name", "excl", "last_w", "readers")

    def __init__(self, name, excl=False):
        self.name = name
        self.excl = excl
        self.last_w = None
        self.readers = []


class Op:
    __slots__ = ("eng", "fn", "deps", "signal", "done", "is_dma", "chan", "chan_prev", "grp", "guard")

    def __init__(self, eng, fn):
        self.eng = eng
        self.fn = fn
        self.deps = []
        self.signal = False
        self.done = None
        self.is_dma = False
        self.chan = None
        self.chan_prev = None
        self.grp = None
        self.guard = None


class Chan:
    def __init__(self, name):
        self.name = name
        self.sem = None
        self.last_grp = None


class Sched:
    def __init__(self, nc):
        self.nc = nc
        self.ops = {e: [] for e in ENGS}
        self.chans = []
        self.final_waits = []
        self.nrec = 0
        self.cur_guard = None
        self.cnt_ap = None
        self.cnt_op = None

    def chan(self, name):
        c = Chan(name)
        self.chans.append(c)
        return c

    def _deps(self, op, reads, writes):
        for b in reads:
            if b.last_w is not None:
                op.deps.append((b.last_w, "RAW"))
            if b.excl:
                for r in b.readers:
                    op.deps.append((r, "RAR"))
        for b in writes:
            if b.last_w is not None:
                op.deps.append((b.last_w, "WAW"))
            for r in b.readers:
                op.deps.append((r, "WAR"))
        for b in reads:
            b.readers.append(op)
        for b in writes:
            b.last_w = op
            b.readers = []

    def op(self, eng, fn, reads=(), writes=()):
        o = Op(eng, fn)
        o.guard = self.cur_guard
        self._deps(o, list(reads), list(writes))
        self.ops[eng].append(o)
        self.nrec += 1
        return o

    def dma(self, eng, chan, fn, reads=(), writes=(), cont=False):
        o = Op(eng, fn)
        o.guard = self.cur_guard
        o.is_dma = True
        o.chan = chan
        if cont and chan.last_grp is not None:
            o.grp = chan.last_grp
            o.chan_prev = o.grp[0].chan_prev
        else:
            o.chan_prev = chan.last_grp
            o.grp = []
            chan.last_grp = o.grp
        o.grp.append(o)
        self._deps(o, list(reads), list(writes))
        self.ops[eng].append(o)
        self.nrec += 1
        return o

    def barrier(self):
        lasts = []
        for e in ENGS:
            for o in reversed(self.ops[e]):
                if not o.is_dma and o.fn is not None:
                    lasts.append(o)
                    break
        for c in self.chans:
            if c.last_grp:
                lasts.append(c.last_grp[0])
        for e in ENGS:
            o = Op(e, None)
            for d in lasts:
                o.deps.append((d, "RAW"))
            self.ops[e].append(o)

    def wait_final(self, eng, ops):
        self.final_waits.append((eng, list(ops)))

    def emit(self):
        nc = self.nc
        for e in ENGS:
            for o in self.ops[e]:
                for (d, kind) in o.deps:
                    if d.is_dma:
                        continue
                    if d.eng != o.eng or kind == "RAW":
                        d.signal = True
        for (e, ops) in self.final_waits:
            for d in ops:
                if not d.is_dma:
                    d.signal = True
        if self.cnt_op is not None:
            self.cnt_op.signal = True
        sems = []
        for e in ENGS:
            cnt = 0
            sem = None
            for o in self.ops[e]:
                if o.is_dma or o.fn is None:
                    continue
                if o.signal:
                    if sem is None or cnt >= EPOCH:
                        sem = nc.alloc_semaphore(f"s_{e}_{len(sems)}")
                        sems.append(sem)
                        cnt = 0
                    cnt += 1
                    o.done = (sem, cnt)
        for c in self.chans:
            if c.last_grp is None:
                continue
            c.sem = nc.alloc_semaphore(f"c_{c.name}")
            chain = []
            g = c.last_grp
            while g is not None:
                chain.append(g)
                g = g[0].chan_prev
            chain.reverse()
            v = 0
            for g in chain:
                v += 16 * len(g)
                for o in g:
                    o.done = (c.sem, v)
        lists = self.ops
        finals = self.final_waits

        cnt_ap = self.cnt_ap
        cnt_op = self.cnt_op

        def run(e, h):
            seen = {}
            state = {"greg": None, "loaded": None, "last_sig": None}

            def need(sem, val):
                k = id(sem)
                if seen.get(k, 0) < val:
                    h.wait_ge(sem, val)
                    seen[k] = val

            def emit_op(o):
                if o.is_dma and o.chan_prev is not None and o is o.grp[0]:
                    need(*o.chan_prev[0].done)
                for (d, kind) in o.deps:
                    if d.is_dma:
                        if d.grp is o.grp:
                            continue
                        need(*d.done)
                    elif d.eng != e or kind == "RAW":
                        need(*d.done)
                if o.fn is None:
                    return
                ins = o.fn(h)
                if o.is_dma:
                    ins.then_inc(o.chan.sem, 16)
                elif o.signal:
                    ins.then_inc(o.done[0], 1)
                    state["last_sig"] = o.done

            ops = lists[e]
            i = 0
            n = len(ops)
            while i < n:
                o = ops[i]
                if o.guard is None:
                    emit_op(o)
                    i += 1
                    continue
                j = i
                while j < n and ops[j].guard == o.guard:
                    j += 1
                grp = ops[i:j]
                (ge, thr) = o.guard
                if state["greg"] is None:
                    state["greg"] = h.alloc_register(f"greg_{e}")
                if state["loaded"] != ge:
                    need(*cnt_op.done)
                    h.reg_load(state["greg"], cnt_ap(ge))
                    state["loaded"] = ge
                saved = dict(seen)
                pre_sig = state["last_sig"]
                with h.If_cmp(state["greg"], thr, "IS_GT"):
                    for g in grp:
                        emit_op(g)
                nsig = 0
                sig_sem = None
                ndma = 0
                for g in grp:
                    if g.fn is None:
                        continue
                    if g.is_dma:
                        ndma += 1
                    elif g.signal:
                        nsig += 1
                        sig_sem = g.done[0]
                        last_in = g.done
                if nsig or ndma:
                    with h.Else():
                        if nsig:
                            if pre_sig is not None:
                                h.wait_ge(*pre_sig)
                            h.sem_inc(sig_sem, nsig)
                        for g in grp:
                            if g.fn is not None and g.is_dma:
                                if g is g.grp[0] and g.chan_prev is not None:
                                    h.wait_ge(*g.chan_prev[0].done)
                                h.sem_inc(g.chan.sem, 16)
                if nsig:
                    state["last_sig"] = last_in
                seen.clear()
                seen.update(saved)
                i = j
            for (fe, fops) in finals:
                if fe == e:
                    for d in fops:
                        need(*d.done)

        with nc.Block() as block:
            @block.tensor
            def _(h):
                run("pe", h)

            @block.scalar
            def _(h):
                run("act", h)

            @block.vector
            def _(h):
                run("dve", h)

            @block.gpsimd
            def _(h):
                run("pool", h)

            @block.sync
            def _(h):
                run("sp", h)


class Arena:
    def __init__(self, nc, nbytes):
        self.nc = nc
        left = nc._sbuf_addr_for_side("left")
        self.base = (left + 63) // 64 * 64
        nbytes = nbytes // 64 * 64
        self.slab = nc.alloc_sbuf_tensor("arena", [128, nbytes // 4], F32)
        self.size = nbytes - 64
        self.top = 0
        self.n = 0

    def alloc(self, name, shape, dtype):
        esz = 2 if dtype == BF16 else 4
        nb = esz
        for s in shape[1:]:
            nb *= s
        nb = (nb + 63) // 64 * 64
        off = self.top
        assert off + nb <= self.size, (name, off, nb, self.size)
        self.top += nb
        self.n += 1
        return self.nc.alloc_sbuf_tensor_at(f"{name}_{self.n}", list(shape), dtype, offset=self.base + off)

    def alloc_at(self, name, shape, dtype, off):
        self.n += 1
        return self.nc.alloc_sbuf_tensor_at(f"{name}_{self.n}", list(shape), dtype, offset=self.base + off)

    def mark(self):
        return self.top

    def reset(self, m):
        self.top = m


def build_program(stage=99, debug=False, ne_decl=NE):
    nc = bass.Bass("TRN2", target_bir_lowering=False)
    S = Sched(nc)

    def din(name, shape, dt=F32):
        return nc.dram_tensor(name, list(shape), dt, kind="ExternalInput").ap()

    x_d = din("x", [SEQ, D])
    c_d = din("c", [16, 128])
    ctx_d = din("ctx", [NCTX, D])
    cctx_d = din("c_ctx", [16, 128])
    adaw_d = din("ada_w", [D, 6 * D])
    adab_d = din("ada_b", [1, 6 * D])
    pmw_d = din("pre_mix_norm", [1, D])
    pomw_d = din("post_mix_norm", [1, D])
    pfw_d = din("pre_ffn_norm", [1, D])
    pofw_d = din("post_ffn_norm", [1, D])
    win_d = din("w_in", [D, DIN])
    dec_d = din("ret_decay", [1, 16])
    gnw_d = din("ret_gn_w", [1, DRET])
    convw_d = din("conv_w", [CW, DCONV])
    cvec_d = din("conv_vecs", [24, 128])
    wout_d = din("w_out", [D, D])
    rw_d = din("router_w", [D, NE])
    rb_d = din("router_b", [1, NE])
    w1_d = din("w1", [ne_decl, D, 2 * DFF])
    b1_d = din("b1", [NE * 32, 128])
    w2_d = din("w2", [ne_decl, DFF, D])
    b2_d = din("b2", [NE, D])
    cos_d = din("rope_cos", [NCTX + SEQ, 64])
    sin_d = din("rope_sin", [NCTX + SEQ, 64])
    out_d = nc.dram_tensor("out", [SEQ, D], F32, kind="ExternalOutput").ap()
    dbg = {}

    def dbg_out(name, shape, dt=F32):
        t = nc.dram_tensor("dbg_" + name, list(shape), dt, kind="ExternalOutput").ap()
        dbg[name] = t
        return t

    mod_s = nc.dram_tensor("mod_s", [2, 6 * D], F32).ap()
    yT_s = nc.dram_tensor("yT_s", [D, SEQ], BF16).ap()
    x1_s = nc.dram_tensor("x1_s", [SEQ, D], F32).ap()
    hsel_s = [nc.dram_tensor(f"hsel_s{j}", [(NE // NHS) * CAP, D], BF16).ap() for j in range(NHS)]
    y_s = [nc.dram_tensor(f"y_s{j}", [(NE // NYS) * CAP, D], F32).ap() for j in range(NYS)]

    AR = Arena(nc, nc.sbuf_bytes_remaining - 6144)

    psf = [nc.alloc_psum_tensor(f"psf{i}", [128, 512], F32) for i in range(6)]
    psb = [nc.alloc_psum_tensor(f"psb{i}", [128, 1024], BF16) for i in range(2)]
    Bpsf = [Buf(f"psf{i}", excl=True) for i in range(6)]
    Bpsb = [Buf(f"psb{i}", excl=True) for i in range(2)]
    final_ops = []
    _regs = {}

    def bound_reg(h, val):
        if val not in _regs:
            _regs[val] = h.to_reg(val)
        return _regs[val]

    ident = AR.alloc("ident", [128, 128], F32)
    identb = AR.alloc("identb", [128, 128], BF16)
    onesb = AR.alloc("onesb", [128, 128], BF16)
    ones32 = AR.alloc("ones32", [128, 128], F32)
    trib = AR.alloc("trib", [128, 128], BF16)
    iorow = AR.alloc("iorow", [128, 128], F32)
    iocol = AR.alloc("iocol", [128, 1], F32)
    b1T = AR.alloc("b1T", [128, NE * 32], F32)
    gate4 = AR.alloc("gate4", [128, NT, 4], F32)
    idxH = [AR.alloc(f"idxH{j}", [128, NT * 4], I32) for j in range(NHS)]
    idxY = [AR.alloc(f"idxY{j}", [128, NT * 4], I32) for j in range(NYS)]
    cnt_i = AR.alloc("cnt_i", [128, NE], I32)
    Bcnt = Buf("cnt_i")
    gatesA = AR.alloc("gatesA", [128, NT, NE], F32)
    Bc = Buf("consts")
    Bb1T = Buf("b1T")
    Bgate4 = Buf("gate4")
    Bidx = [Buf(f"idx{i}") for i in range(NT)]
    BgatesA = Buf("gatesA")

    S.op("pool", lambda h: h.memset(ident[:], 0.0), writes=[Bc])
    S.op("pool", lambda h: h.affine_select(out=ident[:], in_=ident[:], pattern=[[-1, 128]], compare_op=ALU.not_equal,
                                            fill=1.0, base=0, channel_multiplier=1), reads=[Bc], writes=[Bc])
    S.op("pool", lambda h: h.memset(ones32[:], 1.0), writes=[Bc])
    S.op("pool", lambda h: h.affine_select(out=iorow[:], in_=ones32[:], pattern=[[1, 128]], compare_op=ALU.is_gt,
                                            fill=0.0, base=0, channel_multiplier=-1), reads=[Bc], writes=[Bc])
    S.op("dve", lambda h: h.tensor_copy(out=trib[:], in_=iorow[:]), reads=[Bc], writes=[Bc])
    S.op("dve", lambda h: h.tensor_copy(out=identb[:], in_=ident[:]), reads=[Bc], writes=[Bc])
    S.op("dve", lambda h: h.tensor_copy(out=onesb[:], in_=ones32[:]), reads=[Bc], writes=[Bc])
    ioi = AR.alloc("ioi", [128, 128], I32)
    S.op("pool", lambda h: h.iota(ioi[:], pattern=[[1, 128]], base=0, channel_multiplier=0), writes=[Bc])
    S.op("dve", lambda h: h.tensor_copy(out=iorow[:], in_=ioi[:]), reads=[Bc], writes=[Bc])
    S.op("pool", lambda h: h.iota(ioi[:, 0:1], pattern=[[0, 1]], base=0, channel_multiplier=1), reads=[Bc], writes=[Bc])
    S.op("dve", lambda h: h.tensor_copy(out=iocol[:], in_=ioi[:, 0:1]), reads=[Bc], writes=[Bc])

    ch_small = S.chan("small")
    persist_mark = AR.mark()

    def transpose_rows(rows_ap, nrows, out_ap, bank, Bbank, reads, writes, evac="act"):
        S.op("pe", lambda h: h.transpose(out=psf[bank][:, 0:nrows], in_=rows_ap, identity=ident[0:nrows, 0:nrows]),
             reads=reads + [Bc], writes=[Bbank])
        if evac == "act":
            S.op("act", lambda h: h.copy(out=out_ap, in_=psf[bank][:, 0:nrows]), reads=[Bbank], writes=writes)
        else:
            S.op("dve", lambda h: h.tensor_copy(out=out_ap, in_=psf[bank][:, 0:nrows]), reads=[Bbank], writes=writes)

    m0 = AR.mark()
    rows32 = AR.alloc("rows32", [32, 128], F32)
    cT = AR.alloc("cT", [128, 32], F32)
    sil = AR.alloc("sil", [128, 32], F32)
    adab = AR.alloc("adab", [2, 6 * D], F32)
    modsb = AR.alloc("modsb", [2, 6 * D], F32)
    adaw = [AR.alloc(f"adaw{i}", [128, KC, 512], F32) for i in range(2)]
    b1rows = AR.alloc("b1rows", [128, 8, 128], F32)
    Brows32, BcT, Bsil, Badab, Bmodsb, Bb1rows = Buf("rows32"), Buf("cT"), Buf("sil"), Buf("adab"), Buf("modsb"), Buf("b1rows")
    Badaw = [Buf("adaw0"), Buf("adaw1")]
    ch_adaw = [S.chan("adaw0"), S.chan("adaw1")]

    S.dma("sp", ch_small, lambda h: h.dma_start(out=rows32[0:16, :], in_=c_d), writes=[Brows32])
    S.dma("sp", ch_small, lambda h: h.dma_start(out=rows32[16:32, :], in_=cctx_d), writes=[Brows32], cont=True)
    S.dma("sp", ch_small, lambda h: h.dma_start(out=adab[:], in_=adab_d.partition_broadcast(2)), writes=[Badab], cont=True)
    S.dma("sp", ch_small, lambda h: h.dma_start(out=b1rows[:], in_=b1_d.rearrange("(t p) f -> p t f", p=128)), writes=[Bb1rows], cont=True)
    transpose_rows(rows32[:], 32, cT[:], 0, Bpsf[0], [Brows32], [BcT])
    S.op("act", lambda h: h.activation(out=sil[:], in_=cT[:], func=AF.Silu), reads=[BcT], writes=[Bsil])
    for t in range(8):
        S.op("pe", lambda h, t=t: h.transpose(out=psf[1 + (t % 2)][:, 0:128], in_=b1rows[:, t, :], identity=ident[:]),
             reads=[Bb1rows, Bc], writes=[Bpsf[1 + (t % 2)]])
        S.op("act", lambda h, t=t: h.copy(out=b1T[:, t * 128:(t + 1) * 128], in_=psf[1 + (t % 2)][:, 0:128]),
             reads=[Bpsf[1 + (t % 2)]], writes=[Bb1T])
    b1T3 = b1T[:, :].rearrange("p (e f) -> p e f", e=NE)
    S.op("dve", lambda h: h.tensor_scalar(out=b1T3[:, :, 16:32], in0=b1T3[:, :, 16:32], scalar1=1.0, scalar2=None, op0=ALU.add), reads=[Bb1T], writes=[Bb1T])
    sil2 = AR.alloc("sil2", [128, KC, 2], F32)
    Bsil2 = Buf("sil2")
    S.op("dve", lambda h: h.tensor_copy(out=sil2[:, :, 0], in_=sil[:, 0:16]), reads=[Bsil], writes=[Bsil2])
    S.op("dve", lambda h: h.tensor_copy(out=sil2[:, :, 1], in_=sil[:, 16:32]), reads=[Bsil], writes=[Bsil2])
    adaw_v = adaw_d.rearrange("(k p) n -> p k n", p=128)
    for j in range(24):
        bi = j % 2
        S.dma("sp", ch_adaw[bi], lambda h, j=j, bi=bi: h.dma_start(out=adaw[bi][:], in_=adaw_v[:, :, j * 512:(j + 1) * 512]),
              writes=[Badaw[bi]])
        bank = 2 + (j % 2)
        for kc in range(KC):
            S.op("pe", lambda h, kc=kc, bi=bi, bank=bank: h.matmul(psf[bank][0:2, :], lhsT=sil2[:, kc, :], rhs=adaw[bi][:, kc, :],
                                                                     start=(kc == 0), stop=(kc == KC - 1)),
                 reads=[Bsil2, Badaw[bi]], writes=[Bpsf[bank]])
        S.op("dve", lambda h, j=j, bank=bank: h.tensor_tensor(out=modsb[:, j * 512:(j + 1) * 512], in0=psf[bank][0:2, :],
                                                               in1=adab[:, j * 512:(j + 1) * 512], op=ALU.add),
             reads=[Bpsf[bank], Badab], writes=[Bmodsb])
    Bmod_s = Buf("mod_s")
    S.dma("sp", ch_small, lambda h: h.dma_start(out=mod_s, in_=modsb[:]), reads=[Bmodsb], writes=[Bmod_s])
    if debug:
        d_mod = dbg_out("mod", [2, 6 * D])
        final_ops.append(S.dma("sp", S.chan("dbgmod"), lambda h: h.dma_start(out=d_mod, in_=modsb[:]), reads=[Bmodsb]))
    S.barrier()
    AR.reset(m0)
    if stage <= 0:
        return finish(nc, S, final_ops), dbg

    def load_mod_bcast(dst, row, g, chan, Bdst):
        return S.dma("sp", chan, lambda h: h.dma_start(out=dst[:], in_=mod_s[row:row + 1, g * D:(g + 1) * D].partition_broadcast(128)),
                     reads=[Bmod_s], writes=[Bdst])

    def load_row_bcast(dst, row_ap, chan, Bdst, cont=False):
        return S.dma("sp", chan, lambda h: h.dma_start(out=dst[:], in_=row_ap.partition_broadcast(128)), writes=[Bdst], cont=cont)

    hT = AR.alloc("hT", [128, KC, SEQ], BF16)
    hcT_off = AR.mark()
    hcT = AR.alloc("hcT", [128, KC, NCTX], BF16)
    BhT = [Buf(f"hT{i}") for i in range(NT)]
    BhcT = Buf("hcT")
    m1 = AR.mark()
    A1 = AR.alloc("A1", [128, D], F32)
    S1 = AR.alloc("S1", [128, D], F32)
    A1c = AR.alloc("A1c", [128, D], F32)
    S1c = AR.alloc("S1c", [128, D], F32)
    wtmp = AR.alloc("wtmp", [128, D], F32)
    xb = [AR.alloc(f"xb{i}", [128, D], F32) for i in range(2)]
    t32 = AR.alloc("t32", [128, D], F32)
    hb = [AR.alloc(f"hb{i}", [128, D], BF16) for i in range(2)]
    junkb = AR.alloc("junkb", [128, D], BF16)
    stat = AR.alloc("stat", [128, 8], F32)
    BA1, BS1, BA1c, BS1c, Bwtmp, Bt32, Bjunk, Bstat = (Buf(n) for n in ["A1", "S1", "A1c", "S1c", "wtmp", "t32", "junk", "stat"])
    Bxb = [Buf("xb0"), Buf("xb1")]
    Bhb = [Buf("hb0"), Buf("hb1")]
    ch_x = [S.chan("x0"), S.chan("x1")]
    ch_v = S.chan("vecs")

    load_row_bcast(wtmp, pmw_d, ch_v, Bwtmp)
    load_mod_bcast(A1, 0, 1, ch_v, BA1)
    load_mod_bcast(S1, 0, 0, ch_v, BS1)
    load_mod_bcast(A1c, 1, 1, ch_v, BA1c)
    load_mod_bcast(S1c, 1, 0, ch_v, BS1c)
    S.op("dve", lambda h: h.scalar_tensor_tensor(out=A1[:], in0=A1[:], scalar=1.0, in1=wtmp[:], op0=ALU.add, op1=ALU.mult),
         reads=[BA1, Bwtmp], writes=[BA1])
    S.op("dve", lambda h: h.scalar_tensor_tensor(out=A1c[:], in0=A1c[:], scalar=1.0, in1=wtmp[:], op0=ALU.add, op1=ALU.mult),
         reads=[BA1c, Bwtmp], writes=[BA1c])

    def norm_mod_tile(src_ap_dram, xbuf, Bx, chan, Avec, BAv, Svec, BSv, hbuf, Bh, sidx):
        S.dma("sp", chan, lambda h: h.dma_start(out=xbuf[:], in_=src_ap_dram), writes=[Bx])
        S.op("act", lambda h: h.activation(out=junkb[:], in_=xbuf[:], func=AF.Square, accum_out=stat[:, sidx:sidx + 1]),
             reads=[Bx], writes=[Bjunk, Bstat])
        S.op("act", lambda h: h.activation(out=stat[:, sidx:sidx + 1], in_=stat[:, sidx:sidx + 1], func=AF.Sqrt, scale=1.0 / D, bias=EPS),
             reads=[Bstat], writes=[Bstat])
        S.op("dve", lambda h: h.reciprocal(out=stat[:, sidx:sidx + 1], in_=stat[:, sidx:sidx + 1]), reads=[Bstat], writes=[Bstat])
        S.op("dve", lambda h: h.scalar_tensor_tensor(out=t32[:], in0=xbuf[:], scalar=stat[:, sidx:sidx + 1], in1=Avec[:],
                                                     op0=ALU.mult, op1=ALU.mult), reads=[Bx, Bstat, BAv], writes=[Bt32])
        S.op("pool", lambda h: h.tensor_tensor(out=hbuf[:], in0=t32[:], in1=Svec[:], op=ALU.add), reads=[Bt32, BSv], writes=[Bh])

    def transpose_tile_bf16(hbuf, Bh, dstT, col0, Bdst):
        for half in range(2):
            pb = half
            for q in range(8):
                kc = half * 8 + q
                S.op("pe", lambda h, kc=kc, q=q, pb=pb: h.transpose(out=psb[pb][:, q * 128:(q + 1) * 128], in_=hbuf[:, kc * 128:(kc + 1) * 128],
                                                                       identity=identb[:]),
                     reads=[Bh, Bc], writes=[Bpsb[pb]])
            eng = "act" if half == 0 else "dve"
            if eng == "act":
                S.op("act", lambda h, half=half, pb=pb: h.copy(out=dstT[:, half * 8:(half + 1) * 8, col0:col0 + 128],
                                                                 in_=psb[pb][:, :].rearrange("p (q t) -> p q t", q=8)),
                     reads=[Bpsb[pb]], writes=[Bdst])
            else:
                S.op("dve", lambda h, half=half, pb=pb: h.tensor_copy(out=dstT[:, half * 8:(half + 1) * 8, col0:col0 + 128],
                                                                        in_=psb[pb][:, :].rearrange("p (q t) -> p q t", q=8)),
                     reads=[Bpsb[pb]], writes=[Bdst])

    for i in range(2):
        bi = i % 2
        norm_mod_tile(ctx_d[i * 128:(i + 1) * 128, :], xb[bi], Bxb[bi], ch_x[bi], A1c, BA1c, S1c, BS1c, hb[bi], Bhb[bi], i % 8)
        transpose_tile_bf16(hb[bi], Bhb[bi], hcT, i * 128, BhcT)
    for i in range(NT):
        bi = i % 2
        norm_mod_tile(x_d[i * 128:(i + 1) * 128, :], xb[bi], Bxb[bi], ch_x[bi], A1, BA1, S1, BS1, hb[bi], Bhb[bi], i % 8)
        transpose_tile_bf16(hb[bi], Bhb[bi], hT, i * 128, BhT[i])
    if debug:
        d_hT = dbg_out("hT", [128, KC, SEQ], BF16)
        final_ops.append(S.dma("sp", S.chan("dbghT"), lambda h: h.dma_start(out=d_hT, in_=hT[:]), reads=BhT))
    S.barrier()
    AR.reset(m1)
    if stage <= 1:
        return finish(nc, S, final_ops), dbg

    ByT_s = Buf("yT_s")
    win_v = win_d.rearrange("(k p) n -> p k n", p=128)

    m2 = AR.mark()
    decb = AR.alloc("decb", [128, 16], F32)
    Mh = AR.alloc("Mh", [128, H, 128], F32)
    dcol = AR.alloc("dcol", [128, H, 6], F32)
    wctx = AR.alloc("wctx", [128, H, 4], F32)
    Bdecb, BMh, Bdcol, Bwctx = Buf("decb"), Buf("Mh"), Buf("dcol"), Buf("wctx")
    tA = AR.alloc("tA", [128, 128], F32)
    tB = AR.alloc("tB", [128, 128], F32)
    tC = AR.alloc("tC", [128, 128], F32)
    tD = AR.alloc("tD", [128, 128], F32)
    cols = AR.alloc("cols", [128, 8], F32)
    BtA, BtB, BtC, BtD, Bcols = Buf("tA"), Buf("tB"), Buf("tC"), Buf("tD"), Buf("cols")
    S.dma("sp", ch_v, lambda h: h.dma_start(out=decb[:], in_=dec_d.partition_broadcast(128)), writes=[Bdecb])
    S.op("act", lambda h: h.activation(out=decb[:], in_=decb[:], func=AF.Exp, scale=-1.0), reads=[Bdecb], writes=[Bdecb])
    S.op("act", lambda h: h.activation(out=decb[:], in_=decb[:], func=AF.Ln, bias=1.0), reads=[Bdecb], writes=[Bdecb])
    S.op("dve", lambda h: h.tensor_scalar(out=decb[:], in0=decb[:], scalar1=-1.0, scalar2=None, op0=ALU.mult), reads=[Bdecb], writes=[Bdecb])
    S.op("dve", lambda h: h.tensor_scalar(out=tA[:], in0=iorow[:], scalar1=iocol[:, 0:1], scalar2=0.0, op0=ALU.subtract, op1=ALU.max),
         reads=[Bc], writes=[BtA])
    S.op("dve", lambda h: h.tensor_scalar(out=tB[:], in0=iorow[:], scalar1=iocol[:, 0:1], scalar2=-1.0, op0=ALU.subtract, op1=ALU.mult),
         reads=[Bc], writes=[BtB])
    S.op("dve", lambda h: h.tensor_scalar(out=tB[:], in0=tB[:], scalar1=0.0, scalar2=None, op0=ALU.max), reads=[BtB], writes=[BtB])
    S.op("dve", lambda h: h.tensor_scalar(out=tC[:], in0=iorow[:], scalar1=iocol[:, 0:1], scalar2=None, op0=ALU.is_ge), reads=[Bc], writes=[BtC])
    S.op("dve", lambda h: h.tensor_scalar(out=tD[:], in0=iorow[:], scalar1=iocol[:, 0:1], scalar2=None, op0=ALU.is_le), reads=[Bc], writes=[BtD])
    for ci, (mul, add) in enumerate([(1.0, 1.0), (-1.0, 128.0), (-1.0, 127.0), (1.0, 0.0), (0.0, 128.0), (-1.0, 255.0), (-1.0, 127.0), (1.0, 128.0)]):
        S.op("dve", lambda h, ci=ci, mul=mul, add=add: h.tensor_scalar(out=cols[:, ci:ci + 1], in0=iocol[:, 0:1], scalar1=mul, scalar2=add,
                                                                      op0=ALU.mult, op1=ALU.add), reads=[Bc], writes=[Bcols])
    ex1 = AR.alloc("ex1", [128, 128], F32)
    ex2 = AR.alloc("ex2", [128, 128], F32)
    Bex1, Bex2 = Buf("ex1"), Buf("ex2")
    for hh in range(H):
        lf = decb[:, hh:hh + 1]
        lb = decb[:, 8 + hh:9 + hh]
        S.op("act", lambda h, lf=lf: h.activation(out=ex1[:], in_=tA[:], func=AF.Exp, scale=lf), reads=[BtA, Bdecb], writes=[Bex1])
        S.op("act", lambda h, lb=lb: h.activation(out=ex2[:], in_=tB[:], func=AF.Exp, scale=lb), reads=[BtB, Bdecb], writes=[Bex2])
        S.op("dve", lambda h: h.tensor_tensor(out=ex1[:], in0=ex1[:], in1=tC[:], op=ALU.mult), reads=[Bex1, BtC], writes=[Bex1])
        S.op("dve", lambda h: h.tensor_tensor(out=ex2[:], in0=ex2[:], in1=tD[:], op=ALU.mult), reads=[Bex2, BtD], writes=[Bex2])
        S.op("dve", lambda h, hh=hh: h.tensor_tensor(out=Mh[:, hh, :], in0=ex1[:], in1=ex2[:], op=ALU.add), reads=[Bex1, Bex2], writes=[BMh])
        for (dst, ci, lg) in [(0, 0, lf), (1, 1, lb), (2, 2, lf), (3, 3, lb), (4, 4, lf), (5, 4, lb)]:
            S.op("act", lambda h, hh=hh, dst=dst, ci=ci, lg=lg: h.activation(out=dcol[:, hh, dst:dst + 1], in_=cols[:, ci:ci + 1], func=AF.Exp, scale=lg),
                 reads=[Bcols, Bdecb], writes=[Bdcol])
        for (dst, ci, lg) in [(0, 5, lf), (1, 6, lf), (2, 3, lb), (3, 7, lb)]:
            S.op("act", lambda h, hh=hh, dst=dst, ci=ci, lg=lg: h.activation(out=wctx[:, hh, dst:dst + 1], in_=cols[:, ci:ci + 1], func=AF.Exp, scale=lg),
                 reads=[Bcols, Bdecb], writes=[Bwctx])

    cosL = AR.alloc("cosL", [128, NT, 64], F32)
    sinL = AR.alloc("sinL", [128, NT, 64], F32)
    cosT = cosL[:, :, :].unsqueeze(2).to_broadcast([128, NT, 2, 64])
    sinT = sinL[:, :, :].unsqueeze(2).to_broadcast([128, NT, 2, 64])
    cosC = AR.alloc("cosC", [128, 2, 64], F32)
    sinC = AR.alloc("sinC", [128, 2, 64], F32)
    Brope = Buf("rope")
    cos_lat = cos_d[NCTX:NCTX + SEQ, :].rearrange("(t p) f -> p t f", p=128)
    sin_lat = sin_d[NCTX:NCTX + SEQ, :].rearrange("(t p) f -> p t f", p=128)
    S.dma("sp", ch_v, lambda h: h.dma_start(out=cosL[:], in_=cos_lat), writes=[Brope])
    S.dma("sp", ch_v, lambda h: h.dma_start(out=sinL[:], in_=sin_lat), writes=[Brope], cont=True)
    S.dma("sp", ch_v, lambda h: h.dma_start(out=cosC[:], in_=cos_d[0:NCTX, :].rearrange("(t p) f -> p t f", p=128)), writes=[Brope], cont=True)
    S.dma("sp", ch_v, lambda h: h.dma_start(out=sinC[:], in_=sin_d[0:NCTX, :].rearrange("(t p) f -> p t f", p=128)), writes=[Brope], cont=True)
    gnwb = AR.alloc("gnwb", [128, DRET], F32)
    Bgnwb = Buf("gnwb")
    load_row_bcast(gnwb, gnw_d, ch_v, Bgnwb)

    R0 = AR.alloc("R0", [128, H, 2, 128], F32)
    BR0 = Buf("R0")
    m2b = AR.mark()
    wkv = [AR.alloc(f"wkv{i}", [128, KC, 256], BF16) for i in range(2)]
    Bwkv = [Buf("wkv0"), Buf("wkv1")]
    ch_wkv = [S.chan("wkv0"), S.chan("wkv1")]
    kc32 = AR.alloc("kc32", [128, 2, 128], F32)
    kcr = AR.alloc("kcr", [128, 2, 128], BF16)
    vwf = AR.alloc("vwf", [128, 2, 128], BF16)
    vwb = AR.alloc("vwb", [128, 2, 128], BF16)
    rt1 = AR.alloc("rt1", [128, 2, 64], F32)
    rt2 = AR.alloc("rt2", [128, 2, 64], F32)
    Bkc32, Bkcr, Bvwf, Bvwb, Brt1, Brt2 = (Buf(n) for n in ["kc32", "kcr", "vwf", "vwb", "rt1", "rt2"])
    for hh in range(H):
        bi = hh % 2
        S.dma("pool", ch_wkv[bi], lambda h, hh=hh, bi=bi: h.dma_start(out=wkv[bi][:, :, 0:128], in_=win_v[:, :, DRET + hh * 128:DRET + (hh + 1) * 128]),
              writes=[Bwkv[bi]])
        S.dma("pool", ch_wkv[bi], lambda h, hh=hh, bi=bi: h.dma_start(out=wkv[bi][:, :, 128:256], in_=win_v[:, :, 2 * DRET + hh * 128:2 * DRET + (hh + 1) * 128]),
              writes=[Bwkv[bi]], cont=True)
        for t in range(2):
            bank = t
            for kc in range(KC):
                S.op("pe", lambda h, kc=kc, t=t, bi=bi, bank=bank: h.matmul(psf[bank][:, 0:256], lhsT=hcT[:, kc, t * 128:(t + 1) * 128], rhs=wkv[bi][:, kc, :],
                                                                             start=(kc == 0), stop=(kc == KC - 1)),
                     reads=[BhcT, Bwkv[bi]], writes=[Bpsf[bank]])
            S.op("act", lambda h, t=t, bank=bank: h.copy(out=kc32[:, t, :], in_=psf[bank][:, 0:128]), reads=[Bpsf[bank]], writes=[Bkc32])
            S.op("dve", lambda h, t=t, bank=bank, hh=hh: h.tensor_scalar(out=vwf[:, t, :], in0=psf[bank][:, 128:256], scalar1=wctx[:, hh, t:t + 1], scalar2=None, op0=ALU.mult),
                 reads=[Bpsf[bank], Bwctx], writes=[Bvwf])
            S.op("dve", lambda h, t=t, bank=bank, hh=hh: h.tensor_scalar(out=vwb[:, t, :], in0=psf[bank][:, 128:256], scalar1=wctx[:, hh, 2 + t:3 + t], scalar2=None, op0=ALU.mult),
                 reads=[Bpsf[bank], Bwctx], writes=[Bvwb])
        k1 = kc32[:, :, 0:64]
        k2 = kc32[:, :, 64:128]
        S.op("dve", lambda h: h.tensor_tensor(out=rt1[:], in0=k1, in1=cosC[:], op=ALU.mult), reads=[Bkc32, Brope], writes=[Brt1])
        S.op("pool", lambda h: h.tensor_tensor(out=rt2[:], in0=k2, in1=sinC[:], op=ALU.mult), reads=[Bkc32, Brope], writes=[Brt2])
        S.op("dve", lambda h: h.tensor_tensor(out=kcr[:, :, 0:64], in0=rt1[:], in1=rt2[:], op=ALU.subtract), reads=[Brt1, Brt2], writes=[Bkcr])
        S.op("dve", lambda h: h.tensor_tensor(out=rt1[:], in0=k1, in1=sinC[:], op=ALU.mult), reads=[Bkc32, Brope, Bkcr], writes=[Brt1])
        S.op("pool", lambda h: h.tensor_tensor(out=rt2[:], in0=k2, in1=cosC[:], op=ALU.mult), reads=[Bkc32, Brope, Bkcr], writes=[Brt2])
        S.op("dve", lambda h: h.tensor_tensor(out=kcr[:, :, 64:128], in0=rt1[:], in1=rt2[:], op=ALU.add), reads=[Brt1, Brt2], writes=[Bkcr])
        for di, vw, Bvw in [(0, vwf, Bvwf), (1, vwb, Bvwb)]:
            bank = 2 + di
            for t in range(2):
                S.op("pe", lambda h, t=t, bank=bank, vw=vw: h.matmul(psf[bank][:, 0:128], lhsT=kcr[:, t, :], rhs=vw[:, t, :], start=(t == 0), stop=(t == 1)),
                     reads=[Bkcr, Bvw], writes=[Bpsf[bank]])
            S.op("act", lambda h, hh=hh, di=di, bank=bank: h.copy(out=R0[:, hh, di, :], in_=psf[bank][:, 0:128]), reads=[Bpsf[bank]], writes=[BR0])
    if debug:
        d_R0 = dbg_out("R0", [128, H, 2, 128])
        final_ops.append(S.dma("sp", S.chan("dbgR0"), lambda h: h.dma_start(out=d_R0, in_=R0[:]), reads=[BR0]))
    S.barrier()
    AR.reset(m2b)
    if stage <= 2:
        return finish(nc, S, final_ops), dbg

    m2c = AR.mark()
    wq_off = AR.mark()
    wq1 = AR.alloc("wq", [128, KC, 512], BF16)
    wq = [wq1, wq1]
    Bwq1 = Buf("wq")
    Bwq = [Bwq1, Bwq1]
    ch_wq1 = S.chan("wq")
    ch_wq = [ch_wq1, ch_wq1]
    qT = AR.alloc_at("qT", [128, SEQ], BF16, wq_off)
    qdfT = AR.alloc_at("qdfT", [128, SEQ], BF16, wq_off + 4096)
    qdbT = AR.alloc_at("qdbT", [128, SEQ], BF16, wq_off + 8192)
    kT = AR.alloc_at("kT", [128, SEQ], BF16, wq_off + 12288)
    BqT = BqdfT = BqdbT = BkT = Bwq1
    qk_off = AR.mark()
    qk32 = AR.alloc("qk32", [128, NT, 2, 2, 64], F32)
    Bqk32 = Buf("qk32")
    o32 = AR.alloc_at("o32", [128, NT, 128], F32, qk_off)
    ytok = AR.alloc_at("ytok", [128, NT, 128], BF16, qk_off + 8192)
    yTh = AR.alloc_at("yTh", [128, SEQ], BF16, qk_off + 12288)
    Bo32 = Bytok = ByTh = Bqk32
    ta_off = AR.mark()
    ta = AR.alloc("ta", [128, NT, 2, 64], F32)
    Bta = Buf("ta")
    Sfb = AR.alloc_at("Sfb", [128, NT, 128], BF16, ta_off)
    Sbb = AR.alloc_at("Sbb", [128, NT, 128], BF16, ta_off + 4096)
    BSfb = BSbb = Bta
    tb = AR.alloc("tb", [128, NT, 2, 64], F32)
    rotb = AR.alloc("rotb", [128, NT, 2, 2, 64], BF16)
    qdf = AR.alloc("qdf", [128, NT, 2, 64], BF16)
    qdb = AR.alloc("qdb", [128, NT, 2, 64], BF16)
    kdf = AR.alloc("kdf", [128, NT, 2, 64], BF16)
    kdb = AR.alloc("kdb", [128, NT, 2, 64], BF16)
    vtok = AR.alloc("vtok", [128, NT, 128], BF16)
    sg = AR.alloc_at("sg", [128, NT, 128], F32, hcT_off)
    Rf = AR.alloc("Rf", [128, 128], F32)
    Rb = AR.alloc("Rb", [128, 128], F32)
    SM = [AR.alloc(f"SM{i}", [128, 128], BF16) for i in range(2)]
    bnst = AR.alloc("bnst", [128, NT, 6], F32)
    mv = AR.alloc("mv", [128, NT, 2], F32)
    rstd = AR.alloc("rstd", [128, NT], F32)
    (Btb, Brotb, Bqdf, Bqdb, Bkdf, Bkdb, Bvtok, Bsg, BRf, BRb, Bbnst, Bmv, Brstd) = (
        Buf(n) for n in ["tb", "rotb", "qdf", "qdb", "kdf", "kdb", "vtok", "sg", "Rf", "Rb", "bnst", "mv", "rstd"])
    BSM = [Buf("SM0"), Buf("SM1")]
    ch_yT = S.chan("yTout")

    for hh in range(H):
        bi = hh % 2
        for seg in range(4):
            S.dma("pool", ch_wq[bi], lambda h, hh=hh, bi=bi, seg=seg: h.dma_start(out=wq[bi][:, :, seg * 128:(seg + 1) * 128],
                                                                                    in_=win_v[:, :, seg * DRET + hh * 128:seg * DRET + (hh + 1) * 128]),
                  writes=[Bwq[bi]], cont=(seg > 0))
        for i in range(NT):
            bank = i % 4
            for kc in range(KC):
                S.op("pe", lambda h, kc=kc, i=i, bi=bi, bank=bank: h.matmul(psf[bank][:, :], lhsT=hT[:, kc, i * 128:(i + 1) * 128], rhs=wq[bi][:, kc, :],
                                                                             start=(kc == 0), stop=(kc == KC - 1)),
                     reads=[BhT[i], Bwq[bi]], writes=[Bpsf[bank]])
            S.op("act", lambda h, i=i, bank=bank: h.copy(out=qk32[:, i, :, :, :], in_=psf[bank][:, 0:256].rearrange("p (a b c) -> p a b c", a=2, b=2)),
                 reads=[Bpsf[bank]], writes=[Bqk32])
            S.op("act", lambda h, i=i, bank=bank: h.copy(out=vtok[:, i, :], in_=psf[bank][:, 256:384]), reads=[Bpsf[bank]], writes=[Bvtok])
            S.op("act", lambda h, i=i, bank=bank: h.activation(out=sg[:, i, :], in_=psf[bank][:, 384:512], func=AF.Silu), reads=[Bpsf[bank]], writes=[Bsg])
        X1 = qk32[:, :, :, 0, :]
        X2 = qk32[:, :, :, 1, :]
        S.op("dve", lambda h: h.tensor_tensor(out=ta[:], in0=X1, in1=cosT, op=ALU.mult), reads=[Bqk32, Brope], writes=[Bta])
        S.op("pool", lambda h: h.tensor_tensor(out=tb[:], in0=X2, in1=sinT, op=ALU.mult), reads=[Bqk32, Brope], writes=[Btb])
        S.op("dve", lambda h: h.tensor_tensor(out=ta[:], in0=ta[:], in1=tb[:], op=ALU.subtract), reads=[Bta, Btb], writes=[Bta])
        S.op("pool", lambda h: h.tensor_tensor(out=tb[:], in0=X1, in1=sinT, op=ALU.mult), reads=[Bqk32, Brope, Bta], writes=[Btb])
        S.op("dve", lambda h: h.tensor_tensor(out=X1, in0=X2, in1=cosT, op=ALU.mult), reads=[Bqk32, Brope, Btb], writes=[Bqk32])
        S.op("dve", lambda h: h.tensor_tensor(out=tb[:], in0=tb[:], in1=X1, op=ALU.add), reads=[Btb, Bqk32], writes=[Btb])
        S.op("act", lambda h: h.mul(out=rotb[:, :, 0, 0, :], in_=ta[:, :, 0, :], mul=QSCALE), reads=[Bta], writes=[Brotb])
        S.op("act", lambda h: h.copy(out=rotb[:, :, 1, 0, :], in_=ta[:, :, 1, :]), reads=[Bta], writes=[Brotb])
        S.op("act", lambda h: h.mul(out=rotb[:, :, 0, 1, :], in_=tb[:, :, 0, :], mul=QSCALE), reads=[Btb], writes=[Brotb])
        S.op("act", lambda h: h.copy(out=rotb[:, :, 1, 1, :], in_=tb[:, :, 1, :]), reads=[Btb], writes=[Brotb])
        for (dst, Bd, src_i, col, sc) in [(qdf, Bqdf, 0, 0, QSCALE), (qdb, Bqdb, 0, 1, QSCALE), (kdf, Bkdf, 1, 2, 1.0), (kdb, Bkdb, 1, 3, 1.0)]:
            S.op("dve", lambda h, dst=dst, src_i=src_i, col=col, hh=hh, sc=sc: h.tensor_scalar(out=dst[:, :, 0, :], in0=ta[:, :, src_i, :], scalar1=dcol[:, hh, col:col + 1],
                                                                                              scalar2=sc, op0=ALU.mult, op1=ALU.mult), reads=[Bta, Bdcol], writes=[Bd])
            S.op("pool", lambda h, dst=dst, src_i=src_i, col=col, hh=hh, sc=sc: h.tensor_scalar(out=dst[:, :, 1, :], in0=tb[:, :, src_i, :], scalar1=dcol[:, hh, col:col + 1],
                                                                                               scalar2=sc, op0=ALU.mult, op1=ALU.mult), reads=[Btb, Bdcol], writes=[Bd])
        srcs = [(lambda i: rotb[:, i, 0, :, :].rearrange("p a b -> p (a b)"), Brotb, qT, BqT),
                (lambda i: qdf[:, i, :, :].rearrange("p a b -> p (a b)"), Bqdf, qdfT, BqdfT),
                (lambda i: qdb[:, i, :, :].rearrange("p a b -> p (a b)"), Bqdb, qdbT, BqdbT),
                (lambda i: rotb[:, i, 1, :, :].rearrange("p a b -> p (a b)"), Brotb, kT, BkT)]
        cnt = 0
        for (srcf, Bsrc, dstT, BdstT) in srcs:
            for half in range(2):
                pb = cnt % 2
                cnt += 1
                for q in range(8):
                    i = half * 8 + q
                    S.op("pe", lambda h, i=i, q=q, pb=pb, srcf=srcf: h.transpose(out=psb[pb][:, q * 128:(q + 1) * 128], in_=srcf(i), identity=identb[:]),
                         reads=[Bsrc, Bc], writes=[Bpsb[pb]])
                if pb == 0:
                    S.op("act", lambda h, half=half, pb=pb, dstT=dstT: h.copy(out=dstT[:, half * 1024:(half + 1) * 1024], in_=psb[pb][:, :]),
                         reads=[Bpsb[pb]], writes=[BdstT])
                else:
                    S.op("dve", lambda h, half=half, pb=pb, dstT=dstT: h.tensor_copy(out=dstT[:, half * 1024:(half + 1) * 1024], in_=psb[pb][:, :]),
                         reads=[Bpsb[pb]], writes=[BdstT])
        S.op("dve", lambda h, hh=hh: h.tensor_copy(out=Rf[:], in_=R0[:, hh, 0, :]), reads=[BR0], writes=[BRf])
        S.op("dve", lambda h, hh=hh: h.tensor_copy(out=Rb[:], in_=R0[:, hh, 1, :]), reads=[BR0], writes=[BRb])
        S.op("act", lambda h: h.copy(out=Sfb[:, 0, :], in_=Rf[:]), reads=[BRf], writes=[BSfb])
        S.op("act", lambda h: h.copy(out=Sbb[:, NT - 1, :], in_=Rb[:]), reads=[BRb], writes=[BSbb])
        for step in range(NT - 1):
            i_f = step
            i_b = NT - 1 - step
            S.op("pe", lambda h, i=i_f: h.matmul(psf[4][:, 0:128], lhsT=kdf[:, i, :, :].rearrange("p a b -> p (a b)"), rhs=vtok[:, i, :], start=True, stop=True),
                 reads=[Bkdf, Bvtok], writes=[Bpsf[4]])
            S.op("dve", lambda h, hh=hh: h.scalar_tensor_tensor(out=Rf[:], in0=Rf[:], scalar=dcol[:, hh, 4:5], in1=psf[4][:, 0:128], op0=ALU.mult, op1=ALU.add),
                 reads=[BRf, Bdcol, Bpsf[4]], writes=[BRf])
            S.op("act", lambda h, i=i_f: h.copy(out=Sfb[:, i + 1, :], in_=Rf[:]), reads=[BRf], writes=[BSfb])
            S.op("pe", lambda h, i=i_b: h.matmul(psf[5][:, 0:128], lhsT=kdb[:, i, :, :].rearrange("p a b -> p (a b)"), rhs=vtok[:, i, :], start=True, stop=True),
                 reads=[Bkdb, Bvtok], writes=[Bpsf[5]])
            S.op("dve", lambda h, hh=hh: h.scalar_tensor_tensor(out=Rb[:], in0=Rb[:], scalar=dcol[:, hh, 5:6], in1=psf[5][:, 0:128], op0=ALU.mult, op1=ALU.add),
                 reads=[BRb, Bdcol, Bpsf[5]], writes=[BRb])
            S.op("act", lambda h, i=i_b: h.copy(out=Sbb[:, i - 1, :], in_=Rb[:]), reads=[BRb], writes=[BSbb])
        for i in range(NT):
            sb_ = i % 2
            bs = i % 2
            bo = 2 + (i % 2)
            cs = slice(i * 128, (i + 1) * 128)
            S.op("pe", lambda h, cs=cs, bs=bs: h.matmul(psf[bs][:, 0:128], lhsT=kT[:, cs], rhs=qT[:, cs], start=True, stop=True),
                 reads=[BkT, BqT], writes=[Bpsf[bs]])
            S.op("dve", lambda h, bs=bs, sb_=sb_, hh=hh: h.tensor_tensor(out=SM[sb_][:], in0=psf[bs][:, 0:128], in1=Mh[:, hh, :], op=ALU.mult),
                 reads=[Bpsf[bs], BMh], writes=[BSM[sb_]])
            S.op("pe", lambda h, i=i, sb_=sb_, bo=bo: h.matmul(psf[bo][:, 0:128], lhsT=SM[sb_][:], rhs=vtok[:, i, :], start=True, stop=False),
                 reads=[BSM[sb_], Bvtok], writes=[Bpsf[bo]])
            S.op("pe", lambda h, i=i, cs=cs, bo=bo: h.matmul(psf[bo][:, 0:128], lhsT=qdfT[:, cs], rhs=Sfb[:, i, :], start=False, stop=False),
                 reads=[BqdfT, BSfb], writes=[Bpsf[bo]])
            S.op("pe", lambda h, i=i, cs=cs, bo=bo: h.matmul(psf[bo][:, 0:128], lhsT=qdbT[:, cs], rhs=Sbb[:, i, :], start=False, stop=True),
                 reads=[BqdbT, BSbb], writes=[Bpsf[bo]])
            S.op("act", lambda h, i=i, bo=bo: h.copy(out=o32[:, i, :], in_=psf[bo][:, 0:128]), reads=[Bpsf[bo]], writes=[Bo32])
            S.op("dve", lambda h, i=i: h.bn_stats(out=bnst[:, i, :], in_=o32[:, i, :]), reads=[Bo32], writes=[Bbnst])
            S.op("dve", lambda h, i=i: h.bn_aggr(out=mv[:, i, :], in_=bnst[:, i, :]), reads=[Bbnst], writes=[Bmv])
        S.op("act", lambda h: h.activation(out=rstd[:], in_=mv[:, :, 1], func=AF.Sqrt, bias=GN_EPS), reads=[Bmv], writes=[Brstd])
        S.op("dve", lambda h: h.reciprocal(out=rstd[:], in_=rstd[:]), reads=[Brstd], writes=[Brstd])
        S.op("pool", lambda h, hh=hh: h.tensor_tensor(out=sg[:], in0=sg[:], in1=gnwb[:, hh * 128:(hh + 1) * 128].unsqueeze(1).to_broadcast([128, NT, 128]), op=ALU.mult),
             reads=[Bsg, Bgnwb], writes=[Bsg])
        for i in range(NT):
            S.op("dve", lambda h, i=i: h.tensor_scalar(out=o32[:, i, :], in0=o32[:, i, :], scalar1=mv[:, i, 0:1], scalar2=rstd[:, i:i + 1], op0=ALU.subtract, op1=ALU.mult),
                 reads=[Bo32, Bmv, Brstd], writes=[Bo32])
        S.op("pool", lambda h: h.tensor_tensor(out=ytok[:], in0=o32[:], in1=sg[:], op=ALU.mult), reads=[Bo32, Bsg], writes=[Bytok])
        for half in range(2):
            pb = half
            for q in range(8):
                i = half * 8 + q
                S.op("pe", lambda h, i=i, q=q, pb=pb: h.transpose(out=psb[pb][:, q * 128:(q + 1) * 128], in_=ytok[:, i, :], identity=identb[:]),
                     reads=[Bytok, Bc], writes=[Bpsb[pb]])
            if half == 0:
                S.op("act", lambda h, half=half, pb=pb: h.copy(out=yTh[:, half * 1024:(half + 1) * 1024], in_=psb[pb][:, :]), reads=[Bpsb[pb]], writes=[ByTh])
            else:
                S.op("dve", lambda h, half=half, pb=pb: h.tensor_copy(out=yTh[:, half * 1024:(half + 1) * 1024], in_=psb[pb][:, :]), reads=[Bpsb[pb]], writes=[ByTh])
        S.dma("sp", ch_yT, lambda h, hh=hh: h.dma_start(out=yT_s[hh * 128:(hh + 1) * 128, :], in_=yTh[:]), reads=[ByTh], writes=[ByT_s])
    S.barrier()
    AR.reset(m2c)
    if stage <= 3:
        if debug:
            d_yT = dbg_out("yT", [D, SEQ], BF16)
            ld = AR.alloc("dbgld", [128, KC, SEQ], BF16)
            Bld = Buf("dbgld")
            S.dma("sp", S.chan("dbgy1"), lambda h: h.dma_start(out=ld[:], in_=yT_s.rearrange("(c p) t -> p c t", p=128)), reads=[ByT_s], writes=[Bld])
            final_ops.append(S.dma("sp", S.chan("dbgy2"), lambda h: h.dma_start(out=d_yT.rearrange("(c p) t -> p c t", p=128), in_=ld[:]), reads=[Bld]))
        return finish(nc, S, final_ops), dbg

    m2d = AR.mark()
    cvrows = AR.alloc("cvrows", [24, 128], F32)
    cvT = AR.alloc("cvT", [128, 24], F32)
    cwrows = AR.alloc("cwrows", [CW, DCONV], F32)
    cwT = AR.alloc("cwT", [128, 8, CW], F32)
    Bcvrows, BcvT, Bcwrows, BcwT = Buf("cvrows"), Buf("cvT"), Buf("cwrows"), Buf("cwT")
    S.dma("sp", ch_v, lambda h: h.dma_start(out=cvrows[:], in_=cvec_d), writes=[Bcvrows])
    S.dma("sp", ch_v, lambda h: h.dma_start(out=cwrows[:], in_=convw_d), writes=[Bcwrows], cont=True)
    transpose_rows(cvrows[:], 24, cvT[:], 0, Bpsf[0], [Bcvrows], [BcvT])
    for cc in range(8):
        S.op("pe", lambda h, cc=cc: h.transpose(out=psf[1][:, 0:CW], in_=cwrows[:, cc * 128:(cc + 1) * 128], identity=ident[0:CW, 0:CW]),
             reads=[Bcwrows, Bc], writes=[Bpsf[1]])
        S.op("act", lambda h, cc=cc: h.copy(out=cwT[:, cc, :], in_=psf[1][:, 0:CW]), reads=[Bpsf[1]], writes=[BcwT])
    wc = [AR.alloc(f"wc{i}", [128, KC, 256], BF16) for i in range(2)]
    Bwc = [Buf("wc0"), Buf("wc1")]
    ch_wc = [S.chan("wc0"), S.chan("wc1")]
    sig = AR.alloc("sig", [128, 512], F32)
    uu = AR.alloc("uu", [128, 8, 64], F32)
    cvo = AR.alloc("cvo", [128, 8, 8, 64], F32)
    sq = AR.alloc("sq", [128, 512], F32)
    mean = AR.alloc("mean", [128, 512], F32)
    msq = AR.alloc("msq", [128, 512], F32)
    rsd = AR.alloc("rsd", [128, 512], F32)
    tn = AR.alloc("tn", [128, 512], F32)
    ycv = [AR.alloc(f"ycv{i}", [128, 512], BF16) for i in range(2)]
    Bsig, Buu, Bcvo, Bsq, Bmean, Bmsq, Brsd, Btn = (Buf(n) for n in ["sig", "uu", "cvo", "sq", "mean", "msq", "rsd", "tn"])
    Bycv = [Buf("ycv0"), Buf("ycv1")]
    ch_ycv = [S.chan("ycv0"), S.chan("ycv1")]
    pcount = 0
    for tbk in range(4):
        tsl = slice(tbk * 512, (tbk + 1) * 512)
        for cc in range(8):
            bi = pcount % 2
            pcount += 1
            S.dma("pool", ch_wc[bi], lambda h, cc=cc, bi=bi: h.dma_start(out=wc[bi][:, :, 0:128], in_=win_v[:, :, 4 * DRET + cc * 128:4 * DRET + (cc + 1) * 128]),
                  writes=[Bwc[bi]])
            S.dma("pool", ch_wc[bi], lambda h, cc=cc, bi=bi: h.dma_start(out=wc[bi][:, :, 128:256],
                                                                          in_=win_v[:, :, 4 * DRET + DCONV + cc * 128:4 * DRET + DCONV + (cc + 1) * 128]),
                  writes=[Bwc[bi]], cont=True)
            ba, bb = 0 + 2 * (cc % 2), 1 + 2 * (cc % 2)
            for kc in range(KC):
                S.op("pe", lambda h, kc=kc, bi=bi, ba=ba, tsl=tsl: h.matmul(psf[ba][:, :], lhsT=wc[bi][:, kc, 0:128], rhs=hT[:, kc, tsl], start=(kc == 0), stop=(kc == KC - 1)),
                     reads=[Bwc[bi]] + BhT[tbk * 4:(tbk + 1) * 4], writes=[Bpsf[ba]])
            for kc in range(KC):
                S.op("pe", lambda h, kc=kc, bi=bi, bb=bb, tsl=tsl: h.matmul(psf[bb][:, :], lhsT=wc[bi][:, kc, 128:256], rhs=hT[:, kc, tsl], start=(kc == 0), stop=(kc == KC - 1)),
                     reads=[Bwc[bi]] + BhT[tbk * 4:(tbk + 1) * 4], writes=[Bpsf[bb]])
            S.op("act", lambda h, bb=bb: h.activation(out=sig[:], in_=psf[bb][:, :], func=AF.Sigmoid), reads=[Bpsf[bb]], writes=[Bsig])
            S.op("dve", lambda h, ba=ba: h.tensor_tensor(out=uu[:].rearrange("p a b -> p (a b)"), in0=psf[ba][:, :], in1=sig[:], op=ALU.mult),
                 reads=[Bpsf[ba], Bsig], writes=[Buu])
            acc = cvo[:, cc, :, :]
            S.op("dve", lambda h, cc=cc, acc=acc: h.tensor_scalar(out=acc, in0=uu[:], scalar1=cwT[:, cc, 15:16], scalar2=cvT[:, cc:cc + 1], op0=ALU.mult, op1=ALU.add),
                 reads=[Buu, BcwT, BcvT], writes=[Bcvo])
            for k in range(CW):
                o = k - 15
                if o == 0:
                    continue
                lo, hi = max(0, -o), min(64, 64 - o)
                S.op("dve", lambda h, cc=cc, k=k, o=o, lo=lo, hi=hi: h.scalar_tensor_tensor(out=cvo[:, cc, :, lo:hi], in0=uu[:, :, lo + o:hi + o], scalar=cwT[:, cc, k:k + 1],
                                                                                             in1=cvo[:, cc, :, lo:hi], op0=ALU.mult, op1=ALU.add),
                     reads=[Buu, BcwT, Bcvo], writes=[Bcvo])
            accf = cvo[:, cc, :, :].rearrange("p a b -> p (a b)")
            S.op("act", lambda h, accf=accf: h.activation(out=sq[:], in_=accf, func=AF.Square), reads=[Bcvo], writes=[Bsq])
            S.op("pe", lambda h, accf=accf, cc=cc: h.matmul(psf[4][:, :], lhsT=ones32[:], rhs=accf, start=(cc == 0), stop=(cc == 7)), reads=[Bcvo, Bc], writes=[Bpsf[4]])
            S.op("pe", lambda h, cc=cc: h.matmul(psf[5][:, :], lhsT=ones32[:], rhs=sq[:], start=(cc == 0), stop=(cc == 7)), reads=[Bsq, Bc], writes=[Bpsf[5]])
        S.op("act", lambda h: h.activation(out=mean[:], in_=psf[4][:, :], func=AF.Identity, scale=1.0 / DCONV), reads=[Bpsf[4]], writes=[Bmean])
        S.op("dve", lambda h: h.tensor_tensor(out=msq[:], in0=mean[:], in1=mean[:], op=ALU.mult), reads=[Bmean], writes=[Bmsq])
        S.op("dve", lambda h: h.scalar_tensor_tensor(out=rsd[:], in0=psf[5][:, :], scalar=1.0 / DCONV, in1=msq[:], op0=ALU.mult, op1=ALU.subtract),
             reads=[Bpsf[5], Bmsq], writes=[Brsd])
        S.op("act", lambda h: h.activation(out=rsd[:], in_=rsd[:], func=AF.Sqrt, bias=EPS), reads=[Brsd], writes=[Brsd])
        S.op("dve", lambda h: h.reciprocal(out=rsd[:], in_=rsd[:]), reads=[Brsd], writes=[Brsd])
        for cc in range(8):
            yb = cc % 2
            accf = cvo[:, cc, :, :].rearrange("p a b -> p (a b)")
            S.op("dve", lambda h, accf=accf: h.tensor_tensor(out=tn[:], in0=accf, in1=mean[:], op=ALU.subtract), reads=[Bcvo, Bmean], writes=[Btn])
            S.op("pool", lambda h: h.tensor_tensor(out=tn[:], in0=tn[:], in1=rsd[:], op=ALU.mult), reads=[Btn, Brsd], writes=[Btn])
            S.op("act", lambda h, cc=cc, yb=yb: h.activation(out=ycv[yb][:], in_=tn[:], func=AF.Silu, scale=cvT[:, 8 + cc:9 + cc], bias=cvT[:, 16 + cc:17 + cc]),
                 reads=[Btn, BcvT], writes=[Bycv[yb]])
            S.dma("sp", ch_ycv[yb], lambda h, cc=cc, yb=yb, tsl=tsl: h.dma_start(out=yT_s[DRET + cc * 128:DRET + (cc + 1) * 128, tsl], in_=ycv[yb][:]),
                  reads=[Bycv[yb]], writes=[ByT_s])
    S.barrier()
    AR.reset(m2)
    AR.reset(persist_mark)
    if stage <= 4:
        if debug:
            d_yT = dbg_out("yT", [D, SEQ], BF16)
            ld = AR.alloc("dbgld", [128, KC, SEQ], BF16)
            Bld = Buf("dbgld")
            S.dma("sp", S.chan("dbgy1"), lambda h: h.dma_start(out=ld[:], in_=yT_s.rearrange("(c p) t -> p c t", p=128)), reads=[ByT_s], writes=[Bld])
            final_ops.append(S.dma("sp", S.chan("dbgy2"), lambda h: h.dma_start(out=d_yT.rearrange("(c p) t -> p c t", p=128), in_=ld[:]), reads=[Bld]))
        return finish(nc, S, final_ops), dbg

    m3 = AR.mark()
    wo = AR.alloc("wo", [128, KC, D], BF16)
    Bwo = Buf("wo")
    ch_wo = S.chan("wo")
    wout_v = wout_d.rearrange("(k p) n -> p k n", p=128)
    for j in range(4):
        S.dma("pool", ch_wo, lambda h, j=j: h.dma_start(out=wo[:, :, j * 512:(j + 1) * 512], in_=wout_v[:, :, j * 512:(j + 1) * 512]), writes=[Bwo], cont=(j > 0))
    G1 = AR.alloc("G1", [128, D], F32)
    A2 = AR.alloc("A2", [128, D], F32)
    S2 = AR.alloc("S2", [128, D], F32)
    wt3 = AR.alloc("wt3", [128, D], F32)
    BG1, BA2, BS2, Bwt3 = Buf("G1"), Buf("A2"), Buf("S2"), Buf("wt3")
    load_mod_bcast(G1, 0, 2, ch_v, BG1)
    load_row_bcast(wt3, pomw_d, ch_v, Bwt3)
    S.op("dve", lambda h: h.tensor_tensor(out=G1[:], in0=G1[:], in1=wt3[:], op=ALU.mult), reads=[BG1, Bwt3], writes=[BG1])
    load_mod_bcast(A2, 0, 4, ch_v, BA2)
    load_row_bcast(wt3, pfw_d, ch_v, Bwt3)
    S.op("dve", lambda h: h.scalar_tensor_tensor(out=A2[:], in0=A2[:], scalar=1.0, in1=wt3[:], op0=ALU.add, op1=ALU.mult), reads=[BA2, Bwt3], writes=[BA2])
    load_mod_bcast(S2, 0, 3, ch_v, BS2)
    rw32 = AR.alloc("rw32", [128, KC, NE], F32)
    rbb = AR.alloc("rbb", [128, NE], F32)
    ebase = AR.alloc("ebase", [128, NE], F32)
    Brw, Brbb, Bebase = Buf("rw32"), Buf("rbb"), Buf("ebase")
    S.dma("sp", ch_v, lambda h: h.dma_start(out=rw32[:], in_=rw_d.rearrange("(k p) e -> p k e", p=128)), writes=[Brw])
    S.dma("sp", ch_v, lambda h: h.dma_start(out=rbb[:], in_=rb_d.partition_broadcast(128)), writes=[Brbb], cont=True)
    S.op("dve", lambda h: h.tensor_scalar(out=ebase[:], in0=iorow[:, 0:NE], scalar1=float(CAP), scalar2=None, op0=ALU.mult), reads=[Bc], writes=[Bebase])
    yTt = [AR.alloc(f"yTt{i}", [128, KC, 128], BF16) for i in range(2)]
    ByTt = [Buf("yTt0"), Buf("yTt1")]
    ch_yTt = [S.chan("yTt0"), S.chan("yTt1")]
    xr = [AR.alloc(f"xr{i}", [128, D], F32) for i in range(2)]
    Bxr = [Buf("xr0"), Buf("xr1")]
    ch_xr = [S.chan("xr0"), S.chan("xr1")]
    t3 = AR.alloc("t3", [128, D], F32)
    x1t = [AR.alloc(f"x1t{i}", [128, D], F32) for i in range(2)]
    h2f = AR.alloc("h2f", [128, D], F32)
    h2b = [AR.alloc(f"h2b{i}", [128, D], BF16) for i in range(2)]
    h2T = AR.alloc("h2T", [128, KC, 128], F32)
    junk3 = AR.alloc("junk3", [128, D], BF16)
    st3 = AR.alloc("st3", [128, 8], F32)
    lg = AR.alloc("lg", [128, NE], F32)
    mx8 = AR.alloc("mx8", [128, 8], F32)
    nmx = AR.alloc("nmx", [128, 1], F32)
    msk = AR.alloc("msk", [128, NE], F32)
    mskb = AR.alloc("mskb", [128, NT, NE], BF16)
    exv = AR.alloc("exv", [128, NE], F32)
    den = AR.alloc("den", [128, 1], F32)
    posC = AR.alloc("posC", [128, NE], F32)
    ovf = AR.alloc("ovf", [128, NE], F32)
    oh = AR.alloc("oh", [128, NE], F32)
    jk = AR.alloc("jk", [128, NE], F32)
    idxf = AR.alloc("idxf", [128, 4], F32)
    idl = AR.alloc("idl", [128, 4], F32)
    idn = AR.alloc("idn", [128, 4], F32)
    Bidl, Bidn = Buf("idl"), Buf("idn")
    (Bt3, Bh2f, Bh2T, Bjunk3, Bst3, Blg, Bmx8, Bnmx, Bmsk, Bmskb, Bexv, Bden, BposC, Bovf, Boh, Bjk, Bidxf) = (
        Buf(n) for n in ["t3", "h2f", "h2T", "junk3", "st3", "lg", "mx8", "nmx", "msk", "mskb", "exv", "den", "posC", "ovf", "oh", "jk", "idxf"])
    Bx1t = [Buf("x1t0"), Buf("x1t1")]
    Bh2b = [Buf("h2b0"), Buf("h2b1")]
    ch_x1 = [S.chan("x1w0"), S.chan("x1w1")]
    ch_sc = [S.chan(f"scat{i}") for i in range(2)]
    Bx1_s = [Buf(f"x1_s{i}") for i in range(NT)]
    Bhsel = Buf("hsel_s")
    yT_v = yT_s.rearrange("(c p) t -> p c t", p=128)
    mixps = [psf[0], psf[1], psf[2], psf[3]]
    for i in range(NT):
        bi = i % 2
        S.dma("sp", ch_yTt[bi], lambda h, i=i, bi=bi: h.dma_start(out=yTt[bi][:], in_=yT_v[:, :, i * 128:(i + 1) * 128]), reads=[ByT_s], writes=[ByTt[bi]])
        S.dma("sp", ch_xr[bi], lambda h, i=i, bi=bi: h.dma_start(out=xr[bi][:], in_=x_d[i * 128:(i + 1) * 128, :]), writes=[Bxr[bi]])
        for cb in range(4):
            for c in range(KC):
                S.op("pe", lambda h, c=c, cb=cb, bi=bi: h.matmul(psf[cb][:, :], lhsT=yTt[bi][:, c, :], rhs=wo[:, c, cb * 512:(cb + 1) * 512], start=(c == 0), stop=(c == KC - 1)),
                     reads=[ByTt[bi], Bwo], writes=[Bpsf[cb]])
        for cb in range(4):
            S.op("act", lambda h, cb=cb: h.activation(out=junk3[:, cb * 512:(cb + 1) * 512], in_=psf[cb][:, :], func=AF.Square, accum_out=st3[:, cb:cb + 1]),
                 reads=[Bpsf[cb]], writes=[Bjunk3, Bst3])
        S.op("dve", lambda h: h.tensor_reduce(out=st3[:, 4:5], in_=st3[:, 0:4], axis=AX.X, op=ALU.add), reads=[Bst3], writes=[Bst3])
        S.op("act", lambda h: h.activation(out=st3[:, 4:5], in_=st3[:, 4:5], func=AF.Sqrt, scale=1.0 / D, bias=EPS), reads=[Bst3], writes=[Bst3])
        S.op("dve", lambda h: h.reciprocal(out=st3[:, 4:5], in_=st3[:, 4:5]), reads=[Bst3], writes=[Bst3])
        for cb in range(4):
            S.op("dve", lambda h, cb=cb: h.scalar_tensor_tensor(out=t3[:, cb * 512:(cb + 1) * 512], in0=psf[cb][:, :], scalar=st3[:, 4:5], in1=G1[:, cb * 512:(cb + 1) * 512],
                                                                op0=ALU.mult, op1=ALU.mult), reads=[Bpsf[cb], Bst3, BG1], writes=[Bt3])
        S.op("pool", lambda h, bi=bi: h.tensor_tensor(out=x1t[bi][:], in0=t3[:], in1=xr[bi][:], op=ALU.add), reads=[Bt3, Bxr[bi]], writes=[Bx1t[bi]])
        S.dma("sp", ch_x1[bi], lambda h, i=i, bi=bi: h.dma_start(out=x1_s[i * 128:(i + 1) * 128, :], in_=x1t[bi][:]), reads=[Bx1t[bi]], writes=[Bx1_s[i]])
        S.op("act", lambda h, bi=bi: h.activation(out=junk3[:], in_=x1t[bi][:], func=AF.Square, accum_out=st3[:, 5:6]), reads=[Bx1t[bi]], writes=[Bjunk3, Bst3])
        S.op("act", lambda h: h.activation(out=st3[:, 5:6], in_=st3[:, 5:6], func=AF.Sqrt, scale=1.0 / D, bias=EPS), reads=[Bst3], writes=[Bst3])
        S.op("dve", lambda h: h.reciprocal(out=st3[:, 5:6], in_=st3[:, 5:6]), reads=[Bst3], writes=[Bst3])
        S.op("dve", lambda h, bi=bi: h.scalar_tensor_tensor(out=t3[:], in0=x1t[bi][:], scalar=st3[:, 5:6], in1=A2[:], op0=ALU.mult, op1=ALU.mult),
             reads=[Bx1t[bi], Bst3, BA2], writes=[Bt3])
        S.op("pool", lambda h: h.tensor_tensor(out=h2f[:], in0=t3[:], in1=S2[:], op=ALU.add), reads=[Bt3, BS2], writes=[Bh2f])
        S.op("act", lambda h, bi=bi: h.copy(out=h2b[bi][:], in_=h2f[:]), reads=[Bh2f], writes=[Bh2b[bi]])
        for q4 in range(4):
            bank = 4 + (q4 % 2)
            for q in range(4):
                kc = q4 * 4 + q
                S.op("pe", lambda h, kc=kc, q=q, bank=bank: h.transpose(out=psf[bank][:, q * 128:(q + 1) * 128], in_=h2f[:, kc * 128:(kc + 1) * 128], identity=ident[:]),
                     reads=[Bh2f, Bc], writes=[Bpsf[bank]])
            if q4 % 2 == 0:
                S.op("act", lambda h, q4=q4, bank=bank: h.copy(out=h2T[:, q4 * 4:(q4 + 1) * 4, :], in_=psf[bank][:, :].rearrange("p (q t) -> p q t", q=4)),
                     reads=[Bpsf[bank]], writes=[Bh2T])
            else:
                S.op("dve", lambda h, q4=q4, bank=bank: h.tensor_copy(out=h2T[:, q4 * 4:(q4 + 1) * 4, :], in_=psf[bank][:, :].rearrange("p (q t) -> p q t", q=4)),
                     reads=[Bpsf[bank]], writes=[Bh2T])
        for kc in range(KC):
            S.op("pe", lambda h, kc=kc: h.matmul(psf[4][:, 0:NE], lhsT=h2T[:, kc, :], rhs=rw32[:, kc, :], start=(kc == 0), stop=(kc == KC - 1)),
                 reads=[Bh2T, Brw], writes=[Bpsf[4]])
        S.op("dve", lambda h: h.tensor_tensor(out=lg[:], in0=psf[4][:, 0:NE], in1=rbb[:], op=ALU.add), reads=[Bpsf[4], Brbb], writes=[Blg])
        S.op("dve", lambda h: h.max(out=mx8[:], in_=lg[:]), reads=[Blg], writes=[Bmx8])
        S.op("dve", lambda h: h.tensor_scalar(out=msk[:], in0=lg[:], scalar1=mx8[:, 3:4], scalar2=None, op0=ALU.is_ge), reads=[Blg, Bmx8], writes=[Bmsk])
        S.op("dve", lambda h, i=i: h.tensor_copy(out=mskb[:, i, :], in_=msk[:]), reads=[Bmsk], writes=[Bmskb])
        S.op("dve", lambda h: h.tensor_scalar(out=nmx[:], in0=mx8[:, 0:1], scalar1=-1.0, scalar2=None, op0=ALU.mult), reads=[Bmx8], writes=[Bnmx])
        S.op("act", lambda h: h.activation(out=exv[:], in_=lg[:], func=AF.Exp, bias=nmx[:, 0:1]), reads=[Blg, Bnmx], writes=[Bexv])
        S.op("dve", lambda h: h.tensor_tensor(out=exv[:], in0=exv[:], in1=msk[:], op=ALU.mult), reads=[Bexv, Bmsk], writes=[Bexv])
        S.op("dve", lambda h: h.tensor_reduce(out=den[:], in_=exv[:], axis=AX.X, op=ALU.add), reads=[Bexv], writes=[Bden])
        S.op("dve", lambda h: h.reciprocal(out=den[:], in_=den[:]), reads=[Bden], writes=[Bden])
        S.op("pe", lambda h, i=i: h.matmul(psf[5][:, 0:NE], lhsT=trib[:], rhs=mskb[:, i, :], start=True, stop=(i == 0)), reads=[Bmskb, Bc], writes=[Bpsf[5]])
        for j in range(i):
            S.op("pe", lambda h, j=j, i=i: h.matmul(psf[5][:, 0:NE], lhsT=onesb[:], rhs=mskb[:, j, :], start=False, stop=(j == i - 1)), reads=[Bmskb, Bc], writes=[Bpsf[5]])
        S.op("dve", lambda h: h.tensor_scalar(out=ovf[:], in0=psf[5][:, 0:NE], scalar1=float(CAP) - 0.5, scalar2=None, op0=ALU.is_gt), reads=[Bpsf[5]], writes=[Bovf])
        S.op("dve", lambda h: h.tensor_tensor(out=posC[:], in0=psf[5][:, 0:NE], in1=ebase[:], op=ALU.add), reads=[Bpsf[5], Bebase], writes=[BposC])
        S.op("dve", lambda h: h.scalar_tensor_tensor(out=posC[:], in0=ovf[:], scalar=BIG, in1=posC[:], op0=ALU.mult, op1=ALU.add), reads=[Bovf, BposC], writes=[BposC])
        S.op("dve", lambda h: h.tensor_scalar(out=ovf[:], in0=ovf[:], scalar1=-1.0, scalar2=1.0, op0=ALU.mult, op1=ALU.add), reads=[Bovf], writes=[Bovf])
        S.op("dve", lambda h, i=i: h.scalar_tensor_tensor(out=gatesA[:, i, :], in0=exv[:], scalar=den[:, 0:1], in1=ovf[:], op0=ALU.mult, op1=ALU.mult),
             reads=[Bexv, Bden, Bovf], writes=[BgatesA])
        for k in range(4):
            S.op("dve", lambda h, k=k: h.tensor_scalar(out=oh[:], in0=lg[:], scalar1=mx8[:, k:k + 1], scalar2=None, op0=ALU.is_equal), reads=[Blg, Bmx8], writes=[Boh])
            S.op("dve", lambda h, k=k: h.scalar_tensor_tensor(out=jk[:], in0=oh[:], scalar=1.0, in1=posC[:], op0=ALU.mult, op1=ALU.mult, accum_out=idxf[:, k:k + 1]),
                 reads=[Boh, BposC], writes=[Bjk, Bidxf])
            S.op("dve", lambda h, k=k, i=i: h.scalar_tensor_tensor(out=jk[:], in0=oh[:], scalar=1.0, in1=gatesA[:, i, :], op0=ALU.mult, op1=ALU.mult,
                                                                 accum_out=gate4[:, i, k:k + 1]), reads=[Boh, BgatesA], writes=[Bjk, Bgate4])
        for (lst, nten, Bl) in [(idxH, NHS, Bidx[i]), (idxY, NYS, Bidx[i])]:
            for j in range(nten):
                shift = float(j * (NE // nten) * CAP)
                S.op("dve", lambda h, shift=shift: h.tensor_scalar(out=idl[:], in0=idxf[:], scalar1=shift, scalar2=None, op0=ALU.subtract), reads=[Bidxf], writes=[Bidl])
                S.op("dve", lambda h: h.tensor_scalar(out=idn[:], in0=idl[:], scalar1=0.0, scalar2=BIG, op0=ALU.is_lt, op1=ALU.mult), reads=[Bidl], writes=[Bidn])
                S.op("dve", lambda h: h.tensor_tensor(out=idl[:], in0=idl[:], in1=idn[:], op=ALU.add), reads=[Bidl, Bidn], writes=[Bidl])
                S.op("dve", lambda h, i=i, t=lst[j]: h.tensor_copy(out=t[:, i * 4:(i + 1) * 4], in_=idl[:]), reads=[Bidl], writes=[Bl])
        first = True
        for k in range(4):
            for j in range(NHS):
                S.dma("pool", ch_sc[bi], lambda h, i=i, k=k, bi=bi, j=j: h.indirect_dma_start(out=hsel_s[j], out_offset=bass.IndirectOffsetOnAxis(ap=idxH[j][:, i * 4 + k:i * 4 + k + 1], axis=0),
                                                                                           in_=h2b[bi][:], in_offset=None, bounds_check=bound_reg(h, (NE // NHS) * CAP - 1), oob_is_err=False),
                      reads=[Bh2b[bi], Bidx[i]], writes=[Bhsel], cont=(not first))
                first = False
    for j in range(NT):
        S.op("pe", lambda h, j=j: h.matmul(psf[5][:, 0:NE], lhsT=onesb[:], rhs=mskb[:, j, :], start=(j == 0), stop=(j == NT - 1)), reads=[Bmskb, Bc], writes=[Bpsf[5]])
    S.cnt_op = S.op("dve", lambda h: h.tensor_copy(out=cnt_i[:], in_=psf[5][:, 0:NE]), reads=[Bpsf[5]], writes=[Bcnt])
    S.cnt_ap = lambda e: cnt_i[0:1, e:e + 1]
    if debug:
        d_lg = dbg_out("gates", [128, NT, NE])
        final_ops.append(S.dma("sp", S.chan("dbglg"), lambda h: h.dma_start(out=d_lg, in_=gatesA[:]), reads=[BgatesA]))
        d_idx = dbg_out("idx4", [128, NT * 4], I32)
        final_ops.append(S.dma("sp", S.chan("dbgidx"), lambda h: h.dma_start(out=d_idx, in_=idxY[0][:]), reads=Bidx))
        d_cnt = dbg_out("cnt", [128, NE], I32)
        final_ops.append(S.dma("sp", S.chan("dbgcnt"), lambda h: h.dma_start(out=d_cnt, in_=cnt_i[:]), reads=[Bcnt]))
        d_g4 = dbg_out("gate4", [128, NT, 4])
        final_ops.append(S.dma("sp", S.chan("dbgg4"), lambda h: h.dma_start(out=d_g4, in_=gate4[:]), reads=[Bgate4]))
    S.barrier()
    AR.reset(m3)
    if stage <= 5:
        if debug:
            d_x1 = dbg_out("x1", [SEQ, D])
            ld = AR.alloc("dbgld", [128, NT, D], F32)
            Bld = Buf("dbgld")
            S.dma("sp", S.chan("dbgx1"), lambda h: h.dma_start(out=ld[:], in_=x1_s.rearrange("(t p) d -> p t d", p=128)), reads=Bx1_s, writes=[Bld])
            final_ops.append(S.dma("sp", S.chan("dbgx2"), lambda h: h.dma_start(out=d_x1.rearrange("(t p) d -> p t d", p=128), in_=ld[:]), reads=[Bld]))
        return finish(nc, S, final_ops), dbg

    m5 = AR.mark()
    hselT = [AR.alloc(f"hselT{i}", [128, KC, RS], BF16) for i in range(2)]
    BhselT = [Buf("hselT0"), Buf("hselT1")]
    actT = AR.alloc("actT", [128, KC, RS], BF16)
    BactT = Buf("actT")
    NW1, NW2 = 4, 3
    w1u = [AR.alloc(f"w1u{i}", [128, KC, 512], BF16) for i in range(NW1)]
    Bw1u = [Buf(f"w1u{i}") for i in range(NW1)]
    ch_w1 = [S.chan(f"w1u{i}") for i in range(NW1)]
    w2p = [AR.alloc(f"w2p{i}", [128, KC, 512], BF16) for i in range(NW2)]
    Bw2p = [Buf(f"w2p{i}") for i in range(NW2)]
    ch_w2 = [S.chan(f"w2p{i}") for i in range(NW2)]
    hrow = [AR.alloc(f"hrow{i}", [128, D], BF16) for i in range(2)]
    Bhrow = [Buf("hrow0"), Buf("hrow1")]
    ch_hrow = [S.chan("hrow0"), S.chan("hrow1")]
    ysb = [AR.alloc(f"ysb{i}", [128, 512], F32) for i in range(4)]
    Bysb = [Buf(f"ysb{i}") for i in range(4)]
    ch_y = [S.chan(f"yw{i}") for i in range(4)]
    g1 = [AR.alloc(f"g1_{i}", [128, BLK], F32) for i in range(2)]
    sgm = [AR.alloc(f"sgm{i}", [128, BLK], F32) for i in range(2)]
    l2 = [AR.alloc(f"l2_{i}", [128, BLK], F32) for i in range(2)]
    wv = [AR.alloc(f"wv_{i}", [128, BLK], F32) for i in range(2)]
    Bg1 = [Buf("g1_0"), Buf("g1_1")]
    Bsgm = [Buf("sgm0"), Buf("sgm1")]
    Bl2 = [Buf("l2_0"), Buf("l2_1")]
    Bwv = [Buf("wv_0"), Buf("wv_1")]
    By_s = Buf("y_s")
    w1_v = [w1_d[e].rearrange("(k p) n -> p k n", p=128) for e in range(NE)]
    w2_v = [w2_d[e].rearrange("(k p) n -> p k n", p=128) for e in range(NE)]

    def blk_guard(e, r, b):
        return (e, b * BLK) if r == 0 else (e, r * RS)

    def rnd_guard(e, r):
        return (e, r * RS) if r > 0 else None

    def build_hselT(e, r, buf_i):
        for b in range(NBLK):
            S.cur_guard = blk_guard(e, r, b)
            for st2 in range(BLK // 128):
                st = b * (BLK // 128) + st2
                hb_i = st % 2
                row0 = (e % (NE // NHS)) * CAP + r * RS + st * 128
                src = hsel_s[e // (NE // NHS)]
                S.dma("sp", ch_hrow[hb_i], lambda h, row0=row0, hb_i=hb_i, src=src: h.dma_start(out=hrow[hb_i][:], in_=src[row0:row0 + 128, :]), reads=[Bhsel], writes=[Bhrow[hb_i]])
                for half in range(2):
                    pb = half
                    for q in range(8):
                        kc = half * 8 + q
                        S.op("pe", lambda h, kc=kc, q=q, pb=pb, hb_i=hb_i: h.transpose(out=psb[pb][:, q * 128:(q + 1) * 128], in_=hrow[hb_i][:, kc * 128:(kc + 1) * 128], identity=identb[:]),
                             reads=[Bhrow[hb_i], Bc], writes=[Bpsb[pb]])
                    if half == 0:
                        S.op("act", lambda h, half=half, pb=pb, st=st, buf_i=buf_i: h.copy(out=hselT[buf_i][:, half * 8:(half + 1) * 8, st * 128:(st + 1) * 128],
                                                                                        in_=psb[pb][:, :].rearrange("p (q t) -> p q t", q=8)), reads=[Bpsb[pb]], writes=[BhselT[buf_i]])
                    else:
                        S.op("dve", lambda h, half=half, pb=pb, st=st, buf_i=buf_i: h.tensor_copy(out=hselT[buf_i][:, half * 8:(half + 1) * 8, st * 128:(st + 1) * 128],
                                                                                               in_=psb[pb][:, :].rearrange("p (q t) -> p q t", q=8)), reads=[Bpsb[pb]], writes=[BhselT[buf_i]])
        S.cur_guard = None

    ucount = 0
    pcount2 = 0
    acnt = 0
    ycnt = 0
    er_list = [(e, r) for e in range(NE) for r in range(ROUNDS)]
    build_hselT(er_list[0][0], er_list[0][1], 0)
    for n, (e, r) in enumerate(er_list):
        hb_cur = n % 2
        for u in range(8):
            bi = ucount % NW1
            ucount += 1
            S.cur_guard = rnd_guard(e, r)
            S.dma("pool", ch_w1[bi], lambda h, e=e, u=u, bi=bi: h.dma_start(out=w1u[bi][:, :, 0:256], in_=w1_v[e][:, :, u * 256:(u + 1) * 256]), writes=[Bw1u[bi]])
            S.dma("pool", ch_w1[bi], lambda h, e=e, u=u, bi=bi: h.dma_start(out=w1u[bi][:, :, 256:512], in_=w1_v[e][:, :, DFF + u * 256:DFF + (u + 1) * 256]),
                  writes=[Bw1u[bi]], cont=True)
            for b in range(NBLK):
                S.cur_guard = blk_guard(e, r, b)
                for j in range(2):
                    fc = u * 2 + j
                    ab = acnt % 2
                    bank = acnt % 4
                    acnt += 1
                    bs = slice(b * BLK, (b + 1) * BLK)
                    for kc in range(KC):
                        S.op("pe", lambda h, kc=kc, bi=bi, j=j, bs=bs, bank=bank, hb_cur=hb_cur: h.matmul(psf[bank][:, 0:BLK], lhsT=w1u[bi][:, kc, j * 128:(j + 1) * 128],
                                                                                                     rhs=hselT[hb_cur][:, kc, bs], start=(kc == 0), stop=(kc == KC - 1)),
                             reads=[Bw1u[bi], BhselT[hb_cur]], writes=[Bpsf[bank]])
                    for kc in range(KC):
                        S.op("pe", lambda h, kc=kc, bi=bi, j=j, bs=bs, bank=bank, hb_cur=hb_cur: h.matmul(psf[bank][:, BLK:2 * BLK], lhsT=w1u[bi][:, kc, 256 + j * 128:256 + (j + 1) * 128],
                                                                                                     rhs=hselT[hb_cur][:, kc, bs], start=(kc == 0), stop=(kc == KC - 1)),
                             reads=[Bw1u[bi], BhselT[hb_cur]], writes=[Bpsf[bank]])
                    cg = e * 32 + fc
                    cl = e * 32 + 16 + fc
                    S.op("dve", lambda h, ab=ab, bank=bank, cg=cg: h.tensor_scalar(out=g1[ab][:], in0=psf[bank][:, 0:BLK], scalar1=b1T[:, cg:cg + 1], scalar2=LIMIT, op0=ALU.add, op1=ALU.min),
                         reads=[Bpsf[bank], Bb1T], writes=[Bg1[ab]])
                    S.op("act", lambda h, ab=ab: h.activation(out=sgm[ab][:], in_=g1[ab][:], func=AF.Sigmoid, scale=ALPHA), reads=[Bg1[ab]], writes=[Bsgm[ab]])
                    S.op("dve", lambda h, ab=ab, bank=bank, cl=cl: h.tensor_scalar(out=l2[ab][:], in0=psf[bank][:, BLK:2 * BLK], scalar1=b1T[:, cl:cl + 1], scalar2=LIMIT + 1.0, op0=ALU.add, op1=ALU.min),
                         reads=[Bpsf[bank], Bb1T], writes=[Bl2[ab]])
                    S.op("dve", lambda h, ab=ab: h.scalar_tensor_tensor(out=wv[ab][:], in0=l2[ab][:], scalar=1.0 - LIMIT, in1=g1[ab][:], op0=ALU.max, op1=ALU.mult),
                         reads=[Bl2[ab], Bg1[ab]], writes=[Bwv[ab]])
                    S.op("dve", lambda h, ab=ab, fc=fc, bs=bs: h.tensor_tensor(out=actT[:, fc, bs], in0=wv[ab][:], in1=sgm[ab][:], op=ALU.mult),
                         reads=[Bwv[ab], Bsgm[ab]], writes=[BactT])
        S.cur_guard = None
        if n + 1 < len(er_list):
            build_hselT(er_list[n + 1][0], er_list[n + 1][1], (n + 1) % 2)
        ydst = y_s[e // (NE // NYS)]
        for db in range(4):
            bi = pcount2 % NW2
            pcount2 += 1
            S.cur_guard = rnd_guard(e, r)
            S.dma("pool", ch_w2[bi], lambda h, e=e, db=db, bi=bi: h.dma_start(out=w2p[bi][:], in_=w2_v[e][:, :, db * 512:(db + 1) * 512]), writes=[Bw2p[bi]])
            for b in range(NBLK):
                S.cur_guard = blk_guard(e, r, b)
                for st2 in range(BLK // 128):
                    st = b * (BLK // 128) + st2
                    bank = 4 + (ycnt % 2)
                    yb = ycnt % 4
                    ycnt += 1
                    for fc in range(KC):
                        S.op("pe", lambda h, fc=fc, st=st, bi=bi, bank=bank: h.matmul(psf[bank][:, :], lhsT=actT[:, fc, st * 128:(st + 1) * 128], rhs=w2p[bi][:, fc, :],
                                                                                     start=(fc == 0), stop=(fc == KC - 1)),
                             reads=[BactT, Bw2p[bi]], writes=[Bpsf[bank]])
                    S.op("act", lambda h, bank=bank, yb=yb: h.copy(out=ysb[yb][:], in_=psf[bank][:, :]), reads=[Bpsf[bank]], writes=[Bysb[yb]])
                    row0 = (e % (NE // NYS)) * CAP + r * RS + st * 128
                    S.dma("sp", ch_y[yb], lambda h, row0=row0, db=db, yb=yb, ydst=ydst: h.dma_start(out=ydst[row0:row0 + 128, db * 512:(db + 1) * 512], in_=ysb[yb][:]),
                          reads=[Bysb[yb]], writes=[By_s])
        S.cur_guard = None
    S.barrier()
    AR.reset(m5)

    G2 = AR.alloc("G2", [128, D], F32)
    wt6 = AR.alloc("wt6", [128, D], F32)
    b2sb = AR.alloc("b2sb", [NE, D], F32)
    BG2, Bwt6, Bb2 = Buf("G2"), Buf("wt6"), Buf("b2sb")
    load_mod_bcast(G2, 0, 5, ch_v, BG2)
    load_row_bcast(wt6, pofw_d, ch_v, Bwt6)
    S.op("dve", lambda h: h.tensor_tensor(out=G2[:], in0=G2[:], in1=wt6[:], op=ALU.mult), reads=[BG2, Bwt6], writes=[BG2])
    S.dma("sp", ch_v, lambda h: h.dma_start(out=b2sb[:], in_=b2_d), writes=[Bb2])
    yk = [[AR.alloc(f"yk{b}_{k}", [128, D], F32) for k in range(4)] for b in range(2)]
    Byk = [[Buf(f"yk{b}_{k}") for k in range(4)] for b in range(2)]
    ch_g = [S.chan("gath0"), S.chan("gath1")]
    x1r = [AR.alloc(f"x1r{i}", [128, D], F32) for i in range(2)]
    Bx1r = [Buf("x1r0"), Buf("x1r1")]
    ch_x1r = [S.chan("x1r0"), S.chan("x1r1")]
    ff = AR.alloc("ff", [128, D], F32)
    ot = [AR.alloc(f"ot{i}", [128, D], F32) for i in range(2)]
    gT = AR.alloc("gT", [NE, 128], F32)
    junk6 = AR.alloc("junk6", [128, D], BF16)
    st6 = AR.alloc("st6", [128, 2], F32)
    Bff, BgT, Bjunk6, Bst6 = Buf("ff"), Buf("gT"), Buf("junk6"), Buf("st6")
    Bot = [Buf("ot0"), Buf("ot1")]
    ch_out = [S.chan("out0"), S.chan("out1")]
    for i in range(NT):
        bi = i % 2
        first = True
        for k in range(4):
            for j in range(NYS):
                S.dma("pool", ch_g[bi], lambda h, i=i, k=k, bi=bi, j=j: h.indirect_dma_start(out=yk[bi][k][:], out_offset=None, in_=y_s[j],
                                                                                          in_offset=bass.IndirectOffsetOnAxis(ap=idxY[j][:, i * 4 + k:i * 4 + k + 1], axis=0),
                                                                                          bounds_check=bound_reg(h, (NE // NYS) * CAP - 1), oob_is_err=False),
                      reads=[By_s, Bidx[i]], writes=[Byk[bi][k]], cont=(not first))
                first = False
        S.dma("sp", ch_x1r[bi], lambda h, i=i, bi=bi: h.dma_start(out=x1r[bi][:], in_=x1_s[i * 128:(i + 1) * 128, :]), reads=[Bx1_s[i]], writes=[Bx1r[bi]])
        S.op("pe", lambda h, i=i: h.transpose(out=psf[4][0:NE, 0:128], in_=gatesA[:, i, :], identity=ident[:]), reads=[BgatesA, Bc], writes=[Bpsf[4]])
        S.op("act", lambda h: h.copy(out=gT[:], in_=psf[4][0:NE, 0:128]), reads=[Bpsf[4]], writes=[BgT])
        for cb in range(4):
            S.op("pe", lambda h, cb=cb: h.matmul(psf[cb][:, :], lhsT=gT[:], rhs=b2sb[:, cb * 512:(cb + 1) * 512], start=True, stop=True), reads=[BgT, Bb2], writes=[Bpsf[cb]])
            S.op("dve", lambda h, cb=cb, bi=bi, i=i: h.scalar_tensor_tensor(out=ff[:, cb * 512:(cb + 1) * 512], in0=yk[bi][0][:, cb * 512:(cb + 1) * 512], scalar=gate4[:, i, 0:1],
                                                                          in1=psf[cb][:, :], op0=ALU.mult, op1=ALU.add), reads=[Byk[bi][0], Bgate4, Bpsf[cb]], writes=[Bff])
        for k in range(1, 4):
            S.op("dve", lambda h, k=k, bi=bi, i=i: h.scalar_tensor_tensor(out=ff[:], in0=yk[bi][k][:], scalar=gate4[:, i, k:k + 1], in1=ff[:], op0=ALU.mult, op1=ALU.add),
                 reads=[Byk[bi][k], Bgate4, Bff], writes=[Bff])
        S.op("act", lambda h: h.activation(out=junk6[:], in_=ff[:], func=AF.Square, accum_out=st6[:, 0:1]), reads=[Bff], writes=[Bjunk6, Bst6])
        S.op("act", lambda h: h.activation(out=st6[:, 0:1], in_=st6[:, 0:1], func=AF.Sqrt, scale=1.0 / D, bias=EPS), reads=[Bst6], writes=[Bst6])
        S.op("dve", lambda h: h.reciprocal(out=st6[:, 0:1], in_=st6[:, 0:1]), reads=[Bst6], writes=[Bst6])
        S.op("dve", lambda h: h.scalar_tensor_tensor(out=ff[:], in0=ff[:], scalar=st6[:, 0:1], in1=G2[:], op0=ALU.mult, op1=ALU.mult), reads=[Bff, Bst6, BG2], writes=[Bff])
        S.op("pool", lambda h, bi=bi: h.tensor_tensor(out=ot[bi][:], in0=ff[:], in1=x1r[bi][:], op=ALU.add), reads=[Bff, Bx1r[bi]], writes=[Bot[bi]])
        final_ops.append(S.dma("sp", ch_out[bi], lambda h, i=i, bi=bi: h.dma_start(out=out_d[i * 128:(i + 1) * 128, :], in_=ot[bi][:]), reads=[Bot[bi]]))
    return finish(nc, S, final_ops), dbg


def finish(nc, S, final_ops):
    S.wait_final("sp", final_ops)
    S.emit()
    return nc


def _rope_tables():
    half = 64
    inv = (10000.0 ** (-np.arange(half, dtype=np.float32) / np.float32(half))).astype(np.float32)
    pos = np.arange(NCTX + SEQ, dtype=np.float32)
    ang = (pos[:, None] * inv[None, :]).astype(np.float32)
    return np.cos(ang).astype(np.float32), np.sin(ang).astype(np.float32)


def make_in_maps(inp, ne_decl=NE):
    f = lambda a: np.ascontiguousarray(np.asarray(a, dtype=np.float32))
    cos, sin = _rope_tables()
    shared = {
        "c_ctx": f(inp["c_ctx"]).reshape(16, 128),
        "ada_w": f(inp["ada_w"][0]),
        "ada_b": f(inp["ada_b"][0]).reshape(1, 6 * D),
        "pre_mix_norm": f(inp["pre_mix_norm"][0]).reshape(1, D),
        "post_mix_norm": f(inp["post_mix_norm"][0]).reshape(1, D),
        "pre_ffn_norm": f(inp["pre_ffn_norm"][0]).reshape(1, D),
        "post_ffn_norm": f(inp["post_ffn_norm"][0]).reshape(1, D),
        "w_in": f(inp["w_in"][0]),
        "ret_decay": np.concatenate([f(inp["ret_decay_fwd"][0]), f(inp["ret_decay_bwd"][0])]).reshape(1, 16),
        "ret_gn_w": f(inp["ret_gn_w"][0]).reshape(1, DRET),
        "conv_w": f(inp["conv_w"][0]),
        "conv_vecs": np.concatenate([f(inp["conv_b"][0]).reshape(8, 128), f(inp["conv_ln_w"][0]).reshape(8, 128), f(inp["conv_ln_b"][0]).reshape(8, 128)], axis=0),
        "w_out": f(inp["w_out"][0]),
        "router_w": f(inp["router_w"][0]),
        "router_b": f(inp["router_b"][0]).reshape(1, NE),
        "w1": f(inp["w1"][0][:ne_decl]),
        "b1": f(inp["b1"][0]).reshape(NE * 32, 128),
        "w2": f(inp["w2"][0][:ne_decl]),
        "b2": f(inp["b2"][0]),
        "rope_cos": cos,
        "rope_sin": sin,
    }
    maps = []
    for b in range(NB):
        m = dict(shared)
        m["x"] = f(inp["x"][b])
        m["c"] = f(inp["c"][b]).reshape(16, 128)
        m["ctx"] = f(inp["ctx"][b])
        maps.append(m)
    return maps


_NC_CACHE = {}


def kernel(**inputs):
    if "nc" not in _NC_CACHE:
        _NC_CACHE["nc"] = build_program()[0]
    nc = _NC_CACHE["nc"]
    in_maps = make_in_maps(inputs)
    res = run_bass_kernel_spmd(nc, in_maps, core_ids=list(range(NB)))
    out = np.stack([np.asarray(res.results[b]["out"], dtype=np.float32) for b in range(NB)], axis=0)
    return out
```

```python
import numpy as np
import concourse.bass as bass
import concourse.mybir as mybir
from concourse.alu_op_type import AluOpType as ALU
from concourse.bass_utils import run_bass_kernel_spmd

F32 = mybir.dt.float32
BF16 = mybir.dt.bfloat16
I32 = mybir.dt.int32
AF = mybir.ActivationFunctionType
AX = mybir.AxisListType

D = 2048
SEQ = 2048
NB = 8
NCTX = 256
H = 8
HD = 128
DRET = 1024
DCONV = 1024
DIN = 6144
CW = 31
NE = 32
DFF = 2048
NT = SEQ // 128
KC = D // 128
EPS = 1e-6
GN_EPS = 1e-5
ALPHA = 1.702
LIMIT = 7.0
QSCALE = HD ** -0.5

ROUNDS = 4
RS = 512
CAP = ROUNDS * RS
BLK = 256
NBLK = RS // BLK
NHS = 1
NYS = 2
BIG = 4.0e6

ENGS = ("pe", "act", "dve", "pool", "sp")
EPOCH = 1 << 30


class Buf:
    __slots__ = ("name", "excl", "last_w", "readers")

    def __init__(self, name, excl=False):
        self.name = name
        self.excl = excl
        self.last_w = None
        self.readers = []


class Op:
    __slots__ = ("eng", "fn", "deps", "signal", "done", "is_dma", "chan", "chan_prev", "grp", "guard")

    def __init__(self, eng, fn):
        self.eng = eng
        self.fn = fn
        self.deps = []
        self.signal = False
        self.done = None
        self.is_dma = False
        self.chan = None
        self.chan_prev = None
        self.grp = None
        self.guard = None


class Chan:
    def __init__(self, name):
        self.name = name
        self.sem = None
        self.last_grp = None


class Sched:
    def __init__(self, nc):
        self.nc = nc
        self.ops = {e: [] for e in ENGS}
        self.chans = []
        self.final_waits = []
        self.nrec = 0
        self.cur_guard = None
        self.cnt_ap = None
        self.cnt_op = None

    def chan(self, name):
        c = Chan(name)
        self.chans.append(c)
        return c

    def _deps(self, op, reads, writes):
        for b in reads:
            if b.last_w is not None:
                op.deps.append((b.last_w, "RAW"))
            if b.excl:
                for r in b.readers:
                    op.deps.append((r, "RAR"))
        for b in writes:
            if b.last_w is not None:
                op.deps.append((b.last_w, "WAW"))
            for r in b.readers:
                op.deps.append((r, "WAR"))
        for b in reads:
            b.readers.append(op)
        for b in writes:
            b.last_w = op
            b.readers = []

    def op(self, eng, fn, reads=(), writes=()):
        o = Op(eng, fn)
        o.guard = self.cur_guard
        self._deps(o, list(reads), list(writes))
        self.ops[eng].append(o)
        self.nrec += 1
        return o

    def dma(self, eng, chan, fn, reads=(), writes=(), cont=False):
        o = Op(eng, fn)
        o.guard = self.cur_guard
        o.is_dma = True
        o.chan = chan
        if cont and chan.last_grp is not None:
            o.grp = chan.last_grp
            o.chan_prev = o.grp[0].chan_prev
        else:
            o.chan_prev = chan.last_grp
            o.grp = []
            chan.last_grp = o.grp
        o.grp.append(o)
        self._deps(o, list(reads), list(writes))
        self.ops[eng].append(o)
        self.nrec += 1
        return o

    def barrier(self):
        lasts = []
        for e in ENGS:
            for o in reversed(self.ops[e]):
                if not o.is_dma and o.fn is not None:
                    lasts.append(o)
                    break
        for c in self.chans:
            if c.last_grp:
                lasts.append(c.last_grp[0])
        for e in ENGS:
            o = Op(e, None)
            for d in lasts:
                o.deps.append((d, "RAW"))
            self.ops[e].append(o)

    def wait_final(self, eng, ops):
        self.final_waits.append((eng, list(ops)))

    def emit(self):
        nc = self.nc
        for e in ENGS:
            for o in self.ops[e]:
                for (d, kind) in o.deps:
                    if d.is_dma:
                        continue
                    if d.eng != o.eng or kind == "RAW":
                        d.signal = True
        for (e, ops) in self.final_waits:
            for d in ops:
                if not d.is_dma:
                    d.signal = True
        if self.cnt_op is not None:
            self.cnt_op.signal = True
        sems = []
        for e in ENGS:
            cnt = 0
            sem = None
            for o in self.ops[e]:
                if o.is_dma or o.fn is None:
                    continue
                if o.signal:
                    if sem is None or cnt >= EPOCH:
                        sem = nc.alloc_semaphore(f"s_{e}_{len(sems)}")
                        sems.append(sem)
                        cnt = 0
                    cnt += 1
                    o.done = (sem, cnt)
        for c in self.chans:
            if c.last_grp is None:
                continue
            c.sem = nc.alloc_semaphore(f"c_{c.name}")
            chain = []
            g = c.last_grp
            while g is not None:
                chain.append(g)
                g = g[0].chan_prev
            chain.reverse()
            v = 0
            for g in chain:
                v += 16 * len(g)
                for o in g:
                    o.done = (c.sem, v)
        lists = self.ops
        finals = self.final_waits

        cnt_ap = self.cnt_ap
        cnt_op = self.cnt_op

        def run(e, h):
            seen = {}
            state = {"greg": None, "loaded": None, "last_sig": None}

            def need(sem, val):
                k = id(sem)
                if seen.get(k, 0) < val:
                    h.wait_ge(sem, val)
                    seen[k] = val

            def emit_op(o):
                if o.is_dma and o.chan_prev is not None and o is o.grp[0]:
                    need(*o.chan_prev[0].done)
                for (d, kind) in o.deps:
                    if d.is_dma:
                        if d.grp is o.grp:
                            continue
                        need(*d.done)
                    elif d.eng != e or kind == "RAW":
                        need(*d.done)
                if o.fn is None:
                    return
                ins = o.fn(h)
                if o.is_dma:
                    ins.then_inc(o.chan.sem, 16)
                elif o.signal:
                    ins.then_inc(o.done[0], 1)
                    state["last_sig"] = o.done

            ops = lists[e]
            i = 0
            n = len(ops)
            while i < n:
                o = ops[i]
                if o.guard is None:
                    emit_op(o)
                    i += 1
                    continue
                j = i
                while j < n and ops[j].guard == o.guard:
                    j += 1
                grp = ops[i:j]
                (ge, thr) = o.guard
                if state["greg"] is None:
                    state["greg"] = h.alloc_register(f"greg_{e}")
                if state["loaded"] != ge:
                    need(*cnt_op.done)
                    h.reg_load(state["greg"], cnt_ap(ge))
                    state["loaded"] = ge
                saved = dict(seen)
                pre_sig = state["last_sig"]
                with h.If_cmp(state["greg"], thr, "IS_GT"):
                    for g in grp:
                        emit_op(g)
                nsig = 0
                sig_sem = None
                ndma = 0
                for g in grp:
                    if g.fn is None:
                        continue
                    if g.is_dma:
                        ndma += 1
                    elif g.signal:
                        nsig += 1
                        sig_sem = g.done[0]
                        last_in = g.done
                if nsig or ndma:
                    with h.Else():
                        if nsig:
                            if pre_sig is not None:
                                h.wait_ge(*pre_sig)
                            h.sem_inc(sig_sem, nsig)
                        for g in grp:
                            if g.fn is not None and g.is_dma:
                                if g is g.grp[0] and g.chan_prev is not None:
                                    h.wait_ge(*g.chan_prev[0].done)
                                h.sem_inc(g.chan.sem, 16)
                if nsig:
                    state["last_sig"] = last_in
                seen.clear()
                seen.update(saved)
                i = j
            for (fe, fops) in finals:
                if fe == e:
                    for d in fops:
                        need(*d.done)

        with nc.Block() as block:
            @block.tensor
            def _(h):
                run("pe", h)

            @block.scalar
            def _(h):
                run("act", h)

            @block.vector
            def _(h):
                run("dve", h)

            @block.gpsimd
            def _(h):
                run("pool", h)

            @block.sync
            def _(h):
                run("sp", h)


class Arena:
    def __init__(self, nc, nbytes):
        self.nc = nc
        left = nc._sbuf_addr_for_side("left")
        self.base = (left + 63) // 64 * 64
        nbytes = nbytes // 64 * 64
        self.slab = nc.alloc_sbuf_tensor("arena", [128, nbytes // 4], F32)
        self.size = nbytes - 64
        self.top = 0
        self.n = 0

    def alloc(self, name, shape, dtype):
        esz = 2 if dtype == BF16 else 4
        nb = esz
        for s in shape[1:]:
            nb *= s
        nb = (nb + 63) // 64 * 64
        off = self.top
        assert off + nb <= self.size, (name, off, nb, self.size)
        self.top += nb
        self.n += 1
        return self.nc.alloc_sbuf_tensor_at(f"{name}_{self.n}", list(shape), dtype, offset=self.base + off)

    def alloc_at(self, name, shape, dtype, off):
        self.n += 1
        return self.nc.alloc_sbuf_tensor_at(f"{name}_{self.n}", list(shape), dtype, offset=self.base + off)

    def mark(self):
        return self.top

    def reset(self, m):
        self.top = m


def build_program(stage=99, debug=False, ne_decl=NE):
    nc = bass.Bass("TRN2", target_bir_lowering=False)
    S = Sched(nc)

    def din(name, shape, dt=F32):
        return nc.dram_tensor(name, list(shape), dt, kind="ExternalInput").ap()

    x_d = din("x", [SEQ, D])
    c_d = din("c", [16, 128])
    ctx_d = din("ctx", [NCTX, D])
    cctx_d = din("c_ctx", [16, 128])
    adaw_d = din("ada_w", [D, 6 * D])
    adab_d = din("ada_b", [1, 6 * D])
    pmw_d = din("pre_mix_norm", [1, D])
    pomw_d = din("post_mix_norm", [1, D])
    pfw_d = din("pre_ffn_norm", [1, D])
    pofw_d = din("post_ffn_norm", [1, D])
    win_d = din("w_in", [D, DIN])
    dec_d = din("ret_decay", [1, 16])
    gnw_d = din("ret_gn_w", [1, DRET])
    convw_d = din("conv_w", [CW, DCONV])
    cvec_d = din("conv_vecs", [24, 128])
    wout_d = din("w_out", [D, D])
    rw_d = din("router_w", [D, NE])
    rb_d = din("router_b", [1, NE])
    w1_d = din("w1", [ne_decl, D, 2 * DFF])
    b1_d = din("b1", [NE * 32, 128])
    w2_d = din("w2", [ne_decl, DFF, D])
    b2_d = din("b2", [NE, D])
    cos_d = din("rope_cos", [NCTX + SEQ, 64])
    sin_d = din("rope_sin", [NCTX + SEQ, 64])
    out_d = nc.dram_tensor("out", [SEQ, D], F32, kind="ExternalOutput").ap()
    dbg = {}

    def dbg_out(name, shape, dt=F32):
        t = nc.dram_tensor("dbg_" + name, list(shape), dt, kind="ExternalOutput").ap()
        dbg[name] = t
        return t

    mod_s = nc.dram_tensor("mod_s", [2, 6 * D], F32).ap()
    yT_s = nc.dram_tensor("yT_s", [D, SEQ], BF16).ap()
    x1_s = nc.dram_tensor("x1_s", [SEQ, D], F32).ap()
    hsel_s = [nc.dram_tensor(f"hsel_s{j}", [(NE // NHS) * CAP, D], BF16).ap() for j in range(NHS)]
    y_s = [nc.dram_tensor(f"y_s{j}", [(NE // NYS) * CAP, D], F32).ap() for j in range(NYS)]

    AR = Arena(nc, nc.sbuf_bytes_remaining - 6144)

    psf = [nc.alloc_psum_tensor(f"psf{i}", [128, 512], F32) for i in range(6)]
    psb = [nc.alloc_psum_tensor(f"psb{i}", [128, 1024], BF16) for i in range(2)]
    Bpsf = [Buf(f"psf{i}", excl=True) for i in range(6)]
    Bpsb = [Buf(f"psb{i}", excl=True) for i in range(2)]
    final_ops = []
    _regs = {}

    def bound_reg(h, val):
        if val not in _regs:
            _regs[val] = h.to_reg(val)
        return _regs[val]

    ident = AR.alloc("ident", [128, 128], F32)
    identb = AR.alloc("identb", [128, 128], BF16)
    onesb = AR.alloc("onesb", [128, 128], BF16)
    ones32 = AR.alloc("ones32", [128, 128], F32)
    trib = AR.alloc("trib", [128, 128], BF16)
    iorow = AR.alloc("iorow", [128, 128], F32)
    iocol = AR.alloc("iocol", [128, 1], F32)
    b1T = AR.alloc("b1T", [128, NE * 32], F32)
    gate4 = AR.alloc("gate4", [128, NT, 4], F32)
    idxH = [AR.alloc(f"idxH{j}", [128, NT * 4], I32) for j in range(NHS)]
    idxY = [AR.alloc(f"idxY{j}", [128, NT * 4], I32) for j in range(NYS)]
    cnt_i = AR.alloc("cnt_i", [128, NE], I32)
    Bcnt = Buf("cnt_i")
    gatesA = AR.alloc("gatesA", [128, NT, NE], F32)
    Bc = Buf("consts")
    Bb1T = Buf("b1T")
    Bgate4 = Buf("gate4")
    Bidx = [Buf(f"idx{i}") for i in range(NT)]
    BgatesA = Buf("gatesA")

    S.op("pool", lambda h: h.memset(ident[:], 0.0), writes=[Bc])
    S.op("pool", lambda h: h.affine_select(out=ident[:], in_=ident[:], pattern=[[-1, 128]], compare_op=ALU.not_equal,
                                            fill=1.0, base=0, channel_multiplier=1), reads=[Bc], writes=[Bc])
    S.op("pool", lambda h: h.memset(ones32[:], 1.0), writes=[Bc])
    S.op("pool", lambda h: h.affine_select(out=iorow[:], in_=ones32[:], pattern=[[1, 128]], compare_op=ALU.is_gt,
                                            fill=0.0, base=0, channel_multiplier=-1), reads=[Bc], writes=[Bc])
    S.op("dve", lambda h: h.tensor_copy(out=trib[:], in_=iorow[:]), reads=[Bc], writes=[Bc])
    S.op("dve", lambda h: h.tensor_copy(out=identb[:], in_=ident[:]), reads=[Bc], writes=[Bc])
    S.op("dve", lambda h: h.tensor_copy(out=onesb[:], in_=ones32[:]), reads=[Bc], writes=[Bc])
    ioi = AR.alloc("ioi", [128, 128], I32)
    S.op("pool", lambda h: h.iota(ioi[:], pattern=[[1, 128]], base=0, channel_multiplier=0), writes=[Bc])
    S.op("dve", lambda h: h.tensor_copy(out=iorow[:], in_=ioi[:]), reads=[Bc], writes=[Bc])
    S.op("pool", lambda h: h.iota(ioi[:, 0:1], pattern=[[0, 1]], base=0, channel_multiplier=1), reads=[Bc], writes=[Bc])
    S.op("dve", lambda h: h.tensor_copy(out=iocol[:], in_=ioi[:, 0:1]), reads=[Bc], writes=[Bc])

    ch_small = S.chan("small")
    persist_mark = AR.mark()

    def transpose_rows(rows_ap, nrows, out_ap, bank, Bbank, reads, writes, evac="act"):
        S.op("pe", lambda h: h.transpose(out=psf[bank][:, 0:nrows], in_=rows_ap, identity=ident[0:nrows, 0:nrows]),
             reads=reads + [Bc], writes=[Bbank])
        if evac == "act":
            S.op("act", lambda h: h.copy(out=out_ap, in_=psf[bank][:, 0:nrows]), reads=[Bbank], writes=writes)
        else:
            S.op("dve", lambda h: h.tensor_copy(out=out_ap, in_=psf[bank][:, 0:nrows]), reads=[Bbank], writes=writes)

    m0 = AR.mark()
    rows32 = AR.alloc("rows32", [32, 128], F32)
    cT = AR.alloc("cT", [128, 32], F32)
    sil = AR.alloc("sil", [128, 32], F32)
    adab = AR.alloc("adab", [2, 6 * D], F32)
    modsb = AR.alloc("modsb", [2, 6 * D], F32)
    adaw = [AR.alloc(f"adaw{i}", [128, KC, 512], F32) for i in range(2)]
    b1rows = AR.alloc("b1rows", [128, 8, 128], F32)
    Brows32, BcT, Bsil, Badab, Bmodsb, Bb1rows = Buf("rows32"), Buf("cT"), Buf("sil"), Buf("adab"), Buf("modsb"), Buf("b1rows")
    Badaw = [Buf("adaw0"), Buf("adaw1")]
    ch_adaw = [S.chan("adaw0"), S.chan("adaw1")]

    S.dma("sp", ch_small, lambda h: h.dma_start(out=rows32[0:16, :], in_=c_d), writes=[Brows32])
    S.dma("sp", ch_small, lambda h: h.dma_start(out=rows32[16:32, :], in_=cctx_d), writes=[Brows32], cont=True)
    S.dma("sp", ch_small, lambda h: h.dma_start(out=adab[:], in_=adab_d.partition_broadcast(2)), writes=[Badab], cont=True)
    S.dma("sp", ch_small, lambda h: h.dma_start(out=b1rows[:], in_=b1_d.rearrange("(t p) f -> p t f", p=128)), writes=[Bb1rows], cont=True)
    transpose_rows(rows32[:], 32, cT[:], 0, Bpsf[0], [Brows32], [BcT])
    S.op("act", lambda h: h.activation(out=sil[:], in_=cT[:], func=AF.Silu), reads=[BcT], writes=[Bsil])
    for t in range(8):
        S.op("pe", lambda h, t=t: h.transpose(out=psf[1 + (t % 2)][:, 0:128], in_=b1rows[:, t, :], identity=ident[:]),
             reads=[Bb1rows, Bc], writes=[Bpsf[1 + (t % 2)]])
        S.op("act", lambda h, t=t: h.copy(out=b1T[:, t * 128:(t + 1) * 128], in_=psf[1 + (t % 2)][:, 0:128]),
             reads=[Bpsf[1 + (t % 2)]], writes=[Bb1T])
    b1T3 = b1T[:, :].rearrange("p (e f) -> p e f", e=NE)
    S.op("dve", lambda h: h.tensor_scalar(out=b1T3[:, :, 16:32], in0=b1T3[:, :, 16:32], scalar1=1.0, scalar2=None, op0=ALU.add), reads=[Bb1T], writes=[Bb1T])
    sil2 = AR.alloc("sil2", [128, KC, 2], F32)
    Bsil2 = Buf("sil2")
    S.op("dve", lambda h: h.tensor_copy(out=sil2[:, :, 0], in_=sil[:, 0:16]), reads=[Bsil], writes=[Bsil2])
    S.op("dve", lambda h: h.tensor_copy(out=sil2[:, :, 1], in_=sil[:, 16:32]), reads=[Bsil], writes=[Bsil2])
    adaw_v = adaw_d.rearrange("(k p) n -> p k n", p=128)
    for j in range(24):
        bi = j % 2
        S.dma("sp", ch_adaw[bi], lambda h, j=j, bi=bi: h.dma_start(out=adaw[bi][:], in_=adaw_v[:, :, j * 512:(j + 1) * 512]),
              writes=[Badaw[bi]])
        bank = 2 + (j % 2)
        for kc in range(KC):
            S.op("pe", lambda h, kc=kc, bi=bi, bank=bank: h.matmul(psf[bank][0:2, :], lhsT=sil2[:, kc, :], rhs=adaw[bi][:, kc, :],
                                                                     start=(kc == 0), stop=(kc == KC - 1)),
                 reads=[Bsil2, Badaw[bi]], writes=[Bpsf[bank]])
        S.op("dve", lambda h, j=j, bank=bank: h.tensor_tensor(out=modsb[:, j * 512:(j + 1) * 512], in0=psf[bank][0:2, :],
                                                               in1=adab[:, j * 512:(j + 1) * 512], op=ALU.add),
             reads=[Bpsf[bank], Badab], writes=[Bmodsb])
    Bmod_s = Buf("mod_s")
    S.dma("sp", ch_small, lambda h: h.dma_start(out=mod_s, in_=modsb[:]), reads=[Bmodsb], writes=[Bmod_s])
    if debug:
        d_mod = dbg_out("mod", [2, 6 * D])
        final_ops.append(S.dma("sp", S.chan("dbgmod"), lambda h: h.dma_start(out=d_mod, in_=modsb[:]), reads=[Bmodsb]))
    S.barrier()
    AR.reset(m0)
    if stage <= 0:
        return finish(nc, S, final_ops), dbg

    def load_mod_bcast(dst, row, g, chan, Bdst):
        return S.dma("sp", chan, lambda h: h.dma_start(out=dst[:], in_=mod_s[row:row + 1, g * D:(g + 1) * D].partition_broadcast(128)),
                     reads=[Bmod_s], writes=[Bdst])

    def load_row_bcast(dst, row_ap, chan, Bdst, cont=False):
        return S.dma("sp", chan, lambda h: h.dma_start(out=dst[:], in_=row_ap.partition_broadcast(128)), writes=[Bdst], cont=cont)

    hT = AR.alloc("hT", [128, KC, SEQ], BF16)
    hcT_off = AR.mark()
    hcT = AR.alloc("hcT", [128, KC, NCTX], BF16)
    BhT = [Buf(f"hT{i}") for i in range(NT)]
    BhcT = Buf("hcT")
    m1 = AR.mark()
    A1 = AR.alloc("A1", [128, D], F32)
    S1 = AR.alloc("S1", [128, D], F32)
    A1c = AR.alloc("A1c", [128, D], F32)
    S1c = AR.alloc("S1c", [128, D], F32)
    wtmp = AR.alloc("wtmp", [128, D], F32)
    xb = [AR.alloc(f"xb{i}", [128, D], F32) for i in range(2)]
    t32s = [AR.alloc(f"t32_{i}", [128, D], F32) for i in range(2)]
    Bt32s = [Buf("t32_0"), Buf("t32_1")]
    hb = [AR.alloc(f"hb{i}", [128, D], BF16) for i in range(2)]
    junkb = AR.alloc("junkb", [128, D], BF16)
    stat = AR.alloc("stat", [128, 8], F32)
    BA1, BS1, BA1c, BS1c, Bwtmp, Bjunk, Bstat = (Buf(n) for n in ["A1", "S1", "A1c", "S1c", "wtmp", "junk", "stat"])
    Bxb = [Buf("xb0"), Buf("xb1")]
    Bhb = [Buf("hb0"), Buf("hb1")]
    ch_x = [S.chan("x0"), S.chan("x1")]
    ch_v = S.chan("vecs")

    load_row_bcast(wtmp, pmw_d, ch_v, Bwtmp)
    load_mod_bcast(A1, 0, 1, ch_v, BA1)
    load_mod_bcast(S1, 0, 0, ch_v, BS1)
    load_mod_bcast(A1c, 1, 1, ch_v, BA1c)
    load_mod_bcast(S1c, 1, 0, ch_v, BS1c)
    S.op("dve", lambda h: h.scalar_tensor_tensor(out=A1[:], in0=A1[:], scalar=1.0, in1=wtmp[:], op0=ALU.add, op1=ALU.mult),
         reads=[BA1, Bwtmp], writes=[BA1])
    S.op("dve", lambda h: h.scalar_tensor_tensor(out=A1c[:], in0=A1c[:], scalar=1.0, in1=wtmp[:], op0=ALU.add, op1=ALU.mult),
         reads=[BA1c, Bwtmp], writes=[BA1c])

    def norm_mod_tile(src_ap_dram, xbuf, Bx, chan, Avec, BAv, Svec, BSv, hbuf, Bh, sidx):
        t32 = t32s[sidx % 2]
        Bt32 = Bt32s[sidx % 2]
        S.dma("sp", chan, lambda h: h.dma_start(out=xbuf[:], in_=src_ap_dram), writes=[Bx])
        S.op("act", lambda h: h.activation(out=junkb[:], in_=xbuf[:], func=AF.Square, accum_out=stat[:, sidx:sidx + 1]),
             reads=[Bx], writes=[Bjunk, Bstat])
        S.op("act", lambda h: h.activation(out=stat[:, sidx:sidx + 1], in_=stat[:, sidx:sidx + 1], func=AF.Sqrt, scale=1.0 / D, bias=EPS),
             reads=[Bstat], writes=[Bstat])
        S.op("dve", lambda h: h.reciprocal(out=stat[:, sidx:sidx + 1], in_=stat[:, sidx:sidx + 1]), reads=[Bstat], writes=[Bstat])
        S.op("dve", lambda h: h.scalar_tensor_tensor(out=t32[:], in0=xbuf[:], scalar=stat[:, sidx:sidx + 1], in1=Avec[:],
                                                     op0=ALU.mult, op1=ALU.mult), reads=[Bx, Bstat, BAv], writes=[Bt32])
        S.op("pool", lambda h: h.tensor_tensor(out=hbuf[:], in0=t32[:], in1=Svec[:], op=ALU.add), reads=[Bt32, BSv], writes=[Bh])

    def transpose_tile_bf16(hbuf, Bh, dstT, col0, Bdst):
        for half in range(2):
            pb = half
            for q in range(8):
                kc = half * 8 + q
                S.op("pe", lambda h, kc=kc, q=q, pb=pb: h.transpose(out=psb[pb][:, q * 128:(q + 1) * 128], in_=hbuf[:, kc * 128:(kc + 1) * 128],
                                                                       identity=identb[:]),
                     reads=[Bh, Bc], writes=[Bpsb[pb]])
            eng = "act" if half == 0 else "dve"
            if eng == "act":
                S.op("act", lambda h, half=half, pb=pb: h.copy(out=dstT[:, half * 8:(half + 1) * 8, col0:col0 + 128],
                                                                 in_=psb[pb][:, :].rearrange("p (q t) -> p q t", q=8)),
                     reads=[Bpsb[pb]], writes=[Bdst])
            else:
                S.op("dve", lambda h, half=half, pb=pb: h.tensor_copy(out=dstT[:, half * 8:(half + 1) * 8, col0:col0 + 128],
                                                                        in_=psb[pb][:, :].rearrange("p (q t) -> p q t", q=8)),
                     reads=[Bpsb[pb]], writes=[Bdst])

    for i in range(2):
        bi = i % 2
        norm_mod_tile(ctx_d[i * 128:(i + 1) * 128, :], xb[bi], Bxb[bi], ch_x[bi], A1c, BA1c, S1c, BS1c, hb[bi], Bhb[bi], i % 8)
        transpose_tile_bf16(hb[bi], Bhb[bi], hcT, i * 128, BhcT)
    for i in range(NT):
        bi = i % 2
        norm_mod_tile(x_d[i * 128:(i + 1) * 128, :], xb[bi], Bxb[bi], ch_x[bi], A1, BA1, S1, BS1, hb[bi], Bhb[bi], i % 8)
        transpose_tile_bf16(hb[bi], Bhb[bi], hT, i * 128, BhT[i])
    if debug:
        d_hT = dbg_out("hT", [128, KC, SEQ], BF16)
        final_ops.append(S.dma("sp", S.chan("dbghT"), lambda h: h.dma_start(out=d_hT, in_=hT[:]), reads=BhT))
    S.barrier()
    AR.reset(m1)
    if stage <= 1:
        return finish(nc, S, final_ops), dbg

    ByT_s = Buf("yT_s")
    win_v = win_d.rearrange("(k p) n -> p k n", p=128)

    m2 = AR.mark()
    decb = AR.alloc("decb", [128, 16], F32)
    Mh = AR.alloc("Mh", [128, H, 128], F32)
    dcol = AR.alloc("dcol", [128, H, 6], F32)
    wctx = AR.alloc("wctx", [128, H, 4], F32)
    Bdecb, BMh, Bdcol, Bwctx = Buf("decb"), Buf("Mh"), Buf("dcol"), Buf("wctx")
    tA = AR.alloc("tA", [128, 128], F32)
    tB = AR.alloc("tB", [128, 128], F32)
    tC = AR.alloc("tC", [128, 128], F32)
    tD = AR.alloc("tD", [128, 128], F32)
    cols = AR.alloc("cols", [128, 8], F32)
    BtA, BtB, BtC, BtD, Bcols = Buf("tA"), Buf("tB"), Buf("tC"), Buf("tD"), Buf("cols")
    S.dma("sp", ch_v, lambda h: h.dma_start(out=decb[:], in_=dec_d.partition_broadcast(128)), writes=[Bdecb])
    S.op("act", lambda h: h.activation(out=decb[:], in_=decb[:], func=AF.Exp, scale=-1.0), reads=[Bdecb], writes=[Bdecb])
    S.op("act", lambda h: h.activation(out=decb[:], in_=decb[:], func=AF.Ln, bias=1.0), reads=[Bdecb], writes=[Bdecb])
    S.op("dve", lambda h: h.tensor_scalar(out=decb[:], in0=decb[:], scalar1=-1.0, scalar2=None, op0=ALU.mult), reads=[Bdecb], writes=[Bdecb])
    S.op("dve", lambda h: h.tensor_scalar(out=tA[:], in0=iorow[:], scalar1=iocol[:, 0:1], scalar2=0.0, op0=ALU.subtract, op1=ALU.max),
         reads=[Bc], writes=[BtA])
    S.op("dve", lambda h: h.tensor_scalar(out=tB[:], in0=iorow[:], scalar1=iocol[:, 0:1], scalar2=-1.0, op0=ALU.subtract, op1=ALU.mult),
         reads=[Bc], writes=[BtB])
    S.op("dve", lambda h: h.tensor_scalar(out=tB[:], in0=tB[:], scalar1=0.0, scalar2=None, op0=ALU.max), reads=[BtB], writes=[BtB])
    S.op("dve", lambda h: h.tensor_scalar(out=tC[:], in0=iorow[:], scalar1=iocol[:, 0:1], scalar2=None, op0=ALU.is_ge), reads=[Bc], writes=[BtC])
    S.op("dve", lambda h: h.tensor_scalar(out=tD[:], in0=iorow[:], scalar1=iocol[:, 0:1], scalar2=None, op0=ALU.is_le), reads=[Bc], writes=[BtD])
    for ci, (mul, add) in enumerate([(1.0, 1.0), (-1.0, 128.0), (-1.0, 127.0), (1.0, 0.0), (0.0, 128.0), (-1.0, 255.0), (-1.0, 127.0), (1.0, 128.0)]):
        S.op("dve", lambda h, ci=ci, mul=mul, add=add: h.tensor_scalar(out=cols[:, ci:ci + 1], in0=iocol[:, 0:1], scalar1=mul, scalar2=add,
                                                                      op0=ALU.mult, op1=ALU.add), reads=[Bc], writes=[Bcols])
    ex1 = AR.alloc("ex1", [128, 128], F32)
    ex2 = AR.alloc("ex2", [128, 128], F32)
    Bex1, Bex2 = Buf("ex1"), Buf("ex2")
    for hh in range(H):
        lf = decb[:, hh:hh + 1]
        lb = decb[:, 8 + hh:9 + hh]
        S.op("act", lambda h, lf=lf: h.activation(out=ex1[:], in_=tA[:], func=AF.Exp, scale=lf), reads=[BtA, Bdecb], writes=[Bex1])
        S.op("act", lambda h, lb=lb: h.activation(out=ex2[:], in_=tB[:], func=AF.Exp, scale=lb), reads=[BtB, Bdecb], writes=[Bex2])
        S.op("dve", lambda h: h.tensor_tensor(out=ex1[:], in0=ex1[:], in1=tC[:], op=ALU.mult), reads=[Bex1, BtC], writes=[Bex1])
        S.op("dve", lambda h: h.tensor_tensor(out=ex2[:], in0=ex2[:], in1=tD[:], op=ALU.mult), reads=[Bex2, BtD], writes=[Bex2])
        S.op("dve", lambda h, hh=hh: h.tensor_tensor(out=Mh[:, hh, :], in0=ex1[:], in1=ex2[:], op=ALU.add), reads=[Bex1, Bex2], writes=[BMh])
        for (dst, ci, lg) in [(0, 0, lf), (1, 1, lb), (2, 2, lf), (3, 3, lb), (4, 4, lf), (5, 4, lb)]:
            S.op("act", lambda h, hh=hh, dst=dst, ci=ci, lg=lg: h.activation(out=dcol[:, hh, dst:dst + 1], in_=cols[:, ci:ci + 1], func=AF.Exp, scale=lg),
                 reads=[Bcols, Bdecb], writes=[Bdcol])
        for (dst, ci, lg) in [(0, 5, lf), (1, 6, lf), (2, 3, lb), (3, 7, lb)]:
            S.op("act", lambda h, hh=hh, dst=dst, ci=ci, lg=lg: h.activation(out=wctx[:, hh, dst:dst + 1], in_=cols[:, ci:ci + 1], func=AF.Exp, scale=lg),
                 reads=[Bcols, Bdecb], writes=[Bwctx])

    cosL = AR.alloc("cosL", [128, NT, 64], F32)
    sinL = AR.alloc("sinL", [128, NT, 64], F32)
    cosT = cosL[:, :, :].unsqueeze(2).to_broadcast([128, NT, 2, 64])
    sinT = sinL[:, :, :].unsqueeze(2).to_broadcast([128, NT, 2, 64])
    cosC = AR.alloc("cosC", [128, 2, 64], F32)
    sinC = AR.alloc("sinC", [128, 2, 64], F32)
    Brope = Buf("rope")
    cos_lat = cos_d[NCTX:NCTX + SEQ, :].rearrange("(t p) f -> p t f", p=128)
    sin_lat = sin_d[NCTX:NCTX + SEQ, :].rearrange("(t p) f -> p t f", p=128)
    S.dma("sp", ch_v, lambda h: h.dma_start(out=cosL[:], in_=cos_lat), writes=[Brope])
    S.dma("sp", ch_v, lambda h: h.dma_start(out=sinL[:], in_=sin_lat), writes=[Brope], cont=True)
    S.dma("sp", ch_v, lambda h: h.dma_start(out=cosC[:], in_=cos_d[0:NCTX, :].rearrange("(t p) f -> p t f", p=128)), writes=[Brope], cont=True)
    S.dma("sp", ch_v, lambda h: h.dma_start(out=sinC[:], in_=sin_d[0:NCTX, :].rearrange("(t p) f -> p t f", p=128)), writes=[Brope], cont=True)
    gnwb = AR.alloc("gnwb", [128, DRET], F32)
    Bgnwb = Buf("gnwb")
    load_row_bcast(gnwb, gnw_d, ch_v, Bgnwb)

    R0 = AR.alloc("R0", [128, H, 2, 128], F32)
    BR0 = Buf("R0")
    m2b = AR.mark()
    wkv = [AR.alloc(f"wkv{i}", [128, KC, 256], BF16) for i in range(2)]
    Bwkv = [Buf("wkv0"), Buf("wkv1")]
    ch_wkv = [S.chan("wkv0"), S.chan("wkv1")]
    kc32 = AR.alloc("kc32", [128, 2, 128], F32)
    kcr = AR.alloc("kcr", [128, 2, 128], BF16)
    vwf = AR.alloc("vwf", [128, 2, 128], BF16)
    vwb = AR.alloc("vwb", [128, 2, 128], BF16)
    rt1 = AR.alloc("rt1", [128, 2, 64], F32)
    rt2 = AR.alloc("rt2", [128, 2, 64], F32)
    Bkc32, Bkcr, Bvwf, Bvwb, Brt1, Brt2 = (Buf(n) for n in ["kc32", "kcr", "vwf", "vwb", "rt1", "rt2"])
    for hh in range(H):
        bi = hh % 2
        S.dma("pool", ch_wkv[bi], lambda h, hh=hh, bi=bi: h.dma_start(out=wkv[bi][:, :, 0:128], in_=win_v[:, :, DRET + hh * 128:DRET + (hh + 1) * 128]),
              writes=[Bwkv[bi]])
        S.dma("pool", ch_wkv[bi], lambda h, hh=hh, bi=bi: h.dma_start(out=wkv[bi][:, :, 128:256], in_=win_v[:, :, 2 * DRET + hh * 128:2 * DRET + (hh + 1) * 128]),
              writes=[Bwkv[bi]], cont=True)
        for t in range(2):
            bank = t
            for kc in range(KC):
                S.op("pe", lambda h, kc=kc, t=t, bi=bi, bank=bank: h.matmul(psf[bank][:, 0:256], lhsT=hcT[:, kc, t * 128:(t + 1) * 128], rhs=wkv[bi][:, kc, :],
                                                                             start=(kc == 0), stop=(kc == KC - 1)),
                     reads=[BhcT, Bwkv[bi]], writes=[Bpsf[bank]])
            S.op("act", lambda h, t=t, bank=bank: h.copy(out=kc32[:, t, :], in_=psf[bank][:, 0:128]), reads=[Bpsf[bank]], writes=[Bkc32])
            S.op("dve", lambda h, t=t, bank=bank, hh=hh: h.tensor_scalar(out=vwf[:, t, :], in0=psf[bank][:, 128:256], scalar1=wctx[:, hh, t:t + 1], scalar2=None, op0=ALU.mult),
                 reads=[Bpsf[bank], Bwctx], writes=[Bvwf])
            S.op("dve", lambda h, t=t, bank=bank, hh=hh: h.tensor_scalar(out=vwb[:, t, :], in0=psf[bank][:, 128:256], scalar1=wctx[:, hh, 2 + t:3 + t], scalar2=None, op0=ALU.mult),
                 reads=[Bpsf[bank], Bwctx], writes=[Bvwb])
        k1 = kc32[:, :, 0:64]
        k2 = kc32[:, :, 64:128]
        S.op("dve", lambda h: h.tensor_tensor(out=rt1[:], in0=k1, in1=cosC[:], op=ALU.mult), reads=[Bkc32, Brope], writes=[Brt1])
        S.op("pool", lambda h: h.tensor_tensor(out=rt2[:], in0=k2, in1=sinC[:], op=ALU.mult), reads=[Bkc32, Brope], writes=[Brt2])
        S.op("dve", lambda h: h.tensor_tensor(out=kcr[:, :, 0:64], in0=rt1[:], in1=rt2[:], op=ALU.subtract), reads=[Brt1, Brt2], writes=[Bkcr])
        S.op("dve", lambda h: h.tensor_tensor(out=rt1[:], in0=k1, in1=sinC[:], op=ALU.mult), reads=[Bkc32, Brope, Bkcr], writes=[Brt1])
        S.op("pool", lambda h: h.tensor_tensor(out=rt2[:], in0=k2, in1=cosC[:], op=ALU.mult), reads=[Bkc32, Brope, Bkcr], writes=[Brt2])
        S.op("dve", lambda h: h.tensor_tensor(out=kcr[:, :, 64:128], in0=rt1[:], in1=rt2[:], op=ALU.add), reads=[Brt1, Brt2], writes=[Bkcr])
        for di, vw, Bvw in [(0, vwf, Bvwf), (1, vwb, Bvwb)]:
            bank = 2 + di
            for t in range(2):
                S.op("pe", lambda h, t=t, bank=bank, vw=vw: h.matmul(psf[bank][:, 0:128], lhsT=kcr[:, t, :], rhs=vw[:, t, :], start=(t == 0), stop=(t == 1)),
                     reads=[Bkcr, Bvw], writes=[Bpsf[bank]])
            S.op("act", lambda h, hh=hh, di=di, bank=bank: h.copy(out=R0[:, hh, di, :], in_=psf[bank][:, 0:128]), reads=[Bpsf[bank]], writes=[BR0])
    if debug:
        d_R0 = dbg_out("R0", [128, H, 2, 128])
        final_ops.append(S.dma("sp", S.chan("dbgR0"), lambda h: h.dma_start(out=d_R0, in_=R0[:]), reads=[BR0]))
    S.barrier()
    AR.reset(m2b)
    if stage <= 2:
        return finish(nc, S, final_ops), dbg

    m2c = AR.mark()
    wq_off = AR.mark()
    wq1 = AR.alloc("wq", [128, KC, 512], BF16)
    wq = [wq1, wq1]
    Bwq1 = Buf("wq")
    Bwq = [Bwq1, Bwq1]
    ch_wq1 = S.chan("wq")
    ch_wq = [ch_wq1, ch_wq1]
    qT = AR.alloc_at("qT", [128, SEQ], BF16, wq_off)
    qdfT = AR.alloc_at("qdfT", [128, SEQ], BF16, wq_off + 4096)
    qdbT = AR.alloc_at("qdbT", [128, SEQ], BF16, wq_off + 8192)
    kT = AR.alloc_at("kT", [128, SEQ], BF16, wq_off + 12288)
    BqT = BqdfT = BqdbT = BkT = Bwq1
    qk_off = AR.mark()
    qk32 = AR.alloc("qk32", [128, NT, 2, 2, 64], F32)
    Bqk32 = Buf("qk32")
    o32 = AR.alloc_at("o32", [128, NT, 128], F32, qk_off)
    ytok = AR.alloc_at("ytok", [128, NT, 128], BF16, qk_off + 8192)
    yTh = AR.alloc_at("yTh", [128, SEQ], BF16, qk_off + 12288)
    Bo32 = Bytok = ByTh = Bqk32
    ta_off = AR.mark()
    ta = AR.alloc("ta", [128, NT, 2, 64], F32)
    Bta = Buf("ta")
    Sfb = AR.alloc_at("Sfb", [128, NT, 128], BF16, ta_off)
    Sbb = AR.alloc_at("Sbb", [128, NT, 128], BF16, ta_off + 4096)
    BSfb = BSbb = Bta
    tb = AR.alloc("tb", [128, NT, 2, 64], F32)
    rotb = AR.alloc("rotb", [128, NT, 2, 2, 64], BF16)
    qdf = AR.alloc("qdf", [128, NT, 2, 64], BF16)
    qdb = AR.alloc("qdb", [128, NT, 2, 64], BF16)
    kdf = AR.alloc("kdf", [128, NT, 2, 64], BF16)
    kdb = AR.alloc("kdb", [128, NT, 2, 64], BF16)
    vtok = AR.alloc("vtok", [128, NT, 128], BF16)
    sg = AR.alloc_at("sg", [128, NT, 128], F32, hcT_off)
    Rf = AR.alloc("Rf", [128, 128], F32)
    Rb = AR.alloc("Rb", [128, 128], F32)
    SM = [AR.alloc(f"SM{i}", [128, 128], BF16) for i in range(2)]
    bnst = AR.alloc("bnst", [128, NT, 6], F32)
    mv = AR.alloc("mv", [128, NT, 2], F32)
    rstd = AR.alloc("rstd", [128, NT], F32)
    (Btb, Brotb, Bqdf, Bqdb, Bkdf, Bkdb, Bvtok, Bsg, BRf, BRb, Bbnst, Bmv, Brstd) = (
        Buf(n) for n in ["tb", "rotb", "qdf", "qdb", "kdf", "kdb", "vtok", "sg", "Rf", "Rb", "bnst", "mv", "rstd"])
    BSM = [Buf("SM0"), Buf("SM1")]
    ch_yT = S.chan("yTout")

    for hh in range(H):
        bi = hh % 2
        for seg in range(4):
            S.dma("pool", ch_wq[bi], lambda h, hh=hh, bi=bi, seg=seg: h.dma_start(out=wq[bi][:, :, seg * 128:(seg + 1) * 128],
                                                                                    in_=win_v[:, :, seg * DRET + hh * 128:seg * DRET + (hh + 1) * 128]),
                  writes=[Bwq[bi]], cont=(seg > 0))
        for i in range(NT):
            bank = i % 4
            for kc in range(KC):
                S.op("pe", lambda h, kc=kc, i=i, bi=bi, bank=bank: h.matmul(psf[bank][:, :], lhsT=hT[:, kc, i * 128:(i + 1) * 128], rhs=wq[bi][:, kc, :],
                                                                             start=(kc == 0), stop=(kc == KC - 1)),
                     reads=[BhT[i], Bwq[bi]], writes=[Bpsf[bank]])
            S.op("act", lambda h, i=i, bank=bank: h.copy(out=qk32[:, i, :, :, :], in_=psf[bank][:, 0:256].rearrange("p (a b c) -> p a b c", a=2, b=2)),
                 reads=[Bpsf[bank]], writes=[Bqk32])
            S.op("act", lambda h, i=i, bank=bank: h.copy(out=vtok[:, i, :], in_=psf[bank][:, 256:384]), reads=[Bpsf[bank]], writes=[Bvtok])
            S.op("act", lambda h, i=i, bank=bank: h.activation(out=sg[:, i, :], in_=psf[bank][:, 384:512], func=AF.Silu), reads=[Bpsf[bank]], writes=[Bsg])
        X1 = qk32[:, :, :, 0, :]
        X2 = qk32[:, :, :, 1, :]
        S.op("dve", lambda h: h.tensor_tensor(out=ta[:], in0=X1, in1=cosT, op=ALU.mult), reads=[Bqk32, Brope], writes=[Bta])
        S.op("pool", lambda h: h.tensor_tensor(out=tb[:], in0=X2, in1=sinT, op=ALU.mult), reads=[Bqk32, Brope], writes=[Btb])
        S.op("dve", lambda h: h.tensor_tensor(out=ta[:], in0=ta[:], in1=tb[:], op=ALU.subtract), reads=[Bta, Btb], writes=[Bta])
        S.op("pool", lambda h: h.tensor_tensor(out=tb[:], in0=X1, in1=sinT, op=ALU.mult), reads=[Bqk32, Brope, Bta], writes=[Btb])
        S.op("dve", lambda h: h.tensor_tensor(out=X1, in0=X2, in1=cosT, op=ALU.mult), reads=[Bqk32, Brope, Btb], writes=[Bqk32])
        S.op("dve", lambda h: h.tensor_tensor(out=tb[:], in0=tb[:], in1=X1, op=ALU.add), reads=[Btb, Bqk32], writes=[Btb])
        S.op("act", lambda h: h.mul(out=rotb[:, :, 0, 0, :], in_=ta[:, :, 0, :], mul=QSCALE), reads=[Bta], writes=[Brotb])
        S.op("act", lambda h: h.copy(out=rotb[:, :, 1, 0, :], in_=ta[:, :, 1, :]), reads=[Bta], writes=[Brotb])
        S.op("act", lambda h: h.mul(out=rotb[:, :, 0, 1, :], in_=tb[:, :, 0, :], mul=QSCALE), reads=[Btb], writes=[Brotb])
        S.op("act", lambda h: h.copy(out=rotb[:, :, 1, 1, :], in_=tb[:, :, 1, :]), reads=[Btb], writes=[Brotb])
        for (dst, Bd, src_i, col, sc) in [(qdf, Bqdf, 0, 0, QSCALE), (qdb, Bqdb, 0, 1, QSCALE), (kdf, Bkdf, 1, 2, 1.0), (kdb, Bkdb, 1, 3, 1.0)]:
            S.op("dve", lambda h, dst=dst, src_i=src_i, col=col, hh=hh, sc=sc: h.tensor_scalar(out=dst[:, :, 0, :], in0=ta[:, :, src_i, :], scalar1=dcol[:, hh, col:col + 1],
                                                                                              scalar2=sc, op0=ALU.mult, op1=ALU.mult), reads=[Bta, Bdcol], writes=[Bd])
            S.op("pool", lambda h, dst=dst, src_i=src_i, col=col, hh=hh, sc=sc: h.tensor_scalar(out=dst[:, :, 1, :], in0=tb[:, :, src_i, :], scalar1=dcol[:, hh, col:col + 1],
                                                                                               scalar2=sc, op0=ALU.mult, op1=ALU.mult), reads=[Btb, Bdcol], writes=[Bd])
        srcs = [(lambda i: rotb[:, i, 0, :, :].rearrange("p a b -> p (a b)"), Brotb, qT, BqT),
                (lambda i: qdf[:, i, :, :].rearrange("p a b -> p (a b)"), Bqdf, qdfT, BqdfT),
                (lambda i: qdb[:, i, :, :].rearrange("p a b -> p (a b)"), Bqdb, qdbT, BqdbT),
                (lambda i: rotb[:, i, 1, :, :].rearrange("p a b -> p (a b)"), Brotb, kT, BkT)]
        cnt = 0
        for (srcf, Bsrc, dstT, BdstT) in srcs:
            for half in range(2):
                pb = cnt % 2
                cnt += 1
                for q in range(8):
                    i = half * 8 + q
                    S.op("pe", lambda h, i=i, q=q, pb=pb, srcf=srcf: h.transpose(out=psb[pb][:, q * 128:(q + 1) * 128], in_=srcf(i), identity=identb[:]),
                         reads=[Bsrc, Bc], writes=[Bpsb[pb]])
                if pb == 0:
                    S.op("act", lambda h, half=half, pb=pb, dstT=dstT: h.copy(out=dstT[:, half * 1024:(half + 1) * 1024], in_=psb[pb][:, :]),
                         reads=[Bpsb[pb]], writes=[BdstT])
                else:
                    S.op("dve", lambda h, half=half, pb=pb, dstT=dstT: h.tensor_copy(out=dstT[:, half * 1024:(half + 1) * 1024], in_=psb[pb][:, :]),
                         reads=[Bpsb[pb]], writes=[BdstT])
        S.op("dve", lambda h, hh=hh: h.tensor_copy(out=Rf[:], in_=R0[:, hh, 0, :]), reads=[BR0], writes=[BRf])
        S.op("dve", lambda h, hh=hh: h.tensor_copy(out=Rb[:], in_=R0[:, hh, 1, :]), reads=[BR0], writes=[BRb])
        S.op("act", lambda h: h.copy(out=Sfb[:, 0, :], in_=Rf[:]), reads=[BRf], writes=[BSfb])
        S.op("act", lambda h: h.copy(out=Sbb[:, NT - 1, :], in_=Rb[:]), reads=[BRb], writes=[BSbb])
        fbanks = [0, 1, 4]
        bbanks = [2, 3, 5]
        for step in range(NT - 1):
            i_f = step
            i_b = NT - 1 - step
            bf_ = fbanks[step % 3]
            bb_ = bbanks[step % 3]
            S.op("pe", lambda h, i=i_f, bf_=bf_: h.matmul(psf[bf_][:, 0:128], lhsT=kdf[:, i, :, :].rearrange("p a b -> p (a b)"), rhs=vtok[:, i, :], start=True, stop=True),
                 reads=[Bkdf, Bvtok], writes=[Bpsf[bf_]])
            S.op("dve", lambda h, hh=hh, bf_=bf_: h.scalar_tensor_tensor(out=Rf[:], in0=Rf[:], scalar=dcol[:, hh, 4:5], in1=psf[bf_][:, 0:128], op0=ALU.mult, op1=ALU.add),
                 reads=[BRf, Bdcol, Bpsf[bf_]], writes=[BRf])
            S.op("act", lambda h, i=i_f: h.copy(out=Sfb[:, i + 1, :], in_=Rf[:]), reads=[BRf], writes=[BSfb])
            S.op("pe", lambda h, i=i_b, bb_=bb_: h.matmul(psf[bb_][:, 0:128], lhsT=kdb[:, i, :, :].rearrange("p a b -> p (a b)"), rhs=vtok[:, i, :], start=True, stop=True),
                 reads=[Bkdb, Bvtok], writes=[Bpsf[bb_]])
            S.op("dve", lambda h, hh=hh, bb_=bb_: h.scalar_tensor_tensor(out=Rb[:], in0=Rb[:], scalar=dcol[:, hh, 5:6], in1=psf[bb_][:, 0:128], op0=ALU.mult, op1=ALU.add),
                 reads=[BRb, Bdcol, Bpsf[bb_]], writes=[BRb])
            S.op("act", lambda h, i=i_b: h.copy(out=Sbb[:, i - 1, :], in_=Rb[:]), reads=[BRb], writes=[BSbb])
        for i in range(NT):
            sb_ = i % 2
            bs = i % 2
            bo = 2 + (i % 2)
            cs = slice(i * 128, (i + 1) * 128)
            S.op("pe", lambda h, cs=cs, bs=bs: h.matmul(psf[bs][:, 0:128], lhsT=kT[:, cs], rhs=qT[:, cs], start=True, stop=True),
                 reads=[BkT, BqT], writes=[Bpsf[bs]])
            S.op("dve", lambda h, bs=bs, sb_=sb_, hh=hh: h.tensor_tensor(out=SM[sb_][:], in0=psf[bs][:, 0:128], in1=Mh[:, hh, :], op=ALU.mult),
                 reads=[Bpsf[bs], BMh], writes=[BSM[sb_]])
            S.op("pe", lambda h, i=i, sb_=sb_, bo=bo: h.matmul(psf[bo][:, 0:128], lhsT=SM[sb_][:], rhs=vtok[:, i, :], start=True, stop=False),
                 reads=[BSM[sb_], Bvtok], writes=[Bpsf[bo]])
            S.op("pe", lambda h, i=i, cs=cs, bo=bo: h.matmul(psf[bo][:, 0:128], lhsT=qdfT[:, cs], rhs=Sfb[:, i, :], start=False, stop=False),
                 reads=[BqdfT, BSfb], writes=[Bpsf[bo]])
            S.op("pe", lambda h, i=i, cs=cs, bo=bo: h.matmul(psf[bo][:, 0:128], lhsT=qdbT[:, cs], rhs=Sbb[:, i, :], start=False, stop=True),
                 reads=[BqdbT, BSbb], writes=[Bpsf[bo]])
            S.op("act", lambda h, i=i, bo=bo: h.copy(out=o32[:, i, :], in_=psf[bo][:, 0:128]), reads=[Bpsf[bo]], writes=[Bo32])
            S.op("dve", lambda h, i=i: h.bn_stats(out=bnst[:, i, :], in_=o32[:, i, :]), reads=[Bo32], writes=[Bbnst])
            S.op("dve", lambda h, i=i: h.bn_aggr(out=mv[:, i, :], in_=bnst[:, i, :]), reads=[Bbnst], writes=[Bmv])
        S.op("act", lambda h: h.activation(out=rstd[:], in_=mv[:, :, 1], func=AF.Sqrt, bias=GN_EPS), reads=[Bmv], writes=[Brstd])
        S.op("dve", lambda h: h.reciprocal(out=rstd[:], in_=rstd[:]), reads=[Brstd], writes=[Brstd])
        S.op("pool", lambda h, hh=hh: h.tensor_tensor(out=sg[:], in0=sg[:], in1=gnwb[:, hh * 128:(hh + 1) * 128].unsqueeze(1).to_broadcast([128, NT, 128]), op=ALU.mult),
             reads=[Bsg, Bgnwb], writes=[Bsg])
        for i in range(NT):
            S.op("dve", lambda h, i=i: h.tensor_scalar(out=o32[:, i, :], in0=o32[:, i, :], scalar1=mv[:, i, 0:1], scalar2=rstd[:, i:i + 1], op0=ALU.subtract, op1=ALU.mult),
                 reads=[Bo32, Bmv, Brstd], writes=[Bo32])
        S.op("pool", lambda h: h.tensor_tensor(out=ytok[:], in0=o32[:], in1=sg[:], op=ALU.mult), reads=[Bo32, Bsg], writes=[Bytok])
        for half in range(2):
            pb = half
            for q in range(8):
                i = half * 8 + q
                S.op("pe", lambda h, i=i, q=q, pb=pb: h.transpose(out=psb[pb][:, q * 128:(q + 1) * 128], in_=ytok[:, i, :], identity=identb[:]),
                     reads=[Bytok, Bc], writes=[Bpsb[pb]])
            if half == 0:
                S.op("act", lambda h, half=half, pb=pb: h.copy(out=yTh[:, half * 1024:(half + 1) * 1024], in_=psb[pb][:, :]), reads=[Bpsb[pb]], writes=[ByTh])
            else:
                S.op("dve", lambda h, half=half, pb=pb: h.tensor_copy(out=yTh[:, half * 1024:(half + 1) * 1024], in_=psb[pb][:, :]), reads=[Bpsb[pb]], writes=[ByTh])
        S.dma("sp", ch_yT, lambda h, hh=hh: h.dma_start(out=yT_s[hh * 128:(hh + 1) * 128, :], in_=yTh[:]), reads=[ByTh], writes=[ByT_s])
    S.barrier()
    AR.reset(m2c)
    if stage <= 3:
        if debug:
            d_yT = dbg_out("yT", [D, SEQ], BF16)
            ld = AR.alloc("dbgld", [128, KC, SEQ], BF16)
            Bld = Buf("dbgld")
            S.dma("sp", S.chan("dbgy1"), lambda h: h.dma_start(out=ld[:], in_=yT_s.rearrange("(c p) t -> p c t", p=128)), reads=[ByT_s], writes=[Bld])
            final_ops.append(S.dma("sp", S.chan("dbgy2"), lambda h: h.dma_start(out=d_yT.rearrange("(c p) t -> p c t", p=128), in_=ld[:]), reads=[Bld]))
        return finish(nc, S, final_ops), dbg

    m2d = AR.mark()
    cvrows = AR.alloc("cvrows", [24, 128], F32)
    cvT = AR.alloc("cvT", [128, 24], F32)
    cwrows = AR.alloc("cwrows", [CW, DCONV], F32)
    cwT = AR.alloc("cwT", [128, 8, CW], F32)
    Bcvrows, BcvT, Bcwrows, BcwT = Buf("cvrows"), Buf("cvT"), Buf("cwrows"), Buf("cwT")
    S.dma("sp", ch_v, lambda h: h.dma_start(out=cvrows[:], in_=cvec_d), writes=[Bcvrows])
    S.dma("sp", ch_v, lambda h: h.dma_start(out=cwrows[:], in_=convw_d), writes=[Bcwrows], cont=True)
    transpose_rows(cvrows[:], 24, cvT[:], 0, Bpsf[0], [Bcvrows], [BcvT])
    for cc in range(8):
        S.op("pe", lambda h, cc=cc: h.transpose(out=psf[1][:, 0:CW], in_=cwrows[:, cc * 128:(cc + 1) * 128], identity=ident[0:CW, 0:CW]),
             reads=[Bcwrows, Bc], writes=[Bpsf[1]])
        S.op("act", lambda h, cc=cc: h.copy(out=cwT[:, cc, :], in_=psf[1][:, 0:CW]), reads=[Bpsf[1]], writes=[BcwT])
    wc = [AR.alloc(f"wc{i}", [128, KC, 256], BF16) for i in range(2)]
    Bwc = [Buf("wc0"), Buf("wc1")]
    ch_wc = [S.chan("wc0"), S.chan("wc1")]
    sig = AR.alloc("sig", [128, 512], F32)
    uu = AR.alloc("uu", [128, 8, 64], F32)
    cvo = AR.alloc("cvo", [128, 8, 8, 64], F32)
    cv2 = AR.alloc("cv2", [128, 8, 64], F32)
    Bcv2 = Buf("cv2")
    sq = AR.alloc("sq", [128, 512], F32)
    mean = AR.alloc("mean", [128, 512], F32)
    msq = AR.alloc("msq", [128, 512], F32)
    rsd = AR.alloc("rsd", [128, 512], F32)
    tn = AR.alloc("tn", [128, 512], F32)
    ycv = [AR.alloc(f"ycv{i}", [128, 512], BF16) for i in range(2)]
    Bsig, Buu, Bcvo, Bsq, Bmean, Bmsq, Brsd, Btn = (Buf(n) for n in ["sig", "uu", "cvo", "sq", "mean", "msq", "rsd", "tn"])
    Bycv = [Buf("ycv0"), Buf("ycv1")]
    ch_ycv = [S.chan("ycv0"), S.chan("ycv1")]
    pcount = 0
    for tbk in range(4):
        tsl = slice(tbk * 512, (tbk + 1) * 512)
        for cc in range(8):
            bi = pcount % 2
            pcount += 1
            S.dma("pool", ch_wc[bi], lambda h, cc=cc, bi=bi: h.dma_start(out=wc[bi][:, :, 0:128], in_=win_v[:, :, 4 * DRET + cc * 128:4 * DRET + (cc + 1) * 128]),
                  writes=[Bwc[bi]])
            S.dma("pool", ch_wc[bi], lambda h, cc=cc, bi=bi: h.dma_start(out=wc[bi][:, :, 128:256],
                                                                          in_=win_v[:, :, 4 * DRET + DCONV + cc * 128:4 * DRET + DCONV + (cc + 1) * 128]),
                  writes=[Bwc[bi]], cont=True)
            ba, bb = 0 + 2 * (cc % 2), 1 + 2 * (cc % 2)
            for kc in range(KC):
                S.op("pe", lambda h, kc=kc, bi=bi, ba=ba, tsl=tsl: h.matmul(psf[ba][:, :], lhsT=wc[bi][:, kc, 0:128], rhs=hT[:, kc, tsl], start=(kc == 0), stop=(kc == KC - 1)),
                     reads=[Bwc[bi]] + BhT[tbk * 4:(tbk + 1) * 4], writes=[Bpsf[ba]])
            for kc in range(KC):
                S.op("pe", lambda h, kc=kc, bi=bi, bb=bb, tsl=tsl: h.matmul(psf[bb][:, :], lhsT=wc[bi][:, kc, 128:256], rhs=hT[:, kc, tsl], start=(kc == 0), stop=(kc == KC - 1)),
                     reads=[Bwc[bi]] + BhT[tbk * 4:(tbk + 1) * 4], writes=[Bpsf[bb]])
            S.op("act", lambda h, bb=bb: h.activation(out=sig[:], in_=psf[bb][:, :], func=AF.Sigmoid), reads=[Bpsf[bb]], writes=[Bsig])
            S.op("dve", lambda h, ba=ba: h.tensor_tensor(out=uu[:].rearrange("p a b -> p (a b)"), in0=psf[ba][:, :], in1=sig[:], op=ALU.mult),
                 reads=[Bpsf[ba], Bsig], writes=[Buu])
            acc = cvo[:, cc, :, :]
            S.op("dve", lambda h, cc=cc, acc=acc: h.tensor_scalar(out=acc, in0=uu[:], scalar1=cwT[:, cc, 15:16], scalar2=cvT[:, cc:cc + 1], op0=ALU.mult, op1=ALU.add),
                 reads=[Buu, BcwT, BcvT], writes=[Bcvo])
            S.op("dve", lambda h: h.memset(cv2[:], 0.0), writes=[Bcv2])
            taps = [k for k in range(CW) if k != 15]
            for ti, k in enumerate(taps):
                o = k - 15
                lo, hi = max(0, -o), min(64, 64 - o)
                if ti % 2 == 0:
                    S.op("dve", lambda h, cc=cc, k=k, o=o, lo=lo, hi=hi: h.scalar_tensor_tensor(out=cv2[:, :, lo:hi], in0=uu[:, :, lo + o:hi + o], scalar=cwT[:, cc, k:k + 1],
                                                                                                 in1=cv2[:, :, lo:hi], op0=ALU.mult, op1=ALU.add),
                         reads=[Buu, BcwT, Bcv2], writes=[Bcv2])
                else:
                    S.op("dve", lambda h, cc=cc, k=k, o=o, lo=lo, hi=hi: h.scalar_tensor_tensor(out=cvo[:, cc, :, lo:hi], in0=uu[:, :, lo + o:hi + o], scalar=cwT[:, cc, k:k + 1],
                                                                                                 in1=cvo[:, cc, :, lo:hi], op0=ALU.mult, op1=ALU.add),
                         reads=[Buu, BcwT, Bcvo], writes=[Bcvo])
            S.op("dve", lambda h, acc=acc: h.tensor_tensor(out=acc, in0=acc, in1=cv2[:], op=ALU.add), reads=[Bcvo, Bcv2], writes=[Bcvo])
            accf = cvo[:, cc, :, :].rearrange("p a b -> p (a b)")
            S.op("act", lambda h, accf=accf: h.activation(out=sq[:], in_=accf, func=AF.Square), reads=[Bcvo], writes=[Bsq])
            S.op("pe", lambda h, accf=accf, cc=cc: h.matmul(psf[4][:, :], lhsT=ones32[:], rhs=accf, start=(cc == 0), stop=(cc == 7)), reads=[Bcvo, Bc], writes=[Bpsf[4]])
            S.op("pe", lambda h, cc=cc: h.matmul(psf[5][:, :], lhsT=ones32[:], rhs=sq[:], start=(cc == 0), stop=(cc == 7)), reads=[Bsq, Bc], writes=[Bpsf[5]])
        S.op("act", lambda h: h.activation(out=mean[:], in_=psf[4][:, :], func=AF.Identity, scale=1.0 / DCONV), reads=[Bpsf[4]], writes=[Bmean])
        S.op("dve", lambda h: h.tensor_tensor(out=msq[:], in0=mean[:], in1=mean[:], op=ALU.mult), reads=[Bmean], writes=[Bmsq])
        S.op("dve", lambda h: h.scalar_tensor_tensor(out=rsd[:], in0=psf[5][:, :], scalar=1.0 / DCONV, in1=msq[:], op0=ALU.mult, op1=ALU.subtract),
             reads=[Bpsf[5], Bmsq], writes=[Brsd])
        S.op("act", lambda h: h.activation(out=rsd[:], in_=rsd[:], func=AF.Sqrt, bias=EPS), reads=[Brsd], writes=[Brsd])
        S.op("dve", lambda h: h.reciprocal(out=rsd[:], in_=rsd[:]), reads=[Brsd], writes=[Brsd])
        for cc in range(8):
            yb = cc % 2
            accf = cvo[:, cc, :, :].rearrange("p a b -> p (a b)")
            S.op("dve", lambda h, accf=accf: h.tensor_tensor(out=tn[:], in0=accf, in1=mean[:], op=ALU.subtract), reads=[Bcvo, Bmean], writes=[Btn])
            S.op("pool", lambda h: h.tensor_tensor(out=tn[:], in0=tn[:], in1=rsd[:], op=ALU.mult), reads=[Btn, Brsd], writes=[Btn])
            S.op("act", lambda h, cc=cc, yb=yb: h.activation(out=ycv[yb][:], in_=tn[:], func=AF.Silu, scale=cvT[:, 8 + cc:9 + cc], bias=cvT[:, 16 + cc:17 + cc]),
                 reads=[Btn, BcvT], writes=[Bycv[yb]])
            S.dma("sp", ch_ycv[yb], lambda h, cc=cc, yb=yb, tsl=tsl: h.dma_start(out=yT_s[DRET + cc * 128:DRET + (cc + 1) * 128, tsl], in_=ycv[yb][:]),
                  reads=[Bycv[yb]], writes=[ByT_s])
    S.barrier()
    AR.reset(m2)
    AR.reset(persist_mark)
    if stage <= 4:
        if debug:
            d_yT = dbg_out("yT", [D, SEQ], BF16)
            ld = AR.alloc("dbgld", [128, KC, SEQ], BF16)
            Bld = Buf("dbgld")
            S.dma("sp", S.chan("dbgy1"), lambda h: h.dma_start(out=ld[:], in_=yT_s.rearrange("(c p) t -> p c t", p=128)), reads=[ByT_s], writes=[Bld])
            final_ops.append(S.dma("sp", S.chan("dbgy2"), lambda h: h.dma_start(out=d_yT.rearrange("(c p) t -> p c t", p=128), in_=ld[:]), reads=[Bld]))
        return finish(nc, S, final_ops), dbg

    m3 = AR.mark()
    wo = AR.alloc("wo", [128, KC, D], BF16)
    Bwo = Buf("wo")
    ch_wo = S.chan("wo")
    wout_v = wout_d.rearrange("(k p) n -> p k n", p=128)
    for j in range(4):
        S.dma("pool", ch_wo, lambda h, j=j: h.dma_start(out=wo[:, :, j * 512:(j + 1) * 512], in_=wout_v[:, :, j * 512:(j + 1) * 512]), writes=[Bwo], cont=(j > 0))
    G1 = AR.alloc("G1", [128, D], F32)
    A2 = AR.alloc("A2", [128, D], F32)
    S2 = AR.alloc("S2", [128, D], F32)
    wt3 = AR.alloc("wt3", [128, D], F32)
    BG1, BA2, BS2, Bwt3 = Buf("G1"), Buf("A2"), Buf("S2"), Buf("wt3")
    load_mod_bcast(G1, 0, 2, ch_v, BG1)
    load_row_bcast(wt3, pomw_d, ch_v, Bwt3)
    S.op("dve", lambda h: h.tensor_tensor(out=G1[:], in0=G1[:], in1=wt3[:], op=ALU.mult), reads=[BG1, Bwt3], writes=[BG1])
    load_mod_bcast(A2, 0, 4, ch_v, BA2)
    load_row_bcast(wt3, pfw_d, ch_v, Bwt3)
    S.op("dve", lambda h: h.scalar_tensor_tensor(out=A2[:], in0=A2[:], scalar=1.0, in1=wt3[:], op0=ALU.add, op1=ALU.mult), reads=[BA2, Bwt3], writes=[BA2])
    load_mod_bcast(S2, 0, 3, ch_v, BS2)
    rw32 = AR.alloc("rw32", [128, KC, NE], F32)
    rbb = AR.alloc("rbb", [128, NE], F32)
    ebase = AR.alloc("ebase", [128, NE], F32)
    Brw, Brbb, Bebase = Buf("rw32"), Buf("rbb"), Buf("ebase")
    S.dma("sp", ch_v, lambda h: h.dma_start(out=rw32[:], in_=rw_d.rearrange("(k p) e -> p k e", p=128)), writes=[Brw])
    S.dma("sp", ch_v, lambda h: h.dma_start(out=rbb[:], in_=rb_d.partition_broadcast(128)), writes=[Brbb], cont=True)
    S.op("dve", lambda h: h.tensor_scalar(out=ebase[:], in0=iorow[:, 0:NE], scalar1=float(CAP), scalar2=None, op0=ALU.mult), reads=[Bc], writes=[Bebase])
    yTt = [AR.alloc(f"yTt{i}", [128, KC, 128], BF16) for i in range(2)]
    ByTt = [Buf("yTt0"), Buf("yTt1")]
    ch_yTt = [S.chan("yTt0"), S.chan("yTt1")]
    xr = [AR.alloc(f"xr{i}", [128, D], F32) for i in range(2)]
    Bxr = [Buf("xr0"), Buf("xr1")]
    ch_xr = [S.chan("xr0"), S.chan("xr1")]
    t3 = AR.alloc("t3", [128, D], F32)
    x1t = [AR.alloc(f"x1t{i}", [128, D], F32) for i in range(2)]
    h2f = AR.alloc("h2f", [128, D], F32)
    h2b = [AR.alloc(f"h2b{i}", [128, D], BF16) for i in range(2)]
    h2T = AR.alloc("h2T", [128, KC, 128], F32)
    junk3 = AR.alloc("junk3", [128, D], BF16)
    st3 = AR.alloc("st3", [128, 8], F32)
    lg = AR.alloc("lg", [128, NE], F32)
    mx8 = AR.alloc("mx8", [128, 8], F32)
    nmx = AR.alloc("nmx", [128, 1], F32)
    msk = AR.alloc("msk", [128, NE], F32)
    mskb = AR.alloc("mskb", [128, NT, NE], BF16)
    exv = AR.alloc("exv", [128, NE], F32)
    den = AR.alloc("den", [128, 1], F32)
    posC = AR.alloc("posC", [128, NE], F32)
    ovf = AR.alloc("ovf", [128, NE], F32)
    oh = AR.alloc("oh", [128, NE], F32)
    jk = AR.alloc("jk", [128, NE], F32)
    idxf = AR.alloc("idxf", [128, 4], F32)
    idl = AR.alloc("idl", [128, 4], F32)
    idn = AR.alloc("idn", [128, 4], F32)
    Bidl, Bidn = Buf("idl"), Buf("idn")
    (Bt3, Bh2f, Bh2T, Bjunk3, Bst3, Blg, Bmx8, Bnmx, Bmsk, Bmskb, Bexv, Bden, BposC, Bovf, Boh, Bjk, Bidxf) = (
        Buf(n) for n in ["t3", "h2f", "h2T", "junk3", "st3", "lg", "mx8", "nmx", "msk", "mskb", "exv", "den", "posC", "ovf", "oh", "jk", "idxf"])
    Bx1t = [Buf("x1t0"), Buf("x1t1")]
    Bh2b = [Buf("h2b0"), Buf("h2b1")]
    ch_x1 = [S.chan("x1w0"), S.chan("x1w1")]
    ch_sc = [S.chan(f"scat{i}") for i in range(2)]
    Bx1_s = [Buf(f"x1_s{i}") for i in range(NT)]
    Bhsel = Buf("hsel_s")
    yT_v = yT_s.rearrange("(c p) t -> p c t", p=128)
    mixps = [psf[0], psf[1], psf[2], psf[3]]
    for i in range(NT):
        bi = i % 2
        S.dma("sp", ch_yTt[bi], lambda h, i=i, bi=bi: h.dma_start(out=yTt[bi][:], in_=yT_v[:, :, i * 128:(i + 1) * 128]), reads=[ByT_s], writes=[ByTt[bi]])
        S.dma("sp", ch_xr[bi], lambda h, i=i, bi=bi: h.dma_start(out=xr[bi][:], in_=x_d[i * 128:(i + 1) * 128, :]), writes=[Bxr[bi]])
        for cb in range(4):
            for c in range(KC):
                S.op("pe", lambda h, c=c, cb=cb, bi=bi: h.matmul(psf[cb][:, :], lhsT=yTt[bi][:, c, :], rhs=wo[:, c, cb * 512:(cb + 1) * 512], start=(c == 0), stop=(c == KC - 1)),
                     reads=[ByTt[bi], Bwo], writes=[Bpsf[cb]])
        for cb in range(4):
            S.op("act", lambda h, cb=cb: h.activation(out=junk3[:, cb * 512:(cb + 1) * 512], in_=psf[cb][:, :], func=AF.Square, accum_out=st3[:, cb:cb + 1]),
                 reads=[Bpsf[cb]], writes=[Bjunk3, Bst3])
        S.op("dve", lambda h: h.tensor_reduce(out=st3[:, 4:5], in_=st3[:, 0:4], axis=AX.X, op=ALU.add), reads=[Bst3], writes=[Bst3])
        S.op("act", lambda h: h.activation(out=st3[:, 4:5], in_=st3[:, 4:5], func=AF.Sqrt, scale=1.0 / D, bias=EPS), reads=[Bst3], writes=[Bst3])
        S.op("dve", lambda h: h.reciprocal(out=st3[:, 4:5], in_=st3[:, 4:5]), reads=[Bst3], writes=[Bst3])
        for cb in range(4):
            S.op("dve", lambda h, cb=cb: h.scalar_tensor_tensor(out=t3[:, cb * 512:(cb + 1) * 512], in0=psf[cb][:, :], scalar=st3[:, 4:5], in1=G1[:, cb * 512:(cb + 1) * 512],
                                                                op0=ALU.mult, op1=ALU.mult), reads=[Bpsf[cb], Bst3, BG1], writes=[Bt3])
        S.op("pool", lambda h, bi=bi: h.tensor_tensor(out=x1t[bi][:], in0=t3[:], in1=xr[bi][:], op=ALU.add), reads=[Bt3, Bxr[bi]], writes=[Bx1t[bi]])
        S.dma("sp", ch_x1[bi], lambda h, i=i, bi=bi: h.dma_start(out=x1_s[i * 128:(i + 1) * 128, :], in_=x1t[bi][:]), reads=[Bx1t[bi]], writes=[Bx1_s[i]])
        S.op("act", lambda h, bi=bi: h.activation(out=junk3[:], in_=x1t[bi][:], func=AF.Square, accum_out=st3[:, 5:6]), reads=[Bx1t[bi]], writes=[Bjunk3, Bst3])
        S.op("act", lambda h: h.activation(out=st3[:, 5:6], in_=st3[:, 5:6], func=AF.Sqrt, scale=1.0 / D, bias=EPS), reads=[Bst3], writes=[Bst3])
        S.op("dve", lambda h: h.reciprocal(out=st3[:, 5:6], in_=st3[:, 5:6]), reads=[Bst3], writes=[Bst3])
        S.op("dve", lambda h, bi=bi: h.scalar_tensor_tensor(out=t3[:], in0=x1t[bi][:], scalar=st3[:, 5:6], in1=A2[:], op0=ALU.mult, op1=ALU.mult),
             reads=[Bx1t[bi], Bst3, BA2], writes=[Bt3])
        S.op("pool", lambda h: h.tensor_tensor(out=h2f[:], in0=t3[:], in1=S2[:], op=ALU.add), reads=[Bt3, BS2], writes=[Bh2f])
        S.op("act", lambda h, bi=bi: h.copy(out=h2b[bi][:], in_=h2f[:]), reads=[Bh2f], writes=[Bh2b[bi]])
        for q4 in range(4):
            bank = 4 + (q4 % 2)
            for q in range(4):
                kc = q4 * 4 + q
                S.op("pe", lambda h, kc=kc, q=q, bank=bank: h.transpose(out=psf[bank][:, q * 128:(q + 1) * 128], in_=h2f[:, kc * 128:(kc + 1) * 128], identity=ident[:]),
                     reads=[Bh2f, Bc], writes=[Bpsf[bank]])
            if q4 % 2 == 0:
                S.op("act", lambda h, q4=q4, bank=bank: h.copy(out=h2T[:, q4 * 4:(q4 + 1) * 4, :], in_=psf[bank][:, :].rearrange("p (q t) -> p q t", q=4)),
                     reads=[Bpsf[bank]], writes=[Bh2T])
            else:
                S.op("dve", lambda h, q4=q4, bank=bank: h.tensor_copy(out=h2T[:, q4 * 4:(q4 + 1) * 4, :], in_=psf[bank][:, :].rearrange("p (q t) -> p q t", q=4)),
                     reads=[Bpsf[bank]], writes=[Bh2T])
        for kc in range(KC):
            S.op("pe", lambda h, kc=kc: h.matmul(psf[4][:, 0:NE], lhsT=h2T[:, kc, :], rhs=rw32[:, kc, :], start=(kc == 0), stop=(kc == KC - 1)),
                 reads=[Bh2T, Brw], writes=[Bpsf[4]])
        S.op("dve", lambda h: h.tensor_tensor(out=lg[:], in0=psf[4][:, 0:NE], in1=rbb[:], op=ALU.add), reads=[Bpsf[4], Brbb], writes=[Blg])
        S.op("dve", lambda h: h.max(out=mx8[:], in_=lg[:]), reads=[Blg], writes=[Bmx8])
        S.op("dve", lambda h: h.tensor_scalar(out=msk[:], in0=lg[:], scalar1=mx8[:, 3:4], scalar2=None, op0=ALU.is_ge), reads=[Blg, Bmx8], writes=[Bmsk])
        S.op("dve", lambda h, i=i: h.tensor_copy(out=mskb[:, i, :], in_=msk[:]), reads=[Bmsk], writes=[Bmskb])
        S.op("dve", lambda h: h.tensor_scalar(out=nmx[:], in0=mx8[:, 0:1], scalar1=-1.0, scalar2=None, op0=ALU.mult), reads=[Bmx8], writes=[Bnmx])
        S.op("act", lambda h: h.activation(out=exv[:], in_=lg[:], func=AF.Exp, bias=nmx[:, 0:1]), reads=[Blg, Bnmx], writes=[Bexv])
        S.op("dve", lambda h: h.tensor_tensor(out=exv[:], in0=exv[:], in1=msk[:], op=ALU.mult), reads=[Bexv, Bmsk], writes=[Bexv])
        S.op("dve", lambda h: h.tensor_reduce(out=den[:], in_=exv[:], axis=AX.X, op=ALU.add), reads=[Bexv], writes=[Bden])
        S.op("dve", lambda h: h.reciprocal(out=den[:], in_=den[:]), reads=[Bden], writes=[Bden])
        S.op("pe", lambda h, i=i: h.matmul(psf[5][:, 0:NE], lhsT=trib[:], rhs=mskb[:, i, :], start=True, stop=(i == 0)), reads=[Bmskb, Bc], writes=[Bpsf[5]])
        for j in range(i):
            S.op("pe", lambda h, j=j, i=i: h.matmul(psf[5][:, 0:NE], lhsT=onesb[:], rhs=mskb[:, j, :], start=False, stop=(j == i - 1)), reads=[Bmskb, Bc], writes=[Bpsf[5]])
        S.op("dve", lambda h: h.tensor_scalar(out=ovf[:], in0=psf[5][:, 0:NE], scalar1=float(CAP) - 0.5, scalar2=None, op0=ALU.is_gt), reads=[Bpsf[5]], writes=[Bovf])
        S.op("dve", lambda h: h.tensor_tensor(out=posC[:], in0=psf[5][:, 0:NE], in1=ebase[:], op=ALU.add), reads=[Bpsf[5], Bebase], writes=[BposC])
        S.op("dve", lambda h: h.scalar_tensor_tensor(out=posC[:], in0=ovf[:], scalar=BIG, in1=posC[:], op0=ALU.mult, op1=ALU.add), reads=[Bovf, BposC], writes=[BposC])
        S.op("dve", lambda h: h.tensor_scalar(out=ovf[:], in0=ovf[:], scalar1=-1.0, scalar2=1.0, op0=ALU.mult, op1=ALU.add), reads=[Bovf], writes=[Bovf])
        S.op("dve", lambda h, i=i: h.scalar_tensor_tensor(out=gatesA[:, i, :], in0=exv[:], scalar=den[:, 0:1], in1=ovf[:], op0=ALU.mult, op1=ALU.mult),
             reads=[Bexv, Bden, Bovf], writes=[BgatesA])
        for k in range(4):
            S.op("dve", lambda h, k=k: h.tensor_scalar(out=oh[:], in0=lg[:], scalar1=mx8[:, k:k + 1], scalar2=None, op0=ALU.is_equal), reads=[Blg, Bmx8], writes=[Boh])
            S.op("dve", lambda h, k=k: h.scalar_tensor_tensor(out=jk[:], in0=oh[:], scalar=1.0, in1=posC[:], op0=ALU.mult, op1=ALU.mult, accum_out=idxf[:, k:k + 1]),
                 reads=[Boh, BposC], writes=[Bjk, Bidxf])
            S.op("dve", lambda h, k=k, i=i: h.scalar_tensor_tensor(out=jk[:], in0=oh[:], scalar=1.0, in1=gatesA[:, i, :], op0=ALU.mult, op1=ALU.mult,
                                                                 accum_out=gate4[:, i, k:k + 1]), reads=[Boh, BgatesA], writes=[Bjk, Bgate4])
        for (lst, nten, Bl) in [(idxH, NHS, Bidx[i]), (idxY, NYS, Bidx[i])]:
            for j in range(nten):
                shift = float(j * (NE // nten) * CAP)
                S.op("dve", lambda h, shift=shift: h.tensor_scalar(out=idl[:], in0=idxf[:], scalar1=shift, scalar2=None, op0=ALU.subtract), reads=[Bidxf], writes=[Bidl])
                S.op("dve", lambda h: h.tensor_scalar(out=idn[:], in0=idl[:], scalar1=0.0, scalar2=BIG, op0=ALU.is_lt, op1=ALU.mult), reads=[Bidl], writes=[Bidn])
                S.op("dve", lambda h: h.tensor_tensor(out=idl[:], in0=idl[:], in1=idn[:], op=ALU.add), reads=[Bidl, Bidn], writes=[Bidl])
                S.op("dve", lambda h, i=i, t=lst[j]: h.tensor_copy(out=t[:, i * 4:(i + 1) * 4], in_=idl[:]), reads=[Bidl], writes=[Bl])
        first = True
        for k in range(4):
            for j in range(NHS):
                S.dma("pool", ch_sc[bi], lambda h, i=i, k=k, bi=bi, j=j: h.indirect_dma_start(out=hsel_s[j], out_offset=bass.IndirectOffsetOnAxis(ap=idxH[j][:, i * 4 + k:i * 4 + k + 1], axis=0),
                                                                                           in_=h2b[bi][:], in_offset=None, bounds_check=bound_reg(h, (NE // NHS) * CAP - 1), oob_is_err=False),
                      reads=[Bh2b[bi], Bidx[i]], writes=[Bhsel], cont=(not first))
                first = False
    for j in range(NT):
        S.op("pe", lambda h, j=j: h.matmul(psf[5][:, 0:NE], lhsT=onesb[:], rhs=mskb[:, j, :], start=(j == 0), stop=(j == NT - 1)), reads=[Bmskb, Bc], writes=[Bpsf[5]])
    S.cnt_op = S.op("dve", lambda h: h.tensor_copy(out=cnt_i[:], in_=psf[5][:, 0:NE]), reads=[Bpsf[5]], writes=[Bcnt])
    S.cnt_ap = lambda e: cnt_i[0:1, e:e + 1]
    if debug:
        d_lg = dbg_out("gates", [128, NT, NE])
        final_ops.append(S.dma("sp", S.chan("dbglg"), lambda h: h.dma_start(out=d_lg, in_=gatesA[:]), reads=[BgatesA]))
        d_idx = dbg_out("idx4", [128, NT * 4], I32)
        final_ops.append(S.dma("sp", S.chan("dbgidx"), lambda h: h.dma_start(out=d_idx, in_=idxY[0][:]), reads=Bidx))
        d_cnt = dbg_out("cnt", [128, NE], I32)
        final_ops.append(S.dma("sp", S.chan("dbgcnt"), lambda h: h.dma_start(out=d_cnt, in_=cnt_i[:]), reads=[Bcnt]))
        d_g4 = dbg_out("gate4", [128, NT, 4])
        final_ops.append(S.dma("sp", S.chan("dbgg4"), lambda h: h.dma_start(out=d_g4, in_=gate4[:]), reads=[Bgate4]))
    S.barrier()
    AR.reset(m3)
    if stage <= 5:
        if debug:
            d_x1 = dbg_out("x1", [SEQ, D])
            ld = AR.alloc("dbgld", [128, NT, D], F32)
            Bld = Buf("dbgld")
            S.dma("sp", S.chan("dbgx1"), lambda h: h.dma_start(out=ld[:], in_=x1_s.rearrange("(t p) d -> p t d", p=128)), reads=Bx1_s, writes=[Bld])
            final_ops.append(S.dma("sp", S.chan("dbgx2"), lambda h: h.dma_start(out=d_x1.rearrange("(t p) d -> p t d", p=128), in_=ld[:]), reads=[Bld]))
        return finish(nc, S, final_ops), dbg

    m5 = AR.mark()
    hselT = [AR.alloc(f"hselT{i}", [128, KC, RS], BF16) for i in range(2)]
    BhselT = [Buf("hselT0"), Buf("hselT1")]
    actT = AR.alloc("actT", [128, KC, RS], BF16)
    BactT = Buf("actT")
    NW1, NW2 = 4, 3
    w1u = [AR.alloc(f"w1u{i}", [128, KC, 512], BF16) for i in range(NW1)]
    Bw1u = [Buf(f"w1u{i}") for i in range(NW1)]
    ch_w1 = [S.chan(f"w1u{i}") for i in range(NW1)]
    ch_w1g = [S.chan(f"w1ug{i}") for i in range(NW1)]
    w2p = [AR.alloc(f"w2p{i}", [128, KC, 512], BF16) for i in range(NW2)]
    Bw2p = [Buf(f"w2p{i}") for i in range(NW2)]
    ch_w2 = [S.chan(f"w2p{i}") for i in range(NW2)]
    ch_w2g = [S.chan(f"w2pg{i}") for i in range(NW2)]
    hrow = [AR.alloc(f"hrow{i}", [128, D], BF16) for i in range(2)]
    Bhrow = [Buf("hrow0"), Buf("hrow1")]
    NCH_H, NCH_Y = 8, 16
    ch_hrow = [S.chan(f"hrow{i}") for i in range(NCH_H)]
    ysb = [AR.alloc(f"ysb{i}", [128, 512], F32) for i in range(4)]
    Bysb = [Buf(f"ysb{i}") for i in range(4)]
    ch_y = [S.chan(f"yw{i}") for i in range(NCH_Y)]
    hcnt = [0]
    g1 = [AR.alloc(f"g1_{i}", [128, BLK], F32) for i in range(2)]
    sgm = [AR.alloc(f"sgm{i}", [128, BLK], F32) for i in range(2)]
    l2 = [AR.alloc(f"l2_{i}", [128, BLK], F32) for i in range(2)]
    wv = [AR.alloc(f"wv_{i}", [128, BLK], F32) for i in range(2)]
    Bg1 = [Buf("g1_0"), Buf("g1_1")]
    Bsgm = [Buf("sgm0"), Buf("sgm1")]
    Bl2 = [Buf("l2_0"), Buf("l2_1")]
    Bwv = [Buf("wv_0"), Buf("wv_1")]
    By_s = Buf("y_s")
    w1_v = [w1_d[e].rearrange("(k p) n -> p k n", p=128) for e in range(NE)]
    w2_v = [w2_d[e].rearrange("(k p) n -> p k n", p=128) for e in range(NE)]

    def blk_guard(e, r, b):
        return (e, b * BLK) if r == 0 else (e, r * RS)

    def rnd_guard(e, r):
        return (e, r * RS) if r > 0 else None

    def build_hselT(e, r, buf_i):
        for b in range(NBLK):
            S.cur_guard = blk_guard(e, r, b)
            for st2 in range(BLK // 128):
                st = b * (BLK // 128) + st2
                hb_i = st % 2
                row0 = (e % (NE // NHS)) * CAP + r * RS + st * 128
                src = hsel_s[e // (NE // NHS)]
                chh = ch_hrow[hcnt[0] % NCH_H]
                hcnt[0] += 1
                S.dma("sp", chh, lambda h, row0=row0, hb_i=hb_i, src=src: h.dma_start(out=hrow[hb_i][:], in_=src[row0:row0 + 128, :]), reads=[Bhsel], writes=[Bhrow[hb_i]])
                for half in range(2):
                    pb = half
                    for q in range(8):
                        kc = half * 8 + q
                        S.op("pe", lambda h, kc=kc, q=q, pb=pb, hb_i=hb_i: h.transpose(out=psb[pb][:, q * 128:(q + 1) * 128], in_=hrow[hb_i][:, kc * 128:(kc + 1) * 128], identity=identb[:]),
                             reads=[Bhrow[hb_i], Bc], writes=[Bpsb[pb]])
                    if half == 0:
                        S.op("act", lambda h, half=half, pb=pb, st=st, buf_i=buf_i: h.copy(out=hselT[buf_i][:, half * 8:(half + 1) * 8, st * 128:(st + 1) * 128],
                                                                                        in_=psb[pb][:, :].rearrange("p (q t) -> p q t", q=8)), reads=[Bpsb[pb]], writes=[BhselT[buf_i]])
                    else:
                        S.op("dve", lambda h, half=half, pb=pb, st=st, buf_i=buf_i: h.tensor_copy(out=hselT[buf_i][:, half * 8:(half + 1) * 8, st * 128:(st + 1) * 128],
                                                                                               in_=psb[pb][:, :].rearrange("p (q t) -> p q t", q=8)), reads=[Bpsb[pb]], writes=[BhselT[buf_i]])
        S.cur_guard = None

    ucount = 0
    pcount2 = 0
    acnt = 0
    ycnt = 0
    er_list = [(e, r) for e in range(NE) for r in range(ROUNDS)]
    build_hselT(er_list[0][0], er_list[0][1], 0)
    for n, (e, r) in enumerate(er_list):
        hb_cur = n % 2
        for u in range(8):
            bi = ucount % NW1
            ucount += 1
            S.cur_guard = rnd_guard(e, r)
            cw1 = ch_w1[bi] if r == 0 else ch_w1g[bi]
            S.dma("pool", cw1, lambda h, e=e, u=u, bi=bi: h.dma_start(out=w1u[bi][:, :, 0:256], in_=w1_v[e][:, :, u * 256:(u + 1) * 256]), writes=[Bw1u[bi]])
            S.dma("pool", cw1, lambda h, e=e, u=u, bi=bi: h.dma_start(out=w1u[bi][:, :, 256:512], in_=w1_v[e][:, :, DFF + u * 256:DFF + (u + 1) * 256]),
                  writes=[Bw1u[bi]], cont=True)
            for b in range(NBLK):
                S.cur_guard = blk_guard(e, r, b)
                for j in range(2):
                    fc = u * 2 + j
                    ab = acnt % 2
                    bank = acnt % 4
                    acnt += 1
                    bs = slice(b * BLK, (b + 1) * BLK)
                    for kc in range(KC):
                        S.op("pe", lambda h, kc=kc, bi=bi, j=j, bs=bs, bank=bank, hb_cur=hb_cur: h.matmul(psf[bank][:, 0:BLK], lhsT=w1u[bi][:, kc, j * 128:(j + 1) * 128],
                                                                                                     rhs=hselT[hb_cur][:, kc, bs], start=(kc == 0), stop=(kc == KC - 1)),
                             reads=[Bw1u[bi], BhselT[hb_cur]], writes=[Bpsf[bank]])
                    for kc in range(KC):
                        S.op("pe", lambda h, kc=kc, bi=bi, j=j, bs=bs, bank=bank, hb_cur=hb_cur: h.matmul(psf[bank][:, BLK:2 * BLK], lhsT=w1u[bi][:, kc, 256 + j * 128:256 + (j + 1) * 128],
                                                                                                     rhs=hselT[hb_cur][:, kc, bs], start=(kc == 0), stop=(kc == KC - 1)),
                             reads=[Bw1u[bi], BhselT[hb_cur]], writes=[Bpsf[bank]])
                    cg = e * 32 + fc
                    cl = e * 32 + 16 + fc
                    S.op("dve", lambda h, ab=ab, bank=bank, cg=cg: h.tensor_scalar(out=g1[ab][:], in0=psf[bank][:, 0:BLK], scalar1=b1T[:, cg:cg + 1], scalar2=LIMIT, op0=ALU.add, op1=ALU.min),
                         reads=[Bpsf[bank], Bb1T], writes=[Bg1[ab]])
                    S.op("act", lambda h, ab=ab: h.activation(out=sgm[ab][:], in_=g1[ab][:], func=AF.Sigmoid, scale=ALPHA), reads=[Bg1[ab]], writes=[Bsgm[ab]])
                    S.op("dve", lambda h, ab=ab, bank=bank, cl=cl: h.tensor_scalar(out=l2[ab][:], in0=psf[bank][:, BLK:2 * BLK], scalar1=b1T[:, cl:cl + 1], scalar2=LIMIT + 1.0, op0=ALU.add, op1=ALU.min),
                         reads=[Bpsf[bank], Bb1T], writes=[Bl2[ab]])
                    S.op("dve", lambda h, ab=ab: h.scalar_tensor_tensor(out=wv[ab][:], in0=l2[ab][:], scalar=1.0 - LIMIT, in1=g1[ab][:], op0=ALU.max, op1=ALU.mult),
                         reads=[Bl2[ab], Bg1[ab]], writes=[Bwv[ab]])
                    S.op("dve", lambda h, ab=ab, fc=fc, bs=bs: h.tensor_tensor(out=actT[:, fc, bs], in0=wv[ab][:], in1=sgm[ab][:], op=ALU.mult),
                         reads=[Bwv[ab], Bsgm[ab]], writes=[BactT])
        S.cur_guard = None
        if n + 1 < len(er_list):
            build_hselT(er_list[n + 1][0], er_list[n + 1][1], (n + 1) % 2)
        ydst = y_s[e // (NE // NYS)]
        for db in range(4):
            bi = pcount2 % NW2
            pcount2 += 1
            S.cur_guard = rnd_guard(e, r)
            cw2 = ch_w2[bi] if r == 0 else ch_w2g[bi]
            S.dma("pool", cw2, lambda h, e=e, db=db, bi=bi: h.dma_start(out=w2p[bi][:], in_=w2_v[e][:, :, db * 512:(db + 1) * 512]), writes=[Bw2p[bi]])
            for b in range(NBLK):
                S.cur_guard = blk_guard(e, r, b)
                for st2 in range(BLK // 128):
                    st = b * (BLK // 128) + st2
                    bank = 4 + (ycnt % 2)
                    yb = ycnt % 4
                    ych = ch_y[ycnt % NCH_Y]
                    ycnt += 1
                    for fc in range(KC):
                        S.op("pe", lambda h, fc=fc, st=st, bi=bi, bank=bank: h.matmul(psf[bank][:, :], lhsT=actT[:, fc, st * 128:(st + 1) * 128], rhs=w2p[bi][:, fc, :],
                                                                                     start=(fc == 0), stop=(fc == KC - 1)),
                             reads=[BactT, Bw2p[bi]], writes=[Bpsf[bank]])
                    S.op("act", lambda h, bank=bank, yb=yb: h.copy(out=ysb[yb][:], in_=psf[bank][:, :]), reads=[Bpsf[bank]], writes=[Bysb[yb]])
                    row0 = (e % (NE // NYS)) * CAP + r * RS + st * 128
                    S.dma("sp", ych, lambda h, row0=row0, db=db, yb=yb, ydst=ydst: h.dma_start(out=ydst[row0:row0 + 128, db * 512:(db + 1) * 512], in_=ysb[yb][:]),
                          reads=[Bysb[yb]], writes=[By_s])
        S.cur_guard = None
    S.barrier()
    AR.reset(m5)

    G2 = AR.alloc("G2", [128, D], F32)
    wt6 = AR.alloc("wt6", [128, D], F32)
    b2sb = AR.alloc("b2sb", [NE, D], F32)
    BG2, Bwt6, Bb2 = Buf("G2"), Buf("wt6"), Buf("b2sb")
    load_mod_bcast(G2, 0, 5, ch_v, BG2)
    load_row_bcast(wt6, pofw_d, ch_v, Bwt6)
    S.op("dve", lambda h: h.tensor_tensor(out=G2[:], in0=G2[:], in1=wt6[:], op=ALU.mult), reads=[BG2, Bwt6], writes=[BG2])
    S.dma("sp", ch_v, lambda h: h.dma_start(out=b2sb[:], in_=b2_d), writes=[Bb2])
    yk = [[AR.alloc(f"yk{b}_{k}", [128, D], F32) for k in range(4)] for b in range(2)]
    Byk = [[Buf(f"yk{b}_{k}") for k in range(4)] for b in range(2)]
    ch_g = [S.chan("gath0"), S.chan("gath1")]
    x1r = [AR.alloc(f"x1r{i}", [128, D], F32) for i in range(2)]
    Bx1r = [Buf("x1r0"), Buf("x1r1")]
    ch_x1r = [S.chan("x1r0"), S.chan("x1r1")]
    ff = AR.alloc("ff", [128, D], F32)
    ot = [AR.alloc(f"ot{i}", [128, D], F32) for i in range(2)]
    gT = AR.alloc("gT", [NE, 128], F32)
    junk6 = AR.alloc("junk6", [128, D], BF16)
    st6 = AR.alloc("st6", [128, 2], F32)
    Bff, BgT, Bjunk6, Bst6 = Buf("ff"), Buf("gT"), Buf("junk6"), Buf("st6")
    Bot = [Buf("ot0"), Buf("ot1")]
    ch_out = [S.chan("out0"), S.chan("out1")]
    for i in range(NT):
        bi = i % 2
        first = True
        for k in range(4):
            for j in range(NYS):
                S.dma("pool", ch_g[bi], lambda h, i=i, k=k, bi=bi, j=j: h.indirect_dma_start(out=yk[bi][k][:], out_offset=None, in_=y_s[j],
                                                                                          in_offset=bass.IndirectOffsetOnAxis(ap=idxY[j][:, i * 4 + k:i * 4 + k + 1], axis=0),
                                                                                          bounds_check=bound_reg(h, (NE // NYS) * CAP - 1), oob_is_err=False),
                      reads=[By_s, Bidx[i]], writes=[Byk[bi][k]], cont=(not first))
                first = False
        S.dma("sp", ch_x1r[bi], lambda h, i=i, bi=bi: h.dma_start(out=x1r[bi][:], in_=x1_s[i * 128:(i + 1) * 128, :]), reads=[Bx1_s[i]], writes=[Bx1r[bi]])
        S.op("pe", lambda h, i=i: h.transpose(out=psf[4][0:NE, 0:128], in_=gatesA[:, i, :], identity=ident[:]), reads=[BgatesA, Bc], writes=[Bpsf[4]])
        S.op("act", lambda h: h.copy(out=gT[:], in_=psf[4][0:NE, 0:128]), reads=[Bpsf[4]], writes=[BgT])
        for cb in range(4):
            S.op("pe", lambda h, cb=cb: h.matmul(psf[cb][:, :], lhsT=gT[:], rhs=b2sb[:, cb * 512:(cb + 1) * 512], start=True, stop=True), reads=[BgT, Bb2], writes=[Bpsf[cb]])
            S.op("dve", lambda h, cb=cb, bi=bi, i=i: h.scalar_tensor_tensor(out=ff[:, cb * 512:(cb + 1) * 512], in0=yk[bi][0][:, cb * 512:(cb + 1) * 512], scalar=gate4[:, i, 0:1],
                                                                          in1=psf[cb][:, :], op0=ALU.mult, op1=ALU.add), reads=[Byk[bi][0], Bgate4, Bpsf[cb]], writes=[Bff])
        for k in range(1, 4):
            S.op("dve", lambda h, k=k, bi=bi, i=i: h.scalar_tensor_tensor(out=ff[:], in0=yk[bi][k][:], scalar=gate4[:, i, k:k + 1], in1=ff[:], op0=ALU.mult, op1=ALU.add),
                 reads=[Byk[bi][k], Bgate4, Bff], writes=[Bff])
        S.op("act", lambda h: h.activation(out=junk6[:], in_=ff[:], func=AF.Square, accum_out=st6[:, 0:1]), reads=[Bff], writes=[Bjunk6, Bst6])
        S.op("act", lambda h: h.activation(out=st6[:, 0:1], in_=st6[:, 0:1], func=AF.Sqrt, scale=1.0 / D, bias=EPS), reads=[Bst6], writes=[Bst6])
        S.op("dve", lambda h: h.reciprocal(out=st6[:, 0:1], in_=st6[:, 0:1]), reads=[Bst6], writes=[Bst6])
        S.op("dve", lambda h: h.scalar_tensor_tensor(out=ff[:], in0=ff[:], scalar=st6[:, 0:1], in1=G2[:], op0=ALU.mult, op1=ALU.mult), reads=[Bff, Bst6, BG2], writes=[Bff])
        S.op("pool", lambda h, bi=bi: h.tensor_tensor(out=ot[bi][:], in0=ff[:], in1=x1r[bi][:], op=ALU.add), reads=[Bff, Bx1r[bi]], writes=[Bot[bi]])
        final_ops.append(S.dma("sp", ch_out[bi], lambda h, i=i, bi=bi: h.dma_start(out=out_d[i * 128:(i + 1) * 128, :], in_=ot[bi][:]), reads=[Bot[bi]]))
    return finish(nc, S, final_ops), dbg


def finish(nc, S, final_ops):
    S.wait_final("sp", final_ops)
    S.emit()
    return nc


def _rope_tables():
    half = 64
    inv = (10000.0 ** (-np.arange(half, dtype=np.float32) / np.float32(half))).astype(np.float32)
    pos = np.arange(NCTX + SEQ, dtype=np.float32)
    ang = (pos[:, None] * inv[None, :]).astype(np.float32)
    return np.cos(ang).astype(np.float32), np.sin(ang).astype(np.float32)


def make_in_maps(inp, ne_decl=NE):
    f = lambda a: np.ascontiguousarray(np.asarray(a, dtype=np.float32))
    cos, sin = _rope_tables()
    shared = {
        "c_ctx": f(inp["c_ctx"]).reshape(16, 128),
        "ada_w": f(inp["ada_w"][0]),
        "ada_b": f(inp["ada_b"][0]).reshape(1, 6 * D),
        "pre_mix_norm": f(inp["pre_mix_norm"][0]).reshape(1, D),
        "post_mix_norm": f(inp["post_mix_norm"][0]).reshape(1, D),
        "pre_ffn_norm": f(inp["pre_ffn_norm"][0]).reshape(1, D),
        "post_ffn_norm": f(inp["post_ffn_norm"][0]).reshape(1, D),
        "w_in": f(inp["w_in"][0]),
        "ret_decay": np.concatenate([f(inp["ret_decay_fwd"][0]), f(inp["ret_decay_bwd"][0])]).reshape(1, 16),
        "ret_gn_w": f(inp["ret_gn_w"][0]).reshape(1, DRET),
        "conv_w": f(inp["conv_w"][0]),
        "conv_vecs": np.concatenate([f(inp["conv_b"][0]).reshape(8, 128), f(inp["conv_ln_w"][0]).reshape(8, 128), f(inp["conv_ln_b"][0]).reshape(8, 128)], axis=0),
        "w_out": f(inp["w_out"][0]),
        "router_w": f(inp["router_w"][0]),
        "router_b": f(inp["router_b"][0]).reshape(1, NE),
        "w1": f(inp["w1"][0][:ne_decl]),
        "b1": f(inp["b1"][0]).reshape(NE * 32, 128),
        "w2": f(inp["w2"][0][:ne_decl]),
        "b2": f(inp["b2"][0]),
        "rope_cos": cos,
        "rope_sin": sin,
    }
    maps = []
    for b in range(NB):
        m = dict(shared)
        m["x"] = f(inp["x"][b])
        m["c"] = f(inp["c"][b]).reshape(16, 128)
        m["ctx"] = f(inp["ctx"][b])
        maps.append(m)
    return maps


_NC_CACHE = {}


def kernel(**inputs):
    if "nc" not in _NC_CACHE:
        _NC_CACHE["nc"] = build_program()[0]
    nc = _NC_CACHE["nc"]
    in_maps = make_in_maps(inputs)
    res = run_bass_kernel_spmd(nc, in_maps, core_ids=list(range(NB)))
    out = np.stack([np.asarray(res.results[b]["out"], dtype=np.float32) for b in range(NB)], axis=0)
    return out
```

```python
import numpy as np
import concourse.bass as bass
import concourse.mybir as mybir
from concourse.alu_op_type import AluOpType as ALU
from concourse.bass_utils import run_bass_kernel_spmd

F32 = mybir.dt.float32
BF16 = mybir.dt.bfloat16
I32 = mybir.dt.int32
AF = mybir.ActivationFunctionType
AX = mybir.AxisListType

D = 2048
SEQ = 2048
NB = 8
NCTX = 256
H = 8
HD = 128
DRET = 1024
DCONV = 1024
DIN = 6144
CW = 31
NE = 32
DFF = 2048
NT = SEQ // 128
KC = D // 128
EPS = 1e-6
GN_EPS = 1e-5
ALPHA = 1.702
LIMIT = 7.0
QSCALE = HD ** -0.5

ROUNDS = 4
RS = 512
CAP = ROUNDS * RS
BLK = 256
NBLK = RS // BLK
NHS = 1
NYS = 2
BIG = 4.0e6

ENGS = ("pe", "act", "dve", "pool", "sp")
EPOCH = 1 << 30


class Buf:
    __slots__ = ("name", "excl", "last_w", "readers")

    def __init__(self, name, excl=False):
        self.name = name
        self.excl = excl
        self.last_w = None
        self.readers = []


class Op:
    __slots__ = ("eng", "fn", "deps", "signal", "done", "is_dma", "chan", "chan_prev", "grp", "guard")

    def __init__(self, eng, fn):
        self.eng = eng
        self.fn = fn
        self.deps = []
        self.signal = False
        self.done = None
        self.is_dma = False
        self.chan = None
        self.chan_prev = None
        self.grp = None
        self.guard = None


class Chan:
    def __init__(self, name):
        self.name = name
        self.sem = None
        self.last_grp = None


class Sched:
    def __init__(self, nc):
        self.nc = nc
        self.ops = {e: [] for e in ENGS}
        self.chans = []
        self.final_waits = []
        self.nrec = 0
        self.cur_guard = None
        self.cnt_ap = None
        self.cnt_op = None

    def chan(self, name):
        c = Chan(name)
        self.chans.append(c)
        return c

    def _deps(self, op, reads, writes):
        for b in reads:
            if b.last_w is not None:
                op.deps.append((b.last_w, "RAW"))
            if b.excl:
                for r in b.readers:
                    op.deps.append((r, "RAR"))
        for b in writes:
            if b.last_w is not None:
                op.deps.append((b.last_w, "WAW"))
            for r in b.readers:
                op.deps.append((r, "WAR"))
        for b in reads:
            b.readers.append(op)
        for b in writes:
            b.last_w = op
            b.readers = []

    def op(self, eng, fn, reads=(), writes=()):
        o = Op(eng, fn)
        o.guard = self.cur_guard
        self._deps(o, list(reads), list(writes))
        self.ops[eng].append(o)
        self.nrec += 1
        return o

    def dma(self, eng, chan, fn, reads=(), writes=(), cont=False):
        o = Op(eng, fn)
        o.guard = self.cur_guard
        o.is_dma = True
        o.chan = chan
        if cont and chan.last_grp is not None:
            o.grp = chan.last_grp
            o.chan_prev = o.grp[0].chan_prev
        else:
            o.chan_prev = chan.last_grp
            o.grp = []
            chan.last_grp = o.grp
        o.grp.append(o)
        self._deps(o, list(reads), list(writes))
        self.ops[eng].append(o)
        self.nrec += 1
        return o

    def barrier(self):
        lasts = []
        for e in ENGS:
            for o in reversed(self.ops[e]):
                if not o.is_dma and o.fn is not None:
                    lasts.append(o)
                    break
        for c in self.chans:
            if c.last_grp:
                lasts.append(c.last_grp[0])
        for e in ENGS:
            o = Op(e, None)
            for d in lasts:
                o.deps.append((d, "RAW"))
            self.ops[e].append(o)

    def wait_final(self, eng, ops):
        self.final_waits.append((eng, list(ops)))

    def emit(self):
        nc = self.nc
        for e in ENGS:
            for o in self.ops[e]:
                for (d, kind) in o.deps:
                    if d.is_dma:
                        continue
                    if d.eng != o.eng or kind == "RAW":
                        d.signal = True
        for (e, ops) in self.final_waits:
            for d in ops:
                if not d.is_dma:
                    d.signal = True
        if self.cnt_op is not None:
            self.cnt_op.signal = True
        sems = []
        for e in ENGS:
            cnt = 0
            sem = None
            for o in self.ops[e]:
                if o.is_dma or o.fn is None:
                    continue
                if o.signal:
                    if sem is None or cnt >= EPOCH:
                        sem = nc.alloc_semaphore(f"s_{e}_{len(sems)}")
                        sems.append(sem)
                        cnt = 0
                    cnt += 1
                    o.done = (sem, cnt)
        for c in self.chans:
            if c.last_grp is None:
                continue
            c.sem = nc.alloc_semaphore(f"c_{c.name}")
            chain = []
            g = c.last_grp
            while g is not None:
                chain.append(g)
                g = g[0].chan_prev
            chain.reverse()
            v = 0
            for g in chain:
                v += 16 * len(g)
                for o in g:
                    o.done = (c.sem, v)
        lists = self.ops
        finals = self.final_waits

        cnt_ap = self.cnt_ap
        cnt_op = self.cnt_op

        def run(e, h):
            seen = {}
            state = {"greg": None, "loaded": None, "last_sig": None}

            def need(sem, val):
                k = id(sem)
                if seen.get(k, 0) < val:
                    h.wait_ge(sem, val)
                    seen[k] = val

            def emit_op(o):
                if o.is_dma and o.chan_prev is not None and o is o.grp[0]:
                    need(*o.chan_prev[0].done)
                for (d, kind) in o.deps:
                    if d.is_dma:
                        if d.grp is o.grp:
                            continue
                        need(*d.done)
                    elif d.eng != e or kind == "RAW":
                        need(*d.done)
                if o.fn is None:
                    return
                ins = o.fn(h)
                if o.is_dma:
                    ins.then_inc(o.chan.sem, 16)
                elif o.signal:
                    ins.then_inc(o.done[0], 1)
                    state["last_sig"] = o.done

            ops = lists[e]
            i = 0
            n = len(ops)
            while i < n:
                o = ops[i]
                if o.guard is None:
                    emit_op(o)
                    i += 1
                    continue
                j = i
                while j < n and ops[j].guard == o.guard:
                    j += 1
                grp = ops[i:j]
                (ge, thr) = o.guard
                if state["greg"] is None:
                    state["greg"] = h.alloc_register(f"greg_{e}")
                if state["loaded"] != ge:
                    need(*cnt_op.done)
                    h.reg_load(state["greg"], cnt_ap(ge))
                    state["loaded"] = ge
                saved = dict(seen)
                pre_sig = state["last_sig"]
                with h.If_cmp(state["greg"], thr, "IS_GT"):
                    for g in grp:
                        emit_op(g)
                nsig = 0
                sig_sem = None
                ndma = 0
                for g in grp:
                    if g.fn is None:
                        continue
                    if g.is_dma:
                        ndma += 1
                    elif g.signal:
                        nsig += 1
                        sig_sem = g.done[0]
                        last_in = g.done
                if nsig or ndma:
                    with h.Else():
                        if nsig:
                            if pre_sig is not None:
                                h.wait_ge(*pre_sig)
                            h.sem_inc(sig_sem, nsig)
                        for g in grp:
                            if g.fn is not None and g.is_dma:
                                if g is g.grp[0] and g.chan_prev is not None:
                                    h.wait_ge(*g.chan_prev[0].done)
                                h.sem_inc(g.chan.sem, 16)
                if nsig:
                    state["last_sig"] = last_in
                seen.clear()
                seen.update(saved)
                i = j
            for (fe, fops) in finals:
                if fe == e:
                    for d in fops:
                        need(*d.done)

        with nc.Block() as block:
            @block.tensor
            def _(h):
                run("pe", h)

            @block.scalar
            def _(h):
                run("act", h)

            @block.vector
            def _(h):
                run("dve", h)

            @block.gpsimd
            def _(h):
                run("pool", h)

            @block.sync
            def _(h):
                run("sp", h)


class Arena:
    def __init__(self, nc, nbytes):
        self.nc = nc
        left = nc._sbuf_addr_for_side("left")
        self.base = (left + 63) // 64 * 64
        nbytes = nbytes // 64 * 64
        self.slab = nc.alloc_sbuf_tensor("arena", [128, nbytes // 4], F32)
        self.size = nbytes - 64
        self.top = 0
        self.n = 0

    def alloc(self, name, shape, dtype):
        esz = 2 if dtype == BF16 else 4
        nb = esz
        for s in shape[1:]:
            nb *= s
        nb = (nb + 63) // 64 * 64
        off = self.top
        assert off + nb <= self.size, (name, off, nb, self.size)
        self.top += nb
        self.n += 1
        return self.nc.alloc_sbuf_tensor_at(f"{name}_{self.n}", list(shape), dtype, offset=self.base + off)

    def alloc_at(self, name, shape, dtype, off):
        self.n += 1
        return self.nc.alloc_sbuf_tensor_at(f"{name}_{self.n}", list(shape), dtype, offset=self.base + off)

    def mark(self):
        return self.top

    def reset(self, m):
        self.top = m


def build_program(stage=99, debug=False, ne_decl=NE):
    nc = bass.Bass("TRN2", target_bir_lowering=False)
    S = Sched(nc)

    def din(name, shape, dt=F32):
        return nc.dram_tensor(name, list(shape), dt, kind="ExternalInput").ap()

    x_d = din("x", [SEQ, D])
    c_d = din("c", [16, 128])
    ctx_d = din("ctx", [NCTX, D])
    cctx_d = din("c_ctx", [16, 128])
    adaw_d = din("ada_w", [D, 6 * D])
    adab_d = din("ada_b", [1, 6 * D])
    pmw_d = din("pre_mix_norm", [1, D])
    pomw_d = din("post_mix_norm", [1, D])
    pfw_d = din("pre_ffn_norm", [1, D])
    pofw_d = din("post_ffn_norm", [1, D])
    win_d = din("w_in", [D, DIN])
    dec_d = din("ret_decay", [1, 16])
    gnw_d = din("ret_gn_w", [1, DRET])
    convw_d = din("conv_w", [CW, DCONV])
    cvec_d = din("conv_vecs", [24, 128])
    wout_d = din("w_out", [D, D])
    rw_d = din("router_w", [D, NE])
    rb_d = din("router_b", [1, NE])
    w1_d = din("w1", [ne_decl, D, 2 * DFF])
    b1_d = din("b1", [NE * 32, 128])
    w2_d = din("w2", [ne_decl, DFF, D])
    b2_d = din("b2", [NE, D])
    cos_d = din("rope_cos", [NCTX + SEQ, 64])
    sin_d = din("rope_sin", [NCTX + SEQ, 64])
    out_d = nc.dram_tensor("out", [SEQ, D], F32, kind="ExternalOutput").ap()
    dbg = {}

    def dbg_out(name, shape, dt=F32):
        t = nc.dram_tensor("dbg_" + name, list(shape), dt, kind="ExternalOutput").ap()
        dbg[name] = t
        return t

    mod_s = nc.dram_tensor("mod_s", [2, 6 * D], F32).ap()
    yT_s = nc.dram_tensor("yT_s", [D, SEQ], BF16).ap()
    x1_s = nc.dram_tensor("x1_s", [SEQ, D], F32).ap()
    hsel_s = [nc.dram_tensor(f"hsel_s{j}", [(NE // NHS) * CAP, D], BF16).ap() for j in range(NHS)]
    y_s = [nc.dram_tensor(f"y_s{j}", [(NE // NYS) * CAP, D], F32).ap() for j in range(NYS)]

    AR = Arena(nc, nc.sbuf_bytes_remaining - 6144)

    psf = [nc.alloc_psum_tensor(f"psf{i}", [128, 512], F32) for i in range(6)]
    psb = [nc.alloc_psum_tensor(f"psb{i}", [128, 1024], BF16) for i in range(2)]
    Bpsf = [Buf(f"psf{i}", excl=True) for i in range(6)]
    Bpsb = [Buf(f"psb{i}", excl=True) for i in range(2)]
    final_ops = []
    _regs = {}

    def bound_reg(h, val):
        if val not in _regs:
            _regs[val] = h.to_reg(val)
        return _regs[val]

    ident = AR.alloc("ident", [128, 128], F32)
    identb = AR.alloc("identb", [128, 128], BF16)
    onesb = AR.alloc("onesb", [128, 128], BF16)
    ones32 = AR.alloc("ones32", [128, 128], F32)
    trib = AR.alloc("trib", [128, 128], BF16)
    iorow = AR.alloc("iorow", [128, 128], F32)
    iocol = AR.alloc("iocol", [128, 1], F32)
    b1T = AR.alloc("b1T", [128, NE * 32], F32)
    gate4 = AR.alloc("gate4", [128, NT, 4], F32)
    idxH = [AR.alloc(f"idxH{j}", [128, NT * 4], I32) for j in range(NHS)]
    idxY = [AR.alloc(f"idxY{j}", [128, NT * 4], I32) for j in range(NYS)]
    cnt_i = AR.alloc("cnt_i", [128, NE], I32)
    Bcnt = Buf("cnt_i")
    gatesA = AR.alloc("gatesA", [128, NT, NE], F32)
    Bc = Buf("consts")
    Bb1T = Buf("b1T")
    Bgate4 = Buf("gate4")
    Bidx = [Buf(f"idx{i}") for i in range(NT)]
    BgatesA = Buf("gatesA")

    S.op("pool", lambda h: h.memset(ident[:], 0.0), writes=[Bc])
    S.op("pool", lambda h: h.affine_select(out=ident[:], in_=ident[:], pattern=[[-1, 128]], compare_op=ALU.not_equal,
                                            fill=1.0, base=0, channel_multiplier=1), reads=[Bc], writes=[Bc])
    S.op("pool", lambda h: h.memset(ones32[:], 1.0), writes=[Bc])
    S.op("pool", lambda h: h.affine_select(out=iorow[:], in_=ones32[:], pattern=[[1, 128]], compare_op=ALU.is_gt,
                                            fill=0.0, base=0, channel_multiplier=-1), reads=[Bc], writes=[Bc])
    S.op("dve", lambda h: h.tensor_copy(out=trib[:], in_=iorow[:]), reads=[Bc], writes=[Bc])
    S.op("dve", lambda h: h.tensor_copy(out=identb[:], in_=ident[:]), reads=[Bc], writes=[Bc])
    S.op("dve", lambda h: h.tensor_copy(out=onesb[:], in_=ones32[:]), reads=[Bc], writes=[Bc])
    ioi = AR.alloc("ioi", [128, 128], I32)
    S.op("pool", lambda h: h.iota(ioi[:], pattern=[[1, 128]], base=0, channel_multiplier=0), writes=[Bc])
    S.op("dve", lambda h: h.tensor_copy(out=iorow[:], in_=ioi[:]), reads=[Bc], writes=[Bc])
    S.op("pool", lambda h: h.iota(ioi[:, 0:1], pattern=[[0, 1]], base=0, channel_multiplier=1), reads=[Bc], writes=[Bc])
    S.op("dve", lambda h: h.tensor_copy(out=iocol[:], in_=ioi[:, 0:1]), reads=[Bc], writes=[Bc])

    ch_small = S.chan("small")
    persist_mark = AR.mark()

    def transpose_rows(rows_ap, nrows, out_ap, bank, Bbank, reads, writes, evac="act"):
        S.op("pe", lambda h: h.transpose(out=psf[bank][:, 0:nrows], in_=rows_ap, identity=ident[0:nrows, 0:nrows]),
             reads=reads + [Bc], writes=[Bbank])
        if evac == "act":
            S.op("act", lambda h: h.copy(out=out_ap, in_=psf[bank][:, 0:nrows]), reads=[Bbank], writes=writes)
        else:
            S.op("dve", lambda h: h.tensor_copy(out=out_ap, in_=psf[bank][:, 0:nrows]), reads=[Bbank], writes=writes)

    hT = AR.alloc("hT", [128, KC, SEQ], BF16)
    hcT_off = AR.mark()
    hcT = AR.alloc("hcT", [128, KC, NCTX], BF16)
    BhT = [Buf(f"hT{i}") for i in range(NT)]
    BhcT = Buf("hcT")
    m1 = AR.mark()
    sil2 = AR.alloc("sil2", [128, KC, 2], BF16)
    adaw = [AR.alloc(f"adaw{i}", [128, KC, 512], BF16) for i in range(2)]
    adabp = [AR.alloc(f"adabp{i}", [2, 512], F32) for i in range(2)]
    modp = [AR.alloc(f"modp{i}", [2, 512], F32) for i in range(2)]
    Bsil2 = Buf("sil2")
    Badaw = [Buf("adaw0"), Buf("adaw1")]
    Badabp = [Buf("adabp0"), Buf("adabp1")]
    Bmodp = [Buf("modp0"), Buf("modp1")]
    ch_adaw = [S.chan("adaw0"), S.chan("adaw1")]
    ch_adab = [S.chan("adab0"), S.chan("adab1")]
    ch_modw = S.chan("modw")
    Bmod_s = Buf("mod_s")
    adaw_v = adaw_d.rearrange("(k p) n -> p k n", p=128)

    def ada_piece(j):
        bi = j % 2
        S.dma("pool", ch_adaw[bi], lambda h: h.dma_start(out=adaw[bi][:], in_=adaw_v[:, :, j * 512:(j + 1) * 512]), writes=[Badaw[bi]])
        S.dma("sp", ch_adab[bi], lambda h: h.dma_start(out=adabp[bi][:], in_=adab_d[0:1, j * 512:(j + 1) * 512].partition_broadcast(2)), writes=[Badabp[bi]])
        bank = 4 + bi
        for kc in range(KC):
            S.op("pe", lambda h, kc=kc: h.matmul(psf[bank][0:2, :], lhsT=sil2[:, kc, :], rhs=adaw[bi][:, kc, :], start=(kc == 0), stop=(kc == KC - 1)),
                 reads=[Bsil2, Badaw[bi]], writes=[Bpsf[bank]])
        S.op("dve", lambda h: h.tensor_tensor(out=modp[bi][:], in0=psf[bank][0:2, :], in1=adabp[bi][:], op=ALU.add),
             reads=[Bpsf[bank], Badabp[bi]], writes=[Bmodp[bi]])
        S.dma("sp", ch_modw, lambda h: h.dma_start(out=mod_s[:, j * 512:(j + 1) * 512], in_=modp[bi][:]), reads=[Bmodp[bi]], writes=[Bmod_s])

    m0 = AR.mark()
    rows32 = AR.alloc("rows32", [32, 128], F32)
    cT = AR.alloc("cT", [128, 32], F32)
    sil = AR.alloc("sil", [128, 32], F32)
    b1rows = AR.alloc("b1rows", [128, 8, 128], F32)
    Brows32, BcT, Bsil, Bb1rows = Buf("rows32"), Buf("cT"), Buf("sil"), Buf("b1rows")
    S.dma("sp", ch_small, lambda h: h.dma_start(out=rows32[0:16, :], in_=c_d), writes=[Brows32])
    S.dma("sp", ch_small, lambda h: h.dma_start(out=rows32[16:32, :], in_=cctx_d), writes=[Brows32], cont=True)
    S.dma("sp", ch_small, lambda h: h.dma_start(out=b1rows[:], in_=b1_d.rearrange("(t p) f -> p t f", p=128)), writes=[Bb1rows], cont=True)
    transpose_rows(rows32[:], 32, cT[:], 0, Bpsf[0], [Brows32], [BcT])
    S.op("act", lambda h: h.activation(out=sil[:], in_=cT[:], func=AF.Silu), reads=[BcT], writes=[Bsil])
    for t in range(8):
        S.op("pe", lambda h, t=t: h.transpose(out=psf[1 + (t % 2)][:, 0:128], in_=b1rows[:, t, :], identity=ident[:]),
             reads=[Bb1rows, Bc], writes=[Bpsf[1 + (t % 2)]])
        S.op("act", lambda h, t=t: h.copy(out=b1T[:, t * 128:(t + 1) * 128], in_=psf[1 + (t % 2)][:, 0:128]),
             reads=[Bpsf[1 + (t % 2)]], writes=[Bb1T])
    b1T3 = b1T[:, :].rearrange("p (e f) -> p e f", e=NE)
    S.op("dve", lambda h: h.tensor_scalar(out=b1T3[:, :, 16:32], in0=b1T3[:, :, 16:32], scalar1=1.0, scalar2=None, op0=ALU.add), reads=[Bb1T], writes=[Bb1T])
    S.op("dve", lambda h: h.tensor_copy(out=sil2[:, :, 0], in_=sil[:, 0:16]), reads=[Bsil], writes=[Bsil2])
    S.op("dve", lambda h: h.tensor_copy(out=sil2[:, :, 1], in_=sil[:, 16:32]), reads=[Bsil], writes=[Bsil2])
    for j in range(8):
        ada_piece(j)
    ada_next = [8]
    S.barrier()
    AR.reset(m0)
    if stage <= 0:
        while ada_next[0] < 24:
            ada_piece(ada_next[0])
            ada_next[0] += 1
        if debug:
            d_mod = dbg_out("mod", [2, 6 * D])
            mld = AR.alloc("mld", [2, 6 * D], F32)
            Bmld = Buf("mld")
            S.dma("sp", S.chan("dbgm1"), lambda h: h.dma_start(out=mld[:], in_=mod_s), reads=[Bmod_s], writes=[Bmld])
            final_ops.append(S.dma("sp", S.chan("dbgmod"), lambda h: h.dma_start(out=d_mod, in_=mld[:]), reads=[Bmld]))
        return finish(nc, S, final_ops), dbg

    def load_mod_bcast(dst, row, g, chan, Bdst):
        return S.dma("sp", chan, lambda h: h.dma_start(out=dst[:], in_=mod_s[row:row + 1, g * D:(g + 1) * D].partition_broadcast(128)),
                     reads=[Bmod_s], writes=[Bdst])

    def load_row_bcast(dst, row_ap, chan, Bdst, cont=False):
        return S.dma("sp", chan, lambda h: h.dma_start(out=dst[:], in_=row_ap.partition_broadcast(128)), writes=[Bdst], cont=cont)

    A1 = AR.alloc("A1", [128, D], F32)
    S1 = AR.alloc("S1", [128, D], F32)
    A1c = AR.alloc("A1c", [128, D], F32)
    S1c = AR.alloc("S1c", [128, D], F32)
    wtmp = AR.alloc("wtmp", [128, D], F32)
    xb = [AR.alloc(f"xb{i}", [128, D], F32) for i in range(2)]
    t32s = [AR.alloc("t32_0", [128, D], F32)] * 2
    Bt32s = [Buf("t32_0")] * 2
    hb = [AR.alloc(f"hb{i}", [128, D], BF16) for i in range(2)]
    junkb = AR.alloc("junkb", [128, D], BF16)
    stat = AR.alloc("stat", [128, 8], F32)
    BA1, BS1, BA1c, BS1c, Bwtmp, Bjunk, Bstat = (Buf(n) for n in ["A1", "S1", "A1c", "S1c", "wtmp", "junk", "stat"])
    Bxb = [Buf("xb0"), Buf("xb1")]
    Bhb = [Buf("hb0"), Buf("hb1")]
    ch_x = [S.chan("x0"), S.chan("x1")]
    ch_v = S.chan("vecs")

    load_row_bcast(wtmp, pmw_d, ch_v, Bwtmp)
    load_mod_bcast(A1, 0, 1, ch_v, BA1)
    load_mod_bcast(S1, 0, 0, ch_v, BS1)
    load_mod_bcast(A1c, 1, 1, ch_v, BA1c)
    load_mod_bcast(S1c, 1, 0, ch_v, BS1c)
    S.op("dve", lambda h: h.scalar_tensor_tensor(out=A1[:], in0=A1[:], scalar=1.0, in1=wtmp[:], op0=ALU.add, op1=ALU.mult),
         reads=[BA1, Bwtmp], writes=[BA1])
    S.op("dve", lambda h: h.scalar_tensor_tensor(out=A1c[:], in0=A1c[:], scalar=1.0, in1=wtmp[:], op0=ALU.add, op1=ALU.mult),
         reads=[BA1c, Bwtmp], writes=[BA1c])

    def norm_mod_tile(src_ap_dram, xbuf, Bx, chan, Avec, BAv, Svec, BSv, hbuf, Bh, sidx):
        t32 = t32s[sidx % 2]
        Bt32 = Bt32s[sidx % 2]
        S.dma("sp", chan, lambda h: h.dma_start(out=xbuf[:], in_=src_ap_dram), writes=[Bx])
        S.op("act", lambda h: h.activation(out=junkb[:], in_=xbuf[:], func=AF.Square, accum_out=stat[:, sidx:sidx + 1]),
             reads=[Bx], writes=[Bjunk, Bstat])
        S.op("act", lambda h: h.activation(out=stat[:, sidx:sidx + 1], in_=stat[:, sidx:sidx + 1], func=AF.Sqrt, scale=1.0 / D, bias=EPS),
             reads=[Bstat], writes=[Bstat])
        S.op("dve", lambda h: h.reciprocal(out=stat[:, sidx:sidx + 1], in_=stat[:, sidx:sidx + 1]), reads=[Bstat], writes=[Bstat])
        S.op("dve", lambda h: h.scalar_tensor_tensor(out=t32[:], in0=xbuf[:], scalar=stat[:, sidx:sidx + 1], in1=Avec[:],
                                                     op0=ALU.mult, op1=ALU.mult), reads=[Bx, Bstat, BAv], writes=[Bt32])
        S.op("dve", lambda h: h.tensor_tensor(out=hbuf[:], in0=t32[:], in1=Svec[:], op=ALU.add), reads=[Bt32, BSv], writes=[Bh])

    def transpose_tile_bf16(hbuf, Bh, dstT, col0, Bdst):
        for half in range(2):
            pb = half
            for q in range(8):
                kc = half * 8 + q
                S.op("pe", lambda h, kc=kc, q=q, pb=pb: h.transpose(out=psb[pb][:, q * 128:(q + 1) * 128], in_=hbuf[:, kc * 128:(kc + 1) * 128],
                                                                       identity=identb[:]),
                     reads=[Bh, Bc], writes=[Bpsb[pb]])
            eng = "act" if half == 0 else "dve"
            if eng == "act":
                S.op("act", lambda h, half=half, pb=pb: h.copy(out=dstT[:, half * 8:(half + 1) * 8, col0:col0 + 128],
                                                                 in_=psb[pb][:, :].rearrange("p (q t) -> p q t", q=8)),
                     reads=[Bpsb[pb]], writes=[Bdst])
            else:
                S.op("dve", lambda h, half=half, pb=pb: h.tensor_copy(out=dstT[:, half * 8:(half + 1) * 8, col0:col0 + 128],
                                                                        in_=psb[pb][:, :].rearrange("p (q t) -> p q t", q=8)),
                     reads=[Bpsb[pb]], writes=[Bdst])

    for i in range(2):
        bi = i % 2
        norm_mod_tile(ctx_d[i * 128:(i + 1) * 128, :], xb[bi], Bxb[bi], ch_x[bi], A1c, BA1c, S1c, BS1c, hb[bi], Bhb[bi], i % 8)
        transpose_tile_bf16(hb[bi], Bhb[bi], hcT, i * 128, BhcT)
    for i in range(NT):
        bi = i % 2
        if ada_next[0] < 24:
            ada_piece(ada_next[0])
            ada_next[0] += 1
        norm_mod_tile(x_d[i * 128:(i + 1) * 128, :], xb[bi], Bxb[bi], ch_x[bi], A1, BA1, S1, BS1, hb[bi], Bhb[bi], i % 8)
        transpose_tile_bf16(hb[bi], Bhb[bi], hT, i * 128, BhT[i])
    while ada_next[0] < 24:
        ada_piece(ada_next[0])
        ada_next[0] += 1
    if debug:
        d_hT = dbg_out("hT", [128, KC, SEQ], BF16)
        final_ops.append(S.dma("sp", S.chan("dbghT"), lambda h: h.dma_start(out=d_hT, in_=hT[:]), reads=BhT))
    S.barrier()
    AR.reset(m1)
    if stage <= 1:
        return finish(nc, S, final_ops), dbg

    ByT_s = Buf("yT_s")
    win_v = win_d.rearrange("(k p) n -> p k n", p=128)

    m2 = AR.mark()
    decb = AR.alloc("decb", [128, 16], F32)
    Mh = AR.alloc("Mh", [128, H, 128], F32)
    dcol = AR.alloc("dcol", [128, H, 6], F32)
    wctx = AR.alloc("wctx", [128, H, 4], F32)
    Bdecb, BMh, Bdcol, Bwctx = Buf("decb"), Buf("Mh"), Buf("dcol"), Buf("wctx")
    tA = AR.alloc("tA", [128, 128], F32)
    tB = AR.alloc("tB", [128, 128], F32)
    tC = AR.alloc("tC", [128, 128], F32)
    tD = AR.alloc("tD", [128, 128], F32)
    cols = AR.alloc("cols", [128, 8], F32)
    BtA, BtB, BtC, BtD, Bcols = Buf("tA"), Buf("tB"), Buf("tC"), Buf("tD"), Buf("cols")
    S.dma("sp", ch_v, lambda h: h.dma_start(out=decb[:], in_=dec_d.partition_broadcast(128)), writes=[Bdecb])
    S.op("act", lambda h: h.activation(out=decb[:], in_=decb[:], func=AF.Exp, scale=-1.0), reads=[Bdecb], writes=[Bdecb])
    S.op("act", lambda h: h.activation(out=decb[:], in_=decb[:], func=AF.Ln, bias=1.0), reads=[Bdecb], writes=[Bdecb])
    S.op("dve", lambda h: h.tensor_scalar(out=decb[:], in0=decb[:], scalar1=-1.0, scalar2=None, op0=ALU.mult), reads=[Bdecb], writes=[Bdecb])
    S.op("dve", lambda h: h.tensor_scalar(out=tA[:], in0=iorow[:], scalar1=iocol[:, 0:1], scalar2=0.0, op0=ALU.subtract, op1=ALU.max),
         reads=[Bc], writes=[BtA])
    S.op("dve", lambda h: h.tensor_scalar(out=tB[:], in0=iorow[:], scalar1=iocol[:, 0:1], scalar2=-1.0, op0=ALU.subtract, op1=ALU.mult),
         reads=[Bc], writes=[BtB])
    S.op("dve", lambda h: h.tensor_scalar(out=tB[:], in0=tB[:], scalar1=0.0, scalar2=None, op0=ALU.max), reads=[BtB], writes=[BtB])
    S.op("dve", lambda h: h.tensor_scalar(out=tC[:], in0=iorow[:], scalar1=iocol[:, 0:1], scalar2=None, op0=ALU.is_ge), reads=[Bc], writes=[BtC])
    S.op("dve", lambda h: h.tensor_scalar(out=tD[:], in0=iorow[:], scalar1=iocol[:, 0:1], scalar2=None, op0=ALU.is_le), reads=[Bc], writes=[BtD])
    for ci, (mul, add) in enumerate([(1.0, 1.0), (-1.0, 128.0), (-1.0, 127.0), (1.0, 0.0), (0.0, 128.0), (-1.0, 255.0), (-1.0, 127.0), (1.0, 128.0)]):
        S.op("dve", lambda h, ci=ci, mul=mul, add=add: h.tensor_scalar(out=cols[:, ci:ci + 1], in0=iocol[:, 0:1], scalar1=mul, scalar2=add,
                                                                      op0=ALU.mult, op1=ALU.add), reads=[Bc], writes=[Bcols])
    ex1 = AR.alloc("ex1", [128, 128], F32)
    ex2 = AR.alloc("ex2", [128, 128], F32)
    Bex1, Bex2 = Buf("ex1"), Buf("ex2")
    for hh in range(H):
        lf = decb[:, hh:hh + 1]
        lb = decb[:, 8 + hh:9 + hh]
        S.op("act", lambda h, lf=lf: h.activation(out=ex1[:], in_=tA[:], func=AF.Exp, scale=lf), reads=[BtA, Bdecb], writes=[Bex1])
        S.op("act", lambda h, lb=lb: h.activation(out=ex2[:], in_=tB[:], func=AF.Exp, scale=lb), reads=[BtB, Bdecb], writes=[Bex2])
        S.op("dve", lambda h: h.tensor_tensor(out=ex1[:], in0=ex1[:], in1=tC[:], op=ALU.mult), reads=[Bex1, BtC], writes=[Bex1])
        S.op("dve", lambda h: h.tensor_tensor(out=ex2[:], in0=ex2[:], in1=tD[:], op=ALU.mult), reads=[Bex2, BtD], writes=[Bex2])
        S.op("dve", lambda h, hh=hh: h.tensor_tensor(out=Mh[:, hh, :], in0=ex1[:], in1=ex2[:], op=ALU.add), reads=[Bex1, Bex2], writes=[BMh])
        for (dst, ci, lg) in [(0, 0, lf), (1, 1, lb), (2, 2, lf), (3, 3, lb), (4, 4, lf), (5, 4, lb)]:
            S.op("act", lambda h, hh=hh, dst=dst, ci=ci, lg=lg: h.activation(out=dcol[:, hh, dst:dst + 1], in_=cols[:, ci:ci + 1], func=AF.Exp, scale=lg),
                 reads=[Bcols, Bdecb], writes=[Bdcol])
        for (dst, ci, lg) in [(0, 5, lf), (1, 6, lf), (2, 3, lb), (3, 7, lb)]:
            S.op("act", lambda h, hh=hh, dst=dst, ci=ci, lg=lg: h.activation(out=wctx[:, hh, dst:dst + 1], in_=cols[:, ci:ci + 1], func=AF.Exp, scale=lg),
                 reads=[Bcols, Bdecb], writes=[Bwctx])

    cosL = AR.alloc("cosL", [128, NT, 64], F32)
    sinL = AR.alloc("sinL", [128, NT, 64], F32)
    cosT = cosL[:, :, :].unsqueeze(2).to_broadcast([128, NT, 2, 64])
    sinT = sinL[:, :, :].unsqueeze(2).to_broadcast([128, NT, 2, 64])
    cosC = AR.alloc("cosC", [128, 2, 64], F32)
    sinC = AR.alloc("sinC", [128, 2, 64], F32)
    Brope = Buf("rope")
    cos_lat = cos_d[NCTX:NCTX + SEQ, :].rearrange("(t p) f -> p t f", p=128)
    sin_lat = sin_d[NCTX:NCTX + SEQ, :].rearrange("(t p) f -> p t f", p=128)
    S.dma("sp", ch_v, lambda h: h.dma_start(out=cosL[:], in_=cos_lat), writes=[Brope])
    S.dma("sp", ch_v, lambda h: h.dma_start(out=sinL[:], in_=sin_lat), writes=[Brope], cont=True)
    S.dma("sp", ch_v, lambda h: h.dma_start(out=cosC[:], in_=cos_d[0:NCTX, :].rearrange("(t p) f -> p t f", p=128)), writes=[Brope], cont=True)
    S.dma("sp", ch_v, lambda h: h.dma_start(out=sinC[:], in_=sin_d[0:NCTX, :].rearrange("(t p) f -> p t f", p=128)), writes=[Brope], cont=True)
    gnwb = AR.alloc("gnwb", [128, DRET], F32)
    Bgnwb = Buf("gnwb")
    load_row_bcast(gnwb, gnw_d, ch_v, Bgnwb)

    R0 = AR.alloc("R0", [128, H, 2, 128], F32)
    BR0 = Buf("R0")
    m2b = AR.mark()
    wkv = [AR.alloc(f"wkv{i}", [128, KC, 256], BF16) for i in range(2)]
    Bwkv = [Buf("wkv0"), Buf("wkv1")]
    ch_wkv = [S.chan("wkv0"), S.chan("wkv1")]
    kc32 = AR.alloc("kc32", [128, 2, 128], F32)
    kcr = AR.alloc("kcr", [128, 2, 128], BF16)
    vwf = AR.alloc("vwf", [128, 2, 128], BF16)
    vwb = AR.alloc("vwb", [128, 2, 128], BF16)
    rt1 = AR.alloc("rt1", [128, 2, 64], F32)
    rt2 = AR.alloc("rt2", [128, 2, 64], F32)
    Bkc32, Bkcr, Bvwf, Bvwb, Brt1, Brt2 = (Buf(n) for n in ["kc32", "kcr", "vwf", "vwb", "rt1", "rt2"])
    for hh in range(H):
        bi = hh % 2
        S.dma("pool", ch_wkv[bi], lambda h, hh=hh, bi=bi: h.dma_start(out=wkv[bi][:, :, 0:128], in_=win_v[:, :, DRET + hh * 128:DRET + (hh + 1) * 128]),
              writes=[Bwkv[bi]])
        S.dma("pool", ch_wkv[bi], lambda h, hh=hh, bi=bi: h.dma_start(out=wkv[bi][:, :, 128:256], in_=win_v[:, :, 2 * DRET + hh * 128:2 * DRET + (hh + 1) * 128]),
              writes=[Bwkv[bi]], cont=True)
        for t in range(2):
            bank = t
            for kc in range(KC):
                S.op("pe", lambda h, kc=kc, t=t, bi=bi, bank=bank: h.matmul(psf[bank][:, 0:256], lhsT=hcT[:, kc, t * 128:(t + 1) * 128], rhs=wkv[bi][:, kc, :],
                                                                             start=(kc == 0), stop=(kc == KC - 1)),
                     reads=[BhcT, Bwkv[bi]], writes=[Bpsf[bank]])
            S.op("act", lambda h, t=t, bank=bank: h.copy(out=kc32[:, t, :], in_=psf[bank][:, 0:128]), reads=[Bpsf[bank]], writes=[Bkc32])
            S.op("dve", lambda h, t=t, bank=bank, hh=hh: h.tensor_scalar(out=vwf[:, t, :], in0=psf[bank][:, 128:256], scalar1=wctx[:, hh, t:t + 1], scalar2=None, op0=ALU.mult),
                 reads=[Bpsf[bank], Bwctx], writes=[Bvwf])
            S.op("dve", lambda h, t=t, bank=bank, hh=hh: h.tensor_scalar(out=vwb[:, t, :], in0=psf[bank][:, 128:256], scalar1=wctx[:, hh, 2 + t:3 + t], scalar2=None, op0=ALU.mult),
                 reads=[Bpsf[bank], Bwctx], writes=[Bvwb])
        k1 = kc32[:, :, 0:64]
        k2 = kc32[:, :, 64:128]
        S.op("dve", lambda h: h.tensor_tensor(out=rt1[:], in0=k1, in1=cosC[:], op=ALU.mult), reads=[Bkc32, Brope], writes=[Brt1])
        S.op("pool", lambda h: h.tensor_tensor(out=rt2[:], in0=k2, in1=sinC[:], op=ALU.mult), reads=[Bkc32, Brope], writes=[Brt2])
        S.op("dve", lambda h: h.tensor_tensor(out=kcr[:, :, 0:64], in0=rt1[:], in1=rt2[:], op=ALU.subtract), reads=[Brt1, Brt2], writes=[Bkcr])
        S.op("dve", lambda h: h.tensor_tensor(out=rt1[:], in0=k1, in1=sinC[:], op=ALU.mult), reads=[Bkc32, Brope, Bkcr], writes=[Brt1])
        S.op("pool", lambda h: h.tensor_tensor(out=rt2[:], in0=k2, in1=cosC[:], op=ALU.mult), reads=[Bkc32, Brope, Bkcr], writes=[Brt2])
        S.op("dve", lambda h: h.tensor_tensor(out=kcr[:, :, 64:128], in0=rt1[:], in1=rt2[:], op=ALU.add), reads=[Brt1, Brt2], writes=[Bkcr])
        for di, vw, Bvw in [(0, vwf, Bvwf), (1, vwb, Bvwb)]:
            bank = 2 + di
            for t in range(2):
                S.op("pe", lambda h, t=t, bank=bank, vw=vw: h.matmul(psf[bank][:, 0:128], lhsT=kcr[:, t, :], rhs=vw[:, t, :], start=(t == 0), stop=(t == 1)),
                     reads=[Bkcr, Bvw], writes=[Bpsf[bank]])
            S.op("act", lambda h, hh=hh, di=di, bank=bank: h.copy(out=R0[:, hh, di, :], in_=psf[bank][:, 0:128]), reads=[Bpsf[bank]], writes=[BR0])
    if debug:
        d_R0 = dbg_out("R0", [128, H, 2, 128])
        final_ops.append(S.dma("sp", S.chan("dbgR0"), lambda h: h.dma_start(out=d_R0, in_=R0[:]), reads=[BR0]))
    S.barrier()
    AR.reset(m2b)
    if stage <= 2:
        return finish(nc, S, final_ops), dbg

    m2c = AR.mark()
    wq_off = AR.mark()
    wq1 = AR.alloc("wq", [128, KC, 512], BF16)
    wq = [wq1, wq1]
    Bwq1 = Buf("wq")
    Bwq = [Bwq1, Bwq1]
    ch_wq1 = S.chan("wq")
    ch_wq = [ch_wq1, ch_wq1]
    qT = AR.alloc_at("qT", [128, SEQ], BF16, wq_off)
    qdfT = AR.alloc_at("qdfT", [128, SEQ], BF16, wq_off + 4096)
    qdbT = AR.alloc_at("qdbT", [128, SEQ], BF16, wq_off + 8192)
    kT = AR.alloc_at("kT", [128, SEQ], BF16, wq_off + 12288)
    BqT = BqdfT = BqdbT = BkT = Bwq1
    qk_off = AR.mark()
    qk32 = AR.alloc("qk32", [128, NT, 2, 2, 64], F32)
    Bqk32 = Buf("qk32")
    o32 = AR.alloc_at("o32", [128, NT, 128], F32, qk_off)
    ytok = AR.alloc_at("ytok", [128, NT, 128], BF16, qk_off + 8192)
    yTh = AR.alloc_at("yTh", [128, SEQ], BF16, qk_off + 12288)
    Bo32 = Bytok = ByTh = Bqk32
    ta_off = AR.mark()
    ta = AR.alloc("ta", [128, NT, 2, 64], F32)
    Bta = Buf("ta")
    Sfb = AR.alloc_at("Sfb", [128, NT, 128], BF16, ta_off)
    Sbb = AR.alloc_at("Sbb", [128, NT, 128], BF16, ta_off + 4096)
    BSfb = BSbb = Bta
    tb_off = AR.mark()
    tb = AR.alloc("tb", [128, NT, 2, 64], F32)
    Sf32 = AR.alloc_at("Sf32", [128, NT, 128], F32, tb_off)
    rotb_off = AR.mark()
    rotb = AR.alloc("rotb", [128, NT, 2, 2, 64], BF16)
    Sb32 = AR.alloc_at("Sb32", [128, NT, 128], F32, rotb_off)
    qdf = AR.alloc("qdf", [128, NT, 2, 64], BF16)
    qdb = AR.alloc("qdb", [128, NT, 2, 64], BF16)
    kdf = AR.alloc("kdf", [128, NT, 2, 64], BF16)
    kdb = AR.alloc("kdb", [128, NT, 2, 64], BF16)
    vtok = AR.alloc("vtok", [128, NT, 128], BF16)
    sg = AR.alloc_at("sg", [128, NT, 128], F32, hcT_off)
    Rf = AR.alloc("Rf", [128, 128], F32)
    Rb = AR.alloc("Rb", [128, 128], F32)
    SM = [AR.alloc(f"SM{i}", [128, 128], BF16) for i in range(2)]
    bnst = AR.alloc("bnst", [128, NT, 6], F32)
    mv = AR.alloc("mv", [128, NT, 2], F32)
    rstd = AR.alloc("rstd", [128, NT], F32)
    (Btb, Brotb, Bqdf, Bqdb, Bkdf, Bkdb, Bvtok, Bsg, BRf, BRb, Bbnst, Bmv, Brstd) = (
        Buf(n) for n in ["tb", "rotb", "qdf", "qdb", "kdf", "kdb", "vtok", "sg", "Rf", "Rb", "bnst", "mv", "rstd"])
    BSM = [Buf("SM0"), Buf("SM1")]
    ch_yT = S.chan("yTout")

    for hh in range(H):
        bi = hh % 2
        for seg in range(4):
            S.dma("pool", ch_wq[bi], lambda h, hh=hh, bi=bi, seg=seg: h.dma_start(out=wq[bi][:, :, seg * 128:(seg + 1) * 128],
                                                                                    in_=win_v[:, :, seg * DRET + hh * 128:seg * DRET + (hh + 1) * 128]),
                  writes=[Bwq[bi]], cont=(seg > 0))
        for i in range(NT):
            bank = i % 4
            for kc in range(KC):
                S.op("pe", lambda h, kc=kc, i=i, bi=bi, bank=bank: h.matmul(psf[bank][:, :], lhsT=hT[:, kc, i * 128:(i + 1) * 128], rhs=wq[bi][:, kc, :],
                                                                             start=(kc == 0), stop=(kc == KC - 1)),
                     reads=[BhT[i], Bwq[bi]], writes=[Bpsf[bank]])
            S.op("act", lambda h, i=i, bank=bank: h.copy(out=qk32[:, i, :, :, :], in_=psf[bank][:, 0:256].rearrange("p (a b c) -> p a b c", a=2, b=2)),
                 reads=[Bpsf[bank]], writes=[Bqk32])
            S.op("act", lambda h, i=i, bank=bank: h.copy(out=vtok[:, i, :], in_=psf[bank][:, 256:384]), reads=[Bpsf[bank]], writes=[Bvtok])
            S.op("act", lambda h, i=i, bank=bank: h.activation(out=sg[:, i, :], in_=psf[bank][:, 384:512], func=AF.Silu), reads=[Bpsf[bank]], writes=[Bsg])
        X1 = qk32[:, :, :, 0, :]
        X2 = qk32[:, :, :, 1, :]
        S.op("dve", lambda h: h.tensor_tensor(out=ta[:], in0=X1, in1=cosT, op=ALU.mult), reads=[Bqk32, Brope], writes=[Bta])
        S.op("pool", lambda h: h.tensor_tensor(out=tb[:], in0=X2, in1=sinT, op=ALU.mult), reads=[Bqk32, Brope], writes=[Btb])
        S.op("dve", lambda h: h.tensor_tensor(out=ta[:], in0=ta[:], in1=tb[:], op=ALU.subtract), reads=[Bta, Btb], writes=[Bta])
        S.op("pool", lambda h: h.tensor_tensor(out=tb[:], in0=X1, in1=sinT, op=ALU.mult), reads=[Bqk32, Brope, Bta], writes=[Btb])
        S.op("dve", lambda h: h.tensor_tensor(out=X1, in0=X2, in1=cosT, op=ALU.mult), reads=[Bqk32, Brope, Btb], writes=[Bqk32])
        S.op("dve", lambda h: h.tensor_tensor(out=tb[:], in0=tb[:], in1=X1, op=ALU.add), reads=[Btb, Bqk32], writes=[Btb])
        S.op("act", lambda h: h.mul(out=rotb[:, :, 0, 0, :], in_=ta[:, :, 0, :], mul=QSCALE), reads=[Bta], writes=[Brotb])
        S.op("act", lambda h: h.copy(out=rotb[:, :, 1, 0, :], in_=ta[:, :, 1, :]), reads=[Bta], writes=[Brotb])
        S.op("act", lambda h: h.mul(out=rotb[:, :, 0, 1, :], in_=tb[:, :, 0, :], mul=QSCALE), reads=[Btb], writes=[Brotb])
        S.op("act", lambda h: h.copy(out=rotb[:, :, 1, 1, :], in_=tb[:, :, 1, :]), reads=[Btb], writes=[Brotb])
        for (dst, Bd, src_i, col, sc) in [(qdf, Bqdf, 0, 0, QSCALE), (qdb, Bqdb, 0, 1, QSCALE), (kdf, Bkdf, 1, 2, 1.0), (kdb, Bkdb, 1, 3, 1.0)]:
            S.op("dve", lambda h, dst=dst, src_i=src_i, col=col, hh=hh, sc=sc: h.tensor_scalar(out=dst[:, :, 0, :], in0=ta[:, :, src_i, :], scalar1=dcol[:, hh, col:col + 1],
                                                                                              scalar2=sc, op0=ALU.mult, op1=ALU.mult), reads=[Bta, Bdcol], writes=[Bd])
            S.op("pool", lambda h, dst=dst, src_i=src_i, col=col, hh=hh, sc=sc: h.tensor_scalar(out=dst[:, :, 1, :], in0=tb[:, :, src_i, :], scalar1=dcol[:, hh, col:col + 1],
                                                                                               scalar2=sc, op0=ALU.mult, op1=ALU.mult), reads=[Btb, Bdcol], writes=[Bd])
        srcs = [(lambda i: rotb[:, i, 0, :, :].rearrange("p a b -> p (a b)"), Brotb, qT, BqT),
                (lambda i: qdf[:, i, :, :].rearrange("p a b -> p (a b)"), Bqdf, qdfT, BqdfT),
                (lambda i: qdb[:, i, :, :].rearrange("p a b -> p (a b)"), Bqdb, qdbT, BqdbT),
                (lambda i: rotb[:, i, 1, :, :].rearrange("p a b -> p (a b)"), Brotb, kT, BkT)]
        cnt = 0
        for (srcf, Bsrc, dstT, BdstT) in srcs:
            for half in range(2):
                pb = cnt % 2
                cnt += 1
                for q in range(8):
                    i = half * 8 + q
                    S.op("pe", lambda h, i=i, q=q, pb=pb, srcf=srcf: h.transpose(out=psb[pb][:, q * 128:(q + 1) * 128], in_=srcf(i), identity=identb[:]),
                         reads=[Bsrc, Bc], writes=[Bpsb[pb]])
                if pb == 0:
                    S.op("act", lambda h, half=half, pb=pb, dstT=dstT: h.copy(out=dstT[:, half * 1024:(half + 1) * 1024], in_=psb[pb][:, :]),
                         reads=[Bpsb[pb]], writes=[BdstT])
                else:
                    S.op("dve", lambda h, half=half, pb=pb, dstT=dstT: h.tensor_copy(out=dstT[:, half * 1024:(half + 1) * 1024], in_=psb[pb][:, :]),
                         reads=[Bpsb[pb]], writes=[BdstT])
        S.op("act", lambda h, hh=hh: h.copy(out=Sf32[:, 0, :], in_=R0[:, hh, 0, :]), reads=[BR0], writes=[Btb])
        S.op("act", lambda h, hh=hh: h.copy(out=Sb32[:, NT - 1, :], in_=R0[:, hh, 1, :]), reads=[BR0], writes=[Brotb])
        fbanks = [0, 1, 4]
        bbanks = [2, 3, 5]
        for step in range(NT - 1):
            i_f = step
            i_b = NT - 1 - step
            bf_ = fbanks[step % 3]
            bb_ = bbanks[step % 3]
            S.op("pe", lambda h, i=i_f, bf_=bf_: h.matmul(psf[bf_][:, 0:128], lhsT=kdf[:, i, :, :].rearrange("p a b -> p (a b)"), rhs=vtok[:, i, :], start=True, stop=True),
                 reads=[Bkdf, Bvtok], writes=[Bpsf[bf_]])
            S.op("act", lambda h, i=i_f, bf_=bf_: h.copy(out=Sf32[:, i + 1, :], in_=psf[bf_][:, 0:128]), reads=[Bpsf[bf_]], writes=[Btb])
            S.op("pe", lambda h, i=i_b, bb_=bb_: h.matmul(psf[bb_][:, 0:128], lhsT=kdb[:, i, :, :].rearrange("p a b -> p (a b)"), rhs=vtok[:, i, :], start=True, stop=True),
                 reads=[Bkdb, Bvtok], writes=[Bpsf[bb_]])
            S.op("act", lambda h, i=i_b, bb_=bb_: h.copy(out=Sb32[:, i - 1, :], in_=psf[bb_][:, 0:128]), reads=[Bpsf[bb_]], writes=[Brotb])
        for step in range(NT - 1):
            i_f = step
            i_b = NT - 1 - step
            S.op("dve", lambda h, hh=hh, i=i_f: h.scalar_tensor_tensor(out=Sf32[:, i + 1, :], in0=Sf32[:, i, :], scalar=dcol[:, hh, 4:5], in1=Sf32[:, i + 1, :], op0=ALU.mult, op1=ALU.add),
                 reads=[Btb, Bdcol], writes=[Btb])
            S.op("dve", lambda h, hh=hh, i=i_b: h.scalar_tensor_tensor(out=Sb32[:, i - 1, :], in0=Sb32[:, i, :], scalar=dcol[:, hh, 5:6], in1=Sb32[:, i - 1, :], op0=ALU.mult, op1=ALU.add),
                 reads=[Brotb, Bdcol], writes=[Brotb])
        S.op("act", lambda h: h.copy(out=Sfb[:], in_=Sf32[:]), reads=[Btb], writes=[BSfb])
        S.op("act", lambda h: h.copy(out=Sbb[:], in_=Sb32[:]), reads=[Brotb], writes=[BSbb])
        for i in range(NT):
            sb_ = i % 2
            bs = i % 2
            bo = 2 + (i % 2)
            cs = slice(i * 128, (i + 1) * 128)
            S.op("pe", lambda h, cs=cs, bs=bs: h.matmul(psf[bs][:, 0:128], lhsT=kT[:, cs], rhs=qT[:, cs], start=True, stop=True),
                 reads=[BkT, BqT], writes=[Bpsf[bs]])
            S.op("dve", lambda h, bs=bs, sb_=sb_, hh=hh: h.tensor_tensor(out=SM[sb_][:], in0=psf[bs][:, 0:128], in1=Mh[:, hh, :], op=ALU.mult),
                 reads=[Bpsf[bs], BMh], writes=[BSM[sb_]])
            S.op("pe", lambda h, i=i, sb_=sb_, bo=bo: h.matmul(psf[bo][:, 0:128], lhsT=SM[sb_][:], rhs=vtok[:, i, :], start=True, stop=False),
                 reads=[BSM[sb_], Bvtok], writes=[Bpsf[bo]])
            S.op("pe", lambda h, i=i, cs=cs, bo=bo: h.matmul(psf[bo][:, 0:128], lhsT=qdfT[:, cs], rhs=Sfb[:, i, :], start=False, stop=False),
                 reads=[BqdfT, BSfb], writes=[Bpsf[bo]])
            S.op("pe", lambda h, i=i, cs=cs, bo=bo: h.matmul(psf[bo][:, 0:128], lhsT=qdbT[:, cs], rhs=Sbb[:, i, :], start=False, stop=True),
                 reads=[BqdbT, BSbb], writes=[Bpsf[bo]])
            S.op("act", lambda h, i=i, bo=bo: h.copy(out=o32[:, i, :], in_=psf[bo][:, 0:128]), reads=[Bpsf[bo]], writes=[Bo32])
            S.op("dve", lambda h, i=i: h.bn_stats(out=bnst[:, i, :], in_=o32[:, i, :]), reads=[Bo32], writes=[Bbnst])
            S.op("dve", lambda h, i=i: h.bn_aggr(out=mv[:, i, :], in_=bnst[:, i, :]), reads=[Bbnst], writes=[Bmv])
        S.op("act", lambda h: h.activation(out=rstd[:], in_=mv[:, :, 1], func=AF.Sqrt, bias=GN_EPS), reads=[Bmv], writes=[Brstd])
        S.op("dve", lambda h: h.reciprocal(out=rstd[:], in_=rstd[:]), reads=[Brstd], writes=[Brstd])
        S.op("pool", lambda h, hh=hh: h.tensor_tensor(out=sg[:], in0=sg[:], in1=gnwb[:, hh * 128:(hh + 1) * 128].unsqueeze(1).to_broadcast([128, NT, 128]), op=ALU.mult),
             reads=[Bsg, Bgnwb], writes=[Bsg])
        for i in range(NT):
            S.op("dve", lambda h, i=i: h.tensor_scalar(out=o32[:, i, :], in0=o32[:, i, :], scalar1=mv[:, i, 0:1], scalar2=rstd[:, i:i + 1], op0=ALU.subtract, op1=ALU.mult),
                 reads=[Bo32, Bmv, Brstd], writes=[Bo32])
        S.op("pool", lambda h: h.tensor_tensor(out=ytok[:], in0=o32[:], in1=sg[:], op=ALU.mult), reads=[Bo32, Bsg], writes=[Bytok])
        for half in range(2):
            pb = half
            for q in range(8):
                i = half * 8 + q
                S.op("pe", lambda h, i=i, q=q, pb=pb: h.transpose(out=psb[pb][:, q * 128:(q + 1) * 128], in_=ytok[:, i, :], identity=identb[:]),
                     reads=[Bytok, Bc], writes=[Bpsb[pb]])
            if half == 0:
                S.op("act", lambda h, half=half, pb=pb: h.copy(out=yTh[:, half * 1024:(half + 1) * 1024], in_=psb[pb][:, :]), reads=[Bpsb[pb]], writes=[ByTh])
            else:
                S.op("dve", lambda h, half=half, pb=pb: h.tensor_copy(out=yTh[:, half * 1024:(half + 1) * 1024], in_=psb[pb][:, :]), reads=[Bpsb[pb]], writes=[ByTh])
        S.dma("sp", ch_yT, lambda h, hh=hh: h.dma_start(out=yT_s[hh * 128:(hh + 1) * 128, :], in_=yTh[:]), reads=[ByTh], writes=[ByT_s])
    S.barrier()
    AR.reset(m2c)
    if stage <= 3:
        if debug:
            d_yT = dbg_out("yT", [D, SEQ], BF16)
            ld = AR.alloc("dbgld", [128, KC, SEQ], BF16)
            Bld = Buf("dbgld")
            S.dma("sp", S.chan("dbgy1"), lambda h: h.dma_start(out=ld[:], in_=yT_s.rearrange("(c p) t -> p c t", p=128)), reads=[ByT_s], writes=[Bld])
            final_ops.append(S.dma("sp", S.chan("dbgy2"), lambda h: h.dma_start(out=d_yT.rearrange("(c p) t -> p c t", p=128), in_=ld[:]), reads=[Bld]))
        return finish(nc, S, final_ops), dbg

    m2d = AR.mark()
    cvrows = AR.alloc("cvrows", [24, 128], F32)
    cvT = AR.alloc("cvT", [128, 24], F32)
    cwrows = AR.alloc("cwrows", [CW, DCONV], F32)
    cwT = AR.alloc("cwT", [128, 8, CW], F32)
    Bcvrows, BcvT, Bcwrows, BcwT = Buf("cvrows"), Buf("cvT"), Buf("cwrows"), Buf("cwT")
    S.dma("sp", ch_v, lambda h: h.dma_start(out=cvrows[:], in_=cvec_d), writes=[Bcvrows])
    S.dma("sp", ch_v, lambda h: h.dma_start(out=cwrows[:], in_=convw_d), writes=[Bcwrows], cont=True)
    transpose_rows(cvrows[:], 24, cvT[:], 0, Bpsf[0], [Bcvrows], [BcvT])
    for cc in range(8):
        S.op("pe", lambda h, cc=cc: h.transpose(out=psf[1][:, 0:CW], in_=cwrows[:, cc * 128:(cc + 1) * 128], identity=ident[0:CW, 0:CW]),
             reads=[Bcwrows, Bc], writes=[Bpsf[1]])
        S.op("act", lambda h, cc=cc: h.copy(out=cwT[:, cc, :], in_=psf[1][:, 0:CW]), reads=[Bpsf[1]], writes=[BcwT])
    wc = [AR.alloc(f"wc{i}", [128, KC, 256], BF16) for i in range(2)]
    Bwc = [Buf("wc0"), Buf("wc1")]
    ch_wc = [S.chan("wc0"), S.chan("wc1")]
    sig = AR.alloc("sig", [128, 512], F32)
    uu = AR.alloc("uu", [128, 8, 64], F32)
    cvo = AR.alloc("cvo", [128, 8, 8, 64], F32)
    cv2 = AR.alloc("cv2", [128, 8, 64], F32)
    Bcv2 = Buf("cv2")
    sq = AR.alloc("sq", [128, 512], F32)
    mean = AR.alloc("mean", [128, 512], F32)
    msq = AR.alloc("msq", [128, 512], F32)
    rsd = AR.alloc("rsd", [128, 512], F32)
    tn = AR.alloc("tn", [128, 512], F32)
    tns = [tn, AR.alloc("tn2", [128, 512], F32)]
    Btns = [Buf("tn_0"), Buf("tn_1")]
    ycv = [AR.alloc(f"ycv{i}", [128, 512], BF16) for i in range(2)]
    Bsig, Buu, Bcvo, Bsq, Bmean, Bmsq, Brsd, Btn = (Buf(n) for n in ["sig", "uu", "cvo", "sq", "mean", "msq", "rsd", "tn"])
    Bycv = [Buf("ycv0"), Buf("ycv1")]
    ch_ycv = [S.chan("ycv0"), S.chan("ycv1")]
    pcount = 0
    for tbk in range(4):
        tsl = slice(tbk * 512, (tbk + 1) * 512)
        for cc in range(8):
            bi = pcount % 2
            pcount += 1
            S.dma("pool", ch_wc[bi], lambda h, cc=cc, bi=bi: h.dma_start(out=wc[bi][:, :, 0:128], in_=win_v[:, :, 4 * DRET + cc * 128:4 * DRET + (cc + 1) * 128]),
                  writes=[Bwc[bi]])
            S.dma("pool", ch_wc[bi], lambda h, cc=cc, bi=bi: h.dma_start(out=wc[bi][:, :, 128:256],
                                                                          in_=win_v[:, :, 4 * DRET + DCONV + cc * 128:4 * DRET + DCONV + (cc + 1) * 128]),
                  writes=[Bwc[bi]], cont=True)
            ba, bb = 0 + 2 * (cc % 2), 1 + 2 * (cc % 2)
            for kc in range(KC):
                S.op("pe", lambda h, kc=kc, bi=bi, ba=ba, tsl=tsl: h.matmul(psf[ba][:, :], lhsT=wc[bi][:, kc, 0:128], rhs=hT[:, kc, tsl], start=(kc == 0), stop=(kc == KC - 1)),
                     reads=[Bwc[bi]] + BhT[tbk * 4:(tbk + 1) * 4], writes=[Bpsf[ba]])
            for kc in range(KC):
                S.op("pe", lambda h, kc=kc, bi=bi, bb=bb, tsl=tsl: h.matmul(psf[bb][:, :], lhsT=wc[bi][:, kc, 128:256], rhs=hT[:, kc, tsl], start=(kc == 0), stop=(kc == KC - 1)),
                     reads=[Bwc[bi]] + BhT[tbk * 4:(tbk + 1) * 4], writes=[Bpsf[bb]])
            S.op("act", lambda h, bb=bb: h.activation(out=sig[:], in_=psf[bb][:, :], func=AF.Sigmoid), reads=[Bpsf[bb]], writes=[Bsig])
            S.op("dve", lambda h, ba=ba: h.tensor_tensor(out=uu[:].rearrange("p a b -> p (a b)"), in0=psf[ba][:, :], in1=sig[:], op=ALU.mult),
                 reads=[Bpsf[ba], Bsig], writes=[Buu])
            acc = cvo[:, cc, :, :]
            S.op("dve", lambda h, cc=cc, acc=acc: h.tensor_scalar(out=acc, in0=uu[:], scalar1=cwT[:, cc, 15:16], scalar2=cvT[:, cc:cc + 1], op0=ALU.mult, op1=ALU.add),
                 reads=[Buu, BcwT, BcvT], writes=[Bcvo])
            S.op("dve", lambda h: h.memset(cv2[:], 0.0), writes=[Bcv2])
            taps = [k for k in range(CW) if k != 15]
            for ti, k in enumerate(taps):
                o = k - 15
                lo, hi = max(0, -o), min(64, 64 - o)
                if ti % 2 == 0:
                    S.op("dve", lambda h, cc=cc, k=k, o=o, lo=lo, hi=hi: h.scalar_tensor_tensor(out=cv2[:, :, lo:hi], in0=uu[:, :, lo + o:hi + o], scalar=cwT[:, cc, k:k + 1],
                                                                                                 in1=cv2[:, :, lo:hi], op0=ALU.mult, op1=ALU.add),
                         reads=[Buu, BcwT, Bcv2], writes=[Bcv2])
                else:
                    S.op("dve", lambda h, cc=cc, k=k, o=o, lo=lo, hi=hi: h.scalar_tensor_tensor(out=cvo[:, cc, :, lo:hi], in0=uu[:, :, lo + o:hi + o], scalar=cwT[:, cc, k:k + 1],
                                                                                                 in1=cvo[:, cc, :, lo:hi], op0=ALU.mult, op1=ALU.add),
                         reads=[Buu, BcwT, Bcvo], writes=[Bcvo])
            S.op("dve", lambda h, acc=acc: h.tensor_tensor(out=acc, in0=acc, in1=cv2[:], op=ALU.add), reads=[Bcvo, Bcv2], writes=[Bcvo])
            accf = cvo[:, cc, :, :].rearrange("p a b -> p (a b)")
            S.op("act", lambda h, accf=accf: h.activation(out=sq[:], in_=accf, func=AF.Square), reads=[Bcvo], writes=[Bsq])
            S.op("pe", lambda h, accf=accf, cc=cc: h.matmul(psf[4][:, :], lhsT=ones32[:], rhs=accf, start=(cc == 0), stop=(cc == 7)), reads=[Bcvo, Bc], writes=[Bpsf[4]])
            S.op("pe", lambda h, cc=cc: h.matmul(psf[5][:, :], lhsT=ones32[:], rhs=sq[:], start=(cc == 0), stop=(cc == 7)), reads=[Bsq, Bc], writes=[Bpsf[5]])
        S.op("act", lambda h: h.activation(out=mean[:], in_=psf[4][:, :], func=AF.Identity, scale=1.0 / DCONV), reads=[Bpsf[4]], writes=[Bmean])
        S.op("dve", lambda h: h.tensor_tensor(out=msq[:], in0=mean[:], in1=mean[:], op=ALU.mult), reads=[Bmean], writes=[Bmsq])
        S.op("dve", lambda h: h.scalar_tensor_tensor(out=rsd[:], in0=psf[5][:, :], scalar=1.0 / DCONV, in1=msq[:], op0=ALU.mult, op1=ALU.subtract),
             reads=[Bpsf[5], Bmsq], writes=[Brsd])
        S.op("act", lambda h: h.activation(out=rsd[:], in_=rsd[:], func=AF.Sqrt, bias=EPS), reads=[Brsd], writes=[Brsd])
        S.op("dve", lambda h: h.reciprocal(out=rsd[:], in_=rsd[:]), reads=[Brsd], writes=[Brsd])
        for cc in range(8):
            yb = cc % 2
            accf = cvo[:, cc, :, :].rearrange("p a b -> p (a b)")
            tnb = tns[cc % 2]
            Btnb = Btns[cc % 2]
            S.op("dve", lambda h, accf=accf, tnb=tnb: h.tensor_tensor(out=tnb[:], in0=accf, in1=mean[:], op=ALU.subtract), reads=[Bcvo, Bmean], writes=[Btnb])
            S.op("dve", lambda h, tnb=tnb: h.tensor_tensor(out=tnb[:], in0=tnb[:], in1=rsd[:], op=ALU.mult), reads=[Btnb, Brsd], writes=[Btnb])
            S.op("act", lambda h, cc=cc, yb=yb, tnb=tnb: h.activation(out=ycv[yb][:], in_=tnb[:], func=AF.Silu, scale=cvT[:, 8 + cc:9 + cc], bias=cvT[:, 16 + cc:17 + cc]),
                 reads=[Btnb, BcvT], writes=[Bycv[yb]])
            S.dma("sp", ch_ycv[yb], lambda h, cc=cc, yb=yb, tsl=tsl: h.dma_start(out=yT_s[DRET + cc * 128:DRET + (cc + 1) * 128, tsl], in_=ycv[yb][:]),
                  reads=[Bycv[yb]], writes=[ByT_s])
    S.barrier()
    AR.reset(m2)
    AR.reset(persist_mark)
    if stage <= 4:
        if debug:
            d_yT = dbg_out("yT", [D, SEQ], BF16)
            ld = AR.alloc("dbgld", [128, KC, SEQ], BF16)
            Bld = Buf("dbgld")
            S.dma("sp", S.chan("dbgy1"), lambda h: h.dma_start(out=ld[:], in_=yT_s.rearrange("(c p) t -> p c t", p=128)), reads=[ByT_s], writes=[Bld])
            final_ops.append(S.dma("sp", S.chan("dbgy2"), lambda h: h.dma_start(out=d_yT.rearrange("(c p) t -> p c t", p=128), in_=ld[:]), reads=[Bld]))
        return finish(nc, S, final_ops), dbg

    m3 = AR.mark()
    wo = AR.alloc("wo", [128, KC, D], BF16)
    Bwo = Buf("wo")
    ch_wo = S.chan("wo")
    wout_v = wout_d.rearrange("(k p) n -> p k n", p=128)
    for j in range(4):
        S.dma("pool", ch_wo, lambda h, j=j: h.dma_start(out=wo[:, :, j * 512:(j + 1) * 512], in_=wout_v[:, :, j * 512:(j + 1) * 512]), writes=[Bwo], cont=(j > 0))
    G1 = AR.alloc("G1", [128, D], F32)
    A2 = AR.alloc("A2", [128, D], F32)
    S2 = AR.alloc("S2", [128, D], F32)
    wt3 = AR.alloc("wt3", [128, D], F32)
    BG1, BA2, BS2, Bwt3 = Buf("G1"), Buf("A2"), Buf("S2"), Buf("wt3")
    load_mod_bcast(G1, 0, 2, ch_v, BG1)
    load_row_bcast(wt3, pomw_d, ch_v, Bwt3)
    S.op("dve", lambda h: h.tensor_tensor(out=G1[:], in0=G1[:], in1=wt3[:], op=ALU.mult), reads=[BG1, Bwt3], writes=[BG1])
    load_mod_bcast(A2, 0, 4, ch_v, BA2)
    load_row_bcast(wt3, pfw_d, ch_v, Bwt3)
    S.op("dve", lambda h: h.scalar_tensor_tensor(out=A2[:], in0=A2[:], scalar=1.0, in1=wt3[:], op0=ALU.add, op1=ALU.mult), reads=[BA2, Bwt3], writes=[BA2])
    load_mod_bcast(S2, 0, 3, ch_v, BS2)
    rw32 = AR.alloc("rw32", [128, KC, NE], F32)
    rbb = AR.alloc("rbb", [128, NE], F32)
    ebase = AR.alloc("ebase", [128, NE], F32)
    Brw, Brbb, Bebase = Buf("rw32"), Buf("rbb"), Buf("ebase")
    S.dma("sp", ch_v, lambda h: h.dma_start(out=rw32[:], in_=rw_d.rearrange("(k p) e -> p k e", p=128)), writes=[Brw])
    S.dma("sp", ch_v, lambda h: h.dma_start(out=rbb[:], in_=rb_d.partition_broadcast(128)), writes=[Brbb], cont=True)
    S.op("dve", lambda h: h.tensor_scalar(out=ebase[:], in0=iorow[:, 0:NE], scalar1=float(CAP), scalar2=None, op0=ALU.mult), reads=[Bc], writes=[Bebase])
    yTt = [AR.alloc(f"yTt{i}", [128, KC, 128], BF16) for i in range(2)]
    ByTt = [Buf("yTt0"), Buf("yTt1")]
    ch_yTt = [S.chan("yTt0"), S.chan("yTt1")]
    xr = [AR.alloc(f"xr{i}", [128, D], F32) for i in range(2)]
    Bxr = [Buf("xr0"), Buf("xr1")]
    ch_xr = [S.chan("xr0"), S.chan("xr1")]
    t3 = AR.alloc("t3", [128, D], F32)
    x1t = [AR.alloc(f"x1t{i}", [128, D], F32) for i in range(2)]
    h2f = AR.alloc("h2f", [128, D], F32)
    h2b = [AR.alloc(f"h2b{i}", [128, D], BF16) for i in range(2)]
    h2T = AR.alloc("h2T", [128, KC, 128], F32)
    junk3 = AR.alloc("junk3", [128, D], BF16)
    st3 = AR.alloc("st3", [128, 8], F32)
    lg = AR.alloc("lg", [128, NE], F32)
    mx8 = AR.alloc("mx8", [128, 8], F32)
    nmx = AR.alloc("nmx", [128, 1], F32)
    msk = AR.alloc("msk", [128, NE], F32)
    mskb = AR.alloc("mskb", [128, NT, NE], BF16)
    exv = AR.alloc("exv", [128, NE], F32)
    den = AR.alloc("den", [128, 1], F32)
    posC = AR.alloc("posC", [128, NE], F32)
    ovf = AR.alloc("ovf", [128, NE], F32)
    oh = AR.alloc("oh", [128, NE], F32)
    jk = AR.alloc("jk", [128, NE], F32)
    idxf = AR.alloc("idxf", [128, 4], F32)
    idl = AR.alloc("idl", [128, 4], F32)
    idn = AR.alloc("idn", [128, 4], F32)
    Bidl, Bidn = Buf("idl"), Buf("idn")
    (Bt3, Bh2f, Bh2T, Bjunk3, Bst3, Blg, Bmx8, Bnmx, Bmsk, Bmskb, Bexv, Bden, BposC, Bovf, Boh, Bjk, Bidxf) = (
        Buf(n) for n in ["t3", "h2f", "h2T", "junk3", "st3", "lg", "mx8", "nmx", "msk", "mskb", "exv", "den", "posC", "ovf", "oh", "jk", "idxf"])
    Bx1t = [Buf("x1t0"), Buf("x1t1")]
    Bh2b = [Buf("h2b0"), Buf("h2b1")]
    ch_x1 = [S.chan("x1w0"), S.chan("x1w1")]
    ch_sc = [S.chan(f"scat{i}") for i in range(2)]
    Bx1_s = [Buf(f"x1_s{i}") for i in range(NT)]
    Bhsel = Buf("hsel_s")
    yT_v = yT_s.rearrange("(c p) t -> p c t", p=128)
    mixps = [psf[0], psf[1], psf[2], psf[3]]
    for i in range(NT):
        bi = i % 2
        S.dma("sp", ch_yTt[bi], lambda h, i=i, bi=bi: h.dma_start(out=yTt[bi][:], in_=yT_v[:, :, i * 128:(i + 1) * 128]), reads=[ByT_s], writes=[ByTt[bi]])
        S.dma("sp", ch_xr[bi], lambda h, i=i, bi=bi: h.dma_start(out=xr[bi][:], in_=x_d[i * 128:(i + 1) * 128, :]), writes=[Bxr[bi]])
        for cb in range(4):
            for c in range(KC):
                S.op("pe", lambda h, c=c, cb=cb, bi=bi: h.matmul(psf[cb][:, :], lhsT=yTt[bi][:, c, :], rhs=wo[:, c, cb * 512:(cb + 1) * 512], start=(c == 0), stop=(c == KC - 1)),
                     reads=[ByTt[bi], Bwo], writes=[Bpsf[cb]])
        for cb in range(4):
            S.op("act", lambda h, cb=cb: h.activation(out=junk3[:, cb * 512:(cb + 1) * 512], in_=psf[cb][:, :], func=AF.Square, accum_out=st3[:, cb:cb + 1]),
                 reads=[Bpsf[cb]], writes=[Bjunk3, Bst3])
        S.op("dve", lambda h: h.tensor_reduce(out=st3[:, 4:5], in_=st3[:, 0:4], axis=AX.X, op=ALU.add), reads=[Bst3], writes=[Bst3])
        S.op("act", lambda h: h.activation(out=st3[:, 4:5], in_=st3[:, 4:5], func=AF.Sqrt, scale=1.0 / D, bias=EPS), reads=[Bst3], writes=[Bst3])
        S.op("dve", lambda h: h.reciprocal(out=st3[:, 4:5], in_=st3[:, 4:5]), reads=[Bst3], writes=[Bst3])
        for cb in range(4):
            S.op("dve", lambda h, cb=cb: h.scalar_tensor_tensor(out=t3[:, cb * 512:(cb + 1) * 512], in0=psf[cb][:, :], scalar=st3[:, 4:5], in1=G1[:, cb * 512:(cb + 1) * 512],
                                                                op0=ALU.mult, op1=ALU.mult), reads=[Bpsf[cb], Bst3, BG1], writes=[Bt3])
        S.op("pool", lambda h, bi=bi: h.tensor_tensor(out=x1t[bi][:], in0=t3[:], in1=xr[bi][:], op=ALU.add), reads=[Bt3, Bxr[bi]], writes=[Bx1t[bi]])
        S.dma("sp", ch_x1[bi], lambda h, i=i, bi=bi: h.dma_start(out=x1_s[i * 128:(i + 1) * 128, :], in_=x1t[bi][:]), reads=[Bx1t[bi]], writes=[Bx1_s[i]])
        S.op("act", lambda h, bi=bi: h.activation(out=junk3[:], in_=x1t[bi][:], func=AF.Square, accum_out=st3[:, 5:6]), reads=[Bx1t[bi]], writes=[Bjunk3, Bst3])
        S.op("act", lambda h: h.activation(out=st3[:, 5:6], in_=st3[:, 5:6], func=AF.Sqrt, scale=1.0 / D, bias=EPS), reads=[Bst3], writes=[Bst3])
        S.op("dve", lambda h: h.reciprocal(out=st3[:, 5:6], in_=st3[:, 5:6]), reads=[Bst3], writes=[Bst3])
        S.op("dve", lambda h, bi=bi: h.scalar_tensor_tensor(out=t3[:], in0=x1t[bi][:], scalar=st3[:, 5:6], in1=A2[:], op0=ALU.mult, op1=ALU.mult),
             reads=[Bx1t[bi], Bst3, BA2], writes=[Bt3])
        S.op("pool", lambda h: h.tensor_tensor(out=h2f[:], in0=t3[:], in1=S2[:], op=ALU.add), reads=[Bt3, BS2], writes=[Bh2f])
        S.op("act", lambda h, bi=bi: h.copy(out=h2b[bi][:], in_=h2f[:]), reads=[Bh2f], writes=[Bh2b[bi]])
        for q4 in range(4):
            bank = 4 + (q4 % 2)
            for q in range(4):
                kc = q4 * 4 + q
                S.op("pe", lambda h, kc=kc, q=q, bank=bank: h.transpose(out=psf[bank][:, q * 128:(q + 1) * 128], in_=h2f[:, kc * 128:(kc + 1) * 128], identity=ident[:]),
                     reads=[Bh2f, Bc], writes=[Bpsf[bank]])
            if q4 % 2 == 0:
                S.op("act", lambda h, q4=q4, bank=bank: h.copy(out=h2T[:, q4 * 4:(q4 + 1) * 4, :], in_=psf[bank][:, :].rearrange("p (q t) -> p q t", q=4)),
                     reads=[Bpsf[bank]], writes=[Bh2T])
            else:
                S.op("dve", lambda h, q4=q4, bank=bank: h.tensor_copy(out=h2T[:, q4 * 4:(q4 + 1) * 4, :], in_=psf[bank][:, :].rearrange("p (q t) -> p q t", q=4)),
                     reads=[Bpsf[bank]], writes=[Bh2T])
        for kc in range(KC):
            S.op("pe", lambda h, kc=kc: h.matmul(psf[4][:, 0:NE], lhsT=h2T[:, kc, :], rhs=rw32[:, kc, :], start=(kc == 0), stop=(kc == KC - 1)),
                 reads=[Bh2T, Brw], writes=[Bpsf[4]])
        S.op("dve", lambda h: h.tensor_tensor(out=lg[:], in0=psf[4][:, 0:NE], in1=rbb[:], op=ALU.add), reads=[Bpsf[4], Brbb], writes=[Blg])
        S.op("dve", lambda h: h.max(out=mx8[:], in_=lg[:]), reads=[Blg], writes=[Bmx8])
        S.op("dve", lambda h: h.tensor_scalar(out=msk[:], in0=lg[:], scalar1=mx8[:, 3:4], scalar2=None, op0=ALU.is_ge), reads=[Blg, Bmx8], writes=[Bmsk])
        S.op("dve", lambda h, i=i: h.tensor_copy(out=mskb[:, i, :], in_=msk[:]), reads=[Bmsk], writes=[Bmskb])
        S.op("dve", lambda h: h.tensor_scalar(out=nmx[:], in0=mx8[:, 0:1], scalar1=-1.0, scalar2=None, op0=ALU.mult), reads=[Bmx8], writes=[Bnmx])
        S.op("act", lambda h: h.activation(out=exv[:], in_=lg[:], func=AF.Exp, bias=nmx[:, 0:1]), reads=[Blg, Bnmx], writes=[Bexv])
        S.op("dve", lambda h: h.tensor_tensor(out=exv[:], in0=exv[:], in1=msk[:], op=ALU.mult), reads=[Bexv, Bmsk], writes=[Bexv])
        S.op("dve", lambda h: h.tensor_reduce(out=den[:], in_=exv[:], axis=AX.X, op=ALU.add), reads=[Bexv], writes=[Bden])
        S.op("dve", lambda h: h.reciprocal(out=den[:], in_=den[:]), reads=[Bden], writes=[Bden])
        S.op("pe", lambda h, i=i: h.matmul(psf[5][:, 0:NE], lhsT=trib[:], rhs=mskb[:, i, :], start=True, stop=(i == 0)), reads=[Bmskb, Bc], writes=[Bpsf[5]])
        for j in range(i):
            S.op("pe", lambda h, j=j, i=i: h.matmul(psf[5][:, 0:NE], lhsT=onesb[:], rhs=mskb[:, j, :], start=False, stop=(j == i - 1)), reads=[Bmskb, Bc], writes=[Bpsf[5]])
        S.op("dve", lambda h: h.tensor_scalar(out=ovf[:], in0=psf[5][:, 0:NE], scalar1=float(CAP) - 0.5, scalar2=None, op0=ALU.is_gt), reads=[Bpsf[5]], writes=[Bovf])
        S.op("dve", lambda h: h.tensor_tensor(out=posC[:], in0=psf[5][:, 0:NE], in1=ebase[:], op=ALU.add), reads=[Bpsf[5], Bebase], writes=[BposC])
        S.op("dve", lambda h: h.scalar_tensor_tensor(out=posC[:], in0=ovf[:], scalar=BIG, in1=posC[:], op0=ALU.mult, op1=ALU.add), reads=[Bovf, BposC], writes=[BposC])
        S.op("dve", lambda h: h.tensor_scalar(out=ovf[:], in0=ovf[:], scalar1=-1.0, scalar2=1.0, op0=ALU.mult, op1=ALU.add), reads=[Bovf], writes=[Bovf])
        S.op("dve", lambda h, i=i: h.scalar_tensor_tensor(out=gatesA[:, i, :], in0=exv[:], scalar=den[:, 0:1], in1=ovf[:], op0=ALU.mult, op1=ALU.mult),
             reads=[Bexv, Bden, Bovf], writes=[BgatesA])
        for k in range(4):
            S.op("dve", lambda h, k=k: h.tensor_scalar(out=oh[:], in0=lg[:], scalar1=mx8[:, k:k + 1], scalar2=None, op0=ALU.is_equal), reads=[Blg, Bmx8], writes=[Boh])
            S.op("dve", lambda h, k=k: h.scalar_tensor_tensor(out=jk[:], in0=oh[:], scalar=1.0, in1=posC[:], op0=ALU.mult, op1=ALU.mult, accum_out=idxf[:, k:k + 1]),
                 reads=[Boh, BposC], writes=[Bjk, Bidxf])
            S.op("dve", lambda h, k=k, i=i: h.scalar_tensor_tensor(out=jk[:], in0=oh[:], scalar=1.0, in1=gatesA[:, i, :], op0=ALU.mult, op1=ALU.mult,
                                                                 accum_out=gate4[:, i, k:k + 1]), reads=[Boh, BgatesA], writes=[Bjk, Bgate4])
        for (lst, nten, Bl) in [(idxH, NHS, Bidx[i]), (idxY, NYS, Bidx[i])]:
            for j in range(nten):
                shift = float(j * (NE // nten) * CAP)
                S.op("dve", lambda h, shift=shift: h.tensor_scalar(out=idl[:], in0=idxf[:], scalar1=shift, scalar2=None, op0=ALU.subtract), reads=[Bidxf], writes=[Bidl])
                S.op("dve", lambda h: h.tensor_scalar(out=idn[:], in0=idl[:], scalar1=0.0, scalar2=BIG, op0=ALU.is_lt, op1=ALU.mult), reads=[Bidl], writes=[Bidn])
                S.op("dve", lambda h: h.tensor_tensor(out=idl[:], in0=idl[:], in1=idn[:], op=ALU.add), reads=[Bidl, Bidn], writes=[Bidl])
                S.op("dve", lambda h, i=i, t=lst[j]: h.tensor_copy(out=t[:, i * 4:(i + 1) * 4], in_=idl[:]), reads=[Bidl], writes=[Bl])
        first = True
        for k in range(4):
            for j in range(NHS):
                S.dma("pool", ch_sc[bi], lambda h, i=i, k=k, bi=bi, j=j: h.indirect_dma_start(out=hsel_s[j], out_offset=bass.IndirectOffsetOnAxis(ap=idxH[j][:, i * 4 + k:i * 4 + k + 1], axis=0),
                                                                                           in_=h2b[bi][:], in_offset=None, bounds_check=bound_reg(h, (NE // NHS) * CAP - 1), oob_is_err=False),
                      reads=[Bh2b[bi], Bidx[i]], writes=[Bhsel], cont=(not first))
                first = False
    for j in range(NT):
        S.op("pe", lambda h, j=j: h.matmul(psf[5][:, 0:NE], lhsT=onesb[:], rhs=mskb[:, j, :], start=(j == 0), stop=(j == NT - 1)), reads=[Bmskb, Bc], writes=[Bpsf[5]])
    S.cnt_op = S.op("dve", lambda h: h.tensor_copy(out=cnt_i[:], in_=psf[5][:, 0:NE]), reads=[Bpsf[5]], writes=[Bcnt])
    S.cnt_ap = lambda e: cnt_i[0:1, e:e + 1]
    if debug:
        d_lg = dbg_out("gates", [128, NT, NE])
        final_ops.append(S.dma("sp", S.chan("dbglg"), lambda h: h.dma_start(out=d_lg, in_=gatesA[:]), reads=[BgatesA]))
        d_idx = dbg_out("idx4", [128, NT * 4], I32)
        final_ops.append(S.dma("sp", S.chan("dbgidx"), lambda h: h.dma_start(out=d_idx, in_=idxY[0][:]), reads=Bidx))
        d_cnt = dbg_out("cnt", [128, NE], I32)
        final_ops.append(S.dma("sp", S.chan("dbgcnt"), lambda h: h.dma_start(out=d_cnt, in_=cnt_i[:]), reads=[Bcnt]))
        d_g4 = dbg_out("gate4", [128, NT, 4])
        final_ops.append(S.dma("sp", S.chan("dbgg4"), lambda h: h.dma_start(out=d_g4, in_=gate4[:]), reads=[Bgate4]))
    S.barrier()
    AR.reset(m3)
    if stage <= 5:
        if debug:
            d_x1 = dbg_out("x1", [SEQ, D])
            ld = AR.alloc("dbgld", [128, NT, D], F32)
            Bld = Buf("dbgld")
            S.dma("sp", S.chan("dbgx1"), lambda h: h.dma_start(out=ld[:], in_=x1_s.rearrange("(t p) d -> p t d", p=128)), reads=Bx1_s, writes=[Bld])
            final_ops.append(S.dma("sp", S.chan("dbgx2"), lambda h: h.dma_start(out=d_x1.rearrange("(t p) d -> p t d", p=128), in_=ld[:]), reads=[Bld]))
        return finish(nc, S, final_ops), dbg

    m5 = AR.mark()
    hselT = [AR.alloc(f"hselT{i}", [128, KC, RS], BF16) for i in range(2)]
    BhselT = [Buf("hselT0"), Buf("hselT1")]
    actT = AR.alloc("actT", [128, KC, RS], BF16)
    BactT = Buf("actT")
    NW1, NW2 = 4, 3
    w1u = [AR.alloc(f"w1u{i}", [128, KC, 512], BF16) for i in range(NW1)]
    Bw1u = [Buf(f"w1u{i}") for i in range(NW1)]
    ch_w1 = [S.chan(f"w1u{i}") for i in range(NW1)]
    ch_w1g = [S.chan(f"w1ug{i}") for i in range(NW1)]
    w2p = [AR.alloc(f"w2p{i}", [128, KC, 512], BF16) for i in range(NW2)]
    Bw2p = [Buf(f"w2p{i}") for i in range(NW2)]
    ch_w2 = [S.chan(f"w2p{i}") for i in range(NW2)]
    ch_w2g = [S.chan(f"w2pg{i}") for i in range(NW2)]
    hrow = [AR.alloc(f"hrow{i}", [128, D], BF16) for i in range(2)]
    Bhrow = [Buf("hrow0"), Buf("hrow1")]
    NCH_H, NCH_Y = 8, 16
    ch_hrow = [S.chan(f"hrow{i}") for i in range(NCH_H)]
    ysb = [AR.alloc(f"ysb{i}", [128, 512], F32) for i in range(4)]
    Bysb = [Buf(f"ysb{i}") for i in range(4)]
    ch_y = [S.chan(f"yw{i}") for i in range(NCH_Y)]
    hcnt = [0]
    g1 = [AR.alloc(f"g1_{i}", [128, BLK], F32) for i in range(2)]
    sgm = [AR.alloc(f"sgm{i}", [128, BLK], F32) for i in range(2)]
    l2 = [AR.alloc(f"l2_{i}", [128, BLK], F32) for i in range(2)]
    wv = [AR.alloc(f"wv_{i}", [128, BLK], F32) for i in range(2)]
    Bg1 = [Buf("g1_0"), Buf("g1_1")]
    Bsgm = [Buf("sgm0"), Buf("sgm1")]
    Bl2 = [Buf("l2_0"), Buf("l2_1")]
    Bwv = [Buf("wv_0"), Buf("wv_1")]
    By_s = Buf("y_s")
    w1_v = [w1_d[e].rearrange("(k p) n -> p k n", p=128) for e in range(NE)]
    w2_v = [w2_d[e].rearrange("(k p) n -> p k n", p=128) for e in range(NE)]

    def blk_guard(e, r, b):
        return (e, b * BLK) if r == 0 else (e, r * RS)

    def rnd_guard(e, r):
        return (e, r * RS) if r > 0 else None

    def build_hselT(e, r, buf_i):
        for b in range(NBLK):
            S.cur_guard = blk_guard(e, r, b)
            for st2 in range(BLK // 128):
                st = b * (BLK // 128) + st2
                hb_i = st % 2
                row0 = (e % (NE // NHS)) * CAP + r * RS + st * 128
                src = hsel_s[e // (NE // NHS)]
                chh = ch_hrow[hcnt[0] % NCH_H]
                hcnt[0] += 1
                S.dma("sp", chh, lambda h, row0=row0, hb_i=hb_i, src=src: h.dma_start(out=hrow[hb_i][:], in_=src[row0:row0 + 128, :]), reads=[Bhsel], writes=[Bhrow[hb_i]])
                for half in range(2):
                    pb = half
                    for q in range(8):
                        kc = half * 8 + q
                        S.op("pe", lambda h, kc=kc, q=q, pb=pb, hb_i=hb_i: h.transpose(out=psb[pb][:, q * 128:(q + 1) * 128], in_=hrow[hb_i][:, kc * 128:(kc + 1) * 128], identity=identb[:]),
                             reads=[Bhrow[hb_i], Bc], writes=[Bpsb[pb]])
                    if half == 0:
                        S.op("act", lambda h, half=half, pb=pb, st=st, buf_i=buf_i: h.copy(out=hselT[buf_i][:, half * 8:(half + 1) * 8, st * 128:(st + 1) * 128],
                                                                                        in_=psb[pb][:, :].rearrange("p (q t) -> p q t", q=8)), reads=[Bpsb[pb]], writes=[BhselT[buf_i]])
                    else:
                        S.op("dve", lambda h, half=half, pb=pb, st=st, buf_i=buf_i: h.tensor_copy(out=hselT[buf_i][:, half * 8:(half + 1) * 8, st * 128:(st + 1) * 128],
                                                                                               in_=psb[pb][:, :].rearrange("p (q t) -> p q t", q=8)), reads=[Bpsb[pb]], writes=[BhselT[buf_i]])
        S.cur_guard = None

    ucount = 0
    pcount2 = 0
    acnt = 0
    ycnt = 0
    er_list = [(e, r) for e in range(NE) for r in range(ROUNDS)]
    build_hselT(er_list[0][0], er_list[0][1], 0)
    for n, (e, r) in enumerate(er_list):
        hb_cur = n % 2
        for u in range(8):
            bi = ucount % NW1
            ucount += 1
            S.cur_guard = rnd_guard(e, r)
            cw1 = ch_w1[bi] if r == 0 else ch_w1g[bi]
            S.dma("pool", cw1, lambda h, e=e, u=u, bi=bi: h.dma_start(out=w1u[bi][:, :, 0:256], in_=w1_v[e][:, :, u * 256:(u + 1) * 256]), writes=[Bw1u[bi]])
            S.dma("pool", cw1, lambda h, e=e, u=u, bi=bi: h.dma_start(out=w1u[bi][:, :, 256:512], in_=w1_v[e][:, :, DFF + u * 256:DFF + (u + 1) * 256]),
                  writes=[Bw1u[bi]], cont=True)
            for b in range(NBLK):
                S.cur_guard = blk_guard(e, r, b)
                for j in range(2):
                    fc = u * 2 + j
                    ab = acnt % 2
                    bank = acnt % 4
                    acnt += 1
                    bs = slice(b * BLK, (b + 1) * BLK)
                    for kc in range(KC):
                        S.op("pe", lambda h, kc=kc, bi=bi, j=j, bs=bs, bank=bank, hb_cur=hb_cur: h.matmul(psf[bank][:, 0:BLK], lhsT=w1u[bi][:, kc, j * 128:(j + 1) * 128],
                                                                                                     rhs=hselT[hb_cur][:, kc, bs], start=(kc == 0), stop=(kc == KC - 1)),
                             reads=[Bw1u[bi], BhselT[hb_cur]], writes=[Bpsf[bank]])
                    for kc in range(KC):
                        S.op("pe", lambda h, kc=kc, bi=bi, j=j, bs=bs, bank=bank, hb_cur=hb_cur: h.matmul(psf[bank][:, BLK:2 * BLK], lhsT=w1u[bi][:, kc, 256 + j * 128:256 + (j + 1) * 128],
                                                                                                     rhs=hselT[hb_cur][:, kc, bs], start=(kc == 0), stop=(kc == KC - 1)),
                             reads=[Bw1u[bi], BhselT[hb_cur]], writes=[Bpsf[bank]])
                    cg = e * 32 + fc
                    cl = e * 32 + 16 + fc
                    S.op("dve", lambda h, ab=ab, bank=bank, cg=cg: h.tensor_scalar(out=g1[ab][:], in0=psf[bank][:, 0:BLK], scalar1=b1T[:, cg:cg + 1], scalar2=LIMIT, op0=ALU.add, op1=ALU.min),
                         reads=[Bpsf[bank], Bb1T], writes=[Bg1[ab]])
                    S.op("act", lambda h, ab=ab: h.activation(out=sgm[ab][:], in_=g1[ab][:], func=AF.Sigmoid, scale=ALPHA), reads=[Bg1[ab]], writes=[Bsgm[ab]])
                    S.op("dve", lambda h, ab=ab, bank=bank, cl=cl: h.tensor_scalar(out=l2[ab][:], in0=psf[bank][:, BLK:2 * BLK], scalar1=b1T[:, cl:cl + 1], scalar2=LIMIT + 1.0, op0=ALU.add, op1=ALU.min),
                         reads=[Bpsf[bank], Bb1T], writes=[Bl2[ab]])
                    S.op("dve", lambda h, ab=ab: h.scalar_tensor_tensor(out=wv[ab][:], in0=l2[ab][:], scalar=1.0 - LIMIT, in1=g1[ab][:], op0=ALU.max, op1=ALU.mult),
                         reads=[Bl2[ab], Bg1[ab]], writes=[Bwv[ab]])
                    S.op("dve", lambda h, ab=ab, fc=fc, bs=bs: h.tensor_tensor(out=actT[:, fc, bs], in0=wv[ab][:], in1=sgm[ab][:], op=ALU.mult),
                         reads=[Bwv[ab], Bsgm[ab]], writes=[BactT])
        S.cur_guard = None
        if n + 1 < len(er_list):
            build_hselT(er_list[n + 1][0], er_list[n + 1][1], (n + 1) % 2)
        ydst = y_s[e // (NE // NYS)]
        for db in range(4):
            bi = pcount2 % NW2
            pcount2 += 1
            S.cur_guard = rnd_guard(e, r)
            cw2 = ch_w2[bi] if r == 0 else ch_w2g[bi]
            S.dma("pool", cw2, lambda h, e=e, db=db, bi=bi: h.dma_start(out=w2p[bi][:], in_=w2_v[e][:, :, db * 512:(db + 1) * 512]), writes=[Bw2p[bi]])
            for b in range(NBLK):
                S.cur_guard = blk_guard(e, r, b)
                for st2 in range(BLK // 128):
                    st = b * (BLK // 128) + st2
                    bank = 4 + (ycnt % 2)
                    yb = ycnt % 4
                    ych = ch_y[ycnt % NCH_Y]
                    ycnt += 1
                    for fc in range(KC):
                        S.op("pe", lambda h, fc=fc, st=st, bi=bi, bank=bank: h.matmul(psf[bank][:, :], lhsT=actT[:, fc, st * 128:(st + 1) * 128], rhs=w2p[bi][:, fc, :],
                                                                                     start=(fc == 0), stop=(fc == KC - 1)),
                             reads=[BactT, Bw2p[bi]], writes=[Bpsf[bank]])
                    S.op("act", lambda h, bank=bank, yb=yb: h.copy(out=ysb[yb][:], in_=psf[bank][:, :]), reads=[Bpsf[bank]], writes=[Bysb[yb]])
                    row0 = (e % (NE // NYS)) * CAP + r * RS + st * 128
                    S.dma("sp", ych, lambda h, row0=row0, db=db, yb=yb, ydst=ydst: h.dma_start(out=ydst[row0:row0 + 128, db * 512:(db + 1) * 512], in_=ysb[yb][:]),
                          reads=[Bysb[yb]], writes=[By_s])
        S.cur_guard = None
    S.barrier()
    AR.reset(m5)

    G2 = AR.alloc("G2", [128, D], F32)
    wt6 = AR.alloc("wt6", [128, D], F32)
    b2sb = AR.alloc("b2sb", [NE, D], F32)
    BG2, Bwt6, Bb2 = Buf("G2"), Buf("wt6"), Buf("b2sb")
    load_mod_bcast(G2, 0, 5, ch_v, BG2)
    load_row_bcast(wt6, pofw_d, ch_v, Bwt6)
    S.op("dve", lambda h: h.tensor_tensor(out=G2[:], in0=G2[:], in1=wt6[:], op=ALU.mult), reads=[BG2, Bwt6], writes=[BG2])
    S.dma("sp", ch_v, lambda h: h.dma_start(out=b2sb[:], in_=b2_d), writes=[Bb2])
    yk = [[AR.alloc(f"yk{b}_{k}", [128, D], F32) for k in range(4)] for b in range(2)]
    Byk = [[Buf(f"yk{b}_{k}") for k in range(4)] for b in range(2)]
    ch_g = [S.chan("gath0"), S.chan("gath1")]
    x1r = [AR.alloc(f"x1r{i}", [128, D], F32) for i in range(2)]
    Bx1r = [Buf("x1r0"), Buf("x1r1")]
    ch_x1r = [S.chan("x1r0"), S.chan("x1r1")]
    ff = AR.alloc("ff", [128, D], F32)
    ot = [AR.alloc(f"ot{i}", [128, D], F32) for i in range(2)]
    gT = AR.alloc("gT", [NE, 128], F32)
    junk6 = AR.alloc("junk6", [128, D], BF16)
    st6 = AR.alloc("st6", [128, 2], F32)
    Bff, BgT, Bjunk6, Bst6 = Buf("ff"), Buf("gT"), Buf("junk6"), Buf("st6")
    Bot = [Buf("ot0"), Buf("ot1")]
    ch_out = [S.chan("out0"), S.chan("out1")]
    for i in range(NT):
        bi = i % 2
        first = True
        for k in range(4):
            for j in range(NYS):
                S.dma("pool", ch_g[bi], lambda h, i=i, k=k, bi=bi, j=j: h.indirect_dma_start(out=yk[bi][k][:], out_offset=None, in_=y_s[j],
                                                                                          in_offset=bass.IndirectOffsetOnAxis(ap=idxY[j][:, i * 4 + k:i * 4 + k + 1], axis=0),
                                                                                          bounds_check=bound_reg(h, (NE // NYS) * CAP - 1), oob_is_err=False),
                      reads=[By_s, Bidx[i]], writes=[Byk[bi][k]], cont=(not first))
                first = False
        S.dma("sp", ch_x1r[bi], lambda h, i=i, bi=bi: h.dma_start(out=x1r[bi][:], in_=x1_s[i * 128:(i + 1) * 128, :]), reads=[Bx1_s[i]], writes=[Bx1r[bi]])
        S.op("pe", lambda h, i=i: h.transpose(out=psf[4][0:NE, 0:128], in_=gatesA[:, i, :], identity=ident[:]), reads=[BgatesA, Bc], writes=[Bpsf[4]])
        S.op("act", lambda h: h.copy(out=gT[:], in_=psf[4][0:NE, 0:128]), reads=[Bpsf[4]], writes=[BgT])
        for cb in range(4):
            S.op("pe", lambda h, cb=cb: h.matmul(psf[cb][:, :], lhsT=gT[:], rhs=b2sb[:, cb * 512:(cb + 1) * 512], start=True, stop=True), reads=[BgT, Bb2], writes=[Bpsf[cb]])
            S.op("dve", lambda h, cb=cb, bi=bi, i=i: h.scalar_tensor_tensor(out=ff[:, cb * 512:(cb + 1) * 512], in0=yk[bi][0][:, cb * 512:(cb + 1) * 512], scalar=gate4[:, i, 0:1],
                                                                          in1=psf[cb][:, :], op0=ALU.mult, op1=ALU.add), reads=[Byk[bi][0], Bgate4, Bpsf[cb]], writes=[Bff])
        for k in range(1, 4):
            S.op("dve", lambda h, k=k, bi=bi, i=i: h.scalar_tensor_tensor(out=ff[:], in0=yk[bi][k][:], scalar=gate4[:, i, k:k + 1], in1=ff[:], op0=ALU.mult, op1=ALU.add),
                 reads=[Byk[bi][k], Bgate4, Bff], writes=[Bff])
        S.op("act", lambda h: h.activation(out=junk6[:], in_=ff[:], func=AF.Square, accum_out=st6[:, 0:1]), reads=[Bff], writes=[Bjunk6, Bst6])
        S.op("act", lambda h: h.activation(out=st6[:, 0:1], in_=st6[:, 0:1], func=AF.Sqrt, scale=1.0 / D, bias=EPS), reads=[Bst6], writes=[Bst6])
        S.op("dve", lambda h: h.reciprocal(out=st6[:, 0:1], in_=st6[:, 0:1]), reads=[Bst6], writes=[Bst6])
        S.op("dve", lambda h: h.scalar_tensor_tensor(out=ff[:], in0=ff[:], scalar=st6[:, 0:1], in1=G2[:], op0=ALU.mult, op1=ALU.mult), reads=[Bff, Bst6, BG2], writes=[Bff])
        S.op("dve", lambda h, bi=bi: h.tensor_tensor(out=ot[bi][:], in0=ff[:], in1=x1r[bi][:], op=ALU.add), reads=[Bff, Bx1r[bi]], writes=[Bot[bi]])
        final_ops.append(S.dma("sp", ch_out[bi], lambda h, i=i, bi=bi: h.dma_start(out=out_d[i * 128:(i + 1) * 128, :], in_=ot[bi][:]), reads=[Bot[bi]]))
    return finish(nc, S, final_ops), dbg


def finish(nc, S, final_ops):
    S.wait_final("sp", final_ops)
    S.emit()
    return nc


def _rope_tables():
    half = 64
    inv = (10000.0 ** (-np.arange(half, dtype=np.float32) / np.float32(half))).astype(np.float32)
    pos = np.arange(NCTX + SEQ, dtype=np.float32)
    ang = (pos[:, None] * inv[None, :]).astype(np.float32)
    return np.cos(ang).astype(np.float32), np.sin(ang).astype(np.float32)


def make_in_maps(inp, ne_decl=NE):
    f = lambda a: np.ascontiguousarray(np.asarray(a, dtype=np.float32))
    cos, sin = _rope_tables()
    shared = {
        "c_ctx": f(inp["c_ctx"]).reshape(16, 128),
        "ada_w": f(inp["ada_w"][0]),
        "ada_b": f(inp["ada_b"][0]).reshape(1, 6 * D),
        "pre_mix_norm": f(inp["pre_mix_norm"][0]).reshape(1, D),
        "post_mix_norm": f(inp["post_mix_norm"][0]).reshape(1, D),
        "pre_ffn_norm": f(inp["pre_ffn_norm"][0]).reshape(1, D),
        "post_ffn_norm": f(inp["post_ffn_norm"][0]).reshape(1, D),
        "w_in": f(inp["w_in"][0]),
        "ret_decay": np.concatenate([f(inp["ret_decay_fwd"][0]), f(inp["ret_decay_bwd"][0])]).reshape(1, 16),
        "ret_gn_w": f(inp["ret_gn_w"][0]).reshape(1, DRET),
        "conv_w": f(inp["conv_w"][0]),
        "conv_vecs": np.concatenate([f(inp["conv_b"][0]).reshape(8, 128), f(inp["conv_ln_w"][0]).reshape(8, 128), f(inp["conv_ln_b"][0]).reshape(8, 128)], axis=0),
        "w_out": f(inp["w_out"][0]),
        "router_w": f(inp["router_w"][0]),
        "router_b": f(inp["router_b"][0]).reshape(1, NE),
        "w1": f(inp["w1"][0][:ne_decl]),
        "b1": f(inp["b1"][0]).reshape(NE * 32, 128),
        "w2": f(inp["w2"][0][:ne_decl]),
        "b2": f(inp["b2"][0]),
        "rope_cos": cos,
        "rope_sin": sin,
    }
    maps = []
    for b in range(NB):
        m = dict(shared)
        m["x"] = f(inp["x"][b])
        m["c"] = f(inp["c"][b]).reshape(16, 128)
        m["ctx"] = f(inp["ctx"][b])
        maps.append(m)
    return maps


_NC_CACHE = {}


def kernel(**inputs):
    if "nc" not in _NC_CACHE:
        _NC_CACHE["nc"] = build_program()[0]
    nc = _NC_CACHE["nc"]
    in_maps = make_in_maps(inputs)
    res = run_bass_kernel_spmd(nc, in_maps, core_ids=list(range(NB)))
    out = np.stack([np.asarray(res.results[b]["out"], dtype=np.float32) for b in range(NB)], axis=0)
    return out
```

```python
import numpy as np
import concourse.bass as bass
import concourse.mybir as mybir
from concourse.alu_op_type import AluOpType as ALU
from concourse.bass_utils import run_bass_kernel_spmd

F32 = mybir.dt.float32
BF16 = mybir.dt.bfloat16
I32 = mybir.dt.int32
AF = mybir.ActivationFunctionType
AX = mybir.AxisListType

D = 2048
SEQ = 2048
NB = 8
NCTX = 256
H = 8
HD = 128
DRET = 1024
DCONV = 1024
DIN = 6144
CW = 31
NE = 32
DFF = 2048
NT = SEQ // 128
KC = D // 128
EPS = 1e-6
GN_EPS = 1e-5
ALPHA = 1.702
LIMIT = 7.0
QSCALE = HD ** -0.5

ROUNDS = 4
RS = 512
CAP = ROUNDS * RS
BLK = 256
NBLK = RS // BLK
NHS = 1
NYS = 2
BIG = 4.0e6

ENGS = ("pe", "act", "dve", "pool", "sp")
EPOCH = 1 << 30


class Buf:
    __slots__ = ("name", "excl", "last_w", "readers")

    def __init__(self, name, excl=False):
        self.name = name
        self.excl = excl
        self.last_w = None
        self.readers = []


class Op:
    __slots__ = ("eng", "fn", "deps", "signal", "done", "is_dma", "chan", "chan_prev", "grp", "guard")

    def __init__(self, eng, fn):
        self.eng = eng
        self.fn = fn
        self.deps = []
        self.signal = False
        self.done = None
        self.is_dma = False
        self.chan = None
        self.chan_prev = None
        self.grp = None
        self.guard = None


class Chan:
    def __init__(self, name):
        self.name = name
        self.sem = None
        self.last_grp = None


class Sched:
    def __init__(self, nc):
        self.nc = nc
        self.ops = {e: [] for e in ENGS}
        self.chans = []
        self.final_waits = []
        self.nrec = 0
        self.cur_guard = None
        self.cnt_ap = None
        self.cnt_op = None

    def chan(self, name):
        c = Chan(name)
        self.chans.append(c)
        return c

    def _deps(self, op, reads, writes):
        for b in reads:
            if b.last_w is not None:
                op.deps.append((b.last_w, "RAW"))
            if b.excl:
                for r in b.readers:
                    op.deps.append((r, "RAR"))
        for b in writes:
            if b.last_w is not None:
                op.deps.append((b.last_w, "WAW"))
            for r in b.readers:
                op.deps.append((r, "WAR"))
        for b in reads:
            b.readers.append(op)
        for b in writes:
            b.last_w = op
            b.readers = []

    def op(self, eng, fn, reads=(), writes=()):
        o = Op(eng, fn)
        o.guard = self.cur_guard
        self._deps(o, list(reads), list(writes))
        self.ops[eng].append(o)
        self.nrec += 1
        return o

    def dma(self, eng, chan, fn, reads=(), writes=(), cont=False):
        o = Op(eng, fn)
        o.guard = self.cur_guard
        o.is_dma = True
        o.chan = chan
        if cont and chan.last_grp is not None:
            o.grp = chan.last_grp
            o.chan_prev = o.grp[0].chan_prev
        else:
            o.chan_prev = chan.last_grp
            o.grp = []
            chan.last_grp = o.grp
        o.grp.append(o)
        self._deps(o, list(reads), list(writes))
        self.ops[eng].append(o)
        self.nrec += 1
        return o

    def barrier(self):
        lasts = []
        for e in ENGS:
            for o in reversed(self.ops[e]):
                if not o.is_dma and o.fn is not None:
                    lasts.append(o)
                    break
        for c in self.chans:
            if c.last_grp:
                lasts.append(c.last_grp[0])
        for e in ENGS:
            o = Op(e, None)
            for d in lasts:
                o.deps.append((d, "RAW"))
            self.ops[e].append(o)

    def wait_final(self, eng, ops):
        self.final_waits.append((eng, list(ops)))

    def emit(self):
        nc = self.nc
        for e in ENGS:
            for o in self.ops[e]:
                for (d, kind) in o.deps:
                    if d.is_dma:
                        continue
                    if d.eng != o.eng or kind == "RAW":
                        d.signal = True
        for (e, ops) in self.final_waits:
            for d in ops:
                if not d.is_dma:
                    d.signal = True
        if self.cnt_op is not None:
            self.cnt_op.signal = True
        sems = []
        for e in ENGS:
            cnt = 0
            sem = None
            for o in self.ops[e]:
                if o.is_dma or o.fn is None:
                    continue
                if o.signal:
                    if sem is None or cnt >= EPOCH:
                        sem = nc.alloc_semaphore(f"s_{e}_{len(sems)}")
                        sems.append(sem)
                        cnt = 0
                    cnt += 1
                    o.done = (sem, cnt)
        for c in self.chans:
            if c.last_grp is None:
                continue
            c.sem = nc.alloc_semaphore(f"c_{c.name}")
            chain = []
            g = c.last_grp
            while g is not None:
                chain.append(g)
                g = g[0].chan_prev
            chain.reverse()
            v = 0
            for g in chain:
                v += 16 * len(g)
                for o in g:
                    o.done = (c.sem, v)
        lists = self.ops
        finals = self.final_waits

        cnt_ap = self.cnt_ap
        cnt_op = self.cnt_op

        def run(e, h):
            seen = {}
            state = {"greg": None, "loaded": None, "last_sig": None}

            def need(sem, val):
                k = id(sem)
                if seen.get(k, 0) < val:
                    h.wait_ge(sem, val)
                    seen[k] = val

            def emit_op(o):
                if o.is_dma and o.chan_prev is not None and o is o.grp[0]:
                    need(*o.chan_prev[0].done)
                for (d, kind) in o.deps:
                    if d.is_dma:
                        if d.grp is o.grp:
                            continue
                        need(*d.done)
                    elif d.eng != e or kind == "RAW":
                        need(*d.done)
                if o.fn is None:
                    return
                ins = o.fn(h)
                if o.is_dma:
                    ins.then_inc(o.chan.sem, 16)
                elif o.signal:
                    ins.then_inc(o.done[0], 1)
                    state["last_sig"] = o.done

            ops = lists[e]
            i = 0
            n = len(ops)
            while i < n:
                o = ops[i]
                if o.guard is None:
                    emit_op(o)
                    i += 1
                    continue
                j = i
                while j < n and ops[j].guard == o.guard:
                    j += 1
                grp = ops[i:j]
                (ge, thr) = o.guard
                if state["greg"] is None:
                    state["greg"] = h.alloc_register(f"greg_{e}")
                if state["loaded"] != ge:
                    need(*cnt_op.done)
                    h.reg_load(state["greg"], cnt_ap(ge))
                    state["loaded"] = ge
                saved = dict(seen)
                pre_sig = state["last_sig"]
                with h.If_cmp(state["greg"], thr, "IS_GT"):
                    for g in grp:
                        emit_op(g)
                nsig = 0
                sig_sem = None
                ndma = 0
                for g in grp:
                    if g.fn is None:
                        continue
                    if g.is_dma:
                        ndma += 1
                    elif g.signal:
                        nsig += 1
                        sig_sem = g.done[0]
                        last_in = g.done
                if nsig or ndma:
                    with h.Else():
                        if nsig:
                            if pre_sig is not None:
                                h.wait_ge(*pre_sig)
                            h.sem_inc(sig_sem, nsig)
                        for g in grp:
                            if g.fn is not None and g.is_dma:
                                if g is g.grp[0] and g.chan_prev is not None:
                                    h.wait_ge(*g.chan_prev[0].done)
                                h.sem_inc(g.chan.sem, 16)
                if nsig:
                    state["last_sig"] = last_in
                seen.clear()
                seen.update(saved)
                i = j
            for (fe, fops) in finals:
                if fe == e:
                    for d in fops:
                        need(*d.done)

        with nc.Block() as block:
            @block.tensor
            def _(h):
                run("pe", h)

            @block.scalar
            def _(h):
                run("act", h)

            @block.vector
            def _(h):
                run("dve", h)

            @block.gpsimd
            def _(h):
                run("pool", h)

            @block.sync
            def _(h):
                run("sp", h)


class Arena:
    def __init__(self, nc, nbytes):
        self.nc = nc
        left = nc._sbuf_addr_for_side("left")
        self.base = (left + 63) // 64 * 64
        nbytes = nbytes // 64 * 64
        self.slab = nc.alloc_sbuf_tensor("arena", [128, nbytes // 4], F32)
        self.size = nbytes - 64
        self.top = 0
        self.n = 0

    def alloc(self, name, shape, dtype):
        esz = 2 if dtype == BF16 else 4
        nb = esz
        for s in shape[1:]:
            nb *= s
        nb = (nb + 63) // 64 * 64
        off = self.top
        assert off + nb <= self.size, (name, off, nb, self.size)
        self.top += nb
        self.n += 1
        return self.nc.alloc_sbuf_tensor_at(f"{name}_{self.n}", list(shape), dtype, offset=self.base + off)

    def alloc_at(self, name, shape, dtype, off):
        self.n += 1
        return self.nc.alloc_sbuf_tensor_at(f"{name}_{self.n}", list(shape), dtype, offset=self.base + off)

    def mark(self):
        return self.top

    def reset(self, m):
        self.top = m


def build_program(stage=99, debug=False, ne_decl=NE):
    nc = bass.Bass("TRN2", target_bir_lowering=False)
    S = Sched(nc)

    def din(name, shape, dt=F32):
        return nc.dram_tensor(name, list(shape), dt, kind="ExternalInput").ap()

    x_d = din("x", [SEQ, D])
    c_d = din("c", [16, 128])
    ctx_d = din("ctx", [NCTX, D])
    cctx_d = din("c_ctx", [16, 128])
    adaw_d = din("ada_w", [D, 6 * D])
    adab_d = din("ada_b", [1, 6 * D])
    pmw_d = din("pre_mix_norm", [1, D])
    pomw_d = din("post_mix_norm", [1, D])
    pfw_d = din("pre_ffn_norm", [1, D])
    pofw_d = din("post_ffn_norm", [1, D])
    win_d = din("w_in", [D, DIN])
    dec_d = din("ret_decay", [1, 16])
    gnw_d = din("ret_gn_w", [1, DRET])
    convw_d = din("conv_w", [CW, DCONV])
    cvec_d = din("conv_vecs", [24, 128])
    wout_d = din("w_out", [D, D])
    rw_d = din("router_w", [D, NE])
    rb_d = din("router_b", [1, NE])
    w1_d = din("w1", [ne_decl, D, 2 * DFF])
    b1_d = din("b1", [NE * 32, 128])
    w2_d = din("w2", [ne_decl, DFF, D])
    b2_d = din("b2", [NE, D])
    cos_d = din("rope_cos", [NCTX + SEQ, 64])
    sin_d = din("rope_sin", [NCTX + SEQ, 64])
    out_d = nc.dram_tensor("out", [SEQ, D], F32, kind="ExternalOutput").ap()
    dbg = {}

    def dbg_out(name, shape, dt=F32):
        t = nc.dram_tensor("dbg_" + name, list(shape), dt, kind="ExternalOutput").ap()
        dbg[name] = t
        return t

    mod_s = nc.dram_tensor("mod_s", [2, 6 * D], F32).ap()
    yT_s = nc.dram_tensor("yT_s", [D, SEQ], BF16).ap()
    x1_s = nc.dram_tensor("x1_s", [SEQ, D], F32).ap()
    hsel_s = [nc.dram_tensor(f"hsel_s{j}", [(NE // NHS) * CAP, D], BF16).ap() for j in range(NHS)]
    y_s = [nc.dram_tensor(f"y_s{j}", [(NE // NYS) * CAP, D], F32).ap() for j in range(NYS)]

    AR = Arena(nc, nc.sbuf_bytes_remaining - 6144)

    psf = [nc.alloc_psum_tensor(f"psf{i}", [128, 512], F32) for i in range(6)]
    psb = [nc.alloc_psum_tensor(f"psb{i}", [128, 1024], BF16) for i in range(2)]
    Bpsf = [Buf(f"psf{i}", excl=True) for i in range(6)]
    Bpsb = [Buf(f"psb{i}", excl=True) for i in range(2)]
    final_ops = []
    _regs = {}

    def bound_reg(h, val):
        if val not in _regs:
            _regs[val] = h.to_reg(val)
        return _regs[val]

    ident = AR.alloc("ident", [128, 128], F32)
    identb = AR.alloc("identb", [128, 128], BF16)
    onesb = AR.alloc("onesb", [128, 128], BF16)
    ones32 = AR.alloc("ones32", [128, 128], F32)
    trib = AR.alloc("trib", [128, 128], BF16)
    iorow = AR.alloc("iorow", [128, 128], F32)
    iocol = AR.alloc("iocol", [128, 1], F32)
    b1T = AR.alloc("b1T", [128, NE * 32], F32)
    gate4 = AR.alloc("gate4", [128, NT, 4], F32)
    idxH = [AR.alloc(f"idxH{j}", [128, NT * 4], I32) for j in range(NHS)]
    idxY = [AR.alloc(f"idxY{j}", [128, NT * 4], I32) for j in range(NYS)]
    cnt_i = AR.alloc("cnt_i", [128, NE], I32)
    Bcnt = Buf("cnt_i")
    gatesA = AR.alloc("gatesA", [128, NT, NE], F32)
    Bc = Buf("consts")
    Bb1T = Buf("b1T")
    Bgate4 = Buf("gate4")
    Bidx = [Buf(f"idx{i}") for i in range(NT)]
    BgatesA = Buf("gatesA")

    S.op("pool", lambda h: h.memset(ident[:], 0.0), writes=[Bc])
    S.op("pool", lambda h: h.affine_select(out=ident[:], in_=ident[:], pattern=[[-1, 128]], compare_op=ALU.not_equal,
                                            fill=1.0, base=0, channel_multiplier=1), reads=[Bc], writes=[Bc])
    S.op("pool", lambda h: h.memset(ones32[:], 1.0), writes=[Bc])
    S.op("pool", lambda h: h.affine_select(out=iorow[:], in_=ones32[:], pattern=[[1, 128]], compare_op=ALU.is_gt,
                                            fill=0.0, base=0, channel_multiplier=-1), reads=[Bc], writes=[Bc])
    S.op("dve", lambda h: h.tensor_copy(out=trib[:], in_=iorow[:]), reads=[Bc], writes=[Bc])
    S.op("dve", lambda h: h.tensor_copy(out=identb[:], in_=ident[:]), reads=[Bc], writes=[Bc])
    S.op("dve", lambda h: h.tensor_copy(out=onesb[:], in_=ones32[:]), reads=[Bc], writes=[Bc])
    ioi = AR.alloc("ioi", [128, 128], I32)
    S.op("pool", lambda h: h.iota(ioi[:], pattern=[[1, 128]], base=0, channel_multiplier=0), writes=[Bc])
    S.op("dve", lambda h: h.tensor_copy(out=iorow[:], in_=ioi[:]), reads=[Bc], writes=[Bc])
    S.op("pool", lambda h: h.iota(ioi[:, 0:1], pattern=[[0, 1]], base=0, channel_multiplier=1), reads=[Bc], writes=[Bc])
    S.op("dve", lambda h: h.tensor_copy(out=iocol[:], in_=ioi[:, 0:1]), reads=[Bc], writes=[Bc])

    ch_small = S.chan("small")
    persist_mark = AR.mark()

    def transpose_rows(rows_ap, nrows, out_ap, bank, Bbank, reads, writes, evac="act"):
        S.op("pe", lambda h: h.transpose(out=psf[bank][:, 0:nrows], in_=rows_ap, identity=ident[0:nrows, 0:nrows]),
             reads=reads + [Bc], writes=[Bbank])
        if evac == "act":
            S.op("act", lambda h: h.copy(out=out_ap, in_=psf[bank][:, 0:nrows]), reads=[Bbank], writes=writes)
        else:
            S.op("dve", lambda h: h.tensor_copy(out=out_ap, in_=psf[bank][:, 0:nrows]), reads=[Bbank], writes=writes)

    hT = AR.alloc("hT", [128, KC, SEQ], BF16)
    hcT_off = AR.mark()
    hcT = AR.alloc("hcT", [128, KC, NCTX], BF16)
    BhT = [Buf(f"hT{i}") for i in range(NT)]
    BhcT = Buf("hcT")
    m1 = AR.mark()
    sil2 = AR.alloc("sil2", [128, KC, 2], BF16)
    adaw = [AR.alloc(f"adaw{i}", [128, KC, 512], BF16) for i in range(2)]
    adabp = [AR.alloc(f"adabp{i}", [2, 512], F32) for i in range(2)]
    modp = [AR.alloc(f"modp{i}", [2, 512], F32) for i in range(2)]
    Bsil2 = Buf("sil2")
    Badaw = [Buf("adaw0"), Buf("adaw1")]
    Badabp = [Buf("adabp0"), Buf("adabp1")]
    Bmodp = [Buf("modp0"), Buf("modp1")]
    ch_adaw = [S.chan("adaw0"), S.chan("adaw1")]
    ch_adab = [S.chan("adab0"), S.chan("adab1")]
    ch_modw = S.chan("modw")
    Bmod_s = Buf("mod_s")
    adaw_v = adaw_d.rearrange("(k p) n -> p k n", p=128)

    def ada_piece(j):
        bi = j % 2
        S.dma("pool", ch_adaw[bi], lambda h: h.dma_start(out=adaw[bi][:], in_=adaw_v[:, :, j * 512:(j + 1) * 512]), writes=[Badaw[bi]])
        S.dma("sp", ch_adab[bi], lambda h: h.dma_start(out=adabp[bi][:], in_=adab_d[0:1, j * 512:(j + 1) * 512].partition_broadcast(2)), writes=[Badabp[bi]])
        bank = 4 + bi
        for kc in range(KC):
            S.op("pe", lambda h, kc=kc: h.matmul(psf[bank][0:2, :], lhsT=sil2[:, kc, :], rhs=adaw[bi][:, kc, :], start=(kc == 0), stop=(kc == KC - 1)),
                 reads=[Bsil2, Badaw[bi]], writes=[Bpsf[bank]])
        S.op("dve", lambda h: h.tensor_tensor(out=modp[bi][:], in0=psf[bank][0:2, :], in1=adabp[bi][:], op=ALU.add),
             reads=[Bpsf[bank], Badabp[bi]], writes=[Bmodp[bi]])
        S.dma("sp", ch_modw, lambda h: h.dma_start(out=mod_s[:, j * 512:(j + 1) * 512], in_=modp[bi][:]), reads=[Bmodp[bi]], writes=[Bmod_s])

    m0 = AR.mark()
    rows32 = AR.alloc("rows32", [32, 128], F32)
    cT = AR.alloc("cT", [128, 32], F32)
    sil = AR.alloc("sil", [128, 32], F32)
    b1rows = AR.alloc("b1rows", [128, 8, 128], F32)
    Brows32, BcT, Bsil, Bb1rows = Buf("rows32"), Buf("cT"), Buf("sil"), Buf("b1rows")
    S.dma("sp", ch_small, lambda h: h.dma_start(out=rows32[0:16, :], in_=c_d), writes=[Brows32])
    S.dma("sp", ch_small, lambda h: h.dma_start(out=rows32[16:32, :], in_=cctx_d), writes=[Brows32], cont=True)
    S.dma("sp", ch_small, lambda h: h.dma_start(out=b1rows[:], in_=b1_d.rearrange("(t p) f -> p t f", p=128)), writes=[Bb1rows], cont=True)
    transpose_rows(rows32[:], 32, cT[:], 0, Bpsf[0], [Brows32], [BcT])
    S.op("act", lambda h: h.activation(out=sil[:], in_=cT[:], func=AF.Silu), reads=[BcT], writes=[Bsil])
    for t in range(8):
        S.op("pe", lambda h, t=t: h.transpose(out=psf[1 + (t % 2)][:, 0:128], in_=b1rows[:, t, :], identity=ident[:]),
             reads=[Bb1rows, Bc], writes=[Bpsf[1 + (t % 2)]])
        S.op("act", lambda h, t=t: h.copy(out=b1T[:, t * 128:(t + 1) * 128], in_=psf[1 + (t % 2)][:, 0:128]),
             reads=[Bpsf[1 + (t % 2)]], writes=[Bb1T])
    b1T3 = b1T[:, :].rearrange("p (e f) -> p e f", e=NE)
    S.op("dve", lambda h: h.tensor_scalar(out=b1T3[:, :, 16:32], in0=b1T3[:, :, 16:32], scalar1=1.0, scalar2=None, op0=ALU.add), reads=[Bb1T], writes=[Bb1T])
    S.op("dve", lambda h: h.tensor_copy(out=sil2[:, :, 0], in_=sil[:, 0:16]), reads=[Bsil], writes=[Bsil2])
    S.op("dve", lambda h: h.tensor_copy(out=sil2[:, :, 1], in_=sil[:, 16:32]), reads=[Bsil], writes=[Bsil2])
    for j in range(8):
        ada_piece(j)
    ada_next = [8]
    S.barrier()
    AR.reset(m0)
    if stage <= 0:
        while ada_next[0] < 24:
            ada_piece(ada_next[0])
            ada_next[0] += 1
        if debug:
            d_mod = dbg_out("mod", [2, 6 * D])
            mld = AR.alloc("mld", [2, 6 * D], F32)
            Bmld = Buf("mld")
            S.dma("sp", S.chan("dbgm1"), lambda h: h.dma_start(out=mld[:], in_=mod_s), reads=[Bmod_s], writes=[Bmld])
            final_ops.append(S.dma("sp", S.chan("dbgmod"), lambda h: h.dma_start(out=d_mod, in_=mld[:]), reads=[Bmld]))
        return finish(nc, S, final_ops), dbg

    def load_mod_bcast(dst, row, g, chan, Bdst):
        return S.dma("sp", chan, lambda h: h.dma_start(out=dst[:], in_=mod_s[row:row + 1, g * D:(g + 1) * D].partition_broadcast(128)),
                     reads=[Bmod_s], writes=[Bdst])

    def load_row_bcast(dst, row_ap, chan, Bdst, cont=False):
        return S.dma("sp", chan, lambda h: h.dma_start(out=dst[:], in_=row_ap.partition_broadcast(128)), writes=[Bdst], cont=cont)

    A1 = AR.alloc("A1", [128, D], F32)
    S1 = AR.alloc("S1", [128, D], F32)
    A1c = AR.alloc("A1c", [128, D], F32)
    S1c = AR.alloc("S1c", [128, D], F32)
    wtmp = AR.alloc("wtmp", [128, D], F32)
    xb = [AR.alloc(f"xb{i}", [128, D], F32) for i in range(2)]
    t32s = [AR.alloc("t32_0", [128, D], F32)] * 2
    Bt32s = [Buf("t32_0")] * 2
    hb = [AR.alloc(f"hb{i}", [128, D], BF16) for i in range(2)]
    junkb = AR.alloc("junkb", [128, D], BF16)
    stat = AR.alloc("stat", [128, 8], F32)
    BA1, BS1, BA1c, BS1c, Bwtmp, Bjunk, Bstat = (Buf(n) for n in ["A1", "S1", "A1c", "S1c", "wtmp", "junk", "stat"])
    Bxb = [Buf("xb0"), Buf("xb1")]
    Bhb = [Buf("hb0"), Buf("hb1")]
    ch_x = [S.chan("x0"), S.chan("x1")]
    ch_v = S.chan("vecs")

    load_row_bcast(wtmp, pmw_d, ch_v, Bwtmp)
    load_mod_bcast(A1, 0, 1, ch_v, BA1)
    load_mod_bcast(S1, 0, 0, ch_v, BS1)
    load_mod_bcast(A1c, 1, 1, ch_v, BA1c)
    load_mod_bcast(S1c, 1, 0, ch_v, BS1c)
    S.op("dve", lambda h: h.scalar_tensor_tensor(out=A1[:], in0=A1[:], scalar=1.0, in1=wtmp[:], op0=ALU.add, op1=ALU.mult),
         reads=[BA1, Bwtmp], writes=[BA1])
    S.op("dve", lambda h: h.scalar_tensor_tensor(out=A1c[:], in0=A1c[:], scalar=1.0, in1=wtmp[:], op0=ALU.add, op1=ALU.mult),
         reads=[BA1c, Bwtmp], writes=[BA1c])

    def norm_mod_tile(src_ap_dram, xbuf, Bx, chan, Avec, BAv, Svec, BSv, hbuf, Bh, sidx):
        t32 = t32s[sidx % 2]
        Bt32 = Bt32s[sidx % 2]
        S.dma("sp", chan, lambda h: h.dma_start(out=xbuf[:], in_=src_ap_dram), writes=[Bx])
        S.op("act", lambda h: h.activation(out=junkb[:], in_=xbuf[:], func=AF.Square, accum_out=stat[:, sidx:sidx + 1]),
             reads=[Bx], writes=[Bjunk, Bstat])
        S.op("act", lambda h: h.activation(out=stat[:, sidx:sidx + 1], in_=stat[:, sidx:sidx + 1], func=AF.Sqrt, scale=1.0 / D, bias=EPS),
             reads=[Bstat], writes=[Bstat])
        S.op("dve", lambda h: h.reciprocal(out=stat[:, sidx:sidx + 1], in_=stat[:, sidx:sidx + 1]), reads=[Bstat], writes=[Bstat])
        S.op("dve", lambda h: h.scalar_tensor_tensor(out=t32[:], in0=xbuf[:], scalar=stat[:, sidx:sidx + 1], in1=Avec[:],
                                                     op0=ALU.mult, op1=ALU.mult), reads=[Bx, Bstat, BAv], writes=[Bt32])
        S.op("dve", lambda h: h.tensor_tensor(out=hbuf[:], in0=t32[:], in1=Svec[:], op=ALU.add), reads=[Bt32, BSv], writes=[Bh])

    def transpose_tile_bf16(hbuf, Bh, dstT, col0, Bdst):
        for half in range(2):
            pb = half
            for q in range(8):
                kc = half * 8 + q
                S.op("pe", lambda h, kc=kc, q=q, pb=pb: h.transpose(out=psb[pb][:, q * 128:(q + 1) * 128], in_=hbuf[:, kc * 128:(kc + 1) * 128],
                                                                       identity=identb[:]),
                     reads=[Bh, Bc], writes=[Bpsb[pb]])
            eng = "act" if half == 0 else "dve"
            if eng == "act":
                S.op("act", lambda h, half=half, pb=pb: h.copy(out=dstT[:, half * 8:(half + 1) * 8, col0:col0 + 128],
                                                                 in_=psb[pb][:, :].rearrange("p (q t) -> p q t", q=8)),
                     reads=[Bpsb[pb]], writes=[Bdst])
            else:
                S.op("dve", lambda h, half=half, pb=pb: h.tensor_copy(out=dstT[:, half * 8:(half + 1) * 8, col0:col0 + 128],
                                                                        in_=psb[pb][:, :].rearrange("p (q t) -> p q t", q=8)),
                     reads=[Bpsb[pb]], writes=[Bdst])

    for i in range(2):
        bi = i % 2
        norm_mod_tile(ctx_d[i * 128:(i + 1) * 128, :], xb[bi], Bxb[bi], ch_x[bi], A1c, BA1c, S1c, BS1c, hb[bi], Bhb[bi], i % 8)
        transpose_tile_bf16(hb[bi], Bhb[bi], hcT, i * 128, BhcT)
    for i in range(NT):
        bi = i % 2
        if ada_next[0] < 24:
            ada_piece(ada_next[0])
            ada_next[0] += 1
        norm_mod_tile(x_d[i * 128:(i + 1) * 128, :], xb[bi], Bxb[bi], ch_x[bi], A1, BA1, S1, BS1, hb[bi], Bhb[bi], i % 8)
        transpose_tile_bf16(hb[bi], Bhb[bi], hT, i * 128, BhT[i])
    while ada_next[0] < 24:
        ada_piece(ada_next[0])
        ada_next[0] += 1
    if debug:
        d_hT = dbg_out("hT", [128, KC, SEQ], BF16)
        final_ops.append(S.dma("sp", S.chan("dbghT"), lambda h: h.dma_start(out=d_hT, in_=hT[:]), reads=BhT))
    S.barrier()
    AR.reset(m1)
    if stage <= 1:
        return finish(nc, S, final_ops), dbg

    ByT_s = Buf("yT_s")
    win_v = win_d.rearrange("(k p) n -> p k n", p=128)

    m2 = AR.mark()
    decb = AR.alloc("decb", [128, 16], F32)
    Mh = AR.alloc("Mh", [128, H, 128], F32)
    dcol = AR.alloc("dcol", [128, H, 6], F32)
    wctx = AR.alloc("wctx", [128, H, 4], F32)
    Bdecb, BMh, Bdcol, Bwctx = Buf("decb"), Buf("Mh"), Buf("dcol"), Buf("wctx")
    tA = AR.alloc("tA", [128, 128], F32)
    tB = AR.alloc("tB", [128, 128], F32)
    tC = AR.alloc("tC", [128, 128], F32)
    tD = AR.alloc("tD", [128, 128], F32)
    cols = AR.alloc("cols", [128, 8], F32)
    BtA, BtB, BtC, BtD, Bcols = Buf("tA"), Buf("tB"), Buf("tC"), Buf("tD"), Buf("cols")
    S.dma("sp", ch_v, lambda h: h.dma_start(out=decb[:], in_=dec_d.partition_broadcast(128)), writes=[Bdecb])
    S.op("act", lambda h: h.activation(out=decb[:], in_=decb[:], func=AF.Exp, scale=-1.0), reads=[Bdecb], writes=[Bdecb])
    S.op("act", lambda h: h.activation(out=decb[:], in_=decb[:], func=AF.Ln, bias=1.0), reads=[Bdecb], writes=[Bdecb])
    S.op("dve", lambda h: h.tensor_scalar(out=decb[:], in0=decb[:], scalar1=-1.0, scalar2=None, op0=ALU.mult), reads=[Bdecb], writes=[Bdecb])
    S.op("dve", lambda h: h.tensor_scalar(out=tA[:], in0=iorow[:], scalar1=iocol[:, 0:1], scalar2=0.0, op0=ALU.subtract, op1=ALU.max),
         reads=[Bc], writes=[BtA])
    S.op("dve", lambda h: h.tensor_scalar(out=tB[:], in0=iorow[:], scalar1=iocol[:, 0:1], scalar2=-1.0, op0=ALU.subtract, op1=ALU.mult),
         reads=[Bc], writes=[BtB])
    S.op("dve", lambda h: h.tensor_scalar(out=tB[:], in0=tB[:], scalar1=0.0, scalar2=None, op0=ALU.max), reads=[BtB], writes=[BtB])
    S.op("dve", lambda h: h.tensor_scalar(out=tC[:], in0=iorow[:], scalar1=iocol[:, 0:1], scalar2=None, op0=ALU.is_ge), reads=[Bc], writes=[BtC])
    S.op("dve", lambda h: h.tensor_scalar(out=tD[:], in0=iorow[:], scalar1=iocol[:, 0:1], scalar2=None, op0=ALU.is_le), reads=[Bc], writes=[BtD])
    for ci, (mul, add) in enumerate([(1.0, 1.0), (-1.0, 128.0), (-1.0, 127.0), (1.0, 0.0), (0.0, 128.0), (-1.0, 255.0), (-1.0, 127.0), (1.0, 128.0)]):
        S.op("dve", lambda h, ci=ci, mul=mul, add=add: h.tensor_scalar(out=cols[:, ci:ci + 1], in0=iocol[:, 0:1], scalar1=mul, scalar2=add,
                                                                      op0=ALU.mult, op1=ALU.add), reads=[Bc], writes=[Bcols])
    ex1 = AR.alloc("ex1", [128, 128], F32)
    ex2 = AR.alloc("ex2", [128, 128], F32)
    Bex1, Bex2 = Buf("ex1"), Buf("ex2")
    for hh in range(H):
        lf = decb[:, hh:hh + 1]
        lb = decb[:, 8 + hh:9 + hh]
        S.op("act", lambda h, lf=lf: h.activation(out=ex1[:], in_=tA[:], func=AF.Exp, scale=lf), reads=[BtA, Bdecb], writes=[Bex1])
        S.op("act", lambda h, lb=lb: h.activation(out=ex2[:], in_=tB[:], func=AF.Exp, scale=lb), reads=[BtB, Bdecb], writes=[Bex2])
        S.op("dve", lambda h: h.tensor_tensor(out=ex1[:], in0=ex1[:], in1=tC[:], op=ALU.mult), reads=[Bex1, BtC], writes=[Bex1])
        S.op("dve", lambda h: h.tensor_tensor(out=ex2[:], in0=ex2[:], in1=tD[:], op=ALU.mult), reads=[Bex2, BtD], writes=[Bex2])
        S.op("dve", lambda h, hh=hh: h.tensor_tensor(out=Mh[:, hh, :], in0=ex1[:], in1=ex2[:], op=ALU.add), reads=[Bex1, Bex2], writes=[BMh])
        for (dst, ci, lg) in [(0, 0, lf), (1, 1, lb), (2, 2, lf), (3, 3, lb), (4, 4, lf), (5, 4, lb)]:
            S.op("act", lambda h, hh=hh, dst=dst, ci=ci, lg=lg: h.activation(out=dcol[:, hh, dst:dst + 1], in_=cols[:, ci:ci + 1], func=AF.Exp, scale=lg),
                 reads=[Bcols, Bdecb], writes=[Bdcol])
        for (dst, ci, lg) in [(0, 5, lf), (1, 6, lf), (2, 3, lb), (3, 7, lb)]:
            S.op("act", lambda h, hh=hh, dst=dst, ci=ci, lg=lg: h.activation(out=wctx[:, hh, dst:dst + 1], in_=cols[:, ci:ci + 1], func=AF.Exp, scale=lg),
                 reads=[Bcols, Bdecb], writes=[Bwctx])

    cosL = AR.alloc("cosL", [128, NT, 64], F32)
    sinL = AR.alloc("sinL", [128, NT, 64], F32)
    cosT = cosL[:, :, :].unsqueeze(2).to_broadcast([128, NT, 2, 64])
    sinT = sinL[:, :, :].unsqueeze(2).to_broadcast([128, NT, 2, 64])
    cosC = AR.alloc("cosC", [128, 2, 64], F32)
    sinC = AR.alloc("sinC", [128, 2, 64], F32)
    Brope = Buf("rope")
    cos_lat = cos_d[NCTX:NCTX + SEQ, :].rearrange("(t p) f -> p t f", p=128)
    sin_lat = sin_d[NCTX:NCTX + SEQ, :].rearrange("(t p) f -> p t f", p=128)
    S.dma("sp", ch_v, lambda h: h.dma_start(out=cosL[:], in_=cos_lat), writes=[Brope])
    S.dma("sp", ch_v, lambda h: h.dma_start(out=sinL[:], in_=sin_lat), writes=[Brope], cont=True)
    S.dma("sp", ch_v, lambda h: h.dma_start(out=cosC[:], in_=cos_d[0:NCTX, :].rearrange("(t p) f -> p t f", p=128)), writes=[Brope], cont=True)
    S.dma("sp", ch_v, lambda h: h.dma_start(out=sinC[:], in_=sin_d[0:NCTX, :].rearrange("(t p) f -> p t f", p=128)), writes=[Brope], cont=True)
    gnwb = AR.alloc("gnwb", [128, DRET], F32)
    Bgnwb = Buf("gnwb")
    load_row_bcast(gnwb, gnw_d, ch_v, Bgnwb)

    R0 = AR.alloc("R0", [128, H, 2, 128], F32)
    BR0 = Buf("R0")
    m2b = AR.mark()
    wkv = [AR.alloc(f"wkv{i}", [128, KC, 256], BF16) for i in range(2)]
    Bwkv = [Buf("wkv0"), Buf("wkv1")]
    ch_wkv = [S.chan("wkv0"), S.chan("wkv1")]
    kc32 = AR.alloc("kc32", [128, 2, 128], F32)
    kcr = AR.alloc("kcr", [128, 2, 128], BF16)
    vwf = AR.alloc("vwf", [128, 2, 128], BF16)
    vwb = AR.alloc("vwb", [128, 2, 128], BF16)
    rt1 = AR.alloc("rt1", [128, 2, 64], F32)
    rt2 = AR.alloc("rt2", [128, 2, 64], F32)
    Bkc32, Bkcr, Bvwf, Bvwb, Brt1, Brt2 = (Buf(n) for n in ["kc32", "kcr", "vwf", "vwb", "rt1", "rt2"])
    for hh in range(H):
        bi = hh % 2
        S.dma("pool", ch_wkv[bi], lambda h, hh=hh, bi=bi: h.dma_start(out=wkv[bi][:, :, 0:128], in_=win_v[:, :, DRET + hh * 128:DRET + (hh + 1) * 128]),
              writes=[Bwkv[bi]])
        S.dma("pool", ch_wkv[bi], lambda h, hh=hh, bi=bi: h.dma_start(out=wkv[bi][:, :, 128:256], in_=win_v[:, :, 2 * DRET + hh * 128:2 * DRET + (hh + 1) * 128]),
              writes=[Bwkv[bi]], cont=True)
        for t in range(2):
            bank = t
            for kc in range(KC):
                S.op("pe", lambda h, kc=kc, t=t, bi=bi, bank=bank: h.matmul(psf[bank][:, 0:256], lhsT=hcT[:, kc, t * 128:(t + 1) * 128], rhs=wkv[bi][:, kc, :],
                                                                             start=(kc == 0), stop=(kc == KC - 1)),
                     reads=[BhcT, Bwkv[bi]], writes=[Bpsf[bank]])
            S.op("act", lambda h, t=t, bank=bank: h.copy(out=kc32[:, t, :], in_=psf[bank][:, 0:128]), reads=[Bpsf[bank]], writes=[Bkc32])
            S.op("dve", lambda h, t=t, bank=bank, hh=hh: h.tensor_scalar(out=vwf[:, t, :], in0=psf[bank][:, 128:256], scalar1=wctx[:, hh, t:t + 1], scalar2=None, op0=ALU.mult),
                 reads=[Bpsf[bank], Bwctx], writes=[Bvwf])
            S.op("dve", lambda h, t=t, bank=bank, hh=hh: h.tensor_scalar(out=vwb[:, t, :], in0=psf[bank][:, 128:256], scalar1=wctx[:, hh, 2 + t:3 + t], scalar2=None, op0=ALU.mult),
                 reads=[Bpsf[bank], Bwctx], writes=[Bvwb])
        k1 = kc32[:, :, 0:64]
        k2 = kc32[:, :, 64:128]
        S.op("dve", lambda h: h.tensor_tensor(out=rt1[:], in0=k1, in1=cosC[:], op=ALU.mult), reads=[Bkc32, Brope], writes=[Brt1])
        S.op("pool", lambda h: h.tensor_tensor(out=rt2[:], in0=k2, in1=sinC[:], op=ALU.mult), reads=[Bkc32, Brope], writes=[Brt2])
        S.op("dve", lambda h: h.tensor_tensor(out=kcr[:, :, 0:64], in0=rt1[:], in1=rt2[:], op=ALU.subtract), reads=[Brt1, Brt2], writes=[Bkcr])
        S.op("dve", lambda h: h.tensor_tensor(out=rt1[:], in0=k1, in1=sinC[:], op=ALU.mult), reads=[Bkc32, Brope, Bkcr], writes=[Brt1])
        S.op("pool", lambda h: h.tensor_tensor(out=rt2[:], in0=k2, in1=cosC[:], op=ALU.mult), reads=[Bkc32, Brope, Bkcr], writes=[Brt2])
        S.op("dve", lambda h: h.tensor_tensor(out=kcr[:, :, 64:128], in0=rt1[:], in1=rt2[:], op=ALU.add), reads=[Brt1, Brt2], writes=[Bkcr])
        for di, vw, Bvw in [(0, vwf, Bvwf), (1, vwb, Bvwb)]:
            bank = 2 + di
            for t in range(2):
                S.op("pe", lambda h, t=t, bank=bank, vw=vw: h.matmul(psf[bank][:, 0:128], lhsT=kcr[:, t, :], rhs=vw[:, t, :], start=(t == 0), stop=(t == 1)),
                     reads=[Bkcr, Bvw], writes=[Bpsf[bank]])
            S.op("act", lambda h, hh=hh, di=di, bank=bank: h.copy(out=R0[:, hh, di, :], in_=psf[bank][:, 0:128]), reads=[Bpsf[bank]], writes=[BR0])
    if debug:
        d_R0 = dbg_out("R0", [128, H, 2, 128])
        final_ops.append(S.dma("sp", S.chan("dbgR0"), lambda h: h.dma_start(out=d_R0, in_=R0[:]), reads=[BR0]))
    S.barrier()
    AR.reset(m2b)
    if stage <= 2:
        return finish(nc, S, final_ops), dbg

    m2c = AR.mark()
    wq_off = AR.mark()
    wq1 = AR.alloc("wq", [128, KC, 512], BF16)
    wq = [wq1, wq1]
    Bwq1 = Buf("wq")
    Bwq = [Bwq1, Bwq1]
    ch_wq1 = S.chan("wq")
    ch_wq = [ch_wq1, ch_wq1]
    qT = AR.alloc_at("qT", [128, SEQ], BF16, wq_off)
    qdfT = AR.alloc_at("qdfT", [128, SEQ], BF16, wq_off + 4096)
    qdbT = AR.alloc_at("qdbT", [128, SEQ], BF16, wq_off + 8192)
    kT = AR.alloc_at("kT", [128, SEQ], BF16, wq_off + 12288)
    BqT = BqdfT = BqdbT = BkT = Bwq1
    qk_off = AR.mark()
    qk32 = AR.alloc("qk32", [128, NT, 2, 2, 64], F32)
    Bqk32 = Buf("qk32")
    o32 = AR.alloc_at("o32", [128, NT, 128], F32, qk_off)
    ytok = AR.alloc_at("ytok", [128, NT, 128], BF16, qk_off + 8192)
    yTh = AR.alloc_at("yTh", [128, SEQ], BF16, qk_off + 12288)
    Bo32 = Bytok = ByTh = Bqk32
    ta_off = AR.mark()
    ta = AR.alloc("ta", [128, NT, 2, 64], F32)
    Bta = Buf("ta")
    Sfb = AR.alloc_at("Sfb", [128, NT, 128], BF16, ta_off)
    Sbb = AR.alloc_at("Sbb", [128, NT, 128], BF16, ta_off + 4096)
    BSfb = BSbb = Bta
    tb_off = AR.mark()
    tb = AR.alloc("tb", [128, NT, 2, 64], F32)
    Sf32 = AR.alloc_at("Sf32", [128, NT, 128], F32, tb_off)
    rotb_off = AR.mark()
    rotb = AR.alloc("rotb", [128, NT, 2, 2, 64], BF16)
    Sb32 = AR.alloc_at("Sb32", [128, NT, 128], F32, rotb_off)
    qdf = AR.alloc("qdf", [128, NT, 2, 64], BF16)
    qdb = AR.alloc("qdb", [128, NT, 2, 64], BF16)
    kdf = AR.alloc("kdf", [128, NT, 2, 64], BF16)
    kdb = AR.alloc("kdb", [128, NT, 2, 64], BF16)
    vtok = AR.alloc("vtok", [128, NT, 128], BF16)
    sg = AR.alloc_at("sg", [128, NT, 128], F32, hcT_off)
    Rf = AR.alloc("Rf", [128, 128], F32)
    Rb = AR.alloc("Rb", [128, 128], F32)
    SM = [AR.alloc(f"SM{i}", [128, 128], BF16) for i in range(2)]
    bnst = AR.alloc("bnst", [128, NT, 6], F32)
    mv = AR.alloc("mv", [128, NT, 2], F32)
    rstd = AR.alloc("rstd", [128, NT], F32)
    (Btb, Brotb, Bqdf, Bqdb, Bkdf, Bkdb, Bvtok, Bsg, BRf, BRb, Bbnst, Bmv, Brstd) = (
        Buf(n) for n in ["tb", "rotb", "qdf", "qdb", "kdf", "kdb", "vtok", "sg", "Rf", "Rb", "bnst", "mv", "rstd"])
    BSM = [Buf("SM0"), Buf("SM1")]
    ch_yT = S.chan("yTout")

    for hh in range(H):
        bi = hh % 2
        for seg in range(4):
            S.dma("pool", ch_wq[bi], lambda h, hh=hh, bi=bi, seg=seg: h.dma_start(out=wq[bi][:, :, seg * 128:(seg + 1) * 128],
                                                                                    in_=win_v[:, :, seg * DRET + hh * 128:seg * DRET + (hh + 1) * 128]),
                  writes=[Bwq[bi]], cont=(seg > 0))
        for i in range(NT):
            bank = i % 4
            for kc in range(KC):
                S.op("pe", lambda h, kc=kc, i=i, bi=bi, bank=bank: h.matmul(psf[bank][:, :], lhsT=hT[:, kc, i * 128:(i + 1) * 128], rhs=wq[bi][:, kc, :],
                                                                             start=(kc == 0), stop=(kc == KC - 1)),
                     reads=[BhT[i], Bwq[bi]], writes=[Bpsf[bank]])
            S.op("act", lambda h, i=i, bank=bank: h.copy(out=qk32[:, i, :, :, :], in_=psf[bank][:, 0:256].rearrange("p (a b c) -> p a b c", a=2, b=2)),
                 reads=[Bpsf[bank]], writes=[Bqk32])
            S.op("act", lambda h, i=i, bank=bank: h.copy(out=vtok[:, i, :], in_=psf[bank][:, 256:384]), reads=[Bpsf[bank]], writes=[Bvtok])
            S.op("act", lambda h, i=i, bank=bank: h.activation(out=sg[:, i, :], in_=psf[bank][:, 384:512], func=AF.Silu), reads=[Bpsf[bank]], writes=[Bsg])
        X1 = qk32[:, :, :, 0, :]
        X2 = qk32[:, :, :, 1, :]
        S.op("dve", lambda h: h.tensor_tensor(out=ta[:], in0=X1, in1=cosT, op=ALU.mult), reads=[Bqk32, Brope], writes=[Bta])
        S.op("pool", lambda h: h.tensor_tensor(out=tb[:], in0=X2, in1=sinT, op=ALU.mult), reads=[Bqk32, Brope], writes=[Btb])
        S.op("dve", lambda h: h.tensor_tensor(out=ta[:], in0=ta[:], in1=tb[:], op=ALU.subtract), reads=[Bta, Btb], writes=[Bta])
        S.op("pool", lambda h: h.tensor_tensor(out=tb[:], in0=X1, in1=sinT, op=ALU.mult), reads=[Bqk32, Brope, Bta], writes=[Btb])
        S.op("dve", lambda h: h.tensor_tensor(out=X1, in0=X2, in1=cosT, op=ALU.mult), reads=[Bqk32, Brope, Btb], writes=[Bqk32])
        S.op("dve", lambda h: h.tensor_tensor(out=tb[:], in0=tb[:], in1=X1, op=ALU.add), reads=[Btb, Bqk32], writes=[Btb])
        S.op("act", lambda h: h.mul(out=rotb[:, :, 0, 0, :], in_=ta[:, :, 0, :], mul=QSCALE), reads=[Bta], writes=[Brotb])
        S.op("act", lambda h: h.copy(out=rotb[:, :, 1, 0, :], in_=ta[:, :, 1, :]), reads=[Bta], writes=[Brotb])
        S.op("act", lambda h: h.mul(out=rotb[:, :, 0, 1, :], in_=tb[:, :, 0, :], mul=QSCALE), reads=[Btb], writes=[Brotb])
        S.op("act", lambda h: h.copy(out=rotb[:, :, 1, 1, :], in_=tb[:, :, 1, :]), reads=[Btb], writes=[Brotb])
        for (dst, Bd, src_i, col, sc) in [(qdf, Bqdf, 0, 0, QSCALE), (qdb, Bqdb, 0, 1, QSCALE), (kdf, Bkdf, 1, 2, 1.0), (kdb, Bkdb, 1, 3, 1.0)]:
            S.op("dve", lambda h, dst=dst, src_i=src_i, col=col, hh=hh, sc=sc: h.tensor_scalar(out=dst[:, :, 0, :], in0=ta[:, :, src_i, :], scalar1=dcol[:, hh, col:col + 1],
                                                                                              scalar2=sc, op0=ALU.mult, op1=ALU.mult), reads=[Bta, Bdcol], writes=[Bd])
            S.op("pool", lambda h, dst=dst, src_i=src_i, col=col, hh=hh, sc=sc: h.tensor_scalar(out=dst[:, :, 1, :], in0=tb[:, :, src_i, :], scalar1=dcol[:, hh, col:col + 1],
                                                                                               scalar2=sc, op0=ALU.mult, op1=ALU.mult), reads=[Btb, Bdcol], writes=[Bd])
        srcs = [(lambda i: rotb[:, i, 0, :, :].rearrange("p a b -> p (a b)"), Brotb, qT, BqT),
                (lambda i: qdf[:, i, :, :].rearrange("p a b -> p (a b)"), Bqdf, qdfT, BqdfT),
                (lambda i: qdb[:, i, :, :].rearrange("p a b -> p (a b)"), Bqdb, qdbT, BqdbT),
                (lambda i: rotb[:, i, 1, :, :].rearrange("p a b -> p (a b)"), Brotb, kT, BkT)]
        cnt = 0
        for (srcf, Bsrc, dstT, BdstT) in srcs:
            for half in range(2):
                pb = cnt % 2
                cnt += 1
                for q in range(8):
                    i = half * 8 + q
                    S.op("pe", lambda h, i=i, q=q, pb=pb, srcf=srcf: h.transpose(out=psb[pb][:, q * 128:(q + 1) * 128], in_=srcf(i), identity=identb[:]),
                         reads=[Bsrc, Bc], writes=[Bpsb[pb]])
                if pb == 0:
                    S.op("act", lambda h, half=half, pb=pb, dstT=dstT: h.copy(out=dstT[:, half * 1024:(half + 1) * 1024], in_=psb[pb][:, :]),
                         reads=[Bpsb[pb]], writes=[BdstT])
                else:
                    S.op("dve", lambda h, half=half, pb=pb, dstT=dstT: h.tensor_copy(out=dstT[:, half * 1024:(half + 1) * 1024], in_=psb[pb][:, :]),
                         reads=[Bpsb[pb]], writes=[BdstT])
        S.op("act", lambda h, hh=hh: h.copy(out=Sf32[:, 0, :], in_=R0[:, hh, 0, :]), reads=[BR0], writes=[Btb])
        S.op("act", lambda h, hh=hh: h.copy(out=Sb32[:, NT - 1, :], in_=R0[:, hh, 1, :]), reads=[BR0], writes=[Brotb])
        fbanks = [0, 1, 4]
        bbanks = [2, 3, 5]
        for step in range(NT - 1):
            i_f = step
            i_b = NT - 1 - step
            bf_ = fbanks[step % 3]
            bb_ = bbanks[step % 3]
            S.op("pe", lambda h, i=i_f, bf_=bf_: h.matmul(psf[bf_][:, 0:128], lhsT=kdf[:, i, :, :].rearrange("p a b -> p (a b)"), rhs=vtok[:, i, :], start=True, stop=True),
                 reads=[Bkdf, Bvtok], writes=[Bpsf[bf_]])
            S.op("act", lambda h, i=i_f, bf_=bf_: h.copy(out=Sf32[:, i + 1, :], in_=psf[bf_][:, 0:128]), reads=[Bpsf[bf_]], writes=[Btb])
            S.op("pe", lambda h, i=i_b, bb_=bb_: h.matmul(psf[bb_][:, 0:128], lhsT=kdb[:, i, :, :].rearrange("p a b -> p (a b)"), rhs=vtok[:, i, :], start=True, stop=True),
                 reads=[Bkdb, Bvtok], writes=[Bpsf[bb_]])
            S.op("act", lambda h, i=i_b, bb_=bb_: h.copy(out=Sb32[:, i - 1, :], in_=psf[bb_][:, 0:128]), reads=[Bpsf[bb_]], writes=[Brotb])
        for step in range(NT - 1):
            i_f = step
            i_b = NT - 1 - step
            S.op("dve", lambda h, hh=hh, i=i_f: h.scalar_tensor_tensor(out=Sf32[:, i + 1, :], in0=Sf32[:, i, :], scalar=dcol[:, hh, 4:5], in1=Sf32[:, i + 1, :], op0=ALU.mult, op1=ALU.add),
                 reads=[Btb, Bdcol], writes=[Btb])
            S.op("dve", lambda h, hh=hh, i=i_b: h.scalar_tensor_tensor(out=Sb32[:, i - 1, :], in0=Sb32[:, i, :], scalar=dcol[:, hh, 5:6], in1=Sb32[:, i - 1, :], op0=ALU.mult, op1=ALU.add),
                 reads=[Brotb, Bdcol], writes=[Brotb])
        S.op("act", lambda h: h.copy(out=Sfb[:], in_=Sf32[:]), reads=[Btb], writes=[BSfb])
        S.op("act", lambda h: h.copy(out=Sbb[:], in_=Sb32[:]), reads=[Brotb], writes=[BSbb])
        for i in range(NT):
            sb_ = i % 2
            bs = i % 2
            bo = 2 + (i % 2)
            cs = slice(i * 128, (i + 1) * 128)
            S.op("pe", lambda h, cs=cs, bs=bs: h.matmul(psf[bs][:, 0:128], lhsT=kT[:, cs], rhs=qT[:, cs], start=True, stop=True),
                 reads=[BkT, BqT], writes=[Bpsf[bs]])
            S.op("dve", lambda h, bs=bs, sb_=sb_, hh=hh: h.tensor_tensor(out=SM[sb_][:], in0=psf[bs][:, 0:128], in1=Mh[:, hh, :], op=ALU.mult),
                 reads=[Bpsf[bs], BMh], writes=[BSM[sb_]])
            S.op("pe", lambda h, i=i, sb_=sb_, bo=bo: h.matmul(psf[bo][:, 0:128], lhsT=SM[sb_][:], rhs=vtok[:, i, :], start=True, stop=False),
                 reads=[BSM[sb_], Bvtok], writes=[Bpsf[bo]])
            S.op("pe", lambda h, i=i, cs=cs, bo=bo: h.matmul(psf[bo][:, 0:128], lhsT=qdfT[:, cs], rhs=Sfb[:, i, :], start=False, stop=False),
                 reads=[BqdfT, BSfb], writes=[Bpsf[bo]])
            S.op("pe", lambda h, i=i, cs=cs, bo=bo: h.matmul(psf[bo][:, 0:128], lhsT=qdbT[:, cs], rhs=Sbb[:, i, :], start=False, stop=True),
                 reads=[BqdbT, BSbb], writes=[Bpsf[bo]])
            S.op("act", lambda h, i=i, bo=bo: h.copy(out=o32[:, i, :], in_=psf[bo][:, 0:128]), reads=[Bpsf[bo]], writes=[Bo32])
            S.op("dve", lambda h, i=i: h.bn_stats(out=bnst[:, i, :], in_=o32[:, i, :]), reads=[Bo32], writes=[Bbnst])
            S.op("dve", lambda h, i=i: h.bn_aggr(out=mv[:, i, :], in_=bnst[:, i, :]), reads=[Bbnst], writes=[Bmv])
        S.op("act", lambda h: h.activation(out=rstd[:], in_=mv[:, :, 1], func=AF.Sqrt, bias=GN_EPS), reads=[Bmv], writes=[Brstd])
        S.op("dve", lambda h: h.reciprocal(out=rstd[:], in_=rstd[:]), reads=[Brstd], writes=[Brstd])
        S.op("pool", lambda h, hh=hh: h.tensor_tensor(out=sg[:], in0=sg[:], in1=gnwb[:, hh * 128:(hh + 1) * 128].unsqueeze(1).to_broadcast([128, NT, 128]), op=ALU.mult),
             reads=[Bsg, Bgnwb], writes=[Bsg])
        for i in range(NT):
            S.op("dve", lambda h, i=i: h.tensor_scalar(out=o32[:, i, :], in0=o32[:, i, :], scalar1=mv[:, i, 0:1], scalar2=rstd[:, i:i + 1], op0=ALU.subtract, op1=ALU.mult),
                 reads=[Bo32, Bmv, Brstd], writes=[Bo32])
        S.op("pool", lambda h: h.tensor_tensor(out=ytok[:], in0=o32[:], in1=sg[:], op=ALU.mult), reads=[Bo32, Bsg], writes=[Bytok])
        for half in range(2):
            pb = half
            for q in range(8):
                i = half * 8 + q
                S.op("pe", lambda h, i=i, q=q, pb=pb: h.transpose(out=psb[pb][:, q * 128:(q + 1) * 128], in_=ytok[:, i, :], identity=identb[:]),
                     reads=[Bytok, Bc], writes=[Bpsb[pb]])
            if half == 0:
                S.op("act", lambda h, half=half, pb=pb: h.copy(out=yTh[:, half * 1024:(half + 1) * 1024], in_=psb[pb][:, :]), reads=[Bpsb[pb]], writes=[ByTh])
            else:
                S.op("dve", lambda h, half=half, pb=pb: h.tensor_copy(out=yTh[:, half * 1024:(half + 1) * 1024], in_=psb[pb][:, :]), reads=[Bpsb[pb]], writes=[ByTh])
        S.dma("sp", ch_yT, lambda h, hh=hh: h.dma_start(out=yT_s[hh * 128:(hh + 1) * 128, :], in_=yTh[:]), reads=[ByTh], writes=[ByT_s])
    S.barrier()
    AR.reset(m2c)
    if stage <= 3:
        if debug:
            d_yT = dbg_out("yT", [D, SEQ], BF16)
            ld = AR.alloc("dbgld", [128, KC, SEQ], BF16)
            Bld = Buf("dbgld")
            S.dma("sp", S.chan("dbgy1"), lambda h: h.dma_start(out=ld[:], in_=yT_s.rearrange("(c p) t -> p c t", p=128)), reads=[ByT_s], writes=[Bld])
            final_ops.append(S.dma("sp", S.chan("dbgy2"), lambda h: h.dma_start(out=d_yT.rearrange("(c p) t -> p c t", p=128), in_=ld[:]), reads=[Bld]))
        return finish(nc, S, final_ops), dbg

    m2d = AR.mark()
    cvrows = AR.alloc("cvrows", [24, 128], F32)
    cvT = AR.alloc("cvT", [128, 24], F32)
    cwrows = AR.alloc("cwrows", [CW, DCONV], F32)
    cwT = AR.alloc("cwT", [128, 8, CW], F32)
    Bcvrows, BcvT, Bcwrows, BcwT = Buf("cvrows"), Buf("cvT"), Buf("cwrows"), Buf("cwT")
    S.dma("sp", ch_v, lambda h: h.dma_start(out=cvrows[:], in_=cvec_d), writes=[Bcvrows])
    S.dma("sp", ch_v, lambda h: h.dma_start(out=cwrows[:], in_=convw_d), writes=[Bcwrows], cont=True)
    transpose_rows(cvrows[:], 24, cvT[:], 0, Bpsf[0], [Bcvrows], [BcvT])
    for cc in range(8):
        S.op("pe", lambda h, cc=cc: h.transpose(out=psf[1][:, 0:CW], in_=cwrows[:, cc * 128:(cc + 1) * 128], identity=ident[0:CW, 0:CW]),
             reads=[Bcwrows, Bc], writes=[Bpsf[1]])
        S.op("act", lambda h, cc=cc: h.copy(out=cwT[:, cc, :], in_=psf[1][:, 0:CW]), reads=[Bpsf[1]], writes=[BcwT])
    wc = [AR.alloc(f"wc{i}", [128, KC, 256], BF16) for i in range(2)]
    Bwc = [Buf("wc0"), Buf("wc1")]
    ch_wc = [S.chan("wc0"), S.chan("wc1")]
    sig = AR.alloc("sig", [128, 512], F32)
    uu = AR.alloc("uu", [128, 8, 64], F32)
    cvo = AR.alloc("cvo", [128, 8, 8, 64], F32)
    cv2 = AR.alloc("cv2", [128, 8, 64], F32)
    Bcv2 = Buf("cv2")
    sq = AR.alloc("sq", [128, 512], F32)
    mean = AR.alloc("mean", [128, 512], F32)
    msq = AR.alloc("msq", [128, 512], F32)
    rsd = AR.alloc("rsd", [128, 512], F32)
    tn = AR.alloc("tn", [128, 512], F32)
    tns = [tn, AR.alloc("tn2", [128, 512], F32)]
    Btns = [Buf("tn_0"), Buf("tn_1")]
    ycv = [AR.alloc(f"ycv{i}", [128, 512], BF16) for i in range(2)]
    Bsig, Buu, Bcvo, Bsq, Bmean, Bmsq, Brsd, Btn = (Buf(n) for n in ["sig", "uu", "cvo", "sq", "mean", "msq", "rsd", "tn"])
    Bycv = [Buf("ycv0"), Buf("ycv1")]
    ch_ycv = [S.chan("ycv0"), S.chan("ycv1")]
    pcount = 0
    for tbk in range(4):
        tsl = slice(tbk * 512, (tbk + 1) * 512)
        for cc in range(8):
            bi = pcount % 2
            pcount += 1
            S.dma("pool", ch_wc[bi], lambda h, cc=cc, bi=bi: h.dma_start(out=wc[bi][:, :, 0:128], in_=win_v[:, :, 4 * DRET + cc * 128:4 * DRET + (cc + 1) * 128]),
                  writes=[Bwc[bi]])
            S.dma("pool", ch_wc[bi], lambda h, cc=cc, bi=bi: h.dma_start(out=wc[bi][:, :, 128:256],
                                                                          in_=win_v[:, :, 4 * DRET + DCONV + cc * 128:4 * DRET + DCONV + (cc + 1) * 128]),
                  writes=[Bwc[bi]], cont=True)
            ba, bb = 0 + 2 * (cc % 2), 1 + 2 * (cc % 2)
            for kc in range(KC):
                S.op("pe", lambda h, kc=kc, bi=bi, ba=ba, tsl=tsl: h.matmul(psf[ba][:, :], lhsT=wc[bi][:, kc, 0:128], rhs=hT[:, kc, tsl], start=(kc == 0), stop=(kc == KC - 1)),
                     reads=[Bwc[bi]] + BhT[tbk * 4:(tbk + 1) * 4], writes=[Bpsf[ba]])
            for kc in range(KC):
                S.op("pe", lambda h, kc=kc, bi=bi, bb=bb, tsl=tsl: h.matmul(psf[bb][:, :], lhsT=wc[bi][:, kc, 128:256], rhs=hT[:, kc, tsl], start=(kc == 0), stop=(kc == KC - 1)),
                     reads=[Bwc[bi]] + BhT[tbk * 4:(tbk + 1) * 4], writes=[Bpsf[bb]])
            S.op("act", lambda h, bb=bb: h.activation(out=sig[:], in_=psf[bb][:, :], func=AF.Sigmoid), reads=[Bpsf[bb]], writes=[Bsig])
            S.op("dve", lambda h, ba=ba: h.tensor_tensor(out=uu[:].rearrange("p a b -> p (a b)"), in0=psf[ba][:, :], in1=sig[:], op=ALU.mult),
                 reads=[Bpsf[ba], Bsig], writes=[Buu])
            acc = cvo[:, cc, :, :]
            S.op("dve", lambda h, cc=cc, acc=acc: h.tensor_scalar(out=acc, in0=uu[:], scalar1=cwT[:, cc, 15:16], scalar2=cvT[:, cc:cc + 1], op0=ALU.mult, op1=ALU.add),
                 reads=[Buu, BcwT, BcvT], writes=[Bcvo])
            S.op("dve", lambda h: h.memset(cv2[:], 0.0), writes=[Bcv2])
            taps = [k for k in range(CW) if k != 15]
            for ti, k in enumerate(taps):
                o = k - 15
                lo, hi = max(0, -o), min(64, 64 - o)
                if ti % 2 == 0:
                    S.op("dve", lambda h, cc=cc, k=k, o=o, lo=lo, hi=hi: h.scalar_tensor_tensor(out=cv2[:, :, lo:hi], in0=uu[:, :, lo + o:hi + o], scalar=cwT[:, cc, k:k + 1],
                                                                                                 in1=cv2[:, :, lo:hi], op0=ALU.mult, op1=ALU.add),
                         reads=[Buu, BcwT, Bcv2], writes=[Bcv2])
                else:
                    S.op("dve", lambda h, cc=cc, k=k, o=o, lo=lo, hi=hi: h.scalar_tensor_tensor(out=cvo[:, cc, :, lo:hi], in0=uu[:, :, lo + o:hi + o], scalar=cwT[:, cc, k:k + 1],
                                                                                                 in1=cvo[:, cc, :, lo:hi], op0=ALU.mult, op1=ALU.add),
                         reads=[Buu, BcwT, Bcvo], writes=[Bcvo])
            S.op("dve", lambda h, acc=acc: h.tensor_tensor(out=acc, in0=acc, in1=cv2[:], op=ALU.add), reads=[Bcvo, Bcv2], writes=[Bcvo])
            accf = cvo[:, cc, :, :].rearrange("p a b -> p (a b)")
            S.op("act", lambda h, accf=accf: h.activation(out=sq[:], in_=accf, func=AF.Square), reads=[Bcvo], writes=[Bsq])
            S.op("pe", lambda h, accf=accf, cc=cc: h.matmul(psf[4][:, :], lhsT=ones32[:], rhs=accf, start=(cc == 0), stop=(cc == 7)), reads=[Bcvo, Bc], writes=[Bpsf[4]])
            S.op("pe", lambda h, cc=cc: h.matmul(psf[5][:, :], lhsT=ones32[:], rhs=sq[:], start=(cc == 0), stop=(cc == 7)), reads=[Bsq, Bc], writes=[Bpsf[5]])
        S.op("act", lambda h: h.activation(out=mean[:], in_=psf[4][:, :], func=AF.Identity, scale=1.0 / DCONV), reads=[Bpsf[4]], writes=[Bmean])
        S.op("dve", lambda h: h.tensor_tensor(out=msq[:], in0=mean[:], in1=mean[:], op=ALU.mult), reads=[Bmean], writes=[Bmsq])
        S.op("dve", lambda h: h.scalar_tensor_tensor(out=rsd[:], in0=psf[5][:, :], scalar=1.0 / DCONV, in1=msq[:], op0=ALU.mult, op1=ALU.subtract),
             reads=[Bpsf[5], Bmsq], writes=[Brsd])
        S.op("act", lambda h: h.activation(out=rsd[:], in_=rsd[:], func=AF.Sqrt, bias=EPS), reads=[Brsd], writes=[Brsd])
        S.op("dve", lambda h: h.reciprocal(out=rsd[:], in_=rsd[:]), reads=[Brsd], writes=[Brsd])
        for cc in range(8):
            yb = cc % 2
            accf = cvo[:, cc, :, :].rearrange("p a b -> p (a b)")
            tnb = tns[cc % 2]
            Btnb = Btns[cc % 2]
            S.op("dve", lambda h, accf=accf, tnb=tnb: h.tensor_tensor(out=tnb[:], in0=accf, in1=mean[:], op=ALU.subtract), reads=[Bcvo, Bmean], writes=[Btnb])
            S.op("dve", lambda h, tnb=tnb: h.tensor_tensor(out=tnb[:], in0=tnb[:], in1=rsd[:], op=ALU.mult), reads=[Btnb, Brsd], writes=[Btnb])
            S.op("act", lambda h, cc=cc, yb=yb, tnb=tnb: h.activation(out=ycv[yb][:], in_=tnb[:], func=AF.Silu, scale=cvT[:, 8 + cc:9 + cc], bias=cvT[:, 16 + cc:17 + cc]),
                 reads=[Btnb, BcvT], writes=[Bycv[yb]])
            S.dma("sp", ch_ycv[yb], lambda h, cc=cc, yb=yb, tsl=tsl: h.dma_start(out=yT_s[DRET + cc * 128:DRET + (cc + 1) * 128, tsl], in_=ycv[yb][:]),
                  reads=[Bycv[yb]], writes=[ByT_s])
    S.barrier()
    AR.reset(m2)
    AR.reset(persist_mark)
    if stage <= 4:
        if debug:
            d_yT = dbg_out("yT", [D, SEQ], BF16)
            ld = AR.alloc("dbgld", [128, KC, SEQ], BF16)
            Bld = Buf("dbgld")
            S.dma("sp", S.chan("dbgy1"), lambda h: h.dma_start(out=ld[:], in_=yT_s.rearrange("(c p) t -> p c t", p=128)), reads=[ByT_s], writes=[Bld])
            final_ops.append(S.dma("sp", S.chan("dbgy2"), lambda h: h.dma_start(out=d_yT.rearrange("(c p) t -> p c t", p=128), in_=ld[:]), reads=[Bld]))
        return finish(nc, S, final_ops), dbg

    m3 = AR.mark()
    wo = AR.alloc("wo", [128, KC, D], BF16)
    Bwo = Buf("wo")
    ch_wo = S.chan("wo")
    wout_v = wout_d.rearrange("(k p) n -> p k n", p=128)
    for j in range(4):
        S.dma("pool", ch_wo, lambda h, j=j: h.dma_start(out=wo[:, :, j * 512:(j + 1) * 512], in_=wout_v[:, :, j * 512:(j + 1) * 512]), writes=[Bwo], cont=(j > 0))
    G1 = AR.alloc("G1", [128, D], F32)
    A2 = AR.alloc("A2", [128, D], F32)
    S2 = AR.alloc("S2", [128, D], F32)
    wt3 = AR.alloc("wt3", [128, D], F32)
    BG1, BA2, BS2, Bwt3 = Buf("G1"), Buf("A2"), Buf("S2"), Buf("wt3")
    load_mod_bcast(G1, 0, 2, ch_v, BG1)
    load_row_bcast(wt3, pomw_d, ch_v, Bwt3)
    S.op("dve", lambda h: h.tensor_tensor(out=G1[:], in0=G1[:], in1=wt3[:], op=ALU.mult), reads=[BG1, Bwt3], writes=[BG1])
    load_mod_bcast(A2, 0, 4, ch_v, BA2)
    load_row_bcast(wt3, pfw_d, ch_v, Bwt3)
    S.op("dve", lambda h: h.scalar_tensor_tensor(out=A2[:], in0=A2[:], scalar=1.0, in1=wt3[:], op0=ALU.add, op1=ALU.mult), reads=[BA2, Bwt3], writes=[BA2])
    load_mod_bcast(S2, 0, 3, ch_v, BS2)
    rw32 = AR.alloc("rw32", [128, KC, NE], F32)
    rbb = AR.alloc("rbb", [128, NE], F32)
    ebase = AR.alloc("ebase", [128, NE], F32)
    Brw, Brbb, Bebase = Buf("rw32"), Buf("rbb"), Buf("ebase")
    S.dma("sp", ch_v, lambda h: h.dma_start(out=rw32[:], in_=rw_d.rearrange("(k p) e -> p k e", p=128)), writes=[Brw])
    S.dma("sp", ch_v, lambda h: h.dma_start(out=rbb[:], in_=rb_d.partition_broadcast(128)), writes=[Brbb], cont=True)
    S.op("dve", lambda h: h.tensor_scalar(out=ebase[:], in0=iorow[:, 0:NE], scalar1=float(CAP), scalar2=None, op0=ALU.mult), reads=[Bc], writes=[Bebase])
    yTt = [AR.alloc(f"yTt{i}", [128, KC, 128], BF16) for i in range(2)]
    ByTt = [Buf("yTt0"), Buf("yTt1")]
    ch_yTt = [S.chan("yTt0"), S.chan("yTt1")]
    xr = [AR.alloc(f"xr{i}", [128, D], F32) for i in range(2)]
    Bxr = [Buf("xr0"), Buf("xr1")]
    ch_xr = [S.chan("xr0"), S.chan("xr1")]
    t3 = AR.alloc("t3", [128, D], F32)
    x1t = [AR.alloc(f"x1t{i}", [128, D], F32) for i in range(2)]
    h2f = AR.alloc("h2f", [128, D], F32)
    h2b = [AR.alloc(f"h2b{i}", [128, D], BF16) for i in range(2)]
    h2T = AR.alloc("h2T", [128, KC, 128], F32)
    junk3 = AR.alloc("junk3", [128, D], BF16)
    st3 = AR.alloc("st3", [128, 8], F32)
    lg = AR.alloc("lg", [128, NE], F32)
    mx8 = AR.alloc("mx8", [128, 8], F32)
    nmx = AR.alloc("nmx", [128, 1], F32)
    msk = AR.alloc("msk", [128, NE], F32)
    mskb = AR.alloc("mskb", [128, NT, NE], BF16)
    exv = AR.alloc("exv", [128, NE], F32)
    den = AR.alloc("den", [128, 1], F32)
    posC = AR.alloc("posC", [128, NE], F32)
    ovf = AR.alloc("ovf", [128, NE], F32)
    oh = AR.alloc("oh", [128, NE], F32)
    jk = AR.alloc("jk", [128, NE], F32)
    idxf = AR.alloc("idxf", [128, 4], F32)
    idl = AR.alloc("idl", [128, 4], F32)
    idn = AR.alloc("idn", [128, 4], F32)
    Bidl, Bidn = Buf("idl"), Buf("idn")
    (Bt3, Bh2f, Bh2T, Bjunk3, Bst3, Blg, Bmx8, Bnmx, Bmsk, Bmskb, Bexv, Bden, BposC, Bovf, Boh, Bjk, Bidxf) = (
        Buf(n) for n in ["t3", "h2f", "h2T", "junk3", "st3", "lg", "mx8", "nmx", "msk", "mskb", "exv", "den", "posC", "ovf", "oh", "jk", "idxf"])
    Bx1t = [Buf("x1t0"), Buf("x1t1")]
    Bh2b = [Buf("h2b0"), Buf("h2b1")]
    ch_x1 = [S.chan("x1w0"), S.chan("x1w1")]
    ch_sc = [S.chan(f"scat{i}") for i in range(2)]
    Bx1_s = [Buf(f"x1_s{i}") for i in range(NT)]
    Bhsel = Buf("hsel_s")
    yT_v = yT_s.rearrange("(c p) t -> p c t", p=128)
    mixps = [psf[0], psf[1], psf[2], psf[3]]
    for i in range(NT):
        bi = i % 2
        S.dma("sp", ch_yTt[bi], lambda h, i=i, bi=bi: h.dma_start(out=yTt[bi][:], in_=yT_v[:, :, i * 128:(i + 1) * 128]), reads=[ByT_s], writes=[ByTt[bi]])
        S.dma("sp", ch_xr[bi], lambda h, i=i, bi=bi: h.dma_start(out=xr[bi][:], in_=x_d[i * 128:(i + 1) * 128, :]), writes=[Bxr[bi]])
        for cb in range(4):
            for c in range(KC):
                S.op("pe", lambda h, c=c, cb=cb, bi=bi: h.matmul(psf[cb][:, :], lhsT=yTt[bi][:, c, :], rhs=wo[:, c, cb * 512:(cb + 1) * 512], start=(c == 0), stop=(c == KC - 1)),
                     reads=[ByTt[bi], Bwo], writes=[Bpsf[cb]])
        for cb in range(4):
            S.op("act", lambda h, cb=cb: h.activation(out=junk3[:, cb * 512:(cb + 1) * 512], in_=psf[cb][:, :], func=AF.Square, accum_out=st3[:, cb:cb + 1]),
                 reads=[Bpsf[cb]], writes=[Bjunk3, Bst3])
        S.op("dve", lambda h: h.tensor_reduce(out=st3[:, 4:5], in_=st3[:, 0:4], axis=AX.X, op=ALU.add), reads=[Bst3], writes=[Bst3])
        S.op("act", lambda h: h.activation(out=st3[:, 4:5], in_=st3[:, 4:5], func=AF.Sqrt, scale=1.0 / D, bias=EPS), reads=[Bst3], writes=[Bst3])
        S.op("dve", lambda h: h.reciprocal(out=st3[:, 4:5], in_=st3[:, 4:5]), reads=[Bst3], writes=[Bst3])
        for cb in range(4):
            S.op("dve", lambda h, cb=cb: h.scalar_tensor_tensor(out=t3[:, cb * 512:(cb + 1) * 512], in0=psf[cb][:, :], scalar=st3[:, 4:5], in1=G1[:, cb * 512:(cb + 1) * 512],
                                                                op0=ALU.mult, op1=ALU.mult), reads=[Bpsf[cb], Bst3, BG1], writes=[Bt3])
        S.op("dve", lambda h, bi=bi: h.tensor_tensor(out=x1t[bi][:], in0=t3[:], in1=xr[bi][:], op=ALU.add), reads=[Bt3, Bxr[bi]], writes=[Bx1t[bi]])
        S.dma("sp", ch_x1[bi], lambda h, i=i, bi=bi: h.dma_start(out=x1_s[i * 128:(i + 1) * 128, :], in_=x1t[bi][:]), reads=[Bx1t[bi]], writes=[Bx1_s[i]])
        S.op("act", lambda h, bi=bi: h.activation(out=junk3[:], in_=x1t[bi][:], func=AF.Square, accum_out=st3[:, 5:6]), reads=[Bx1t[bi]], writes=[Bjunk3, Bst3])
        S.op("act", lambda h: h.activation(out=st3[:, 5:6], in_=st3[:, 5:6], func=AF.Sqrt, scale=1.0 / D, bias=EPS), reads=[Bst3], writes=[Bst3])
        S.op("dve", lambda h: h.reciprocal(out=st3[:, 5:6], in_=st3[:, 5:6]), reads=[Bst3], writes=[Bst3])
        S.op("dve", lambda h, bi=bi: h.scalar_tensor_tensor(out=t3[:], in0=x1t[bi][:], scalar=st3[:, 5:6], in1=A2[:], op0=ALU.mult, op1=ALU.mult),
             reads=[Bx1t[bi], Bst3, BA2], writes=[Bt3])
        S.op("dve", lambda h: h.tensor_tensor(out=h2f[:], in0=t3[:], in1=S2[:], op=ALU.add), reads=[Bt3, BS2], writes=[Bh2f])
        S.op("act", lambda h, bi=bi: h.copy(out=h2b[bi][:], in_=h2f[:]), reads=[Bh2f], writes=[Bh2b[bi]])
        for q4 in range(4):
            bank = 4 + (q4 % 2)
            for q in range(4):
                kc = q4 * 4 + q
                S.op("pe", lambda h, kc=kc, q=q, bank=bank: h.transpose(out=psf[bank][:, q * 128:(q + 1) * 128], in_=h2f[:, kc * 128:(kc + 1) * 128], identity=ident[:]),
                     reads=[Bh2f, Bc], writes=[Bpsf[bank]])
            if q4 % 2 == 0:
                S.op("act", lambda h, q4=q4, bank=bank: h.copy(out=h2T[:, q4 * 4:(q4 + 1) * 4, :], in_=psf[bank][:, :].rearrange("p (q t) -> p q t", q=4)),
                     reads=[Bpsf[bank]], writes=[Bh2T])
            else:
                S.op("dve", lambda h, q4=q4, bank=bank: h.tensor_copy(out=h2T[:, q4 * 4:(q4 + 1) * 4, :], in_=psf[bank][:, :].rearrange("p (q t) -> p q t", q=4)),
                     reads=[Bpsf[bank]], writes=[Bh2T])
        for kc in range(KC):
            S.op("pe", lambda h, kc=kc: h.matmul(psf[4][:, 0:NE], lhsT=h2T[:, kc, :], rhs=rw32[:, kc, :], start=(kc == 0), stop=(kc == KC - 1)),
                 reads=[Bh2T, Brw], writes=[Bpsf[4]])
        S.op("dve", lambda h: h.tensor_tensor(out=lg[:], in0=psf[4][:, 0:NE], in1=rbb[:], op=ALU.add), reads=[Bpsf[4], Brbb], writes=[Blg])
        S.op("dve", lambda h: h.max(out=mx8[:], in_=lg[:]), reads=[Blg], writes=[Bmx8])
        S.op("dve", lambda h: h.tensor_scalar(out=msk[:], in0=lg[:], scalar1=mx8[:, 3:4], scalar2=None, op0=ALU.is_ge), reads=[Blg, Bmx8], writes=[Bmsk])
        S.op("dve", lambda h, i=i: h.tensor_copy(out=mskb[:, i, :], in_=msk[:]), reads=[Bmsk], writes=[Bmskb])
        S.op("dve", lambda h: h.tensor_scalar(out=nmx[:], in0=mx8[:, 0:1], scalar1=-1.0, scalar2=None, op0=ALU.mult), reads=[Bmx8], writes=[Bnmx])
        S.op("act", lambda h: h.activation(out=exv[:], in_=lg[:], func=AF.Exp, bias=nmx[:, 0:1]), reads=[Blg, Bnmx], writes=[Bexv])
        S.op("dve", lambda h: h.tensor_tensor(out=exv[:], in0=exv[:], in1=msk[:], op=ALU.mult), reads=[Bexv, Bmsk], writes=[Bexv])
        S.op("dve", lambda h: h.tensor_reduce(out=den[:], in_=exv[:], axis=AX.X, op=ALU.add), reads=[Bexv], writes=[Bden])
        S.op("dve", lambda h: h.reciprocal(out=den[:], in_=den[:]), reads=[Bden], writes=[Bden])
        S.op("pe", lambda h, i=i: h.matmul(psf[5][:, 0:NE], lhsT=trib[:], rhs=mskb[:, i, :], start=True, stop=(i == 0)), reads=[Bmskb, Bc], writes=[Bpsf[5]])
        for j in range(i):
            S.op("pe", lambda h, j=j, i=i: h.matmul(psf[5][:, 0:NE], lhsT=onesb[:], rhs=mskb[:, j, :], start=False, stop=(j == i - 1)), reads=[Bmskb, Bc], writes=[Bpsf[5]])
        S.op("dve", lambda h: h.tensor_scalar(out=ovf[:], in0=psf[5][:, 0:NE], scalar1=float(CAP) - 0.5, scalar2=None, op0=ALU.is_gt), reads=[Bpsf[5]], writes=[Bovf])
        S.op("dve", lambda h: h.tensor_tensor(out=posC[:], in0=psf[5][:, 0:NE], in1=ebase[:], op=ALU.add), reads=[Bpsf[5], Bebase], writes=[BposC])
        S.op("dve", lambda h: h.scalar_tensor_tensor(out=posC[:], in0=ovf[:], scalar=BIG, in1=posC[:], op0=ALU.mult, op1=ALU.add), reads=[Bovf, BposC], writes=[BposC])
        S.op("dve", lambda h: h.tensor_scalar(out=ovf[:], in0=ovf[:], scalar1=-1.0, scalar2=1.0, op0=ALU.mult, op1=ALU.add), reads=[Bovf], writes=[Bovf])
        S.op("dve", lambda h, i=i: h.scalar_tensor_tensor(out=gatesA[:, i, :], in0=exv[:], scalar=den[:, 0:1], in1=ovf[:], op0=ALU.mult, op1=ALU.mult),
             reads=[Bexv, Bden, Bovf], writes=[BgatesA])
        for k in range(4):
            S.op("dve", lambda h, k=k: h.tensor_scalar(out=oh[:], in0=lg[:], scalar1=mx8[:, k:k + 1], scalar2=None, op0=ALU.is_equal), reads=[Blg, Bmx8], writes=[Boh])
            S.op("dve", lambda h, k=k: h.scalar_tensor_tensor(out=jk[:], in0=oh[:], scalar=1.0, in1=posC[:], op0=ALU.mult, op1=ALU.mult, accum_out=idxf[:, k:k + 1]),
                 reads=[Boh, BposC], writes=[Bjk, Bidxf])
            S.op("dve", lambda h, k=k, i=i: h.scalar_tensor_tensor(out=jk[:], in0=oh[:], scalar=1.0, in1=gatesA[:, i, :], op0=ALU.mult, op1=ALU.mult,
                                                                 accum_out=gate4[:, i, k:k + 1]), reads=[Boh, BgatesA], writes=[Bjk, Bgate4])
        for (lst, nten, Bl) in [(idxH, NHS, Bidx[i]), (idxY, NYS, Bidx[i])]:
            for j in range(nten):
                shift = float(j * (NE // nten) * CAP)
                S.op("dve", lambda h, shift=shift: h.tensor_scalar(out=idl[:], in0=idxf[:], scalar1=shift, scalar2=None, op0=ALU.subtract), reads=[Bidxf], writes=[Bidl])
                S.op("dve", lambda h: h.tensor_scalar(out=idn[:], in0=idl[:], scalar1=0.0, scalar2=BIG, op0=ALU.is_lt, op1=ALU.mult), reads=[Bidl], writes=[Bidn])
                S.op("dve", lambda h: h.tensor_tensor(out=idl[:], in0=idl[:], in1=idn[:], op=ALU.add), reads=[Bidl, Bidn], writes=[Bidl])
                S.op("dve", lambda h, i=i, t=lst[j]: h.tensor_copy(out=t[:, i * 4:(i + 1) * 4], in_=idl[:]), reads=[Bidl], writes=[Bl])
        first = True
        for k in range(4):
            for j in range(NHS):
                S.dma("pool", ch_sc[bi], lambda h, i=i, k=k, bi=bi, j=j: h.indirect_dma_start(out=hsel_s[j], out_offset=bass.IndirectOffsetOnAxis(ap=idxH[j][:, i * 4 + k:i * 4 + k + 1], axis=0),
                                                                                           in_=h2b[bi][:], in_offset=None, bounds_check=bound_reg(h, (NE // NHS) * CAP - 1), oob_is_err=False),
                      reads=[Bh2b[bi], Bidx[i]], writes=[Bhsel], cont=(not first))
                first = False
    for j in range(NT):
        S.op("pe", lambda h, j=j: h.matmul(psf[5][:, 0:NE], lhsT=onesb[:], rhs=mskb[:, j, :], start=(j == 0), stop=(j == NT - 1)), reads=[Bmskb, Bc], writes=[Bpsf[5]])
    S.cnt_op = S.op("dve", lambda h: h.tensor_copy(out=cnt_i[:], in_=psf[5][:, 0:NE]), reads=[Bpsf[5]], writes=[Bcnt])
    S.cnt_ap = lambda e: cnt_i[0:1, e:e + 1]
    if debug:
        d_lg = dbg_out("gates", [128, NT, NE])
        final_ops.append(S.dma("sp", S.chan("dbglg"), lambda h: h.dma_start(out=d_lg, in_=gatesA[:]), reads=[BgatesA]))
        d_idx = dbg_out("idx4", [128, NT * 4], I32)
        final_ops.append(S.dma("sp", S.chan("dbgidx"), lambda h: h.dma_start(out=d_idx, in_=idxY[0][:]), reads=Bidx))
        d_cnt = dbg_out("cnt", [128, NE], I32)
        final_ops.append(S.dma("sp", S.chan("dbgcnt"), lambda h: h.dma_start(out=d_cnt, in_=cnt_i[:]), reads=[Bcnt]))
        d_g4 = dbg_out("gate4", [128, NT, 4])
        final_ops.append(S.dma("sp", S.chan("dbgg4"), lambda h: h.dma_start(out=d_g4, in_=gate4[:]), reads=[Bgate4]))
    S.barrier()
    AR.reset(m3)
    if stage <= 5:
        if debug:
            d_x1 = dbg_out("x1", [SEQ, D])
            ld = AR.alloc("dbgld", [128, NT, D], F32)
            Bld = Buf("dbgld")
            S.dma("sp", S.chan("dbgx1"), lambda h: h.dma_start(out=ld[:], in_=x1_s.rearrange("(t p) d -> p t d", p=128)), reads=Bx1_s, writes=[Bld])
            final_ops.append(S.dma("sp", S.chan("dbgx2"), lambda h: h.dma_start(out=d_x1.rearrange("(t p) d -> p t d", p=128), in_=ld[:]), reads=[Bld]))
        return finish(nc, S, final_ops), dbg

    m5 = AR.mark()
    hselT = [AR.alloc(f"hselT{i}", [128, KC, RS], BF16) for i in range(2)]
    BhselT = [Buf("hselT0"), Buf("hselT1")]
    actT = AR.alloc("actT", [128, KC, RS], BF16)
    BactT = Buf("actT")
    NW1, NW2 = 4, 3
    w1u = [AR.alloc(f"w1u{i}", [128, KC, 512], BF16) for i in range(NW1)]
    Bw1u = [Buf(f"w1u{i}") for i in range(NW1)]
    ch_w1 = [S.chan(f"w1u{i}") for i in range(NW1)]
    ch_w1g = [S.chan(f"w1ug{i}") for i in range(NW1)]
    w2p = [AR.alloc(f"w2p{i}", [128, KC, 512], BF16) for i in range(NW2)]
    Bw2p = [Buf(f"w2p{i}") for i in range(NW2)]
    ch_w2 = [S.chan(f"w2p{i}") for i in range(NW2)]
    ch_w2g = [S.chan(f"w2pg{i}") for i in range(NW2)]
    hrow = [AR.alloc(f"hrow{i}", [128, D], BF16) for i in range(2)]
    Bhrow = [Buf("hrow0"), Buf("hrow1")]
    NCH_H, NCH_Y = 8, 16
    ch_hrow = [S.chan(f"hrow{i}") for i in range(NCH_H)]
    ysb = [AR.alloc(f"ysb{i}", [128, 512], F32) for i in range(4)]
    Bysb = [Buf(f"ysb{i}") for i in range(4)]
    ch_y = [S.chan(f"yw{i}") for i in range(NCH_Y)]
    hcnt = [0]
    g1 = [AR.alloc(f"g1_{i}", [128, BLK], F32) for i in range(2)]
    sgm = [AR.alloc(f"sgm{i}", [128, BLK], F32) for i in range(2)]
    l2 = [AR.alloc(f"l2_{i}", [128, BLK], F32) for i in range(2)]
    wv = [AR.alloc(f"wv_{i}", [128, BLK], F32) for i in range(2)]
    Bg1 = [Buf("g1_0"), Buf("g1_1")]
    Bsgm = [Buf("sgm0"), Buf("sgm1")]
    Bl2 = [Buf("l2_0"), Buf("l2_1")]
    Bwv = [Buf("wv_0"), Buf("wv_1")]
    By_s = Buf("y_s")
    w1_v = [w1_d[e].rearrange("(k p) n -> p k n", p=128) for e in range(NE)]
    w2_v = [w2_d[e].rearrange("(k p) n -> p k n", p=128) for e in range(NE)]

    def blk_guard(e, r, b):
        return (e, b * BLK) if r == 0 else (e, r * RS)

    def rnd_guard(e, r):
        return (e, r * RS) if r > 0 else None

    def build_hselT(e, r, buf_i):
        for b in range(NBLK):
            S.cur_guard = blk_guard(e, r, b)
            for st2 in range(BLK // 128):
                st = b * (BLK // 128) + st2
                hb_i = st % 2
                row0 = (e % (NE // NHS)) * CAP + r * RS + st * 128
                src = hsel_s[e // (NE // NHS)]
                chh = ch_hrow[hcnt[0] % NCH_H]
                hcnt[0] += 1
                S.dma("sp", chh, lambda h, row0=row0, hb_i=hb_i, src=src: h.dma_start(out=hrow[hb_i][:], in_=src[row0:row0 + 128, :]), reads=[Bhsel], writes=[Bhrow[hb_i]])
                for half in range(2):
                    pb = half
                    for q in range(8):
                        kc = half * 8 + q
                        S.op("pe", lambda h, kc=kc, q=q, pb=pb, hb_i=hb_i: h.transpose(out=psb[pb][:, q * 128:(q + 1) * 128], in_=hrow[hb_i][:, kc * 128:(kc + 1) * 128], identity=identb[:]),
                             reads=[Bhrow[hb_i], Bc], writes=[Bpsb[pb]])
                    if half == 0:
                        S.op("act", lambda h, half=half, pb=pb, st=st, buf_i=buf_i: h.copy(out=hselT[buf_i][:, half * 8:(half + 1) * 8, st * 128:(st + 1) * 128],
                                                                                        in_=psb[pb][:, :].rearrange("p (q t) -> p q t", q=8)), reads=[Bpsb[pb]], writes=[BhselT[buf_i]])
                    else:
                        S.op("dve", lambda h, half=half, pb=pb, st=st, buf_i=buf_i: h.tensor_copy(out=hselT[buf_i][:, half * 8:(half + 1) * 8, st * 128:(st + 1) * 128],
                                                                                               in_=psb[pb][:, :].rearrange("p (q t) -> p q t", q=8)), reads=[Bpsb[pb]], writes=[BhselT[buf_i]])
        S.cur_guard = None

    ucount = 0
    pcount2 = 0
    acnt = 0
    ycnt = 0
    er_list = [(e, r) for e in range(NE) for r in range(ROUNDS)]
    build_hselT(er_list[0][0], er_list[0][1], 0)
    for n, (e, r) in enumerate(er_list):
        hb_cur = n % 2
        for u in range(8):
            bi = ucount % NW1
            ucount += 1
            S.cur_guard = rnd_guard(e, r)
            cw1 = ch_w1[bi] if r == 0 else ch_w1g[bi]
            S.dma("pool", cw1, lambda h, e=e, u=u, bi=bi: h.dma_start(out=w1u[bi][:, :, 0:256], in_=w1_v[e][:, :, u * 256:(u + 1) * 256]), writes=[Bw1u[bi]])
            S.dma("pool", cw1, lambda h, e=e, u=u, bi=bi: h.dma_start(out=w1u[bi][:, :, 256:512], in_=w1_v[e][:, :, DFF + u * 256:DFF + (u + 1) * 256]),
                  writes=[Bw1u[bi]], cont=True)
            for b in range(NBLK):
                S.cur_guard = blk_guard(e, r, b)
                for j in range(2):
                    fc = u * 2 + j
                    ab = acnt % 2
                    bank = acnt % 4
                    acnt += 1
                    bs = slice(b * BLK, (b + 1) * BLK)
                    for kc in range(KC):
                        S.op("pe", lambda h, kc=kc, bi=bi, j=j, bs=bs, bank=bank, hb_cur=hb_cur: h.matmul(psf[bank][:, 0:BLK], lhsT=w1u[bi][:, kc, j * 128:(j + 1) * 128],
                                                                                                     rhs=hselT[hb_cur][:, kc, bs], start=(kc == 0), stop=(kc == KC - 1)),
                             reads=[Bw1u[bi], BhselT[hb_cur]], writes=[Bpsf[bank]])
                    for kc in range(KC):
                        S.op("pe", lambda h, kc=kc, bi=bi, j=j, bs=bs, bank=bank, hb_cur=hb_cur: h.matmul(psf[bank][:, BLK:2 * BLK], lhsT=w1u[bi][:, kc, 256 + j * 128:256 + (j + 1) * 128],
                                                                                                     rhs=hselT[hb_cur][:, kc, bs], start=(kc == 0), stop=(kc == KC - 1)),
                             reads=[Bw1u[bi], BhselT[hb_cur]], writes=[Bpsf[bank]])
                    cg = e * 32 + fc
                    cl = e * 32 + 16 + fc
                    S.op("dve", lambda h, ab=ab, bank=bank, cg=cg: h.tensor_scalar(out=g1[ab][:], in0=psf[bank][:, 0:BLK], scalar1=b1T[:, cg:cg + 1], scalar2=LIMIT, op0=ALU.add, op1=ALU.min),
                         reads=[Bpsf[bank], Bb1T], writes=[Bg1[ab]])
                    S.op("act", lambda h, ab=ab: h.activation(out=sgm[ab][:], in_=g1[ab][:], func=AF.Sigmoid, scale=ALPHA), reads=[Bg1[ab]], writes=[Bsgm[ab]])
                    S.op("dve", lambda h, ab=ab, bank=bank, cl=cl: h.tensor_scalar(out=l2[ab][:], in0=psf[bank][:, BLK:2 * BLK], scalar1=b1T[:, cl:cl + 1], scalar2=LIMIT + 1.0, op0=ALU.add, op1=ALU.min),
                         reads=[Bpsf[bank], Bb1T], writes=[Bl2[ab]])
                    S.op("dve", lambda h, ab=ab: h.scalar_tensor_tensor(out=wv[ab][:], in0=l2[ab][:], scalar=1.0 - LIMIT, in1=g1[ab][:], op0=ALU.max, op1=ALU.mult),
                         reads=[Bl2[ab], Bg1[ab]], writes=[Bwv[ab]])
                    S.op("dve", lambda h, ab=ab, fc=fc, bs=bs: h.tensor_tensor(out=actT[:, fc, bs], in0=wv[ab][:], in1=sgm[ab][:], op=ALU.mult),
                         reads=[Bwv[ab], Bsgm[ab]], writes=[BactT])
        S.cur_guard = None
        if n + 1 < len(er_list):
            build_hselT(er_list[n + 1][0], er_list[n + 1][1], (n + 1) % 2)
        ydst = y_s[e // (NE // NYS)]
        for db in range(4):
            bi = pcount2 % NW2
            pcount2 += 1
            S.cur_guard = rnd_guard(e, r)
            cw2 = ch_w2[bi] if r == 0 else ch_w2g[bi]
            S.dma("pool", cw2, lambda h, e=e, db=db, bi=bi: h.dma_start(out=w2p[bi][:], in_=w2_v[e][:, :, db * 512:(db + 1) * 512]), writes=[Bw2p[bi]])
            for b in range(NBLK):
                S.cur_guard = blk_guard(e, r, b)
                for st2 in range(BLK // 128):
                    st = b * (BLK // 128) + st2
                    bank = 4 + (ycnt % 2)
                    yb = ycnt % 4
                    ych = ch_y[ycnt % NCH_Y]
                    ycnt += 1
                    for fc in range(KC):
                        S.op("pe", lambda h, fc=fc, st=st, bi=bi, bank=bank: h.matmul(psf[bank][:, :], lhsT=actT[:, fc, st * 128:(st + 1) * 128], rhs=w2p[bi][:, fc, :],
                                                                                     start=(fc == 0), stop=(fc == KC - 1)),
                             reads=[BactT, Bw2p[bi]], writes=[Bpsf[bank]])
                    S.op("act", lambda h, bank=bank, yb=yb: h.copy(out=ysb[yb][:], in_=psf[bank][:, :]), reads=[Bpsf[bank]], writes=[Bysb[yb]])
                    row0 = (e % (NE // NYS)) * CAP + r * RS + st * 128
                    S.dma("sp", ych, lambda h, row0=row0, db=db, yb=yb, ydst=ydst: h.dma_start(out=ydst[row0:row0 + 128, db * 512:(db + 1) * 512], in_=ysb[yb][:]),
                          reads=[Bysb[yb]], writes=[By_s])
        S.cur_guard = None
    S.barrier()
    AR.reset(m5)

    G2 = AR.alloc("G2", [128, D], F32)
    wt6 = AR.alloc("wt6", [128, D], F32)
    b2sb = AR.alloc("b2sb", [NE, D], F32)
    BG2, Bwt6, Bb2 = Buf("G2"), Buf("wt6"), Buf("b2sb")
    load_mod_bcast(G2, 0, 5, ch_v, BG2)
    load_row_bcast(wt6, pofw_d, ch_v, Bwt6)
    S.op("dve", lambda h: h.tensor_tensor(out=G2[:], in0=G2[:], in1=wt6[:], op=ALU.mult), reads=[BG2, Bwt6], writes=[BG2])
    S.dma("sp", ch_v, lambda h: h.dma_start(out=b2sb[:], in_=b2_d), writes=[Bb2])
    yk = [[AR.alloc(f"yk{b}_{k}", [128, D], F32) for k in range(4)] for b in range(2)]
    Byk = [[Buf(f"yk{b}_{k}") for k in range(4)] for b in range(2)]
    ch_g = [S.chan("gath0"), S.chan("gath1")]
    x1r = [AR.alloc(f"x1r{i}", [128, D], F32) for i in range(2)]
    Bx1r = [Buf("x1r0"), Buf("x1r1")]
    ch_x1r = [S.chan("x1r0"), S.chan("x1r1")]
    ff = AR.alloc("ff", [128, D], F32)
    ot = [AR.alloc(f"ot{i}", [128, D], F32) for i in range(2)]
    gT = AR.alloc("gT", [NE, 128], F32)
    junk6 = AR.alloc("junk6", [128, D], BF16)
    st6 = AR.alloc("st6", [128, 2], F32)
    Bff, BgT, Bjunk6, Bst6 = Buf("ff"), Buf("gT"), Buf("junk6"), Buf("st6")
    Bot = [Buf("ot0"), Buf("ot1")]
    ch_out = [S.chan("out0"), S.chan("out1")]
    for i in range(NT):
        bi = i % 2
        first = True
        for k in range(4):
            for j in range(NYS):
                S.dma("pool", ch_g[bi], lambda h, i=i, k=k, bi=bi, j=j: h.indirect_dma_start(out=yk[bi][k][:], out_offset=None, in_=y_s[j],
                                                                                          in_offset=bass.IndirectOffsetOnAxis(ap=idxY[j][:, i * 4 + k:i * 4 + k + 1], axis=0),
                                                                                          bounds_check=bound_reg(h, (NE // NYS) * CAP - 1), oob_is_err=False),
                      reads=[By_s, Bidx[i]], writes=[Byk[bi][k]], cont=(not first))
                first = False
        S.dma("sp", ch_x1r[bi], lambda h, i=i, bi=bi: h.dma_start(out=x1r[bi][:], in_=x1_s[i * 128:(i + 1) * 128, :]), reads=[Bx1_s[i]], writes=[Bx1r[bi]])
        S.op("pe", lambda h, i=i: h.transpose(out=psf[4][0:NE, 0:128], in_=gatesA[:, i, :], identity=ident[:]), reads=[BgatesA, Bc], writes=[Bpsf[4]])
        S.op("act", lambda h: h.copy(out=gT[:], in_=psf[4][0:NE, 0:128]), reads=[Bpsf[4]], writes=[BgT])
        for cb in range(4):
            S.op("pe", lambda h, cb=cb: h.matmul(psf[cb][:, :], lhsT=gT[:], rhs=b2sb[:, cb * 512:(cb + 1) * 512], start=True, stop=True), reads=[BgT, Bb2], writes=[Bpsf[cb]])
            S.op("dve", lambda h, cb=cb, bi=bi, i=i: h.scalar_tensor_tensor(out=ff[:, cb * 512:(cb + 1) * 512], in0=yk[bi][0][:, cb * 512:(cb + 1) * 512], scalar=gate4[:, i, 0:1],
                                                                          in1=psf[cb][:, :], op0=ALU.mult, op1=ALU.add), reads=[Byk[bi][0], Bgate4, Bpsf[cb]], writes=[Bff])
        for k in range(1, 4):
            S.op("dve", lambda h, k=k, bi=bi, i=i: h.scalar_tensor_tensor(out=ff[:], in0=yk[bi][k][:], scalar=gate4[:, i, k:k + 1], in1=ff[:], op0=ALU.mult, op1=ALU.add),
                 reads=[Byk[bi][k], Bgate4, Bff], writes=[Bff])
        S.op("act", lambda h: h.activation(out=junk6[:], in_=ff[:], func=AF.Square, accum_out=st6[:, 0:1]), reads=[Bff], writes=[Bjunk6, Bst6])
        S.op("act", lambda h: h.activation(out=st6[:, 0:1], in_=st6[:, 0:1], func=AF.Sqrt, scale=1.0 / D, bias=EPS), reads=[Bst6], writes=[Bst6])
        S.op("dve", lambda h: h.reciprocal(out=st6[:, 0:1], in_=st6[:, 0:1]), reads=[Bst6], writes=[Bst6])
        S.op("dve", lambda h: h.scalar_tensor_tensor(out=ff[:], in0=ff[:], scalar=st6[:, 0:1], in1=G2[:], op0=ALU.mult, op1=ALU.mult), reads=[Bff, Bst6, BG2], writes=[Bff])
        S.op("dve", lambda h, bi=bi: h.tensor_tensor(out=ot[bi][:], in0=ff[:], in1=x1r[bi][:], op=ALU.add), reads=[Bff, Bx1r[bi]], writes=[Bot[bi]])
        final_ops.append(S.dma("sp", ch_out[bi], lambda h, i=i, bi=bi: h.dma_start(out=out_d[i * 128:(i + 1) * 128, :], in_=ot[bi][:]), reads=[Bot[bi]]))
    return finish(nc, S, final_ops), dbg


def finish(nc, S, final_ops):
    S.wait_final("sp", final_ops)
    S.emit()
    return nc


def _rope_tables():
    half = 64
    inv = (10000.0 ** (-np.arange(half, dtype=np.float32) / np.float32(half))).astype(np.float32)
    pos = np.arange(NCTX + SEQ, dtype=np.float32)
    ang = (pos[:, None] * inv[None, :]).astype(np.float32)
    return np.cos(ang).astype(np.float32), np.sin(ang).astype(np.float32)


def make_in_maps(inp, ne_decl=NE):
    f = lambda a: np.ascontiguousarray(np.asarray(a, dtype=np.float32))
    cos, sin = _rope_tables()
    shared = {
        "c_ctx": f(inp["c_ctx"]).reshape(16, 128),
        "ada_w": f(inp["ada_w"][0]),
        "ada_b": f(inp["ada_b"][0]).reshape(1, 6 * D),
        "pre_mix_norm": f(inp["pre_mix_norm"][0]).reshape(1, D),
        "post_mix_norm": f(inp["post_mix_norm"][0]).reshape(1, D),
        "pre_ffn_norm": f(inp["pre_ffn_norm"][0]).reshape(1, D),
        "post_ffn_norm": f(inp["post_ffn_norm"][0]).reshape(1, D),
        "w_in": f(inp["w_in"][0]),
        "ret_decay": np.concatenate([f(inp["ret_decay_fwd"][0]), f(inp["ret_decay_bwd"][0])]).reshape(1, 16),
        "ret_gn_w": f(inp["ret_gn_w"][0]).reshape(1, DRET),
        "conv_w": f(inp["conv_w"][0]),
        "conv_vecs": np.concatenate([f(inp["conv_b"][0]).reshape(8, 128), f(inp["conv_ln_w"][0]).reshape(8, 128), f(inp["conv_ln_b"][0]).reshape(8, 128)], axis=0),
        "w_out": f(inp["w_out"][0]),
        "router_w": f(inp["router_w"][0]),
        "router_b": f(inp["router_b"][0]).reshape(1, NE),
        "w1": f(inp["w1"][0][:ne_decl]),
        "b1": f(inp["b1"][0]).reshape(NE * 32, 128),
        "w2": f(inp["w2"][0][:ne_decl]),
        "b2": f(inp["b2"][0]),
        "rope_cos": cos,
        "rope_sin": sin,
    }
    maps = []
    for b in range(NB):
        m = dict(shared)
        m["x"] = f(inp["x"][b])
        m["c"] = f(inp["c"][b]).reshape(16, 128)
        m["ctx"] = f(inp["ctx"][b])
        maps.append(m)
    return maps


_NC_CACHE = {}


def kernel(**inputs):
    if "nc" not in _NC_CACHE:
        _NC_CACHE["nc"] = build_program()[0]
    nc = _NC_CACHE["nc"]
    in_maps = make_in_maps(inputs)
    res = run_bass_kernel_spmd(nc, in_maps, core_ids=list(range(NB)))
    out = np.stack([np.asarray(res.results[b]["out"], dtype=np.float32) for b in range(NB)], axis=0)
    return out
```

```python
import numpy as np
import concourse.bass as bass
import concourse.mybir as mybir
from concourse.alu_op_type import AluOpType as ALU
from concourse.bass_utils import run_bass_kernel_spmd

F32 = mybir.dt.float32
BF16 = mybir.dt.bfloat16
I32 = mybir.dt.int32
AF = mybir.ActivationFunctionType
AX = mybir.AxisListType

D = 2048
SEQ = 2048
NB = 8
NCTX = 256
H = 8
HD = 128
DRET = 1024
DCONV = 1024
DIN = 6144
CW = 31
NE = 32
DFF = 2048
NT = SEQ // 128
KC = D // 128
EPS = 1e-6
GN_EPS = 1e-5
ALPHA = 1.702
LIMIT = 7.0
QSCALE = HD ** -0.5

ROUNDS = 4
RS = 512
CAP = ROUNDS * RS
BLK = 256
NBLK = RS // BLK
NHS = 1
NYS = 2
BIG = 4.0e6

ENGS = ("pe", "act", "dve", "pool", "sp")
EPOCH = 1 << 30


class Buf:
    __slots__ = ("name", "excl", "last_w", "readers")

    def __init__(self, name, excl=False):
        self.name = name
        self.excl = excl
        self.last_w = None
        self.readers = []


class Op:
    __slots__ = ("eng", "fn", "deps", "signal", "done", "is_dma", "chan", "chan_prev", "grp", "guard")

    def __init__(self, eng, fn):
        self.eng = eng
        self.fn = fn
        self.deps = []
        self.signal = False
        self.done = None
        self.is_dma = False
        self.chan = None
        self.chan_prev = None
        self.grp = None
        self.guard = None


class Chan:
    def __init__(self, name):
        self.name = name
        self.sem = None
        self.last_grp = None


class Sched:
    def __init__(self, nc):
        self.nc = nc
        self.ops = {e: [] for e in ENGS}
        self.chans = []
        self.final_waits = []
        self.nrec = 0
        self.cur_guard = None
        self.cnt_ap = None
        self.cnt_op = None

    def chan(self, name):
        c = Chan(name)
        self.chans.append(c)
        return c

    def _deps(self, op, reads, writes):
        for b in reads:
            if b.last_w is not None:
                op.deps.append((b.last_w, "RAW"))
            if b.excl:
                for r in b.readers:
                    op.deps.append((r, "RAR"))
        for b in writes:
            if b.last_w is not None:
                op.deps.append((b.last_w, "WAW"))
            for r in b.readers:
                op.deps.append((r, "WAR"))
        for b in reads:
            b.readers.append(op)
        for b in writes:
            b.last_w = op
            b.readers = []

    def op(self, eng, fn, reads=(), writes=()):
        o = Op(eng, fn)
        o.guard = self.cur_guard
        self._deps(o, list(reads), list(writes))
        self.ops[eng].append(o)
        self.nrec += 1
        return o

    def dma(self, eng, chan, fn, reads=(), writes=(), cont=False):
        o = Op(eng, fn)
        o.guard = self.cur_guard
        o.is_dma = True
        o.chan = chan
        if cont and chan.last_grp is not None:
            o.grp = chan.last_grp
            o.chan_prev = o.grp[0].chan_prev
        else:
            o.chan_prev = chan.last_grp
            o.grp = []
            chan.last_grp = o.grp
        o.grp.append(o)
        self._deps(o, list(reads), list(writes))
        self.ops[eng].append(o)
        self.nrec += 1
        return o

    def barrier(self):
        lasts = []
        for e in ENGS:
            for o in reversed(self.ops[e]):
                if not o.is_dma and o.fn is not None:
                    lasts.append(o)
                    break
        for c in self.chans:
            if c.last_grp:
                lasts.append(c.last_grp[0])
        for e in ENGS:
            o = Op(e, None)
            for d in lasts:
                o.deps.append((d, "RAW"))
            self.ops[e].append(o)

    def wait_final(self, eng, ops):
        self.final_waits.append((eng, list(ops)))

    def emit(self):
        nc = self.nc
        for e in ENGS:
            for o in self.ops[e]:
                for (d, kind) in o.deps:
                    if d.is_dma:
                        continue
                    if d.eng != o.eng or kind == "RAW":
                        d.signal = True
        for (e, ops) in self.final_waits:
            for d in ops:
                if not d.is_dma:
                    d.signal = True
        if self.cnt_op is not None:
            self.cnt_op.signal = True
        sems = []
        for e in ENGS:
            cnt = 0
            sem = None
            for o in self.ops[e]:
                if o.is_dma or o.fn is None:
                    continue
                if o.signal:
                    if sem is None or cnt >= EPOCH:
                        sem = nc.alloc_semaphore(f"s_{e}_{len(sems)}")
                        sems.append(sem)
                        cnt = 0
                    cnt += 1
                    o.done = (sem, cnt)
        for c in self.chans:
            if c.last_grp is None:
                continue
            c.sem = nc.alloc_semaphore(f"c_{c.name}")
            chain = []
            g = c.last_grp
            while g is not None:
                chain.append(g)
                g = g[0].chan_prev
            chain.reverse()
            v = 0
            for g in chain:
                v += 16 * len(g)
                for o in g:
                    o.done = (c.sem, v)
        lists = self.ops
        finals = self.final_waits

        cnt_ap = self.cnt_ap
        cnt_op = self.cnt_op

        def run(e, h):
            seen = {}
            state = {"greg": None, "loaded": None, "last_sig": None}

            def need(sem, val):
                k = id(sem)
                if seen.get(k, 0) < val:
                    h.wait_ge(sem, val)
                    seen[k] = val

            def emit_op(o):
                if o.is_dma and o.chan_prev is not None and o is o.grp[0]:
                    need(*o.chan_prev[0].done)
                for (d, kind) in o.deps:
                    if d.is_dma:
                        if d.grp is o.grp:
                            continue
                        need(*d.done)
                    elif d.eng != e or kind == "RAW":
                        need(*d.done)
                if o.fn is None:
                    return
                ins = o.fn(h)
                if o.is_dma:
                    ins.then_inc(o.chan.sem, 16)
                elif o.signal:
                    ins.then_inc(o.done[0], 1)
                    state["last_sig"] = o.done

            ops = lists[e]
            i = 0
            n = len(ops)
            while i < n:
                o = ops[i]
                if o.guard is None:
                    emit_op(o)
                    i += 1
                    continue
                j = i
                while j < n and ops[j].guard == o.guard:
                    j += 1
                grp = ops[i:j]
                (ge, thr) = o.guard
                if state["greg"] is None:
                    state["greg"] = h.alloc_register(f"greg_{e}")
                if state["loaded"] != ge:
                    need(*cnt_op.done)
                    h.reg_load(state["greg"], cnt_ap(ge))
                    state["loaded"] = ge
                saved = dict(seen)
                pre_sig = state["last_sig"]
                with h.If_cmp(state["greg"], thr, "IS_GT"):
                    for g in grp:
                        emit_op(g)
                nsig = 0
                sig_sem = None
                ndma = 0
                for g in grp:
                    if g.fn is None:
                        continue
                    if g.is_dma:
                        ndma += 1
                    elif g.signal:
                        nsig += 1
                        sig_sem = g.done[0]
                        last_in = g.done
                if nsig or ndma:
                    with h.Else():
                        if nsig:
                            if pre_sig is not None:
                                h.wait_ge(*pre_sig)
                            h.sem_inc(sig_sem, nsig)
                        for g in grp:
                            if g.fn is not None and g.is_dma:
                                if g is g.grp[0] and g.chan_prev is not None:
                                    h.wait_ge(*g.chan_prev[0].done)
                                h.sem_inc(g.chan.sem, 16)
                if nsig:
                    state["last_sig"] = last_in
                seen.clear()
                seen.update(saved)
                i = j
            for (fe, fops) in finals:
                if fe == e:
                    for d in fops:
                        need(*d.done)

        with nc.Block() as block:
            @block.tensor
            def _(h):
                run("pe", h)

            @block.scalar
            def _(h):
                run("act", h)

            @block.vector
            def _(h):
                run("dve", h)

            @block.gpsimd
            def _(h):
                run("pool", h)

            @block.sync
            def _(h):
                run("sp", h)


class Arena:
    def __init__(self, nc, nbytes):
        self.nc = nc
        left = nc._sbuf_addr_for_side("left")
        self.base = (left + 63) // 64 * 64
        nbytes = nbytes // 64 * 64
        self.slab = nc.alloc_sbuf_tensor("arena", [128, nbytes // 4], F32)
        self.size = nbytes - 64
        self.top = 0
        self.n = 0

    def alloc(self, name, shape, dtype):
        esz = 2 if dtype == BF16 else 4
        nb = esz
        for s in shape[1:]:
            nb *= s
        nb = (nb + 63) // 64 * 64
        off = self.top
        assert off + nb <= self.size, (name, off, nb, self.size)
        self.top += nb
        self.n += 1
        return self.nc.alloc_sbuf_tensor_at(f"{name}_{self.n}", list(shape), dtype, offset=self.base + off)

    def alloc_at(self, name, shape, dtype, off):
        self.n += 1
        return self.nc.alloc_sbuf_tensor_at(f"{name}_{self.n}", list(shape), dtype, offset=self.base + off)

    def mark(self):
        return self.top

    def reset(self, m):
        self.top = m


def build_program(stage=99, debug=False, ne_decl=NE):
    nc = bass.Bass("TRN2", target_bir_lowering=False)
    S = Sched(nc)

    def din(name, shape, dt=F32):
        return nc.dram_tensor(name, list(shape), dt, kind="ExternalInput").ap()

    x_d = din("x", [SEQ, D])
    c_d = din("c", [16, 128])
    ctx_d = din("ctx", [NCTX, D])
    cctx_d = din("c_ctx", [16, 128])
    adaw_d = din("ada_w", [D, 6 * D])
    adab_d = din("ada_b", [1, 6 * D])
    pmw_d = din("pre_mix_norm", [1, D])
    pomw_d = din("post_mix_norm", [1, D])
    pfw_d = din("pre_ffn_norm", [1, D])
    pofw_d = din("post_ffn_norm", [1, D])
    win_d = din("w_in", [D, DIN])
    dec_d = din("ret_decay", [1, 16])
    gnw_d = din("ret_gn_w", [1, DRET])
    convw_d = din("conv_w", [CW, DCONV])
    cvec_d = din("conv_vecs", [24, 128])
    wout_d = din("w_out", [D, D])
    rw_d = din("router_w", [D, NE])
    rb_d = din("router_b", [1, NE])
    w1_d = din("w1", [ne_decl, D, 2 * DFF])
    b1_d = din("b1", [NE * 32, 128])
    w2_d = din("w2", [ne_decl, DFF, D])
    b2_d = din("b2", [NE, D])
    cos_d = din("rope_cos", [NCTX + SEQ, 64])
    sin_d = din("rope_sin", [NCTX + SEQ, 64])
    out_d = nc.dram_tensor("out", [SEQ, D], F32, kind="ExternalOutput").ap()
    dbg = {}

    def dbg_out(name, shape, dt=F32):
        t = nc.dram_tensor("dbg_" + name, list(shape), dt, kind="ExternalOutput").ap()
        dbg[name] = t
        return t

    mod_s = nc.dram_tensor("mod_s", [2, 6 * D], F32).ap()
    yT_s = nc.dram_tensor("yT_s", [D, SEQ], BF16).ap()
    x1_s = nc.dram_tensor("x1_s", [SEQ, D], F32).ap()
    hsel_s = [nc.dram_tensor(f"hsel_s{j}", [(NE // NHS) * CAP, D], BF16).ap() for j in range(NHS)]
    y_s = [nc.dram_tensor(f"y_s{j}", [(NE // NYS) * CAP, D], F32).ap() for j in range(NYS)]

    AR = Arena(nc, nc.sbuf_bytes_remaining - 6144)

    psf = [nc.alloc_psum_tensor(f"psf{i}", [128, 512], F32) for i in range(6)]
    psb = [nc.alloc_psum_tensor(f"psb{i}", [128, 1024], BF16) for i in range(2)]
    Bpsf = [Buf(f"psf{i}", excl=True) for i in range(6)]
    Bpsb = [Buf(f"psb{i}", excl=True) for i in range(2)]
    final_ops = []
    _regs = {}

    def bound_reg(h, val):
        if val not in _regs:
            _regs[val] = h.to_reg(val)
        return _regs[val]

    ident = AR.alloc("ident", [128, 128], F32)
    identb = AR.alloc("identb", [128, 128], BF16)
    onesb = AR.alloc("onesb", [128, 128], BF16)
    ones32 = AR.alloc("ones32", [128, 128], F32)
    trib = AR.alloc("trib", [128, 128], BF16)
    iorow = AR.alloc("iorow", [128, 128], F32)
    iocol = AR.alloc("iocol", [128, 1], F32)
    b1T = AR.alloc("b1T", [128, NE * 32], F32)
    gate4 = AR.alloc("gate4", [128, NT, 4], F32)
    idxH = [AR.alloc(f"idxH{j}", [128, NT * 4], I32) for j in range(NHS)]
    idxY = [AR.alloc(f"idxY{j}", [128, NT * 4], I32) for j in range(NYS)]
    cnt_i = AR.alloc("cnt_i", [128, NE], I32)
    Bcnt = Buf("cnt_i")
    gatesA = AR.alloc("gatesA", [128, NT, NE], F32)
    Bc = Buf("consts")
    Bb1T = Buf("b1T")
    Bgate4 = Buf("gate4")
    Bidx = [Buf(f"idx{i}") for i in range(NT)]
    BgatesA = Buf("gatesA")

    S.op("pool", lambda h: h.memset(ident[:], 0.0), writes=[Bc])
    S.op("pool", lambda h: h.affine_select(out=ident[:], in_=ident[:], pattern=[[-1, 128]], compare_op=ALU.not_equal,
                                            fill=1.0, base=0, channel_multiplier=1), reads=[Bc], writes=[Bc])
    S.op("pool", lambda h: h.memset(ones32[:], 1.0), writes=[Bc])
    S.op("pool", lambda h: h.affine_select(out=iorow[:], in_=ones32[:], pattern=[[1, 128]], compare_op=ALU.is_gt,
                                            fill=0.0, base=0, channel_multiplier=-1), reads=[Bc], writes=[Bc])
    S.op("dve", lambda h: h.tensor_copy(out=trib[:], in_=iorow[:]), reads=[Bc], writes=[Bc])
    S.op("dve", lambda h: h.tensor_copy(out=identb[:], in_=ident[:]), reads=[Bc], writes=[Bc])
    S.op("dve", lambda h: h.tensor_copy(out=onesb[:], in_=ones32[:]), reads=[Bc], writes=[Bc])
    ioi = AR.alloc("ioi", [128, 128], I32)
    S.op("pool", lambda h: h.iota(ioi[:], pattern=[[1, 128]], base=0, channel_multiplier=0), writes=[Bc])
    S.op("dve", lambda h: h.tensor_copy(out=iorow[:], in_=ioi[:]), reads=[Bc], writes=[Bc])
    S.op("pool", lambda h: h.iota(ioi[:, 0:1], pattern=[[0, 1]], base=0, channel_multiplier=1), reads=[Bc], writes=[Bc])
    S.op("dve", lambda h: h.tensor_copy(out=iocol[:], in_=ioi[:, 0:1]), reads=[Bc], writes=[Bc])

    ch_small = S.chan("small")
    persist_mark = AR.mark()

    def transpose_rows(rows_ap, nrows, out_ap, bank, Bbank, reads, writes, evac="act"):
        S.op("pe", lambda h: h.transpose(out=psf[bank][:, 0:nrows], in_=rows_ap, identity=ident[0:nrows, 0:nrows]),
             reads=reads + [Bc], writes=[Bbank])
        if evac == "act":
            S.op("act", lambda h: h.copy(out=out_ap, in_=psf[bank][:, 0:nrows]), reads=[Bbank], writes=writes)
        else:
            S.op("dve", lambda h: h.tensor_copy(out=out_ap, in_=psf[bank][:, 0:nrows]), reads=[Bbank], writes=writes)

    hT = AR.alloc("hT", [128, KC, SEQ], BF16)
    hcT_off = AR.mark()
    hcT = AR.alloc("hcT", [128, KC, NCTX], BF16)
    BhT = [Buf(f"hT{i}") for i in range(NT)]
    BhcT = Buf("hcT")
    m1 = AR.mark()
    sil2 = AR.alloc("sil2", [128, KC, 2], BF16)
    adaw = [AR.alloc(f"adaw{i}", [128, KC, 512], BF16) for i in range(2)]
    adabp = [AR.alloc(f"adabp{i}", [2, 512], F32) for i in range(2)]
    modp = [AR.alloc(f"modp{i}", [2, 512], F32) for i in range(2)]
    Bsil2 = Buf("sil2")
    Badaw = [Buf("adaw0"), Buf("adaw1")]
    Badabp = [Buf("adabp0"), Buf("adabp1")]
    Bmodp = [Buf("modp0"), Buf("modp1")]
    ch_adaw = [S.chan("adaw0"), S.chan("adaw1")]
    ch_adab = [S.chan("adab0"), S.chan("adab1")]
    ch_modw = S.chan("modw")
    Bmod_s = Buf("mod_s")
    adaw_v = adaw_d.rearrange("(k p) n -> p k n", p=128)

    def ada_piece(j):
        bi = j % 2
        S.dma("pool", ch_adaw[bi], lambda h: h.dma_start(out=adaw[bi][:], in_=adaw_v[:, :, j * 512:(j + 1) * 512]), writes=[Badaw[bi]])
        S.dma("sp", ch_adab[bi], lambda h: h.dma_start(out=adabp[bi][:], in_=adab_d[0:1, j * 512:(j + 1) * 512].partition_broadcast(2)), writes=[Badabp[bi]])
        bank = 4 + bi
        for kc in range(KC):
            S.op("pe", lambda h, kc=kc: h.matmul(psf[bank][0:2, :], lhsT=sil2[:, kc, :], rhs=adaw[bi][:, kc, :], start=(kc == 0), stop=(kc == KC - 1)),
                 reads=[Bsil2, Badaw[bi]], writes=[Bpsf[bank]])
        S.op("dve", lambda h: h.tensor_tensor(out=modp[bi][:], in0=psf[bank][0:2, :], in1=adabp[bi][:], op=ALU.add),
             reads=[Bpsf[bank], Badabp[bi]], writes=[Bmodp[bi]])
        S.dma("sp", ch_modw, lambda h: h.dma_start(out=mod_s[:, j * 512:(j + 1) * 512], in_=modp[bi][:]), reads=[Bmodp[bi]], writes=[Bmod_s])

    m0 = AR.mark()
    rows32 = AR.alloc("rows32", [32, 128], F32)
    cT = AR.alloc("cT", [128, 32], F32)
    sil = AR.alloc("sil", [128, 32], F32)
    b1rows = AR.alloc("b1rows", [128, 8, 128], F32)
    Brows32, BcT, Bsil, Bb1rows = Buf("rows32"), Buf("cT"), Buf("sil"), Buf("b1rows")
    S.dma("sp", ch_small, lambda h: h.dma_start(out=rows32[0:16, :], in_=c_d), writes=[Brows32])
    S.dma("sp", ch_small, lambda h: h.dma_start(out=rows32[16:32, :], in_=cctx_d), writes=[Brows32], cont=True)
    S.dma("sp", ch_small, lambda h: h.dma_start(out=b1rows[:], in_=b1_d.rearrange("(t p) f -> p t f", p=128)), writes=[Bb1rows], cont=True)
    transpose_rows(rows32[:], 32, cT[:], 0, Bpsf[0], [Brows32], [BcT])
    S.op("act", lambda h: h.activation(out=sil[:], in_=cT[:], func=AF.Silu), reads=[BcT], writes=[Bsil])
    for t in range(8):
        S.op("pe", lambda h, t=t: h.transpose(out=psf[1 + (t % 2)][:, 0:128], in_=b1rows[:, t, :], identity=ident[:]),
             reads=[Bb1rows, Bc], writes=[Bpsf[1 + (t % 2)]])
        S.op("act", lambda h, t=t: h.copy(out=b1T[:, t * 128:(t + 1) * 128], in_=psf[1 + (t % 2)][:, 0:128]),
             reads=[Bpsf[1 + (t % 2)]], writes=[Bb1T])
    b1T3 = b1T[:, :].rearrange("p (e f) -> p e f", e=NE)
    S.op("dve", lambda h: h.tensor_scalar(out=b1T3[:, :, 16:32], in0=b1T3[:, :, 16:32], scalar1=1.0, scalar2=None, op0=ALU.add), reads=[Bb1T], writes=[Bb1T])
    S.op("dve", lambda h: h.tensor_copy(out=sil2[:, :, 0], in_=sil[:, 0:16]), reads=[Bsil], writes=[Bsil2])
    S.op("dve", lambda h: h.tensor_copy(out=sil2[:, :, 1], in_=sil[:, 16:32]), reads=[Bsil], writes=[Bsil2])
    for j in range(8):
        ada_piece(j)
    ada_next = [8]
    S.barrier()
    AR.reset(m0)
    if stage <= 0:
        while ada_next[0] < 24:
            ada_piece(ada_next[0])
            ada_next[0] += 1
        if debug:
            d_mod = dbg_out("mod", [2, 6 * D])
            mld = AR.alloc("mld", [2, 6 * D], F32)
            Bmld = Buf("mld")
            S.dma("sp", S.chan("dbgm1"), lambda h: h.dma_start(out=mld[:], in_=mod_s), reads=[Bmod_s], writes=[Bmld])
            final_ops.append(S.dma("sp", S.chan("dbgmod"), lambda h: h.dma_start(out=d_mod, in_=mld[:]), reads=[Bmld]))
        return finish(nc, S, final_ops), dbg

    def load_mod_bcast(dst, row, g, chan, Bdst):
        return S.dma("sp", chan, lambda h: h.dma_start(out=dst[:], in_=mod_s[row:row + 1, g * D:(g + 1) * D].partition_broadcast(128)),
                     reads=[Bmod_s], writes=[Bdst])

    def load_row_bcast(dst, row_ap, chan, Bdst, cont=False):
        return S.dma("sp", chan, lambda h: h.dma_start(out=dst[:], in_=row_ap.partition_broadcast(128)), writes=[Bdst], cont=cont)

    A1 = AR.alloc("A1", [128, D], F32)
    S1 = AR.alloc("S1", [128, D], F32)
    A1c = AR.alloc("A1c", [128, D], F32)
    S1c = AR.alloc("S1c", [128, D], F32)
    wtmp = AR.alloc("wtmp", [128, D], F32)
    xb = [AR.alloc(f"xb{i}", [128, D], F32) for i in range(2)]
    t32s = [AR.alloc("t32_0", [128, D], F32)] * 2
    Bt32s = [Buf("t32_0")] * 2
    hb = [AR.alloc(f"hb{i}", [128, D], BF16) for i in range(2)]
    junkb = AR.alloc("junkb", [128, D], BF16)
    stat = AR.alloc("stat", [128, 8], F32)
    BA1, BS1, BA1c, BS1c, Bwtmp, Bjunk, Bstat = (Buf(n) for n in ["A1", "S1", "A1c", "S1c", "wtmp", "junk", "stat"])
    Bxb = [Buf("xb0"), Buf("xb1")]
    Bhb = [Buf("hb0"), Buf("hb1")]
    ch_x = [S.chan("x0"), S.chan("x1")]
    ch_v = S.chan("vecs")

    load_row_bcast(wtmp, pmw_d, ch_v, Bwtmp)
    load_mod_bcast(A1, 0, 1, ch_v, BA1)
    load_mod_bcast(S1, 0, 0, ch_v, BS1)
    load_mod_bcast(A1c, 1, 1, ch_v, BA1c)
    load_mod_bcast(S1c, 1, 0, ch_v, BS1c)
    S.op("dve", lambda h: h.scalar_tensor_tensor(out=A1[:], in0=A1[:], scalar=1.0, in1=wtmp[:], op0=ALU.add, op1=ALU.mult),
         reads=[BA1, Bwtmp], writes=[BA1])
    S.op("dve", lambda h: h.scalar_tensor_tensor(out=A1c[:], in0=A1c[:], scalar=1.0, in1=wtmp[:], op0=ALU.add, op1=ALU.mult),
         reads=[BA1c, Bwtmp], writes=[BA1c])

    def norm_mod_tile(src_ap_dram, xbuf, Bx, chan, Avec, BAv, Svec, BSv, hbuf, Bh, sidx):
        t32 = t32s[sidx % 2]
        Bt32 = Bt32s[sidx % 2]
        S.dma("sp", chan, lambda h: h.dma_start(out=xbuf[:], in_=src_ap_dram), writes=[Bx])
        S.op("act", lambda h: h.activation(out=junkb[:], in_=xbuf[:], func=AF.Square, accum_out=stat[:, sidx:sidx + 1]),
             reads=[Bx], writes=[Bjunk, Bstat])
        S.op("act", lambda h: h.activation(out=stat[:, sidx:sidx + 1], in_=stat[:, sidx:sidx + 1], func=AF.Sqrt, scale=1.0 / D, bias=EPS),
             reads=[Bstat], writes=[Bstat])
        S.op("dve", lambda h: h.reciprocal(out=stat[:, sidx:sidx + 1], in_=stat[:, sidx:sidx + 1]), reads=[Bstat], writes=[Bstat])
        S.op("dve", lambda h: h.scalar_tensor_tensor(out=t32[:], in0=xbuf[:], scalar=stat[:, sidx:sidx + 1], in1=Avec[:],
                                                     op0=ALU.mult, op1=ALU.mult), reads=[Bx, Bstat, BAv], writes=[Bt32])
        S.op("dve", lambda h: h.tensor_tensor(out=hbuf[:], in0=t32[:], in1=Svec[:], op=ALU.add), reads=[Bt32, BSv], writes=[Bh])

    def transpose_tile_bf16(hbuf, Bh, dstT, col0, Bdst):
        for half in range(2):
            pb = half
            for q in range(8):
                kc = half * 8 + q
                S.op("pe", lambda h, kc=kc, q=q, pb=pb: h.transpose(out=psb[pb][:, q * 128:(q + 1) * 128], in_=hbuf[:, kc * 128:(kc + 1) * 128],
                                                                       identity=identb[:]),
                     reads=[Bh, Bc], writes=[Bpsb[pb]])
            eng = "act" if half == 0 else "dve"
            if eng == "act":
                S.op("act", lambda h, half=half, pb=pb: h.copy(out=dstT[:, half * 8:(half + 1) * 8, col0:col0 + 128],
                                                                 in_=psb[pb][:, :].rearrange("p (q t) -> p q t", q=8)),
                     reads=[Bpsb[pb]], writes=[Bdst])
            else:
                S.op("dve", lambda h, half=half, pb=pb: h.tensor_copy(out=dstT[:, half * 8:(half + 1) * 8, col0:col0 + 128],
                                                                        in_=psb[pb][:, :].rearrange("p (q t) -> p q t", q=8)),
                     reads=[Bpsb[pb]], writes=[Bdst])

    for i in range(2):
        bi = i % 2
        norm_mod_tile(ctx_d[i * 128:(i + 1) * 128, :], xb[bi], Bxb[bi], ch_x[bi], A1c, BA1c, S1c, BS1c, hb[bi], Bhb[bi], i % 8)
        transpose_tile_bf16(hb[bi], Bhb[bi], hcT, i * 128, BhcT)
    for i in range(NT):
        bi = i % 2
        if ada_next[0] < 24:
            ada_piece(ada_next[0])
            ada_next[0] += 1
        norm_mod_tile(x_d[i * 128:(i + 1) * 128, :], xb[bi], Bxb[bi], ch_x[bi], A1, BA1, S1, BS1, hb[bi], Bhb[bi], i % 8)
        transpose_tile_bf16(hb[bi], Bhb[bi], hT, i * 128, BhT[i])
    while ada_next[0] < 24:
        ada_piece(ada_next[0])
        ada_next[0] += 1
    if debug:
        d_hT = dbg_out("hT", [128, KC, SEQ], BF16)
        final_ops.append(S.dma("sp", S.chan("dbghT"), lambda h: h.dma_start(out=d_hT, in_=hT[:]), reads=BhT))
    S.barrier()
    AR.reset(m1)
    if stage <= 1:
        return finish(nc, S, final_ops), dbg

    ByT_s = Buf("yT_s")
    win_v = win_d.rearrange("(k p) n -> p k n", p=128)

    m2 = AR.mark()
    decb = AR.alloc("decb", [128, 16], F32)
    Mh = AR.alloc("Mh", [128, H, 128], F32)
    dcol = AR.alloc("dcol", [128, H, 6], F32)
    wctx = AR.alloc("wctx", [128, H, 4], F32)
    Bdecb, BMh, Bdcol, Bwctx = Buf("decb"), Buf("Mh"), Buf("dcol"), Buf("wctx")
    tA = AR.alloc("tA", [128, 128], F32)
    tB = AR.alloc("tB", [128, 128], F32)
    tC = AR.alloc("tC", [128, 128], F32)
    tD = AR.alloc("tD", [128, 128], F32)
    cols = AR.alloc("cols", [128, 8], F32)
    BtA, BtB, BtC, BtD, Bcols = Buf("tA"), Buf("tB"), Buf("tC"), Buf("tD"), Buf("cols")
    S.dma("sp", ch_v, lambda h: h.dma_start(out=decb[:], in_=dec_d.partition_broadcast(128)), writes=[Bdecb])
    S.op("act", lambda h: h.activation(out=decb[:], in_=decb[:], func=AF.Exp, scale=-1.0), reads=[Bdecb], writes=[Bdecb])
    S.op("act", lambda h: h.activation(out=decb[:], in_=decb[:], func=AF.Ln, bias=1.0), reads=[Bdecb], writes=[Bdecb])
    S.op("dve", lambda h: h.tensor_scalar(out=decb[:], in0=decb[:], scalar1=-1.0, scalar2=None, op0=ALU.mult), reads=[Bdecb], writes=[Bdecb])
    S.op("dve", lambda h: h.tensor_scalar(out=tA[:], in0=iorow[:], scalar1=iocol[:, 0:1], scalar2=0.0, op0=ALU.subtract, op1=ALU.max),
         reads=[Bc], writes=[BtA])
    S.op("dve", lambda h: h.tensor_scalar(out=tB[:], in0=iorow[:], scalar1=iocol[:, 0:1], scalar2=-1.0, op0=ALU.subtract, op1=ALU.mult),
         reads=[Bc], writes=[BtB])
    S.op("dve", lambda h: h.tensor_scalar(out=tB[:], in0=tB[:], scalar1=0.0, scalar2=None, op0=ALU.max), reads=[BtB], writes=[BtB])
    S.op("dve", lambda h: h.tensor_scalar(out=tC[:], in0=iorow[:], scalar1=iocol[:, 0:1], scalar2=None, op0=ALU.is_ge), reads=[Bc], writes=[BtC])
    S.op("dve", lambda h: h.tensor_scalar(out=tD[:], in0=iorow[:], scalar1=iocol[:, 0:1], scalar2=None, op0=ALU.is_le), reads=[Bc], writes=[BtD])
    for ci, (mul, add) in enumerate([(1.0, 1.0), (-1.0, 128.0), (-1.0, 127.0), (1.0, 0.0), (0.0, 128.0), (-1.0, 255.0), (-1.0, 127.0), (1.0, 128.0)]):
        S.op("dve", lambda h, ci=ci, mul=mul, add=add: h.tensor_scalar(out=cols[:, ci:ci + 1], in0=iocol[:, 0:1], scalar1=mul, scalar2=add,
                                                                      op0=ALU.mult, op1=ALU.add), reads=[Bc], writes=[Bcols])
    ex1 = AR.alloc("ex1", [128, 128], F32)
    ex2 = AR.alloc("ex2", [128, 128], F32)
    Bex1, Bex2 = Buf("ex1"), Buf("ex2")
    for hh in range(H):
        lf = decb[:, hh:hh + 1]
        lb = decb[:, 8 + hh:9 + hh]
        S.op("act", lambda h, lf=lf: h.activation(out=ex1[:], in_=tA[:], func=AF.Exp, scale=lf), reads=[BtA, Bdecb], writes=[Bex1])
        S.op("act", lambda h, lb=lb: h.activation(out=ex2[:], in_=tB[:], func=AF.Exp, scale=lb), reads=[BtB, Bdecb], writes=[Bex2])
        S.op("dve", lambda h: h.tensor_tensor(out=ex1[:], in0=ex1[:], in1=tC[:], op=ALU.mult), reads=[Bex1, BtC], writes=[Bex1])
        S.op("dve", lambda h: h.tensor_tensor(out=ex2[:], in0=ex2[:], in1=tD[:], op=ALU.mult), reads=[Bex2, BtD], writes=[Bex2])
        S.op("dve", lambda h, hh=hh: h.tensor_tensor(out=Mh[:, hh, :], in0=ex1[:], in1=ex2[:], op=ALU.add), reads=[Bex1, Bex2], writes=[BMh])
        for (dst, ci, lg) in [(0, 0, lf), (1, 1, lb), (2, 2, lf), (3, 3, lb), (4, 4, lf), (5, 4, lb)]:
            S.op("act", lambda h, hh=hh, dst=dst, ci=ci, lg=lg: h.activation(out=dcol[:, hh, dst:dst + 1], in_=cols[:, ci:ci + 1], func=AF.Exp, scale=lg),
                 reads=[Bcols, Bdecb], writes=[Bdcol])
        for (dst, ci, lg) in [(0, 5, lf), (1, 6, lf), (2, 3, lb), (3, 7, lb)]:
            S.op("act", lambda h, hh=hh, dst=dst, ci=ci, lg=lg: h.activation(out=wctx[:, hh, dst:dst + 1], in_=cols[:, ci:ci + 1], func=AF.Exp, scale=lg),
                 reads=[Bcols, Bdecb], writes=[Bwctx])

    cosL = AR.alloc("cosL", [128, NT, 64], F32)
    sinL = AR.alloc("sinL", [128, NT, 64], F32)
    cosT = cosL[:, :, :].unsqueeze(2).to_broadcast([128, NT, 2, 64])
    sinT = sinL[:, :, :].unsqueeze(2).to_broadcast([128, NT, 2, 64])
    cosC = AR.alloc("cosC", [128, 2, 64], F32)
    sinC = AR.alloc("sinC", [128, 2, 64], F32)
    Brope = Buf("rope")
    cos_lat = cos_d[NCTX:NCTX + SEQ, :].rearrange("(t p) f -> p t f", p=128)
    sin_lat = sin_d[NCTX:NCTX + SEQ, :].rearrange("(t p) f -> p t f", p=128)
    S.dma("sp", ch_v, lambda h: h.dma_start(out=cosL[:], in_=cos_lat), writes=[Brope])
    S.dma("sp", ch_v, lambda h: h.dma_start(out=sinL[:], in_=sin_lat), writes=[Brope], cont=True)
    S.dma("sp", ch_v, lambda h: h.dma_start(out=cosC[:], in_=cos_d[0:NCTX, :].rearrange("(t p) f -> p t f", p=128)), writes=[Brope], cont=True)
    S.dma("sp", ch_v, lambda h: h.dma_start(out=sinC[:], in_=sin_d[0:NCTX, :].rearrange("(t p) f -> p t f", p=128)), writes=[Brope], cont=True)
    gnwb = AR.alloc("gnwb", [128, DRET], F32)
    Bgnwb = Buf("gnwb")
    load_row_bcast(gnwb, gnw_d, ch_v, Bgnwb)

    R0 = AR.alloc("R0", [128, H, 2, 128], F32)
    BR0 = Buf("R0")
    m2b = AR.mark()
    wkv = [AR.alloc(f"wkv{i}", [128, KC, 256], BF16) for i in range(2)]
    Bwkv = [Buf("wkv0"), Buf("wkv1")]
    ch_wkv = [S.chan("wkv0"), S.chan("wkv1")]
    kc32 = AR.alloc("kc32", [128, 2, 128], F32)
    kcr = AR.alloc("kcr", [128, 2, 128], BF16)
    vwf = AR.alloc("vwf", [128, 2, 128], BF16)
    vwb = AR.alloc("vwb", [128, 2, 128], BF16)
    rt1 = AR.alloc("rt1", [128, 2, 64], F32)
    rt2 = AR.alloc("rt2", [128, 2, 64], F32)
    Bkc32, Bkcr, Bvwf, Bvwb, Brt1, Brt2 = (Buf(n) for n in ["kc32", "kcr", "vwf", "vwb", "rt1", "rt2"])
    for hh in range(H):
        bi = hh % 2
        S.dma("pool", ch_wkv[bi], lambda h, hh=hh, bi=bi: h.dma_start(out=wkv[bi][:, :, 0:128], in_=win_v[:, :, DRET + hh * 128:DRET + (hh + 1) * 128]),
              writes=[Bwkv[bi]])
        S.dma("pool", ch_wkv[bi], lambda h, hh=hh, bi=bi: h.dma_start(out=wkv[bi][:, :, 128:256], in_=win_v[:, :, 2 * DRET + hh * 128:2 * DRET + (hh + 1) * 128]),
              writes=[Bwkv[bi]], cont=True)
        for t in range(2):
            bank = t
            for kc in range(KC):
                S.op("pe", lambda h, kc=kc, t=t, bi=bi, bank=bank: h.matmul(psf[bank][:, 0:256], lhsT=hcT[:, kc, t * 128:(t + 1) * 128], rhs=wkv[bi][:, kc, :],
                                                                             start=(kc == 0), stop=(kc == KC - 1)),
                     reads=[BhcT, Bwkv[bi]], writes=[Bpsf[bank]])
            S.op("act", lambda h, t=t, bank=bank: h.copy(out=kc32[:, t, :], in_=psf[bank][:, 0:128]), reads=[Bpsf[bank]], writes=[Bkc32])
            S.op("dve", lambda h, t=t, bank=bank, hh=hh: h.tensor_scalar(out=vwf[:, t, :], in0=psf[bank][:, 128:256], scalar1=wctx[:, hh, t:t + 1], scalar2=None, op0=ALU.mult),
                 reads=[Bpsf[bank], Bwctx], writes=[Bvwf])
            S.op("dve", lambda h, t=t, bank=bank, hh=hh: h.tensor_scalar(out=vwb[:, t, :], in0=psf[bank][:, 128:256], scalar1=wctx[:, hh, 2 + t:3 + t], scalar2=None, op0=ALU.mult),
                 reads=[Bpsf[bank], Bwctx], writes=[Bvwb])
        k1 = kc32[:, :, 0:64]
        k2 = kc32[:, :, 64:128]
        S.op("dve", lambda h: h.tensor_tensor(out=rt1[:], in0=k1, in1=cosC[:], op=ALU.mult), reads=[Bkc32, Brope], writes=[Brt1])
        S.op("pool", lambda h: h.tensor_tensor(out=rt2[:], in0=k2, in1=sinC[:], op=ALU.mult), reads=[Bkc32, Brope], writes=[Brt2])
        S.op("dve", lambda h: h.tensor_tensor(out=kcr[:, :, 0:64], in0=rt1[:], in1=rt2[:], op=ALU.subtract), reads=[Brt1, Brt2], writes=[Bkcr])
        S.op("dve", lambda h: h.tensor_tensor(out=rt1[:], in0=k1, in1=sinC[:], op=ALU.mult), reads=[Bkc32, Brope, Bkcr], writes=[Brt1])
        S.op("pool", lambda h: h.tensor_tensor(out=rt2[:], in0=k2, in1=cosC[:], op=ALU.mult), reads=[Bkc32, Brope, Bkcr], writes=[Brt2])
        S.op("dve", lambda h: h.tensor_tensor(out=kcr[:, :, 64:128], in0=rt1[:], in1=rt2[:], op=ALU.add), reads=[Brt1, Brt2], writes=[Bkcr])
        for di, vw, Bvw in [(0, vwf, Bvwf), (1, vwb, Bvwb)]:
            bank = 2 + di
            for t in range(2):
                S.op("pe", lambda h, t=t, bank=bank, vw=vw: h.matmul(psf[bank][:, 0:128], lhsT=kcr[:, t, :], rhs=vw[:, t, :], start=(t == 0), stop=(t == 1)),
                     reads=[Bkcr, Bvw], writes=[Bpsf[bank]])
            S.op("act", lambda h, hh=hh, di=di, bank=bank: h.copy(out=R0[:, hh, di, :], in_=psf[bank][:, 0:128]), reads=[Bpsf[bank]], writes=[BR0])
    if debug:
        d_R0 = dbg_out("R0", [128, H, 2, 128])
        final_ops.append(S.dma("sp", S.chan("dbgR0"), lambda h: h.dma_start(out=d_R0, in_=R0[:]), reads=[BR0]))
    S.barrier()
    AR.reset(m2b)
    if stage <= 2:
        return finish(nc, S, final_ops), dbg

    m2c = AR.mark()
    wq_off = AR.mark()
    wq1 = AR.alloc("wq", [128, KC, 512], BF16)
    wq = [wq1, wq1]
    Bwq1 = Buf("wq")
    Bwq = [Bwq1, Bwq1]
    ch_wq1 = S.chan("wq")
    ch_wq = [ch_wq1, ch_wq1]
    qT = AR.alloc_at("qT", [128, SEQ], BF16, wq_off)
    qdfT = AR.alloc_at("qdfT", [128, SEQ], BF16, wq_off + 4096)
    qdbT = AR.alloc_at("qdbT", [128, SEQ], BF16, wq_off + 8192)
    kT = AR.alloc_at("kT", [128, SEQ], BF16, wq_off + 12288)
    BqT = BqdfT = BqdbT = BkT = Bwq1
    qk_off = AR.mark()
    qk32 = AR.alloc("qk32", [128, NT, 2, 2, 64], F32)
    Bqk32 = Buf("qk32")
    o32 = AR.alloc_at("o32", [128, NT, 128], F32, qk_off)
    ytok = AR.alloc_at("ytok", [128, NT, 128], BF16, qk_off + 8192)
    yTh = AR.alloc_at("yTh", [128, SEQ], BF16, qk_off + 12288)
    Bo32 = Bytok = ByTh = Bqk32
    ta_off = AR.mark()
    ta = AR.alloc("ta", [128, NT, 2, 64], F32)
    Bta = Buf("ta")
    Sfb = AR.alloc_at("Sfb", [128, NT, 128], BF16, ta_off)
    Sbb = AR.alloc_at("Sbb", [128, NT, 128], BF16, ta_off + 4096)
    BSfb = BSbb = Bta
    tb_off = AR.mark()
    tb = AR.alloc("tb", [128, NT, 2, 64], F32)
    Sf32 = AR.alloc_at("Sf32", [128, NT, 128], F32, tb_off)
    rotb_off = AR.mark()
    rotb = AR.alloc("rotb", [128, NT, 2, 2, 64], BF16)
    Sb32 = AR.alloc_at("Sb32", [128, NT, 128], F32, rotb_off)
    qdf = AR.alloc("qdf", [128, NT, 2, 64], BF16)
    qdb = AR.alloc("qdb", [128, NT, 2, 64], BF16)
    kdf = AR.alloc("kdf", [128, NT, 2, 64], BF16)
    kdb = AR.alloc("kdb", [128, NT, 2, 64], BF16)
    vtok = AR.alloc("vtok", [128, NT, 128], BF16)
    sg = AR.alloc_at("sg", [128, NT, 128], F32, hcT_off)
    Rf = AR.alloc("Rf", [128, 128], F32)
    Rb = AR.alloc("Rb", [128, 128], F32)
    SM = [AR.alloc(f"SM{i}", [128, 128], BF16) for i in range(2)]
    bnst = AR.alloc("bnst", [128, NT, 6], F32)
    mv = AR.alloc("mv", [128, NT, 2], F32)
    rstd = AR.alloc("rstd", [128, NT], F32)
    (Btb, Brotb, Bqdf, Bqdb, Bkdf, Bkdb, Bvtok, Bsg, BRf, BRb, Bbnst, Bmv, Brstd) = (
        Buf(n) for n in ["tb", "rotb", "qdf", "qdb", "kdf", "kdb", "vtok", "sg", "Rf", "Rb", "bnst", "mv", "rstd"])
    BSM = [Buf("SM0"), Buf("SM1")]
    ch_yT = S.chan("yTout")

    for hh in range(H):
        bi = hh % 2
        for seg in range(4):
            S.dma("pool", ch_wq[bi], lambda h, hh=hh, bi=bi, seg=seg: h.dma_start(out=wq[bi][:, :, seg * 128:(seg + 1) * 128],
                                                                                    in_=win_v[:, :, seg * DRET + hh * 128:seg * DRET + (hh + 1) * 128]),
                  writes=[Bwq[bi]], cont=(seg > 0))
        for i in range(NT):
            bank = i % 4
            for kc in range(KC):
                S.op("pe", lambda h, kc=kc, i=i, bi=bi, bank=bank: h.matmul(psf[bank][:, :], lhsT=hT[:, kc, i * 128:(i + 1) * 128], rhs=wq[bi][:, kc, :],
                                                                             start=(kc == 0), stop=(kc == KC - 1)),
                     reads=[BhT[i], Bwq[bi]], writes=[Bpsf[bank]])
            S.op("act", lambda h, i=i, bank=bank: h.copy(out=qk32[:, i, :, :, :], in_=psf[bank][:, 0:256].rearrange("p (a b c) -> p a b c", a=2, b=2)),
                 reads=[Bpsf[bank]], writes=[Bqk32])
            S.op("act", lambda h, i=i, bank=bank: h.copy(out=vtok[:, i, :], in_=psf[bank][:, 256:384]), reads=[Bpsf[bank]], writes=[Bvtok])
            S.op("act", lambda h, i=i, bank=bank: h.activation(out=sg[:, i, :], in_=psf[bank][:, 384:512], func=AF.Silu), reads=[Bpsf[bank]], writes=[Bsg])
        X1 = qk32[:, :, :, 0, :]
        X2 = qk32[:, :, :, 1, :]
        S.op("dve", lambda h: h.tensor_tensor(out=ta[:], in0=X1, in1=cosT, op=ALU.mult), reads=[Bqk32, Brope], writes=[Bta])
        S.op("pool", lambda h: h.tensor_tensor(out=tb[:], in0=X2, in1=sinT, op=ALU.mult), reads=[Bqk32, Brope], writes=[Btb])
        S.op("dve", lambda h: h.tensor_tensor(out=ta[:], in0=ta[:], in1=tb[:], op=ALU.subtract), reads=[Bta, Btb], writes=[Bta])
        S.op("pool", lambda h: h.tensor_tensor(out=tb[:], in0=X1, in1=sinT, op=ALU.mult), reads=[Bqk32, Brope, Bta], writes=[Btb])
        S.op("dve", lambda h: h.tensor_tensor(out=X1, in0=X2, in1=cosT, op=ALU.mult), reads=[Bqk32, Brope, Btb], writes=[Bqk32])
        S.op("dve", lambda h: h.tensor_tensor(out=tb[:], in0=tb[:], in1=X1, op=ALU.add), reads=[Btb, Bqk32], writes=[Btb])
        S.op("act", lambda h: h.mul(out=rotb[:, :, 0, 0, :], in_=ta[:, :, 0, :], mul=QSCALE), reads=[Bta], writes=[Brotb])
        S.op("act", lambda h: h.copy(out=rotb[:, :, 1, 0, :], in_=ta[:, :, 1, :]), reads=[Bta], writes=[Brotb])
        S.op("act", lambda h: h.mul(out=rotb[:, :, 0, 1, :], in_=tb[:, :, 0, :], mul=QSCALE), reads=[Btb], writes=[Brotb])
        S.op("act", lambda h: h.copy(out=rotb[:, :, 1, 1, :], in_=tb[:, :, 1, :]), reads=[Btb], writes=[Brotb])
        for (dst, Bd, src_i, col, sc) in [(qdf, Bqdf, 0, 0, QSCALE), (qdb, Bqdb, 0, 1, QSCALE), (kdf, Bkdf, 1, 2, 1.0), (kdb, Bkdb, 1, 3, 1.0)]:
            S.op("dve", lambda h, dst=dst, src_i=src_i, col=col, hh=hh, sc=sc: h.tensor_scalar(out=dst[:, :, 0, :], in0=ta[:, :, src_i, :], scalar1=dcol[:, hh, col:col + 1],
                                                                                              scalar2=sc, op0=ALU.mult, op1=ALU.mult), reads=[Bta, Bdcol], writes=[Bd])
            S.op("pool", lambda h, dst=dst, src_i=src_i, col=col, hh=hh, sc=sc: h.tensor_scalar(out=dst[:, :, 1, :], in0=tb[:, :, src_i, :], scalar1=dcol[:, hh, col:col + 1],
                                                                                               scalar2=sc, op0=ALU.mult, op1=ALU.mult), reads=[Btb, Bdcol], writes=[Bd])
        srcs = [(lambda i: rotb[:, i, 0, :, :].rearrange("p a b -> p (a b)"), Brotb, qT, BqT),
                (lambda i: qdf[:, i, :, :].rearrange("p a b -> p (a b)"), Bqdf, qdfT, BqdfT),
                (lambda i: qdb[:, i, :, :].rearrange("p a b -> p (a b)"), Bqdb, qdbT, BqdbT),
                (lambda i: rotb[:, i, 1, :, :].rearrange("p a b -> p (a b)"), Brotb, kT, BkT)]
        cnt = 0
        for (srcf, Bsrc, dstT, BdstT) in srcs:
            for half in range(2):
                pb = cnt % 2
                cnt += 1
                for q in range(8):
                    i = half * 8 + q
                    S.op("pe", lambda h, i=i, q=q, pb=pb, srcf=srcf: h.transpose(out=psb[pb][:, q * 128:(q + 1) * 128], in_=srcf(i), identity=identb[:]),
                         reads=[Bsrc, Bc], writes=[Bpsb[pb]])
                if pb == 0:
                    S.op("act", lambda h, half=half, pb=pb, dstT=dstT: h.copy(out=dstT[:, half * 1024:(half + 1) * 1024], in_=psb[pb][:, :]),
                         reads=[Bpsb[pb]], writes=[BdstT])
                else:
                    S.op("dve", lambda h, half=half, pb=pb, dstT=dstT: h.tensor_copy(out=dstT[:, half * 1024:(half + 1) * 1024], in_=psb[pb][:, :]),
                         reads=[Bpsb[pb]], writes=[BdstT])
        S.op("act", lambda h, hh=hh: h.copy(out=Sf32[:, 0, :], in_=R0[:, hh, 0, :]), reads=[BR0], writes=[Btb])
        S.op("act", lambda h, hh=hh: h.copy(out=Sb32[:, NT - 1, :], in_=R0[:, hh, 1, :]), reads=[BR0], writes=[Brotb])
        fbanks = [0, 1, 4]
        bbanks = [2, 3, 5]
        for step in range(NT - 1):
            i_f = step
            i_b = NT - 1 - step
            bf_ = fbanks[step % 3]
            bb_ = bbanks[step % 3]
            S.op("pe", lambda h, i=i_f, bf_=bf_: h.matmul(psf[bf_][:, 0:128], lhsT=kdf[:, i, :, :].rearrange("p a b -> p (a b)"), rhs=vtok[:, i, :], start=True, stop=True),
                 reads=[Bkdf, Bvtok], writes=[Bpsf[bf_]])
            S.op("act", lambda h, i=i_f, bf_=bf_: h.copy(out=Sf32[:, i + 1, :], in_=psf[bf_][:, 0:128]), reads=[Bpsf[bf_]], writes=[Btb])
            S.op("pe", lambda h, i=i_b, bb_=bb_: h.matmul(psf[bb_][:, 0:128], lhsT=kdb[:, i, :, :].rearrange("p a b -> p (a b)"), rhs=vtok[:, i, :], start=True, stop=True),
                 reads=[Bkdb, Bvtok], writes=[Bpsf[bb_]])
            S.op("act", lambda h, i=i_b, bb_=bb_: h.copy(out=Sb32[:, i - 1, :], in_=psf[bb_][:, 0:128]), reads=[Bpsf[bb_]], writes=[Brotb])
        for step in range(NT - 1):
            i_f = step
            i_b = NT - 1 - step
            S.op("dve", lambda h, hh=hh, i=i_f: h.scalar_tensor_tensor(out=Sf32[:, i + 1, :], in0=Sf32[:, i, :], scalar=dcol[:, hh, 4:5], in1=Sf32[:, i + 1, :], op0=ALU.mult, op1=ALU.add),
                 reads=[Btb, Bdcol], writes=[Btb])
            S.op("dve", lambda h, hh=hh, i=i_b: h.scalar_tensor_tensor(out=Sb32[:, i - 1, :], in0=Sb32[:, i, :], scalar=dcol[:, hh, 5:6], in1=Sb32[:, i - 1, :], op0=ALU.mult, op1=ALU.add),
                 reads=[Brotb, Bdcol], writes=[Brotb])
        S.op("act", lambda h: h.copy(out=Sfb[:], in_=Sf32[:]), reads=[Btb], writes=[BSfb])
        S.op("act", lambda h: h.copy(out=Sbb[:], in_=Sb32[:]), reads=[Brotb], writes=[BSbb])
        for i in range(NT):
            sb_ = i % 2
            bs = i % 2
            bo = 2 + (i % 2)
            cs = slice(i * 128, (i + 1) * 128)
            S.op("pe", lambda h, cs=cs, bs=bs: h.matmul(psf[bs][:, 0:128], lhsT=kT[:, cs], rhs=qT[:, cs], start=True, stop=True),
                 reads=[BkT, BqT], writes=[Bpsf[bs]])
            S.op("dve", lambda h, bs=bs, sb_=sb_, hh=hh: h.tensor_tensor(out=SM[sb_][:], in0=psf[bs][:, 0:128], in1=Mh[:, hh, :], op=ALU.mult),
                 reads=[Bpsf[bs], BMh], writes=[BSM[sb_]])
            S.op("pe", lambda h, i=i, sb_=sb_, bo=bo: h.matmul(psf[bo][:, 0:128], lhsT=SM[sb_][:], rhs=vtok[:, i, :], start=True, stop=False),
                 reads=[BSM[sb_], Bvtok], writes=[Bpsf[bo]])
            S.op("pe", lambda h, i=i, cs=cs, bo=bo: h.matmul(psf[bo][:, 0:128], lhsT=qdfT[:, cs], rhs=Sfb[:, i, :], start=False, stop=False),
                 reads=[BqdfT, BSfb], writes=[Bpsf[bo]])
            S.op("pe", lambda h, i=i, cs=cs, bo=bo: h.matmul(psf[bo][:, 0:128], lhsT=qdbT[:, cs], rhs=Sbb[:, i, :], start=False, stop=True),
                 reads=[BqdbT, BSbb], writes=[Bpsf[bo]])
            S.op("act", lambda h, i=i, bo=bo: h.copy(out=o32[:, i, :], in_=psf[bo][:, 0:128]), reads=[Bpsf[bo]], writes=[Bo32])
            S.op("dve", lambda h, i=i: h.bn_stats(out=bnst[:, i, :], in_=o32[:, i, :]), reads=[Bo32], writes=[Bbnst])
            S.op("dve", lambda h, i=i: h.bn_aggr(out=mv[:, i, :], in_=bnst[:, i, :]), reads=[Bbnst], writes=[Bmv])
        S.op("act", lambda h: h.activation(out=rstd[:], in_=mv[:, :, 1], func=AF.Sqrt, bias=GN_EPS), reads=[Bmv], writes=[Brstd])
        S.op("dve", lambda h: h.reciprocal(out=rstd[:], in_=rstd[:]), reads=[Brstd], writes=[Brstd])
        S.op("pool", lambda h, hh=hh: h.tensor_tensor(out=sg[:], in0=sg[:], in1=gnwb[:, hh * 128:(hh + 1) * 128].unsqueeze(1).to_broadcast([128, NT, 128]), op=ALU.mult),
             reads=[Bsg, Bgnwb], writes=[Bsg])
        for i in range(NT):
            S.op("dve", lambda h, i=i: h.tensor_scalar(out=o32[:, i, :], in0=o32[:, i, :], scalar1=mv[:, i, 0:1], scalar2=rstd[:, i:i + 1], op0=ALU.subtract, op1=ALU.mult),
                 reads=[Bo32, Bmv, Brstd], writes=[Bo32])
        S.op("pool", lambda h: h.tensor_tensor(out=ytok[:], in0=o32[:], in1=sg[:], op=ALU.mult), reads=[Bo32, Bsg], writes=[Bytok])
        for half in range(2):
            pb = half
            for q in range(8):
                i = half * 8 + q
                S.op("pe", lambda h, i=i, q=q, pb=pb: h.transpose(out=psb[pb][:, q * 128:(q + 1) * 128], in_=ytok[:, i, :], identity=identb[:]),
                     reads=[Bytok, Bc], writes=[Bpsb[pb]])
            if half == 0:
                S.op("act", lambda h, half=half, pb=pb: h.copy(out=yTh[:, half * 1024:(half + 1) * 1024], in_=psb[pb][:, :]), reads=[Bpsb[pb]], writes=[ByTh])
            else:
                S.op("dve", lambda h, half=half, pb=pb: h.tensor_copy(out=yTh[:, half * 1024:(half + 1) * 1024], in_=psb[pb][:, :]), reads=[Bpsb[pb]], writes=[ByTh])
        S.dma("sp", ch_yT, lambda h, hh=hh: h.dma_start(out=yT_s[hh * 128:(hh + 1) * 128, :], in_=yTh[:]), reads=[ByTh], writes=[ByT_s])
    S.barrier()
    AR.reset(m2c)
    if stage <= 3:
        if debug:
            d_yT = dbg_out("yT", [D, SEQ], BF16)
            ld = AR.alloc("dbgld", [128, KC, SEQ], BF16)
            Bld = Buf("dbgld")
            S.dma("sp", S.chan("dbgy1"), lambda h: h.dma_start(out=ld[:], in_=yT_s.rearrange("(c p) t -> p c t", p=128)), reads=[ByT_s], writes=[Bld])
            final_ops.append(S.dma("sp", S.chan("dbgy2"), lambda h: h.dma_start(out=d_yT.rearrange("(c p) t -> p c t", p=128), in_=ld[:]), reads=[Bld]))
        return finish(nc, S, final_ops), dbg

    m2d = AR.mark()
    cvrows = AR.alloc("cvrows", [24, 128], F32)
    cvT = AR.alloc("cvT", [128, 24], F32)
    cwrows = AR.alloc("cwrows", [CW, DCONV], F32)
    cwT = AR.alloc("cwT", [128, 8, CW], F32)
    Bcvrows, BcvT, Bcwrows, BcwT = Buf("cvrows"), Buf("cvT"), Buf("cwrows"), Buf("cwT")
    S.dma("sp", ch_v, lambda h: h.dma_start(out=cvrows[:], in_=cvec_d), writes=[Bcvrows])
    S.dma("sp", ch_v, lambda h: h.dma_start(out=cwrows[:], in_=convw_d), writes=[Bcwrows], cont=True)
    transpose_rows(cvrows[:], 24, cvT[:], 0, Bpsf[0], [Bcvrows], [BcvT])
    for cc in range(8):
        S.op("pe", lambda h, cc=cc: h.transpose(out=psf[1][:, 0:CW], in_=cwrows[:, cc * 128:(cc + 1) * 128], identity=ident[0:CW, 0:CW]),
             reads=[Bcwrows, Bc], writes=[Bpsf[1]])
        S.op("act", lambda h, cc=cc: h.copy(out=cwT[:, cc, :], in_=psf[1][:, 0:CW]), reads=[Bpsf[1]], writes=[BcwT])
    wc = [AR.alloc(f"wc{i}", [128, KC, 256], BF16) for i in range(2)]
    Bwc = [Buf("wc0"), Buf("wc1")]
    ch_wc = [S.chan("wc0"), S.chan("wc1")]
    sig = AR.alloc("sig", [128, 512], F32)
    uu = AR.alloc("uu", [128, 8, 64], F32)
    cvo = AR.alloc("cvo", [128, 8, 8, 64], F32)
    cv2 = AR.alloc("cv2", [128, 8, 64], F32)
    Bcv2 = Buf("cv2")
    sq = AR.alloc("sq", [128, 512], F32)
    mean = AR.alloc("mean", [128, 512], F32)
    msq = AR.alloc("msq", [128, 512], F32)
    rsd = AR.alloc("rsd", [128, 512], F32)
    tn = AR.alloc("tn", [128, 512], F32)
    tns = [tn, AR.alloc("tn2", [128, 512], F32)]
    Btns = [Buf("tn_0"), Buf("tn_1")]
    ycv = [AR.alloc(f"ycv{i}", [128, 512], BF16) for i in range(2)]
    Bsig, Buu, Bcvo, Bsq, Bmean, Bmsq, Brsd, Btn = (Buf(n) for n in ["sig", "uu", "cvo", "sq", "mean", "msq", "rsd", "tn"])
    Bycv = [Buf("ycv0"), Buf("ycv1")]
    ch_ycv = [S.chan("ycv0"), S.chan("ycv1")]
    pcount = 0
    for tbk in range(4):
        tsl = slice(tbk * 512, (tbk + 1) * 512)
        for cc in range(8):
            bi = pcount % 2
            pcount += 1
            S.dma("pool", ch_wc[bi], lambda h, cc=cc, bi=bi: h.dma_start(out=wc[bi][:, :, 0:128], in_=win_v[:, :, 4 * DRET + cc * 128:4 * DRET + (cc + 1) * 128]),
                  writes=[Bwc[bi]])
            S.dma("pool", ch_wc[bi], lambda h, cc=cc, bi=bi: h.dma_start(out=wc[bi][:, :, 128:256],
                                                                          in_=win_v[:, :, 4 * DRET + DCONV + cc * 128:4 * DRET + DCONV + (cc + 1) * 128]),
                  writes=[Bwc[bi]], cont=True)
            ba, bb = 0 + 2 * (cc % 2), 1 + 2 * (cc % 2)
            for kc in range(KC):
                S.op("pe", lambda h, kc=kc, bi=bi, ba=ba, tsl=tsl: h.matmul(psf[ba][:, :], lhsT=wc[bi][:, kc, 0:128], rhs=hT[:, kc, tsl], start=(kc == 0), stop=(kc == KC - 1)),
                     reads=[Bwc[bi]] + BhT[tbk * 4:(tbk + 1) * 4], writes=[Bpsf[ba]])
            for kc in range(KC):
                S.op("pe", lambda h, kc=kc, bi=bi, bb=bb, tsl=tsl: h.matmul(psf[bb][:, :], lhsT=wc[bi][:, kc, 128:256], rhs=hT[:, kc, tsl], start=(kc == 0), stop=(kc == KC - 1)),
                     reads=[Bwc[bi]] + BhT[tbk * 4:(tbk + 1) * 4], writes=[Bpsf[bb]])
            S.op("act", lambda h, bb=bb: h.activation(out=sig[:], in_=psf[bb][:, :], func=AF.Sigmoid), reads=[Bpsf[bb]], writes=[Bsig])
            S.op("dve", lambda h, ba=ba: h.tensor_tensor(out=uu[:].rearrange("p a b -> p (a b)"), in0=psf[ba][:, :], in1=sig[:], op=ALU.mult),
                 reads=[Bpsf[ba], Bsig], writes=[Buu])
            acc = cvo[:, cc, :, :]
            S.op("dve", lambda h, cc=cc, acc=acc: h.tensor_scalar(out=acc, in0=uu[:], scalar1=cwT[:, cc, 15:16], scalar2=cvT[:, cc:cc + 1], op0=ALU.mult, op1=ALU.add),
                 reads=[Buu, BcwT, BcvT], writes=[Bcvo])
            S.op("dve", lambda h: h.memset(cv2[:], 0.0), writes=[Bcv2])
            taps = [k for k in range(CW) if k != 15]
            for ti, k in enumerate(taps):
                o = k - 15
                lo, hi = max(0, -o), min(64, 64 - o)
                if ti % 2 == 0:
                    S.op("dve", lambda h, cc=cc, k=k, o=o, lo=lo, hi=hi: h.scalar_tensor_tensor(out=cv2[:, :, lo:hi], in0=uu[:, :, lo + o:hi + o], scalar=cwT[:, cc, k:k + 1],
                                                                                                 in1=cv2[:, :, lo:hi], op0=ALU.mult, op1=ALU.add),
                         reads=[Buu, BcwT, Bcv2], writes=[Bcv2])
                else:
                    S.op("dve", lambda h, cc=cc, k=k, o=o, lo=lo, hi=hi: h.scalar_tensor_tensor(out=cvo[:, cc, :, lo:hi], in0=uu[:, :, lo + o:hi + o], scalar=cwT[:, cc, k:k + 1],
                                                                                                 in1=cvo[:, cc, :, lo:hi], op0=ALU.mult, op1=ALU.add),
                         reads=[Buu, BcwT, Bcvo], writes=[Bcvo])
            S.op("dve", lambda h, acc=acc: h.tensor_tensor(out=acc, in0=acc, in1=cv2[:], op=ALU.add), reads=[Bcvo, Bcv2], writes=[Bcvo])
            accf = cvo[:, cc, :, :].rearrange("p a b -> p (a b)")
            S.op("act", lambda h, accf=accf: h.activation(out=sq[:], in_=accf, func=AF.Square), reads=[Bcvo], writes=[Bsq])
            S.op("pe", lambda h, accf=accf, cc=cc: h.matmul(psf[4][:, :], lhsT=ones32[:], rhs=accf, start=(cc == 0), stop=(cc == 7)), reads=[Bcvo, Bc], writes=[Bpsf[4]])
            S.op("pe", lambda h, cc=cc: h.matmul(psf[5][:, :], lhsT=ones32[:], rhs=sq[:], start=(cc == 0), stop=(cc == 7)), reads=[Bsq, Bc], writes=[Bpsf[5]])
        S.op("act", lambda h: h.activation(out=mean[:], in_=psf[4][:, :], func=AF.Identity, scale=1.0 / DCONV), reads=[Bpsf[4]], writes=[Bmean])
        S.op("dve", lambda h: h.tensor_tensor(out=msq[:], in0=mean[:], in1=mean[:], op=ALU.mult), reads=[Bmean], writes=[Bmsq])
        S.op("dve", lambda h: h.scalar_tensor_tensor(out=rsd[:], in0=psf[5][:, :], scalar=1.0 / DCONV, in1=msq[:], op0=ALU.mult, op1=ALU.subtract),
             reads=[Bpsf[5], Bmsq], writes=[Brsd])
        S.op("act", lambda h: h.activation(out=rsd[:], in_=rsd[:], func=AF.Sqrt, bias=EPS), reads=[Brsd], writes=[Brsd])
        S.op("dve", lambda h: h.reciprocal(out=rsd[:], in_=rsd[:]), reads=[Brsd], writes=[Brsd])
        for cc in range(8):
            yb = cc % 2
            accf = cvo[:, cc, :, :].rearrange("p a b -> p (a b)")
            tnb = tns[cc % 2]
            Btnb = Btns[cc % 2]
            S.op("dve", lambda h, accf=accf, tnb=tnb: h.tensor_tensor(out=tnb[:], in0=accf, in1=mean[:], op=ALU.subtract), reads=[Bcvo, Bmean], writes=[Btnb])
            S.op("dve", lambda h, tnb=tnb: h.tensor_tensor(out=tnb[:], in0=tnb[:], in1=rsd[:], op=ALU.mult), reads=[Btnb, Brsd], writes=[Btnb])
            S.op("act", lambda h, cc=cc, yb=yb, tnb=tnb: h.activation(out=ycv[yb][:], in_=tnb[:], func=AF.Silu, scale=cvT[:, 8 + cc:9 + cc], bias=cvT[:, 16 + cc:17 + cc]),
                 reads=[Btnb, BcvT], writes=[Bycv[yb]])
            S.dma("sp", ch_ycv[yb], lambda h, cc=cc, yb=yb, tsl=tsl: h.dma_start(out=yT_s[DRET + cc * 128:DRET + (cc + 1) * 128, tsl], in_=ycv[yb][:]),
                  reads=[Bycv[yb]], writes=[ByT_s])
    S.barrier()
    AR.reset(m2)
    AR.reset(persist_mark)
    if stage <= 4:
        if debug:
            d_yT = dbg_out("yT", [D, SEQ], BF16)
            ld = AR.alloc("dbgld", [128, KC, SEQ], BF16)
            Bld = Buf("dbgld")
            S.dma("sp", S.chan("dbgy1"), lambda h: h.dma_start(out=ld[:], in_=yT_s.rearrange("(c p) t -> p c t", p=128)), reads=[ByT_s], writes=[Bld])
            final_ops.append(S.dma("sp", S.chan("dbgy2"), lambda h: h.dma_start(out=d_yT.rearrange("(c p) t -> p c t", p=128), in_=ld[:]), reads=[Bld]))
        return finish(nc, S, final_ops), dbg

    m3 = AR.mark()
    wo = AR.alloc("wo", [128, KC, D], BF16)
    Bwo = Buf("wo")
    ch_wo = S.chan("wo")
    wout_v = wout_d.rearrange("(k p) n -> p k n", p=128)
    for j in range(4):
        S.dma("pool", ch_wo, lambda h, j=j: h.dma_start(out=wo[:, :, j * 512:(j + 1) * 512], in_=wout_v[:, :, j * 512:(j + 1) * 512]), writes=[Bwo], cont=(j > 0))
    G1 = AR.alloc("G1", [128, D], F32)
    A2 = AR.alloc("A2", [128, D], F32)
    S2 = AR.alloc("S2", [128, D], F32)
    wt3 = AR.alloc("wt3", [128, D], F32)
    BG1, BA2, BS2, Bwt3 = Buf("G1"), Buf("A2"), Buf("S2"), Buf("wt3")
    load_mod_bcast(G1, 0, 2, ch_v, BG1)
    load_row_bcast(wt3, pomw_d, ch_v, Bwt3)
    S.op("dve", lambda h: h.tensor_tensor(out=G1[:], in0=G1[:], in1=wt3[:], op=ALU.mult), reads=[BG1, Bwt3], writes=[BG1])
    load_mod_bcast(A2, 0, 4, ch_v, BA2)
    load_row_bcast(wt3, pfw_d, ch_v, Bwt3)
    S.op("dve", lambda h: h.scalar_tensor_tensor(out=A2[:], in0=A2[:], scalar=1.0, in1=wt3[:], op0=ALU.add, op1=ALU.mult), reads=[BA2, Bwt3], writes=[BA2])
    load_mod_bcast(S2, 0, 3, ch_v, BS2)
    rw32 = AR.alloc("rw32", [128, KC, NE], F32)
    rbb = AR.alloc("rbb", [128, NE], F32)
    ebase = AR.alloc("ebase", [128, NE], F32)
    Brw, Brbb, Bebase = Buf("rw32"), Buf("rbb"), Buf("ebase")
    S.dma("sp", ch_v, lambda h: h.dma_start(out=rw32[:], in_=rw_d.rearrange("(k p) e -> p k e", p=128)), writes=[Brw])
    S.dma("sp", ch_v, lambda h: h.dma_start(out=rbb[:], in_=rb_d.partition_broadcast(128)), writes=[Brbb], cont=True)
    S.op("dve", lambda h: h.tensor_scalar(out=ebase[:], in0=iorow[:, 0:NE], scalar1=float(CAP), scalar2=None, op0=ALU.mult), reads=[Bc], writes=[Bebase])
    yTt = [AR.alloc(f"yTt{i}", [128, KC, 128], BF16) for i in range(2)]
    ByTt = [Buf("yTt0"), Buf("yTt1")]
    ch_yTt = [S.chan("yTt0"), S.chan("yTt1")]
    xr = [AR.alloc(f"xr{i}", [128, D], F32) for i in range(2)]
    Bxr = [Buf("xr0"), Buf("xr1")]
    ch_xr = [S.chan("xr0"), S.chan("xr1")]
    t3 = AR.alloc("t3", [128, D], F32)
    x1t = [AR.alloc(f"x1t{i}", [128, D], F32) for i in range(2)]
    h2f = AR.alloc("h2f", [128, D], F32)
    h2b = [AR.alloc(f"h2b{i}", [128, D], BF16) for i in range(2)]
    h2T = AR.alloc("h2T", [128, KC, 128], F32)
    junk3 = AR.alloc("junk3", [128, D], BF16)
    st3 = AR.alloc("st3", [128, 8], F32)
    lg = AR.alloc("lg", [128, NE], F32)
    mx8 = AR.alloc("mx8", [128, 8], F32)
    nmx = AR.alloc("nmx", [128, 1], F32)
    msk = AR.alloc("msk", [128, NE], F32)
    mskb = AR.alloc("mskb", [128, NT, NE], BF16)
    exv = AR.alloc("exv", [128, NE], F32)
    den = AR.alloc("den", [128, 1], F32)
    posC = AR.alloc("posC", [128, NE], F32)
    ovf = AR.alloc("ovf", [128, NE], F32)
    oh = AR.alloc("oh", [128, NE], F32)
    jk = AR.alloc("jk", [128, NE], F32)
    idxf = AR.alloc("idxf", [128, 4], F32)
    idl = AR.alloc("idl", [128, 4], F32)
    idn = AR.alloc("idn", [128, 4], F32)
    Bidl, Bidn = Buf("idl"), Buf("idn")
    (Bt3, Bh2f, Bh2T, Bjunk3, Bst3, Blg, Bmx8, Bnmx, Bmsk, Bmskb, Bexv, Bden, BposC, Bovf, Boh, Bjk, Bidxf) = (
        Buf(n) for n in ["t3", "h2f", "h2T", "junk3", "st3", "lg", "mx8", "nmx", "msk", "mskb", "exv", "den", "posC", "ovf", "oh", "jk", "idxf"])
    Bx1t = [Buf("x1t0"), Buf("x1t1")]
    Bh2b = [Buf("h2b0"), Buf("h2b1")]
    ch_x1 = [S.chan("x1w0"), S.chan("x1w1")]
    ch_sc = [S.chan(f"scat{i}") for i in range(2)]
    Bx1_s = [Buf(f"x1_s{i}") for i in range(NT)]
    Bhsel = Buf("hsel_s")
    yT_v = yT_s.rearrange("(c p) t -> p c t", p=128)
    mixps = [psf[0], psf[1], psf[2], psf[3]]
    for i in range(NT):
        bi = i % 2
        S.dma("sp", ch_yTt[bi], lambda h, i=i, bi=bi: h.dma_start(out=yTt[bi][:], in_=yT_v[:, :, i * 128:(i + 1) * 128]), reads=[ByT_s], writes=[ByTt[bi]])
        S.dma("sp", ch_xr[bi], lambda h, i=i, bi=bi: h.dma_start(out=xr[bi][:], in_=x_d[i * 128:(i + 1) * 128, :]), writes=[Bxr[bi]])
        for cb in range(4):
            for c in range(KC):
                S.op("pe", lambda h, c=c, cb=cb, bi=bi: h.matmul(psf[cb][:, :], lhsT=yTt[bi][:, c, :], rhs=wo[:, c, cb * 512:(cb + 1) * 512], start=(c == 0), stop=(c == KC - 1)),
                     reads=[ByTt[bi], Bwo], writes=[Bpsf[cb]])
        for cb in range(4):
            S.op("act", lambda h, cb=cb: h.activation(out=junk3[:, cb * 512:(cb + 1) * 512], in_=psf[cb][:, :], func=AF.Square, accum_out=st3[:, cb:cb + 1]),
                 reads=[Bpsf[cb]], writes=[Bjunk3, Bst3])
        S.op("dve", lambda h: h.tensor_reduce(out=st3[:, 4:5], in_=st3[:, 0:4], axis=AX.X, op=ALU.add), reads=[Bst3], writes=[Bst3])
        S.op("act", lambda h: h.activation(out=st3[:, 4:5], in_=st3[:, 4:5], func=AF.Sqrt, scale=1.0 / D, bias=EPS), reads=[Bst3], writes=[Bst3])
        S.op("dve", lambda h: h.reciprocal(out=st3[:, 4:5], in_=st3[:, 4:5]), reads=[Bst3], writes=[Bst3])
        for cb in range(4):
            S.op("dve", lambda h, cb=cb: h.scalar_tensor_tensor(out=t3[:, cb * 512:(cb + 1) * 512], in0=psf[cb][:, :], scalar=st3[:, 4:5], in1=G1[:, cb * 512:(cb + 1) * 512],
                                                                op0=ALU.mult, op1=ALU.mult), reads=[Bpsf[cb], Bst3, BG1], writes=[Bt3])
        S.op("dve", lambda h, bi=bi: h.tensor_tensor(out=x1t[bi][:], in0=t3[:], in1=xr[bi][:], op=ALU.add), reads=[Bt3, Bxr[bi]], writes=[Bx1t[bi]])
        S.dma("sp", ch_x1[bi], lambda h, i=i, bi=bi: h.dma_start(out=x1_s[i * 128:(i + 1) * 128, :], in_=x1t[bi][:]), reads=[Bx1t[bi]], writes=[Bx1_s[i]])
        S.op("act", lambda h, bi=bi: h.activation(out=junk3[:], in_=x1t[bi][:], func=AF.Square, accum_out=st3[:, 5:6]), reads=[Bx1t[bi]], writes=[Bjunk3, Bst3])
        S.op("act", lambda h: h.activation(out=st3[:, 5:6], in_=st3[:, 5:6], func=AF.Sqrt, scale=1.0 / D, bias=EPS), reads=[Bst3], writes=[Bst3])
        S.op("dve", lambda h: h.reciprocal(out=st3[:, 5:6], in_=st3[:, 5:6]), reads=[Bst3], writes=[Bst3])
        S.op("dve", lambda h, bi=bi: h.scalar_tensor_tensor(out=t3[:], in0=x1t[bi][:], scalar=st3[:, 5:6], in1=A2[:], op0=ALU.mult, op1=ALU.mult),
             reads=[Bx1t[bi], Bst3, BA2], writes=[Bt3])
        S.op("dve", lambda h: h.tensor_tensor(out=h2f[:], in0=t3[:], in1=S2[:], op=ALU.add), reads=[Bt3, BS2], writes=[Bh2f])
        S.op("act", lambda h, bi=bi: h.copy(out=h2b[bi][:], in_=h2f[:]), reads=[Bh2f], writes=[Bh2b[bi]])
        for q4 in range(4):
            bank = 4 + (q4 % 2)
            for q in range(4):
                kc = q4 * 4 + q
                S.op("pe", lambda h, kc=kc, q=q, bank=bank: h.transpose(out=psf[bank][:, q * 128:(q + 1) * 128], in_=h2f[:, kc * 128:(kc + 1) * 128], identity=ident[:]),
                     reads=[Bh2f, Bc], writes=[Bpsf[bank]])
            if q4 % 2 == 0:
                S.op("act", lambda h, q4=q4, bank=bank: h.copy(out=h2T[:, q4 * 4:(q4 + 1) * 4, :], in_=psf[bank][:, :].rearrange("p (q t) -> p q t", q=4)),
                     reads=[Bpsf[bank]], writes=[Bh2T])
            else:
                S.op("dve", lambda h, q4=q4, bank=bank: h.tensor_copy(out=h2T[:, q4 * 4:(q4 + 1) * 4, :], in_=psf[bank][:, :].rearrange("p (q t) -> p q t", q=4)),
                     reads=[Bpsf[bank]], writes=[Bh2T])
        for kc in range(KC):
            S.op("pe", lambda h, kc=kc: h.matmul(psf[4][:, 0:NE], lhsT=h2T[:, kc, :], rhs=rw32[:, kc, :], start=(kc == 0), stop=(kc == KC - 1)),
                 reads=[Bh2T, Brw], writes=[Bpsf[4]])
        S.op("dve", lambda h: h.tensor_tensor(out=lg[:], in0=psf[4][:, 0:NE], in1=rbb[:], op=ALU.add), reads=[Bpsf[4], Brbb], writes=[Blg])
        S.op("dve", lambda h: h.max(out=mx8[:], in_=lg[:]), reads=[Blg], writes=[Bmx8])
        S.op("dve", lambda h: h.tensor_scalar(out=msk[:], in0=lg[:], scalar1=mx8[:, 3:4], scalar2=None, op0=ALU.is_ge), reads=[Blg, Bmx8], writes=[Bmsk])
        S.op("dve", lambda h, i=i: h.tensor_copy(out=mskb[:, i, :], in_=msk[:]), reads=[Bmsk], writes=[Bmskb])
        S.op("dve", lambda h: h.tensor_scalar(out=nmx[:], in0=mx8[:, 0:1], scalar1=-1.0, scalar2=None, op0=ALU.mult), reads=[Bmx8], writes=[Bnmx])
        S.op("act", lambda h: h.activation(out=exv[:], in_=lg[:], func=AF.Exp, bias=nmx[:, 0:1]), reads=[Blg, Bnmx], writes=[Bexv])
        S.op("dve", lambda h: h.tensor_tensor(out=exv[:], in0=exv[:], in1=msk[:], op=ALU.mult), reads=[Bexv, Bmsk], writes=[Bexv])
        S.op("dve", lambda h: h.tensor_reduce(out=den[:], in_=exv[:], axis=AX.X, op=ALU.add), reads=[Bexv], writes=[Bden])
        S.op("dve", lambda h: h.reciprocal(out=den[:], in_=den[:]), reads=[Bden], writes=[Bden])
        S.op("pe", lambda h, i=i: h.matmul(psf[5][:, 0:NE], lhsT=trib[:], rhs=mskb[:, i, :], start=True, stop=(i == 0)), reads=[Bmskb, Bc], writes=[Bpsf[5]])
        for j in range(i):
            S.op("pe", lambda h, j=j, i=i: h.matmul(psf[5][:, 0:NE], lhsT=onesb[:], rhs=mskb[:, j, :], start=False, stop=(j == i - 1)), reads=[Bmskb, Bc], writes=[Bpsf[5]])
        S.op("dve", lambda h: h.tensor_scalar(out=ovf[:], in0=psf[5][:, 0:NE], scalar1=float(CAP) - 0.5, scalar2=None, op0=ALU.is_gt), reads=[Bpsf[5]], writes=[Bovf])
        S.op("dve", lambda h: h.tensor_tensor(out=posC[:], in0=psf[5][:, 0:NE], in1=ebase[:], op=ALU.add), reads=[Bpsf[5], Bebase], writes=[BposC])
        S.op("dve", lambda h: h.scalar_tensor_tensor(out=posC[:], in0=ovf[:], scalar=BIG, in1=posC[:], op0=ALU.mult, op1=ALU.add), reads=[Bovf, BposC], writes=[BposC])
        S.op("dve", lambda h: h.tensor_scalar(out=ovf[:], in0=ovf[:], scalar1=-1.0, scalar2=1.0, op0=ALU.mult, op1=ALU.add), reads=[Bovf], writes=[Bovf])
        S.op("dve", lambda h, i=i: h.scalar_tensor_tensor(out=gatesA[:, i, :], in0=exv[:], scalar=den[:, 0:1], in1=ovf[:], op0=ALU.mult, op1=ALU.mult),
             reads=[Bexv, Bden, Bovf], writes=[BgatesA])
        for k in range(4):
            S.op("dve", lambda h, k=k: h.tensor_scalar(out=oh[:], in0=lg[:], scalar1=mx8[:, k:k + 1], scalar2=None, op0=ALU.is_equal), reads=[Blg, Bmx8], writes=[Boh])
            S.op("dve", lambda h, k=k: h.scalar_tensor_tensor(out=jk[:], in0=oh[:], scalar=1.0, in1=posC[:], op0=ALU.mult, op1=ALU.mult, accum_out=idxf[:, k:k + 1]),
                 reads=[Boh, BposC], writes=[Bjk, Bidxf])
            S.op("dve", lambda h, k=k, i=i: h.scalar_tensor_tensor(out=jk[:], in0=oh[:], scalar=1.0, in1=gatesA[:, i, :], op0=ALU.mult, op1=ALU.mult,
                                                                 accum_out=gate4[:, i, k:k + 1]), reads=[Boh, BgatesA], writes=[Bjk, Bgate4])
        for (lst, nten, Bl) in [(idxH, NHS, Bidx[i]), (idxY, NYS, Bidx[i])]:
            for j in range(nten):
                shift = float(j * (NE // nten) * CAP)
                S.op("dve", lambda h, shift=shift: h.tensor_scalar(out=idl[:], in0=idxf[:], scalar1=shift, scalar2=None, op0=ALU.subtract), reads=[Bidxf], writes=[Bidl])
                S.op("dve", lambda h: h.tensor_scalar(out=idn[:], in0=idl[:], scalar1=0.0, scalar2=BIG, op0=ALU.is_lt, op1=ALU.mult), reads=[Bidl], writes=[Bidn])
                S.op("dve", lambda h: h.tensor_tensor(out=idl[:], in0=idl[:], in1=idn[:], op=ALU.add), reads=[Bidl, Bidn], writes=[Bidl])
                S.op("dve", lambda h, i=i, t=lst[j]: h.tensor_copy(out=t[:, i * 4:(i + 1) * 4], in_=idl[:]), reads=[Bidl], writes=[Bl])
        first = True
        for k in range(4):
            for j in range(NHS):
                S.dma("pool", ch_sc[bi], lambda h, i=i, k=k, bi=bi, j=j: h.indirect_dma_start(out=hsel_s[j], out_offset=bass.IndirectOffsetOnAxis(ap=idxH[j][:, i * 4 + k:i * 4 + k + 1], axis=0),
                                                                                           in_=h2b[bi][:], in_offset=None, bounds_check=bound_reg(h, (NE // NHS) * CAP - 1), oob_is_err=False),
                      reads=[Bh2b[bi], Bidx[i]], writes=[Bhsel], cont=(not first))
                first = False
    for j in range(NT):
        S.op("pe", lambda h, j=j: h.matmul(psf[5][:, 0:NE], lhsT=onesb[:], rhs=mskb[:, j, :], start=(j == 0), stop=(j == NT - 1)), reads=[Bmskb, Bc], writes=[Bpsf[5]])
    S.cnt_op = S.op("dve", lambda h: h.tensor_copy(out=cnt_i[:], in_=psf[5][:, 0:NE]), reads=[Bpsf[5]], writes=[Bcnt])
    S.cnt_ap = lambda e: cnt_i[0:1, e:e + 1]
    if debug:
        d_lg = dbg_out("gates", [128, NT, NE])
        final_ops.append(S.dma("sp", S.chan("dbglg"), lambda h: h.dma_start(out=d_lg, in_=gatesA[:]), reads=[BgatesA]))
        d_idx = dbg_out("idx4", [128, NT * 4], I32)
        final_ops.append(S.dma("sp", S.chan("dbgidx"), lambda h: h.dma_start(out=d_idx, in_=idxY[0][:]), reads=Bidx))
        d_cnt = dbg_out("cnt", [128, NE], I32)
        final_ops.append(S.dma("sp", S.chan("dbgcnt"), lambda h: h.dma_start(out=d_cnt, in_=cnt_i[:]), reads=[Bcnt]))
        d_g4 = dbg_out("gate4", [128, NT, 4])
        final_ops.append(S.dma("sp", S.chan("dbgg4"), lambda h: h.dma_start(out=d_g4, in_=gate4[:]), reads=[Bgate4]))
    S.barrier()
    AR.reset(m3)
    if stage <= 5:
        if debug:
            d_x1 = dbg_out("x1", [SEQ, D])
            ld = AR.alloc("dbgld", [128, NT, D], F32)
            Bld = Buf("dbgld")
            S.dma("sp", S.chan("dbgx1"), lambda h: h.dma_start(out=ld[:], in_=x1_s.rearrange("(t p) d -> p t d", p=128)), reads=Bx1_s, writes=[Bld])
            final_ops.append(S.dma("sp", S.chan("dbgx2"), lambda h: h.dma_start(out=d_x1.rearrange("(t p) d -> p t d", p=128), in_=ld[:]), reads=[Bld]))
        return finish(nc, S, final_ops), dbg

    m5 = AR.mark()
    hselT = [AR.alloc(f"hselT{i}", [128, KC, RS], BF16) for i in range(2)]
    BhselT = [Buf("hselT0"), Buf("hselT1")]
    actT = AR.alloc("actT", [128, KC, RS], BF16)
    BactT = Buf("actT")
    NW1, NW2 = 4, 3
    w1u = [AR.alloc(f"w1u{i}", [128, KC, 512], BF16) for i in range(NW1)]
    Bw1u = [Buf(f"w1u{i}") for i in range(NW1)]
    ch_w1 = [S.chan(f"w1u{i}") for i in range(NW1)]
    ch_w1g = [S.chan(f"w1ug{i}") for i in range(NW1)]
    w2p = [AR.alloc(f"w2p{i}", [128, KC, 512], BF16) for i in range(NW2)]
    Bw2p = [Buf(f"w2p{i}") for i in range(NW2)]
    ch_w2 = [S.chan(f"w2p{i}") for i in range(NW2)]
    ch_w2g = [S.chan(f"w2pg{i}") for i in range(NW2)]
    hrow = [AR.alloc(f"hrow{i}", [128, D], BF16) for i in range(2)]
    Bhrow = [Buf("hrow0"), Buf("hrow1")]
    NCH_H, NCH_Y = 8, 16
    ch_hrow = [S.chan(f"hrow{i}") for i in range(NCH_H)]
    ysb = [AR.alloc(f"ysb{i}", [128, 512], F32) for i in range(4)]
    Bysb = [Buf(f"ysb{i}") for i in range(4)]
    ch_y = [S.chan(f"yw{i}") for i in range(NCH_Y)]
    hcnt = [0]
    g1 = [AR.alloc(f"g1_{i}", [128, BLK], F32) for i in range(2)]
    sgm = [AR.alloc(f"sgm{i}", [128, BLK], F32) for i in range(2)]
    l2 = [AR.alloc(f"l2_{i}", [128, BLK], F32) for i in range(2)]
    wv = [AR.alloc(f"wv_{i}", [128, BLK], F32) for i in range(2)]
    Bg1 = [Buf("g1_0"), Buf("g1_1")]
    Bsgm = [Buf("sgm0"), Buf("sgm1")]
    Bl2 = [Buf("l2_0"), Buf("l2_1")]
    Bwv = [Buf("wv_0"), Buf("wv_1")]
    By_s = Buf("y_s")
    w1_v = [w1_d[e].rearrange("(k p) n -> p k n", p=128) for e in range(NE)]
    w2_v = [w2_d[e].rearrange("(k p) n -> p k n", p=128) for e in range(NE)]

    def blk_guard(e, r, b):
        return (e, b * BLK) if r == 0 else (e, r * RS)

    def rnd_guard(e, r):
        return (e, r * RS) if r > 0 else None

    def build_hselT(e, r, buf_i):
        for b in range(NBLK):
            S.cur_guard = blk_guard(e, r, b)
            for st2 in range(BLK // 128):
                st = b * (BLK // 128) + st2
                hb_i = st % 2
                row0 = (e % (NE // NHS)) * CAP + r * RS + st * 128
                src = hsel_s[e // (NE // NHS)]
                chh = ch_hrow[hcnt[0] % NCH_H]
                hcnt[0] += 1
                S.dma("sp", chh, lambda h, row0=row0, hb_i=hb_i, src=src: h.dma_start(out=hrow[hb_i][:], in_=src[row0:row0 + 128, :]), reads=[Bhsel], writes=[Bhrow[hb_i]])
                for half in range(2):
                    pb = half
                    for q in range(8):
                        kc = half * 8 + q
                        S.op("pe", lambda h, kc=kc, q=q, pb=pb, hb_i=hb_i: h.transpose(out=psb[pb][:, q * 128:(q + 1) * 128], in_=hrow[hb_i][:, kc * 128:(kc + 1) * 128], identity=identb[:]),
                             reads=[Bhrow[hb_i], Bc], writes=[Bpsb[pb]])
                    if half == 0:
                        S.op("act", lambda h, half=half, pb=pb, st=st, buf_i=buf_i: h.copy(out=hselT[buf_i][:, half * 8:(half + 1) * 8, st * 128:(st + 1) * 128],
                                                                                        in_=psb[pb][:, :].rearrange("p (q t) -> p q t", q=8)), reads=[Bpsb[pb]], writes=[BhselT[buf_i]])
                    else:
                        S.op("dve", lambda h, half=half, pb=pb, st=st, buf_i=buf_i: h.tensor_copy(out=hselT[buf_i][:, half * 8:(half + 1) * 8, st * 128:(st + 1) * 128],
                                                                                               in_=psb[pb][:, :].rearrange("p (q t) -> p q t", q=8)), reads=[Bpsb[pb]], writes=[BhselT[buf_i]])
        S.cur_guard = None

    ucount = 0
    pcount2 = 0
    acnt = 0
    ycnt = 0
    er_list = [(e, r) for e in range(NE) for r in range(ROUNDS)]
    build_hselT(0, 0, 0)
    for n, (e, r) in enumerate(er_list):
        hb_cur = e % 2
        if r > 0:
            build_hselT(e, r, hb_cur)
        for u in range(8):
            bi = ucount % NW1
            ucount += 1
            S.cur_guard = rnd_guard(e, r)
            cw1 = ch_w1[bi] if r == 0 else ch_w1g[bi]
            S.dma("pool", cw1, lambda h, e=e, u=u, bi=bi: h.dma_start(out=w1u[bi][:, :, 0:256], in_=w1_v[e][:, :, u * 256:(u + 1) * 256]), writes=[Bw1u[bi]])
            S.dma("pool", cw1, lambda h, e=e, u=u, bi=bi: h.dma_start(out=w1u[bi][:, :, 256:512], in_=w1_v[e][:, :, DFF + u * 256:DFF + (u + 1) * 256]),
                  writes=[Bw1u[bi]], cont=True)
            for b in range(NBLK):
                S.cur_guard = blk_guard(e, r, b)
                for j in range(2):
                    fc = u * 2 + j
                    ab = acnt % 2
                    bank = acnt % 4
                    acnt += 1
                    bs = slice(b * BLK, (b + 1) * BLK)
                    for kc in range(KC):
                        S.op("pe", lambda h, kc=kc, bi=bi, j=j, bs=bs, bank=bank, hb_cur=hb_cur: h.matmul(psf[bank][:, 0:BLK], lhsT=w1u[bi][:, kc, j * 128:(j + 1) * 128],
                                                                                                     rhs=hselT[hb_cur][:, kc, bs], start=(kc == 0), stop=(kc == KC - 1)),
                             reads=[Bw1u[bi], BhselT[hb_cur]], writes=[Bpsf[bank]])
                    for kc in range(KC):
                        S.op("pe", lambda h, kc=kc, bi=bi, j=j, bs=bs, bank=bank, hb_cur=hb_cur: h.matmul(psf[bank][:, BLK:2 * BLK], lhsT=w1u[bi][:, kc, 256 + j * 128:256 + (j + 1) * 128],
                                                                                                     rhs=hselT[hb_cur][:, kc, bs], start=(kc == 0), stop=(kc == KC - 1)),
                             reads=[Bw1u[bi], BhselT[hb_cur]], writes=[Bpsf[bank]])
                    cg = e * 32 + fc
                    cl = e * 32 + 16 + fc
                    S.op("dve", lambda h, ab=ab, bank=bank, cg=cg: h.tensor_scalar(out=g1[ab][:], in0=psf[bank][:, 0:BLK], scalar1=b1T[:, cg:cg + 1], scalar2=LIMIT, op0=ALU.add, op1=ALU.min),
                         reads=[Bpsf[bank], Bb1T], writes=[Bg1[ab]])
                    S.op("act", lambda h, ab=ab: h.activation(out=sgm[ab][:], in_=g1[ab][:], func=AF.Sigmoid, scale=ALPHA), reads=[Bg1[ab]], writes=[Bsgm[ab]])
                    S.op("dve", lambda h, ab=ab, bank=bank, cl=cl: h.tensor_scalar(out=l2[ab][:], in0=psf[bank][:, BLK:2 * BLK], scalar1=b1T[:, cl:cl + 1], scalar2=LIMIT + 1.0, op0=ALU.add, op1=ALU.min),
                         reads=[Bpsf[bank], Bb1T], writes=[Bl2[ab]])
                    S.op("dve", lambda h, ab=ab: h.scalar_tensor_tensor(out=wv[ab][:], in0=l2[ab][:], scalar=1.0 - LIMIT, in1=g1[ab][:], op0=ALU.max, op1=ALU.mult),
                         reads=[Bl2[ab], Bg1[ab]], writes=[Bwv[ab]])
                    S.op("dve", lambda h, ab=ab, fc=fc, bs=bs: h.tensor_tensor(out=actT[:, fc, bs], in0=wv[ab][:], in1=sgm[ab][:], op=ALU.mult),
                         reads=[Bwv[ab], Bsgm[ab]], writes=[BactT])
        S.cur_guard = None
        if r == 0 and e + 1 < NE:
            build_hselT(e + 1, 0, (e + 1) % 2)
        ydst = y_s[e // (NE // NYS)]
        for db in range(4):
            bi = pcount2 % NW2
            pcount2 += 1
            S.cur_guard = rnd_guard(e, r)
            cw2 = ch_w2[bi] if r == 0 else ch_w2g[bi]
            S.dma("pool", cw2, lambda h, e=e, db=db, bi=bi: h.dma_start(out=w2p[bi][:], in_=w2_v[e][:, :, db * 512:(db + 1) * 512]), writes=[Bw2p[bi]])
            for b in range(NBLK):
                S.cur_guard = blk_guard(e, r, b)
                for st2 in range(BLK // 128):
                    st = b * (BLK // 128) + st2
                    bank = 4 + (ycnt % 2)
                    yb = ycnt % 4
                    ych = ch_y[ycnt % NCH_Y]
                    ycnt += 1
                    for fc in range(KC):
                        S.op("pe", lambda h, fc=fc, st=st, bi=bi, bank=bank: h.matmul(psf[bank][:, :], lhsT=actT[:, fc, st * 128:(st + 1) * 128], rhs=w2p[bi][:, fc, :],
                                                                                     start=(fc == 0), stop=(fc == KC - 1)),
                             reads=[BactT, Bw2p[bi]], writes=[Bpsf[bank]])
                    S.op("act", lambda h, bank=bank, yb=yb: h.copy(out=ysb[yb][:], in_=psf[bank][:, :]), reads=[Bpsf[bank]], writes=[Bysb[yb]])
                    row0 = (e % (NE // NYS)) * CAP + r * RS + st * 128
                    S.dma("sp", ych, lambda h, row0=row0, db=db, yb=yb, ydst=ydst: h.dma_start(out=ydst[row0:row0 + 128, db * 512:(db + 1) * 512], in_=ysb[yb][:]),
                          reads=[Bysb[yb]], writes=[By_s])
        S.cur_guard = None
    S.barrier()
    AR.reset(m5)

    G2 = AR.alloc("G2", [128, D], F32)
    wt6 = AR.alloc("wt6", [128, D], F32)
    b2sb = AR.alloc("b2sb", [NE, D], F32)
    BG2, Bwt6, Bb2 = Buf("G2"), Buf("wt6"), Buf("b2sb")
    load_mod_bcast(G2, 0, 5, ch_v, BG2)
    load_row_bcast(wt6, pofw_d, ch_v, Bwt6)
    S.op("dve", lambda h: h.tensor_tensor(out=G2[:], in0=G2[:], in1=wt6[:], op=ALU.mult), reads=[BG2, Bwt6], writes=[BG2])
    S.dma("sp", ch_v, lambda h: h.dma_start(out=b2sb[:], in_=b2_d), writes=[Bb2])
    yk = [[AR.alloc(f"yk{b}_{k}", [128, D], F32) for k in range(4)] for b in range(2)]
    Byk = [[Buf(f"yk{b}_{k}") for k in range(4)] for b in range(2)]
    ch_g = [S.chan("gath0"), S.chan("gath1")]
    x1r = [AR.alloc(f"x1r{i}", [128, D], F32) for i in range(2)]
    Bx1r = [Buf("x1r0"), Buf("x1r1")]
    ch_x1r = [S.chan("x1r0"), S.chan("x1r1")]
    ff = AR.alloc("ff", [128, D], F32)
    ot = [AR.alloc(f"ot{i}", [128, D], F32) for i in range(2)]
    gT = AR.alloc("gT", [NE, 128], F32)
    junk6 = AR.alloc("junk6", [128, D], BF16)
    st6 = AR.alloc("st6", [128, 2], F32)
    Bff, BgT, Bjunk6, Bst6 = Buf("ff"), Buf("gT"), Buf("junk6"), Buf("st6")
    Bot = [Buf("ot0"), Buf("ot1")]
    ch_out = [S.chan("out0"), S.chan("out1")]
    for i in range(NT):
        bi = i % 2
        first = True
        for k in range(4):
            for j in range(NYS):
                S.dma("pool", ch_g[bi], lambda h, i=i, k=k, bi=bi, j=j: h.indirect_dma_start(out=yk[bi][k][:], out_offset=None, in_=y_s[j],
                                                                                          in_offset=bass.IndirectOffsetOnAxis(ap=idxY[j][:, i * 4 + k:i * 4 + k + 1], axis=0),
                                                                                          bounds_check=bound_reg(h, (NE // NYS) * CAP - 1), oob_is_err=False),
                      reads=[By_s, Bidx[i]], writes=[Byk[bi][k]], cont=(not first))
                first = False
        S.dma("sp", ch_x1r[bi], lambda h, i=i, bi=bi: h.dma_start(out=x1r[bi][:], in_=x1_s[i * 128:(i + 1) * 128, :]), reads=[Bx1_s[i]], writes=[Bx1r[bi]])
        S.op("pe", lambda h, i=i: h.transpose(out=psf[4][0:NE, 0:128], in_=gatesA[:, i, :], identity=ident[:]), reads=[BgatesA, Bc], writes=[Bpsf[4]])
        S.op("act", lambda h: h.copy(out=gT[:], in_=psf[4][0:NE, 0:128]), reads=[Bpsf[4]], writes=[BgT])
        for cb in range(4):
            S.op("pe", lambda h, cb=cb: h.matmul(psf[cb][:, :], lhsT=gT[:], rhs=b2sb[:, cb * 512:(cb + 1) * 512], start=True, stop=True), reads=[BgT, Bb2], writes=[Bpsf[cb]])
            S.op("dve", lambda h, cb=cb, bi=bi, i=i: h.scalar_tensor_tensor(out=ff[:, cb * 512:(cb + 1) * 512], in0=yk[bi][0][:, cb * 512:(cb + 1) * 512], scalar=gate4[:, i, 0:1],
                                                                          in1=psf[cb][:, :], op0=ALU.mult, op1=ALU.add), reads=[Byk[bi][0], Bgate4, Bpsf[cb]], writes=[Bff])
        for k in range(1, 4):
            S.op("dve", lambda h, k=k, bi=bi, i=i: h.scalar_tensor_tensor(out=ff[:], in0=yk[bi][k][:], scalar=gate4[:, i, k:k + 1], in1=ff[:], op0=ALU.mult, op1=ALU.add),
                 reads=[Byk[bi][k], Bgate4, Bff], writes=[Bff])
        S.op("act", lambda h: h.activation(out=junk6[:], in_=ff[:], func=AF.Square, accum_out=st6[:, 0:1]), reads=[Bff], writes=[Bjunk6, Bst6])
        S.op("act", lambda h: h.activation(out=st6[:, 0:1], in_=st6[:, 0:1], func=AF.Sqrt, scale=1.0 / D, bias=EPS), reads=[Bst6], writes=[Bst6])
        S.op("dve", lambda h: h.reciprocal(out=st6[:, 0:1], in_=st6[:, 0:1]), reads=[Bst6], writes=[Bst6])
        S.op("dve", lambda h: h.scalar_tensor_tensor(out=ff[:], in0=ff[:], scalar=st6[:, 0:1], in1=G2[:], op0=ALU.mult, op1=ALU.mult), reads=[Bff, Bst6, BG2], writes=[Bff])
        S.op("dve", lambda h, bi=bi: h.tensor_tensor(out=ot[bi][:], in0=ff[:], in1=x1r[bi][:], op=ALU.add), reads=[Bff, Bx1r[bi]], writes=[Bot[bi]])
        final_ops.append(S.dma("sp", ch_out[bi], lambda h, i=i, bi=bi: h.dma_start(out=out_d[i * 128:(i + 1) * 128, :], in_=ot[bi][:]), reads=[Bot[bi]]))
    return finish(nc, S, final_ops), dbg


def finish(nc, S, final_ops):
    S.wait_final("sp", final_ops)
    S.emit()
    return nc


def _rope_tables():
    half = 64
    inv = (10000.0 ** (-np.arange(half, dtype=np.float32) / np.float32(half))).astype(np.float32)
    pos = np.arange(NCTX + SEQ, dtype=np.float32)
    ang = (pos[:, None] * inv[None, :]).astype(np.float32)
    return np.cos(ang).astype(np.float32), np.sin(ang).astype(np.float32)


def make_in_maps(inp, ne_decl=NE):
    f = lambda a: np.ascontiguousarray(np.asarray(a, dtype=np.float32))
    cos, sin = _rope_tables()
    shared = {
        "c_ctx": f(inp["c_ctx"]).reshape(16, 128),
        "ada_w": f(inp["ada_w"][0]),
        "ada_b": f(inp["ada_b"][0]).reshape(1, 6 * D),
        "pre_mix_norm": f(inp["pre_mix_norm"][0]).reshape(1, D),
        "post_mix_norm": f(inp["post_mix_norm"][0]).reshape(1, D),
        "pre_ffn_norm": f(inp["pre_ffn_norm"][0]).reshape(1, D),
        "post_ffn_norm": f(inp["post_ffn_norm"][0]).reshape(1, D),
        "w_in": f(inp["w_in"][0]),
        "ret_decay": np.concatenate([f(inp["ret_decay_fwd"][0]), f(inp["ret_decay_bwd"][0])]).reshape(1, 16),
        "ret_gn_w": f(inp["ret_gn_w"][0]).reshape(1, DRET),
        "conv_w": f(inp["conv_w"][0]),
        "conv_vecs": np.concatenate([f(inp["conv_b"][0]).reshape(8, 128), f(inp["conv_ln_w"][0]).reshape(8, 128), f(inp["conv_ln_b"][0]).reshape(8, 128)], axis=0),
        "w_out": f(inp["w_out"][0]),
        "router_w": f(inp["router_w"][0]),
        "router_b": f(inp["router_b"][0]).reshape(1, NE),
        "w1": f(inp["w1"][0][:ne_decl]),
        "b1": f(inp["b1"][0]).reshape(NE * 32, 128),
        "w2": f(inp["w2"][0][:ne_decl]),
        "b2": f(inp["b2"][0]),
        "rope_cos": cos,
        "rope_sin": sin,
    }
    maps = []
    for b in range(NB):
        m = dict(shared)
        m["x"] = f(inp["x"][b])
        m["c"] = f(inp["c"][b]).reshape(16, 128)
        m["ctx"] = f(inp["ctx"][b])
        maps.append(m)
    return maps


_NC_CACHE = {}


def kernel(**inputs):
    if "nc" not in _NC_CACHE:
        _NC_CACHE["nc"] = build_program()[0]
    nc = _NC_CACHE["nc"]
    in_maps = make_in_maps(inputs)
    res = run_bass_kernel_spmd(nc, in_maps, core_ids=list(range(NB)))
    out = np.stack([np.asarray(res.results[b]["out"], dtype=np.float32) for b in range(NB)], axis=0)
    return out
```
